# Optimizing a Trainium2 kernel written in Bass

```python
import jax, jax.numpy as jnp
from jax import lax
import numpy as np


D_MODEL = 1024
BATCH = 1
SEQ = 16384
DEPTH = 2
DEC_BATCH = 32
DEC_SEQ = 2048
PAST_LEN = 128

N_EVEN = (DEPTH + 1) // 2
N_ODD = DEPTH // 2

HG_HEADS = 4
HG_DK = 128
HG_DV = 128
HG_WIDTH = HG_HEADS * HG_DK
HG_CHUNK = 64

MLA_HEADS = 4
MLA_Q_RANK = 384
MLA_KV_RANK = 256
MLA_NOPE = 128
MLA_ROPE = 64
MLA_V = 128
MLA_QK = MLA_NOPE + MLA_ROPE
ROPE_THETA = 10000.0
Q_BLOCK = 128

EVEN_IN = 5 * HG_WIDTH + MLA_Q_RANK + MLA_KV_RANK + MLA_ROPE
EVEN_MIX = HG_HEADS * HG_DV + MLA_HEADS * MLA_V

LRU_WIDTH = D_MODEL
LRU_BLOCKS = 4
LRU_BW = LRU_WIDTH // LRU_BLOCKS
CONV_W = 4
CONV_LEFT = 2
LRU_C = 8.0

N_EXPERTS = 16
EXPERT_FF = 1024
CAPACITY = 2

EPS = 1e-6

kernel_name = 'hybrid_hgrn2_mla_rglru_ecmoe_encoder'


def rmsnorm(x, g):
    xf = x.astype(jnp.float32)
    y = xf * lax.rsqrt(jnp.mean(xf * xf, axis=-1, keepdims=True) + EPS)
    return (y * g.astype(jnp.float32)).astype(x.dtype)


def _flip(t):
    return jnp.flip(t, axis=1)


def _gla_direction(q, k, v, logf):
    B, S, H, DK = q.shape
    DV = v.shape[-1]
    n = S // HG_CHUNK

    def to_chunks(t):
        return t.astype(jnp.float32).reshape(B, n, HG_CHUNK, H, t.shape[-1]).transpose(1, 0, 3, 2, 4)

    qc, kc, vc, fc = (to_chunks(t) for t in (q, k, v, logf))
    lower = jnp.tril(jnp.ones((HG_CHUNK, HG_CHUNK), dtype=bool))[:, :, None]

    def step(state, inp):
        qi, ki, vi, fi = inp
        b = jnp.cumsum(fi, axis=-2)
        b_last = b[..., -1:, :]
        diff = b[..., :, None, :] - b[..., None, :, :]
        decay = jnp.exp(jnp.where(lower, diff, -jnp.inf))
        scores = jnp.einsum('bhtd,bhsd,bhtsd->bhts', qi, ki, decay)
        o = (jnp.einsum('bhts,bhse->bhte', scores, vi)
             + jnp.einsum('bhtd,bhde->bhte', qi * jnp.exp(b), state))
        state = (state * jnp.exp(b_last)[..., 0, :, None]
                 + jnp.einsum('bhsd,bhse->bhde', ki * jnp.exp(b_last - b), vi))
        return state, o

    s0 = jnp.zeros((B, H, DK, DV), jnp.float32)
    _, o = lax.scan(step, s0, (qc, kc, vc, fc))
    return o.transpose(1, 0, 3, 2, 4).reshape(B, S, H, DV)


def hgrn2_mixer(q_in, i_in, g_in, f_fwd, f_bwd, lb, out_gain):
    B, S, _ = q_in.shape
    shp = (B, S, HG_HEADS, HG_DK)
    q = q_in.reshape(shp) * (HG_DK ** -0.5)
    v = i_in.reshape(B, S, HG_HEADS, HG_DV)
    o = jnp.zeros((B, S, HG_HEADS, HG_DV), jnp.float32)
    for d, fz in enumerate((f_fwd, f_bwd)):
        f = (lb[d] + (1.0 - lb[d]) * jax.nn.sigmoid(fz.astype(jnp.float32))).reshape(shp)
        logf = jnp.log(f)
        k = 1.0 - f
        if d == 0:
            o = o + _gla_direction(q, k, v, logf)
        else:
            o = o + _flip(_gla_direction(_flip(q), _flip(k), _flip(v), _flip(logf)))
    gate = jax.nn.silu(g_in.astype(jnp.float32)).reshape(B, S, HG_HEADS, HG_DV)
    o = rmsnorm(o, out_gain) * gate
    return o.reshape(B, S, HG_HEADS * HG_DV).astype(q_in.dtype)


def rope_tables(S):
    pos = jnp.arange(S, dtype=jnp.float32)
    inv = 1.0 / (ROPE_THETA ** (jnp.arange(0, MLA_ROPE, 2, dtype=jnp.float32) / MLA_ROPE))
    ang = pos[:, None] * inv[None, :]
    return jnp.cos(ang), jnp.sin(ang)


def apply_rope(x, cos, sin):
    half = MLA_ROPE // 2
    x1, x2 = x[..., :half], x[..., half:]
    c = cos[None, :, None, :]
    s = sin[None, :, None, :]
    return jnp.concatenate([x1 * c - x2 * s, x1 * s + x2 * c], axis=-1).astype(x.dtype)


def bidir_block_attention(q, k, v):
    B, S, H, Dh = q.shape
    Dv = v.shape[-1]
    nb = S // Q_BLOCK
    qb = q.reshape(B, nb, Q_BLOCK, H, Dh).transpose(1, 0, 2, 3, 4)
    scale = Dh ** -0.5

    def one_block(qblk):
        s = jnp.einsum('bqhd,bkhd->bhqk', qblk, k, preferred_element_type=jnp.float32) * scale
        p = jax.nn.softmax(s, axis=-1)
        return jnp.einsum('bhqk,bkhe->bqhe', p.astype(v.dtype), v)

    o = lax.map(one_block, qb)
    return o.transpose(1, 0, 2, 3, 4).reshape(B, S, H, Dv)


def mla_mixer(c_q, c_kv, k_pe, q_norm_g, kv_norm_g, w_uq, w_ukv, q_gain, k_gain):
    B, S, _ = c_q.shape
    q = jnp.einsum('bsr,rn->bsn', rmsnorm(c_q, q_norm_g), w_uq).reshape(B, S, MLA_HEADS, MLA_QK)
    kv = jnp.einsum('bsr,rn->bsn', rmsnorm(c_kv, kv_norm_g), w_ukv).reshape(B, S, MLA_HEADS, MLA_NOPE + MLA_V)
    k_nope, v = kv[..., :MLA_NOPE], kv[..., MLA_NOPE:]
    k = jnp.concatenate([k_nope, jnp.broadcast_to(k_pe[:, :, None, :], (B, S, MLA_HEADS, MLA_ROPE))], axis=-1)
    q = rmsnorm(q, q_gain)
    k = rmsnorm(k, k_gain)
    cos, sin = rope_tables(S)
    q = jnp.concatenate([q[..., :MLA_NOPE], apply_rope(q[..., MLA_NOPE:], cos, sin)], axis=-1)
    k = jnp.concatenate([k[..., :MLA_NOPE], apply_rope(k[..., MLA_NOPE:], cos, sin)], axis=-1)
    o = bidir_block_attention(q, k, v)
    return o.reshape(B, S, MLA_HEADS * MLA_V)


def even_mixer(h, w_in, w_out, lb, hg_gain, q_norm_g, kv_norm_g, w_uq, w_ukv, q_gain, k_gain):
    proj = jnp.einsum('bsd,dn->bsn', h, w_in)
    sizes = [HG_WIDTH] * 5 + [MLA_Q_RANK, MLA_KV_RANK, MLA_ROPE]
    cuts = [int(c) for c in np.cumsum(sizes)[:-1]]
    q_in, i_in, g_in, f_f, f_b, c_q, c_kv, k_pe = jnp.split(proj, cuts, axis=-1)
    o_hg = hgrn2_mixer(q_in, i_in, g_in, f_f, f_b, lb, hg_gain)
    o_mla = mla_mixer(c_q, c_kv, k_pe, q_norm_g, kv_norm_g, w_uq, w_ukv, q_gain, k_gain)
    return jnp.einsum('bsn,nd->bsd', jnp.concatenate([o_hg, o_mla.astype(o_hg.dtype)], axis=-1), w_out)


def centred_dwconv(x, w, b):
    S = x.shape[1]
    xp = jnp.pad(x, ((0, 0), (CONV_LEFT, CONV_W - 1 - CONV_LEFT), (0, 0)))
    out = xp[:, 0:S] * w[0]
    for j in range(1, CONV_W):
        out = out + xp[:, j:j + S] * w[j]
    return out + b


def block_diag_linear(x, w, b):
    xb = x.reshape(x.shape[0], x.shape[1], LRU_BLOCKS, LRU_BW)
    return jnp.einsum('bsnk,nkj->bsnj', xb, w).reshape(x.shape) + b


def rglru_direction(x, wa, ba, wx, bx, lam, reverse):
    r = jax.nn.sigmoid(block_diag_linear(x, wa, ba).astype(jnp.float32))
    i = jax.nn.sigmoid(block_diag_linear(x, wx, bx).astype(jnp.float32))
    log_a = -LRU_C * r * jax.nn.softplus(-lam.astype(jnp.float32))
    a = jnp.exp(log_a)
    u = jnp.sqrt(-jnp.expm1(2.0 * log_a)) * (i * x.astype(jnp.float32))

    def step(hc, au):
        a_t, u_t = au
        hc = a_t * hc + u_t
        return hc, hc

    h0 = jnp.zeros((x.shape[0], x.shape[2]), jnp.float32)
    _, hs = lax.scan(step, h0, (a.transpose(1, 0, 2), u.transpose(1, 0, 2)), reverse=reverse)
    return hs.transpose(1, 0, 2)


def odd_mixer(h, w_in, conv_w, conv_b, wa, ba, wx, bx, lam, w_out):
    proj = jnp.einsum('bsd,dn->bsn', h, w_in)
    gate, xb = proj[..., :LRU_WIDTH], proj[..., LRU_WIDTH:]
    xb = centred_dwconv(xb, conv_w, conv_b)
    y = (rglru_direction(xb, wa[0], ba[0], wx[0], bx[0], lam[0], False)
         + rglru_direction(xb, wa[1], ba[1], wx[1], bx[1], lam[1], True))
    y = (jax.nn.gelu(gate.astype(jnp.float32)) * y).astype(h.dtype)
    return jnp.einsum('bsn,nd->bsd', y, w_out)


def expert_choice_moe(h, w_router, w_gate, w_up, w_down):
    B, S, D = h.shape
    xt = h.reshape(B * S, D)
    cap = CAPACITY * (B * S) // N_EXPERTS
    affinity = jax.nn.softmax(jnp.einsum('nd,de->ne', xt, w_router, preferred_element_type=jnp.float32), axis=-1)
    gates, idx = lax.top_k(affinity.T, cap)
    xe = xt[idx]
    hid = jax.nn.silu(jnp.einsum('ecd,edf->ecf', xe, w_gate)) * jnp.einsum('ecd,edf->ecf', xe, w_up)
    ye = jnp.einsum('ecf,efd->ecd', hid, w_down) * gates[..., None].astype(xt.dtype)
    out = jnp.zeros_like(xt).at[idx.reshape(-1)].add(ye.reshape(-1, D))
    return out.reshape(B, S, D)


def trunk(x, norm_mix, norm_ffn, ev_w_in, ev_w_out, hg_lb_logits, hg_out_gain, mla_q_norm, mla_kv_norm,
          mla_w_uq, mla_w_ukv, mla_q_gain, mla_k_gain, od_w_in, od_conv_w, od_conv_b, rg_w_a, rg_b_a,
          rg_w_x, rg_b_x, rg_lambda, od_w_out, moe_router, moe_w_gate, moe_w_up, moe_w_down):
    lb_all = jnp.cumsum(jax.nn.softmax(hg_lb_logits.astype(jnp.float32), axis=1), axis=1)
    for layer in range(DEPTH):
        j = layer // 2
        h = rmsnorm(x, norm_mix[layer])
        if layer % 2 == 0:
            x = x + even_mixer(h, ev_w_in[j], ev_w_out[j], lb_all[:, j], hg_out_gain[j], mla_q_norm[j],
                               mla_kv_norm[j], mla_w_uq[j], mla_w_ukv[j], mla_q_gain[j], mla_k_gain[j])
        else:
            x = x + odd_mixer(h, od_w_in[j], od_conv_w[j], od_conv_b[j], rg_w_a[j], rg_b_a[j],
                              rg_w_x[j], rg_b_x[j], rg_lambda[j], od_w_out[j])
        x = x + expert_choice_moe(rmsnorm(x, norm_ffn[layer]), moe_router[layer], moe_w_gate[layer],
                                  moe_w_up[layer], moe_w_down[layer])
    return x


def setup_inputs(seed: int = 0) -> dict:
    key = jax.random.key(seed)
    ks = jax.random.split(key, 32)
    f32 = jnp.float32

    def nrm(k, shape, fan_in):
        return jax.random.normal(k, shape, f32) * (fan_in ** -0.5)

    def gain(k, shape):
        return 1.0 + 0.02 * jax.random.normal(k, shape, f32)

    def small(k, shape):
        return 0.01 * jax.random.normal(k, shape, f32)

    a0 = jax.random.uniform(ks[21], (N_ODD, 2, LRU_WIDTH), f32, minval=0.9, maxval=0.999)
    s = a0 ** (1.0 / LRU_C)
    rg_lambda = jnp.log(s) - jnp.log1p(-s)
    return {
        'x_prompt': jax.random.normal(ks[0], (BATCH, SEQ, D_MODEL), f32),
        'x_sample': jax.random.normal(ks[1], (DEC_BATCH, DEC_SEQ, D_MODEL), f32),
        'norm_mix': gain(ks[2], (DEPTH, D_MODEL)),
        'norm_ffn': gain(ks[3], (DEPTH, D_MODEL)),
        'ev_w_in': nrm(ks[4], (N_EVEN, D_MODEL, EVEN_IN), D_MODEL),
        'ev_w_out': nrm(ks[5], (N_EVEN, EVEN_MIX, D_MODEL), EVEN_MIX),
        'hg_lb_logits': 0.5 * jax.random.normal(ks[6], (2, N_EVEN + 1, HG_WIDTH), f32),
        'hg_out_gain': gain(ks[7], (N_EVEN, HG_DV)),
        'mla_q_norm': gain(ks[8], (N_EVEN, MLA_Q_RANK)),
        'mla_kv_norm': gain(ks[9], (N_EVEN, MLA_KV_RANK)),
        'mla_w_uq': nrm(ks[10], (N_EVEN, MLA_Q_RANK, MLA_HEADS * MLA_QK), MLA_Q_RANK),
        'mla_w_ukv': nrm(ks[11], (N_EVEN, MLA_KV_RANK, MLA_HEADS * (MLA_NOPE + MLA_V)), MLA_KV_RANK),
        'mla_q_gain': gain(ks[12], (N_EVEN, MLA_QK)),
        'mla_k_gain': gain(ks[13], (N_EVEN, MLA_QK)),
        'od_w_in': nrm(ks[14], (N_ODD, D_MODEL, 2 * LRU_WIDTH), D_MODEL),
        'od_conv_w': nrm(ks[15], (N_ODD, CONV_W, LRU_WIDTH), CONV_W),
        'od_conv_b': small(ks[16], (N_ODD, LRU_WIDTH)),
        'rg_w_a': nrm(ks[17], (N_ODD, 2, LRU_BLOCKS, LRU_BW, LRU_BW), LRU_BW),
        'rg_b_a': small(ks[18], (N_ODD, 2, LRU_WIDTH)),
        'rg_w_x': nrm(ks[19], (N_ODD, 2, LRU_BLOCKS, LRU_BW, LRU_BW), LRU_BW),
        'rg_b_x': small(ks[20], (N_ODD, 2, LRU_WIDTH)),
        'rg_lambda': rg_lambda,
        'od_w_out': nrm(ks[22], (N_ODD, LRU_WIDTH, D_MODEL), LRU_WIDTH),
        'moe_router': nrm(ks[23], (DEPTH, D_MODEL, N_EXPERTS), D_MODEL),
        'moe_w_gate': nrm(ks[24], (DEPTH, N_EXPERTS, D_MODEL, EXPERT_FF), D_MODEL),
        'moe_w_up': nrm(ks[25], (DEPTH, N_EXPERTS, D_MODEL, EXPERT_FF), D_MODEL),
        'moe_w_down': nrm(ks[26], (DEPTH, N_EXPERTS, EXPERT_FF, D_MODEL), EXPERT_FF),
    }


def reference(x_prompt, x_sample, norm_mix, norm_ffn, ev_w_in, ev_w_out, hg_lb_logits, hg_out_gain,
              mla_q_norm, mla_kv_norm, mla_w_uq, mla_w_ukv, mla_q_gain, mla_k_gain, od_w_in, od_conv_w,
              od_conv_b, rg_w_a, rg_b_a, rg_w_x, rg_b_x, rg_lambda, od_w_out, moe_router, moe_w_gate,
              moe_w_up, moe_w_down):
    params = (norm_mix, norm_ffn, ev_w_in, ev_w_out, hg_lb_logits, hg_out_gain, mla_q_norm, mla_kv_norm,
              mla_w_uq, mla_w_ukv, mla_q_gain, mla_k_gain, od_w_in, od_conv_w, od_conv_b, rg_w_a, rg_b_a,
              rg_w_x, rg_b_x, rg_lambda, od_w_out, moe_router, moe_w_gate, moe_w_up, moe_w_down)
    y_prompt = trunk(x_prompt, *params)
    y_sample = trunk(x_sample, *params)
    return (y_prompt, y_sample)
```

```python
import numpy as np
from contextlib import ExitStack
import concourse.bass as bass
import concourse.mybir as mybir
from concourse.bass_utils import run_bass_kernel_spmd

F32 = mybir.dt.float32
BF16 = mybir.dt.bfloat16
I32 = mybir.dt.int32
AF = mybir.ActivationFunctionType
ALU = mybir.AluOpType
AX = mybir.AxisListType

D = 1024
L = 2048
EPS = 1e-6
NE = 16
BIG = 1.0e6
import os
HSTOP = int(os.environ.get('HSTOP', '99'))
MSTOP = int(os.environ.get('MSTOP', '99'))
FSTOP = int(os.environ.get('FSTOP', '99'))
NEXP = int(os.environ.get('NEXP', '16'))


class Cx:
    ENG = ('pe', 'dve', 'act', 'pool')

    def __init__(s, nc, es):
        s.nc = nc
        s.e = {'pe': nc.tensor, 'dve': nc.vector, 'act': nc.scalar, 'pool': nc.gpsimd, 'sp': nc.sync}
        s.sem = {}
        for k in s.ENG:
            s.sem[k] = es.enter_context(nc.semaphore(f"c_{k}"))
        s.cnt = {k: 0 for k in s.ENG}
        s.ch = []
        for q, n in (('sp', 8), ('pool', 8)):
            for i in range(n):
                nm = f"d_{q}{i}"
                s.sem[nm] = es.enter_context(nc.semaphore(nm))
                s.ch.append({'q': q, 'name': nm, 'cnt': 0})
        s.rr = {'sp': 0, 'pool': 0}
        s.bar = [es.enter_context(nc.semaphore(f"bar{i}")) for i in range(4)]
        s.par = 0
        s.seen = {k: {} for k in ('pe', 'dve', 'act', 'pool', 'sp')}
        s.lw = {}
        s.rd = {}
        s.ninst = 0
        s.nwait = 0
        s.npool = 0
        s.nopt = es.enter_context(nc.sbuf_tensor("cx_nop", [128, 1], F32))

    def _wait(s, eng, sn, v):
        if v <= 0:
            return
        if s.seen[eng].get(sn, 0) >= v:
            return
        s.e[eng].wait_ge(s.sem[sn], v)
        s.seen[eng][sn] = v
        s.nwait += 1

    def _deps(s, eng, r, w):
        need = {}
        for k in r:
            ev = s.lw.get(k)
            if ev:
                need[ev[0]] = max(need.get(ev[0], 0), ev[1])
        for k in w:
            ev = s.lw.get(k)
            if ev:
                need[ev[0]] = max(need.get(ev[0], 0), ev[1])
            for sn, v in s.rd.get(k, {}).items():
                need[sn] = max(need.get(sn, 0), v)
        for sn, v in need.items():
            if eng == 'pe' and sn == 'pe':
                continue
            s._wait(eng, sn, v)

    def _rec(s, ev, r, w):
        for k in w:
            s.lw[k] = ev
            s.rd[k] = {}
        for k in r:
            d = s.rd.setdefault(k, {})
            d[ev[0]] = max(d.get(ev[0], 0), ev[1])

    def op(s, eng, fn, r=(), w=()):
        s._deps(eng, r, w)
        inst = fn(s.e[eng])
        s.cnt[eng] += 1
        assert s.cnt[eng] < 32000, "epoch too long"
        inst.then_inc(s.sem[eng], 1)
        s._rec((eng, s.cnt[eng]), r, w)
        s.ninst += 1

    def dma(s, q, fn, r=(), w=()):
        chs = [c for c in s.ch if c['q'] == q]
        c = chs[s.rr[q] % len(chs)]
        s.rr[q] += 1
        nw0 = s.nwait
        s._wait(q, c['name'], c['cnt'])
        s._deps(q, r, w)
        if q == 'pool':
            s.npool += 1
            probe = s.e['pool'].alloc_register(f"cxprobe{s.npool}")
            s.e['pool'].free_register(probe)
            inst = fn(s.e[q])
            try:
                s.e['pool'].free_register(probe)
            except Exception:
                pass
        else:
            inst = fn(s.e[q])
        c['cnt'] += 16
        assert c['cnt'] < 32000, "epoch too long (dma)"
        inst.then_inc(s.sem[c['name']], 16)
        s._rec((c['name'], c['cnt']), r, w)
        s.ninst += 1

    def drain(s):
        for k in s.ENG:
            s._wait(k, k, s.cnt[k])
        for c in s.ch:
            s._wait(c['q'], c['name'], c['cnt'])

    def epoch(s):
        s.drain()
        A, B = s.bar[2 * s.par], s.bar[2 * s.par + 1]
        A2, B2 = s.bar[2 * (1 - s.par)], s.bar[2 * (1 - s.par) + 1]
        allk = ('pe', 'dve', 'act', 'pool', 'sp')
        for k in allk:
            s.e[k].sem_inc(A, 1)
        m = s.e['sp']
        m.wait_ge(A, 5)
        for k in s.ENG:
            m.sem_clear(s.sem[k])
        for c in s.ch:
            if c['q'] != 'pool':
                m.sem_clear(s.sem[c['name']])
        m.sem_clear(A2)
        m.sem_clear(B2)
        m.sem_inc(B, 1)
        for k in allk:
            s.e[k].wait_ge(B, 1)
        s.cnt = {k: 0 for k in s.ENG}
        for c in s.ch:
            if c['q'] != 'pool':
                c['cnt'] = 0
        s.seen = {k: {} for k in allk}
        s.lw = {}
        s.rd = {}
        s.par ^= 1


class Cfg:
    def __init__(s, P, B):
        s.P = P
        s.B = B
        s.NS = P + B
        s.T = s.NS * L
        s.NT = s.T // 128
        s.groups = [(0, P), (P, P + B)]


def chain_of(cfg, sg):
    if sg < cfg.P:
        return list(range(cfg.P))
    return [sg]


def load_w(cx, es, name, src, K, N, gcol=None, stg=None, dtype=BF16):
    nc = cx.nc
    kc = (K + 127) // 128
    wt = es.enter_context(nc.sbuf_tensor(name, [128, kc, N], dtype))
    for k in range(kc):
        rows = min(128, K - k * 128)
        for n0 in range(0, N, 1024):
            nn = min(1024, N - n0)
            sk = f"wstg{(k + n0 // 1024) % 2}"
            st = stg[(k + n0 // 1024) % 2]
            cx.dma('sp', lambda e, st=st, k=k, rows=rows, n0=n0, nn=nn: e.dma_start(out=st[0:rows, 0:nn], in_=src[k * 128:k * 128 + rows, n0:n0 + nn]),
                   w=[sk])
            if gcol is None:
                cx.op('pool', lambda e, st=st, k=k, rows=rows, n0=n0, nn=nn: e.tensor_copy(wt[0:rows, k, n0:n0 + nn], st[0:rows, 0:nn]),
                      r=[sk], w=[name])
            else:
                cx.op('dve', lambda e, st=st, k=k, rows=rows, n0=n0, nn=nn: e.tensor_scalar(wt[0:rows, k, n0:n0 + nn], st[0:rows, 0:nn], gcol[0:rows, k:k + 1], None, op0=ALU.mult),
                      r=[sk], w=[name])
    return wt


def load_cols(cx, es, name, src_vec, n):
    nc = cx.nc
    kc = n // 128
    t = es.enter_context(nc.sbuf_tensor(name, [128, kc], F32))
    cx.dma('sp', lambda e: e.dma_start(out=t[:, :], in_=src_vec.rearrange("(k p) -> p k", p=128), allow_slow_non_contiguous=True), w=[name])
    return t


def rstd_from_ssq(cx, out, ssq, n, tmpk):
    (o_ap, o_k), (s_ap, s_k) = out, ssq
    cx.op('dve', lambda e: e.tensor_scalar(o_ap, s_ap, 1.0 / n, EPS, op0=ALU.mult, op1=ALU.add), r=[s_k], w=[o_k])
    cx.op('act', lambda e: e.activation(out=o_ap, in_=o_ap, func=AF.Sqrt), r=[o_k], w=[o_k])
    cx.op('dve', lambda e: e.reciprocal(o_ap, o_ap), r=[o_k], w=[o_k])


def pass_h(cx, cfg, x_rows, hT_d, consts, ps, tag="a"):
    nc = cx.nc
    with ExitStack() as es:
        xt = [es.enter_context(nc.sbuf_tensor(f"h{tag}_xt{i}", [128, D], F32)) for i in range(2)]
        junk = es.enter_context(nc.sbuf_tensor(f"h{tag}_junk", [128, D], BF16))
        ssq = es.enter_context(nc.sbuf_tensor(f"h{tag}_ssq", [128, 2], F32))
        rs = es.enter_context(nc.sbuf_tensor(f"h{tag}_rs", [128, 2], F32))
        hT = [es.enter_context(nc.sbuf_tensor(f"h{tag}_hT{i}", [128, 8, 512], BF16)) for i in range(2)]
        ident = consts['identF']
        for sg in range(cfg.NS):
            for blk in range(L // 512):
                hb = hT[blk % 2]
                hk = f"h_hT{blk % 2}"
                for j4 in range(4):
                    j = blk * 4 + j4
                    b = j % 2
                    t0 = sg * L + j * 128
                    cx.dma('sp', lambda e, b=b, t0=t0: e.dma_start(out=xt[b][:, :], in_=x_rows(t0, 128)), w=[f"h_xt{b}"])
                    cx.op('act', lambda e, b=b: e.activation(out=junk[:, :], in_=xt[b][:, :], func=AF.Square, accum_out=ssq[:, b:b + 1]),
                          r=[f"h_xt{b}"], w=["h_junk", f"h_ssq{b}"])
                    rstd_from_ssq(cx, (rs[:, b:b + 1], f"h_rs{b}"), (ssq[:, b:b + 1], f"h_ssq{b}"), D, None)
                    cx.op('dve', lambda e, b=b: e.tensor_scalar(xt[b][:, :], xt[b][:, :], rs[:, b:b + 1], None, op0=ALU.mult),
                          r=[f"h_rs{b}", f"h_xt{b}"], w=[f"h_xt{b}"])
                    for half in range(2):
                        pt = ps[half]
                        pk = f"ps{half}"
                        for kk in range(4):
                            k = half * 4 + kk
                            cx.op('pe', lambda e, pt=pt, kk=kk, k=k, b=b: e.transpose(pt[:, kk * 128:(kk + 1) * 128], xt[b][:, k * 128:(k + 1) * 128], ident[:, :]),
                                  r=[f"h_xt{b}", "identF"], w=[pk])
                        eng = 'act' if half == 0 else 'dve'
                        if eng == 'act':
                            cx.op('act', lambda e, pt=pt, half=half, j4=j4, hb=hb: e.activation(
                                out=hb[:, half * 4:(half + 1) * 4, j4 * 128:(j4 + 1) * 128],
                                in_=pt[:, :].rearrange("p (k t) -> p k t", k=4), func=AF.Copy), r=[pk], w=[hk])
                        else:
                            cx.op('dve', lambda e, pt=pt, half=half, j4=j4, hb=hb: e.tensor_copy(
                                hb[:, half * 4:(half + 1) * 4, j4 * 128:(j4 + 1) * 128],
                                pt[:, :].rearrange("p (k t) -> p k t", k=4)), r=[pk], w=[hk])
                c0 = sg * L + blk * 512
                cx.dma('sp', lambda e, hb=hb, c0=c0: e.dma_start(out=hT_d[:, :, c0:c0 + 512].rearrange("k p t -> p k t"), in_=hb[:, :, :]),
                       r=[hk], w=[f"d:hT:{sg}"])
            cx.epoch()


def pass_hgrn(cx, cfg, hT_d, o_d, w_in, lbT, dirn, consts, ps, gmix, stop=99):
    nc = cx.nc
    with ExitStack() as es:
        stg = [es.enter_context(nc.sbuf_tensor(f"g{dirn}_stg{i}", [128, 1024], F32)) for i in range(2)]
        wq = load_w(cx, es, f"g{dirn}_wq", w_in[:, 0:512], D, 512, gcol=gmix, stg=stg)
        wi = load_w(cx, es, f"g{dirn}_wi", w_in[:, 512:1024], D, 512, gcol=gmix, stg=stg)
        wf = load_w(cx, es, f"g{dirn}_wf", w_in[:, 1536 + 512 * dirn:2048 + 512 * dirn], D, 512, gcol=gmix, stg=stg)
        hR = es.enter_context(nc.sbuf_tensor(f"g{dirn}_hR", [128, 8, L], BF16))
        hst = [es.enter_context(nc.sbuf_tensor(f"g{dirn}_hst{i}", [128, L], BF16)) for i in range(2)] if dirn else None
        f = es.enter_context(nc.sbuf_tensor(f"g{dirn}_f", [128, 1024], F32))
        bb = es.enter_context(nc.sbuf_tensor(f"g{dirn}_b", [128, 1024], F32))
        ea = es.enter_context(nc.sbuf_tensor(f"g{dirn}_ea", [128, 1024], F32))
        kk_ = es.enter_context(nc.sbuf_tensor(f"g{dirn}_k", [128, 1024], F32))
        qt = es.enter_context(nc.sbuf_tensor(f"g{dirn}_qt", [128, 4, L], BF16))
        kt = es.enter_context(nc.sbuf_tensor(f"g{dirn}_kt", [128, 4, L], BF16))
        kh = es.enter_context(nc.sbuf_tensor(f"g{dirn}_kh", [128, 4, L], BF16))
        ebl = es.enter_context(nc.sbuf_tensor(f"g{dirn}_ebl", [128, 4, 32], F32))
        v = es.enter_context(nc.sbuf_tensor(f"g{dirn}_v", [64, 2, 512], BF16))
        khT = es.enter_context(nc.sbuf_tensor(f"g{dirn}_khT", [64, 2, 512], BF16))
        pT = es.enter_context(nc.sbuf_tensor(f"g{dirn}_pT", [64, 4, 64], BF16))
        osb = es.enter_context(nc.sbuf_tensor(f"g{dirn}_o", [64, 2, 8, 512], BF16))
        otmp = es.enter_context(nc.sbuf_tensor(f"g{dirn}_otmp", [64, 512], F32))
        S = es.enter_context(nc.sbuf_tensor(f"g{dirn}_S", [128, 512], F32))
        Sb = es.enter_context(nc.sbuf_tensor(f"g{dirn}_Sb", [128, 512], BF16))
        identB = consts['identB']
        cmask = consts['cmask']
        smask = consts['smask']
        lb = lbT[dirn]
        segs = list(range(cfg.NS))
        if dirn:
            segs = list(reversed(range(cfg.P))) + list(range(cfg.P, cfg.NS))
        for si, sg in enumerate(segs):
            first = (sg >= cfg.P) or (sg == (cfg.P - 1 if dirn else 0))
            for k in range(8):
                if dirn:
                    cx.dma('sp', lambda e, k=k, sg=sg: e.dma_start(out=hst[k % 2][:, :], in_=hT_d[k, :, sg * L:(sg + 1) * L]), w=[f"g_hst{k % 2}"])
                    cx.op('dve', lambda e, k=k: e.tensor_copy(hR[:, k, :], hst[k % 2][:, ::-1]), r=[f"g_hst{k % 2}"], w=["g_hR"])
                else:
                    cx.dma('sp', lambda e, k=k, sg=sg: e.dma_start(out=hR[:, k, :], in_=hT_d[k, :, sg * L:(sg + 1) * L]), w=["g_hR"])
            hk = "g_hR"
            if first:
                cx.op('dve', lambda e: e.memset(S[:, :], 0.0), w=["g_S"])
                cx.op('pool', lambda e: e.memset(Sb[:, :], 0.0), w=["g_Sb"])
            if stop <= 0:
                cx.epoch(); continue
            for h in range(4):
                for hf in range(2):
                    c0 = hf * 1024
                    pz = (ps[0], ps[1])
                    pq = (ps[2], ps[3])
                    for nb in range(2):
                        for k in range(8):
                            cx.op('pe', lambda e, nb=nb, k=k, h=h, c0=c0: e.matmul(pz[nb][:, :], lhsT=wf[:, k, h * 128:(h + 1) * 128], rhs=hR[:, k, c0 + nb * 512:c0 + (nb + 1) * 512], start=(k == 0), stop=(k == 7)),
                                  r=[f"g{dirn}_wf", hk], w=[f"ps{nb}"])
                        for k in range(8):
                            cx.op('pe', lambda e, nb=nb, k=k, h=h, c0=c0: e.matmul(pq[nb][:, :], lhsT=wq[:, k, h * 128:(h + 1) * 128], rhs=hR[:, k, c0 + nb * 512:c0 + (nb + 1) * 512], start=(k == 0), stop=(k == 7)),
                                  r=[f"g{dirn}_wq", hk], w=[f"ps{2 + nb}"])
                    for nb in range(2):
                        cx.op('act', lambda e, nb=nb: e.activation(out=f[:, nb * 512:(nb + 1) * 512], in_=pz[nb][:, :], func=AF.Sigmoid), r=[f"ps{nb}"], w=["g_f"])
                    cx.op('dve', lambda e, h=h: e.tensor_scalar(f[:, :], f[:, :], lb[1][:, h:h + 1], lb[0][:, h:h + 1], op0=ALU.mult, op1=ALU.add), r=["g_f", "lbT"], w=["g_f"])
                    cx.op('act', lambda e: e.activation(out=bb[:, :], in_=f[:, :], func=AF.Ln), r=["g_f"], w=["g_b"])
                    cx.op('dve', lambda e: e.tensor_tensor_scan(out=bb[:, :], data0=smask[:, :], data1=bb[:, :], initial=0.0, op0=ALU.mult, op1=ALU.add), r=["g_b", "smask"], w=["g_b"])
                    cx.op('pool', lambda e: e.tensor_scalar(kk_[:, :], f[:, :], -1.0, 1.0, op0=ALU.mult, op1=ALU.add), r=["g_f"], w=["g_k"])
                    cx.op('act', lambda e: e.activation(out=ea[:, :], in_=bb[:, :], func=AF.Exp), r=["g_b"], w=["g_ea"])
                    for nb in range(2):
                        cx.op('dve', lambda e, nb=nb, h=h, c0=c0: e.scalar_tensor_tensor(out=qt[:, h, c0 + nb * 512:c0 + (nb + 1) * 512], in0=pq[nb][:, :], scalar=128.0 ** -0.5, in1=ea[:, nb * 512:(nb + 1) * 512], op0=ALU.mult, op1=ALU.mult),
                              r=[f"ps{2 + nb}", "g_ea"], w=["g_qt"])
                    cx.op('act', lambda e: e.activation(out=ea[:, :], in_=bb[:, :], func=AF.Exp, scale=-1.0), r=["g_b"], w=["g_ea"])
                    cx.op('dve', lambda e, h=h, c0=c0: e.tensor_tensor(out=kt[:, h, c0:c0 + 1024], in0=kk_[:, :], in1=ea[:, :], op=ALU.mult), r=["g_k", "g_ea"], w=["g_kt"])
                    b3 = bb[:, :].rearrange("p (c t) -> p c t", t=64)
                    cx.op('act', lambda e, h=h, hf=hf: e.activation(out=ebl[:, h, hf * 16:(hf + 1) * 16], in_=b3[:, :, 63], func=AF.Exp), r=["g_b"], w=["g_ebl"])
                    cx.op('dve', lambda e: e.tensor_tensor(out=ea[:, :].rearrange("p (c t) -> p c t", t=64), in0=b3[:, :, 63:64].to_broadcast([128, 16, 64]), in1=b3, op=ALU.subtract), r=["g_b"], w=["g_ea"])
                    cx.op('act', lambda e: e.activation(out=ea[:, :], in_=ea[:, :], func=AF.Exp), r=["g_ea"], w=["g_ea"])
                    cx.op('dve', lambda e, h=h, c0=c0: e.tensor_tensor(out=kh[:, h, c0:c0 + 1024], in0=kk_[:, :], in1=ea[:, :], op=ALU.mult), r=["g_k", "g_ea"], w=["g_kh"])
            if stop <= 1:
                cx.epoch(); continue
            if dirn == 0:
                r0 = sg * L
            else:
                r0 = (cfg.P - 1 - sg) * L if sg < cfg.P else sg * L
            for c in range(32):
                cb = c % 2
                pv = ps[4 + cb]
                pvk = f"ps{4 + cb}"
                for k in range(8):
                    cx.op('pe', lambda e, k=k, c=c, pv=pv: e.matmul(pv[0:64, :], lhsT=hR[:, k, c * 64:(c + 1) * 64], rhs=wi[:, k, :], start=(k == 0), stop=(k == 7)),
                          r=[hk, f"g{dirn}_wi"], w=[pvk])
                cx.op('act', lambda e, cb=cb, pv=pv: e.activation(out=v[:, cb, :], in_=pv[0:64, :], func=AF.Copy), r=[pvk], w=[f"g_v{cb}"])
                pk_ = consts['psb'][cb]
                pkk = f"psb{cb}"
                for h in range(4):
                    cx.op('pe', lambda e, h=h, c=c, pk_=pk_: e.transpose(pk_[0:64, h * 128:(h + 1) * 128], kh[:, h, c * 64:(c + 1) * 64], identB[:, :]),
                          r=["g_kh", "identB"], w=[pkk])
                cx.op('pool' if False else 'dve', lambda e, cb=cb, pk_=pk_: e.tensor_copy(khT[:, cb, :], pk_[0:64, :]), r=[pkk], w=[f"g_khT{cb}"])
                psS, pso, psd = ps[0], ps[1], ps[2]
                for h in range(4):
                    cx.op('pe', lambda e, h=h, c=c: e.matmul(psS[0:64, h * 64:(h + 1) * 64], lhsT=kt[:, h, c * 64:(c + 1) * 64], rhs=qt[:, h, c * 64:(c + 1) * 64], start=True, stop=True),
                          r=["g_kt", "g_qt"], w=["ps0"])
                cx.op('dve', lambda e: e.tensor_tensor(out=pT[:, :, :], in0=psS[0:64, 0:256].rearrange("p (h t) -> p h t", h=4), in1=cmask[:, :].unsqueeze(1).to_broadcast([64, 4, 64]), op=ALU.mult),
                      r=["ps0", "cmask"], w=["g_pT"])
                for h in range(4):
                    cx.op('pe', lambda e, h=h, cb=cb: e.matmul(pso[0:64, h * 128:(h + 1) * 128], lhsT=pT[:, h, :], rhs=v[:, cb, h * 128:(h + 1) * 128], start=True, stop=True),
                          r=["g_pT", f"g_v{cb}"], w=["ps1"])
                    cx.op('pe', lambda e, h=h, c=c: e.matmul(ps[3][0:64, h * 128:(h + 1) * 128], lhsT=qt[:, h, c * 64:(c + 1) * 64], rhs=Sb[:, h * 128:(h + 1) * 128], start=True, stop=True),
                          r=["g_qt", "g_Sb"], w=["ps3"])
                ob = (c // 8) % 2
                cx.op('act', lambda e: e.activation(out=otmp[:, :], in_=pso[0:64, :], func=AF.Copy), r=["ps1"], w=["g_otmp"])
                cx.op('dve', lambda e, c=c, ob=ob: e.tensor_tensor(out=osb[:, ob, c % 8, :], in0=ps[3][0:64, :], in1=otmp[:, :], op=ALU.add), r=["ps3", "g_otmp"], w=[f"g_o{ob}"])
                for h in range(4):
                    cx.op('pe', lambda e, h=h, cb=cb: e.matmul(psd[:, h * 128:(h + 1) * 128], lhsT=khT[:, cb, h * 128:(h + 1) * 128], rhs=v[:, cb, h * 128:(h + 1) * 128], start=True, stop=True),
                          r=[f"g_khT{cb}", f"g_v{cb}"], w=["ps2"])
                for h in range(4):
                    cx.op('dve', lambda e, h=h, c=c: e.scalar_tensor_tensor(out=S[:, h * 128:(h + 1) * 128], in0=S[:, h * 128:(h + 1) * 128], scalar=ebl[:, h, c:c + 1], in1=psd[:, h * 128:(h + 1) * 128], op0=ALU.mult, op1=ALU.add),
                          r=["g_S", "g_ebl", "ps2"], w=["g_S"])
                cx.op('act', lambda e: e.activation(out=Sb[:, :], in_=S[:, :], func=AF.Copy), r=["g_S"], w=["g_Sb"])
                if c % 8 == 7:
                    rr0 = r0 + (c - 7) * 64
                    cx.dma('sp', lambda e, rr0=rr0, ob=ob: e.dma_start(out=o_d[rr0:rr0 + 512, :].rearrange("(c t) n -> t c n", t=64), in_=osb[:, ob, :, :]), r=[f"g_o{ob}"], w=[f"d:o{dirn}:{sg}:{c}"])
            cx.epoch()


def rstd_tile(cx, out_ap, out_k, ps_ap, ps_k, n):
    cx.op('dve', lambda e: e.tensor_scalar(out_ap, ps_ap, 1.0 / n, EPS, op0=ALU.mult, op1=ALU.add), r=[ps_k], w=[out_k])
    cx.op('act', lambda e: e.activation(out=out_ap, in_=out_ap, func=AF.Sqrt), r=[out_k], w=[out_k])
    cx.op('dve', lambda e: e.reciprocal(out_ap, out_ap), r=[out_k], w=[out_k])


def seg_pos0(cfg, sg):
    return sg * L if sg < cfg.P else 0


def pass_mla_prep(cx, cfg, hT_d, q_d, k_d, v_d, ins, consts, ps, gmix):
    nc = cx.nc
    w_in = ins["ev_w_in"]
    with ExitStack() as es:
        stg = [es.enter_context(nc.sbuf_tensor(f"m1_stg{i}", [128, 1024], F32)) for i in range(2)]
        wcq = load_w(cx, es, "m1_wcq", w_in[:, 2560:2944], D, 384, gcol=gmix, stg=stg)
        wckv = load_w(cx, es, "m1_wckv", w_in[:, 2944:3200], D, 256, gcol=gmix, stg=stg)
        wkpe = load_w(cx, es, "m1_wkpe", w_in[:, 3200:3264], D, 64, gcol=gmix, stg=stg)
        gqn = load_cols(cx, es, "m1_gqn", ins["mla_q_norm"], 384)
        gkvn = load_cols(cx, es, "m1_gkvn", ins["mla_kv_norm"], 256)
        wuq = load_w(cx, es, "m1_wuq", ins["mla_w_uq"], 384, 768, gcol=gqn, stg=stg)
        wukv = load_w(cx, es, "m1_wukv", ins["mla_w_ukv"], 256, 1024, gcol=gkvn, stg=stg)
        wv = es.enter_context(nc.sbuf_tensor("m1_wv", [128, 2, 512], BF16))
        for h in range(4):
            cx.op('dve', lambda e, h=h: e.tensor_copy(wv[:, :, h * 128:(h + 1) * 128], wukv[:, :, h * 256 + 128:h * 256 + 256]), r=["m1_wukv"], w=["m1_wv"])
        gq = es.enter_context(nc.sbuf_tensor("m1_gq", [128, 2], F32))
        gk = es.enter_context(nc.sbuf_tensor("m1_gk", [128, 2], F32))
        for (t, src, nm) in ((gq, ins["mla_q_gain"], "m1_gq"), (gk, ins["mla_k_gain"], "m1_gk")):
            cx.op('dve', lambda e, t=t: e.memset(t[:, :], 0.0), w=[nm])
            cx.dma('sp', lambda e, t=t, src=src: e.dma_start(out=t[:, 0:1], in_=src[0:128].rearrange("(p o) -> p o", o=1)), w=[nm])
            cx.dma('sp', lambda e, t=t, src=src: e.dma_start(out=t[0:64, 1:2], in_=src[128:192].rearrange("(p o) -> p o", o=1)), w=[nm])
        cx.op('dve', lambda e: e.tensor_scalar(gq[:, :], gq[:, :], 192.0 ** -0.5, None, op0=ALU.mult), r=["m1_gq"], w=["m1_gq"])
        Rm = es.enter_context(nc.sbuf_tensor("m1_Rm", [64, 64], F32))
        Rt = es.enter_context(nc.sbuf_tensor("m1_Rt", [64, 64], F32))
        iot = consts['iot']
        cx.op('dve', lambda e: e.tensor_scalar(Rm[:, :], iot[0:64, 0:64], 32.0, None, op0=ALU.is_equal), r=["c_iot"], w=["m1_Rm"])
        cx.op('dve', lambda e: e.tensor_scalar(Rt[:, :], iot[0:64, 0:64], -32.0, None, op0=ALU.is_equal), r=["c_iot"], w=["m1_Rt"])
        cx.op('dve', lambda e: e.tensor_tensor(out=Rm[:, :], in0=Rm[:, :], in1=Rt[:, :], op=ALU.subtract), r=["m1_Rm", "m1_Rt"], w=["m1_Rm"])
        hb = es.enter_context(nc.sbuf_tensor("m1_hb", [128, 8, 512], BF16))
        cq = es.enter_context(nc.sbuf_tensor("m1_cq", [128, 3, 512], F32))
        cqn = es.enter_context(nc.sbuf_tensor("m1_cqn", [128, 3, 512], BF16))
        ckvn = es.enter_context(nc.sbuf_tensor("m1_ckvn", [128, 2, 512], BF16))
        sqb = es.enter_context(nc.sbuf_tensor("m1_sqb", [128, 3, 512], BF16))
        sqr = es.enter_context(nc.sbuf_tensor("m1_sqr", [128, 512], BF16))
        sqk = es.enter_context(nc.sbuf_tensor("m1_sqk", [128, 512], BF16))
        r1 = es.enter_context(nc.sbuf_tensor("m1_r1", [128, 512], F32))
        nf = es.enter_context(nc.sbuf_tensor("m1_nf", [128, 512], F32))
        rf = es.enter_context(nc.sbuf_tensor("m1_rf", [64, 512], F32))
        t1 = es.enter_context(nc.sbuf_tensor("m1_t1", [64, 512], F32))
        t2 = es.enter_context(nc.sbuf_tensor("m1_t2", [64, 512], F32))
        kpe = es.enter_context(nc.sbuf_tensor("m1_kpe", [64, 512], F32))
        krot = es.enter_context(nc.sbuf_tensor("m1_krot", [64, 512], F32))
        cs = es.enter_context(nc.sbuf_tensor("m1_cos", [64, 512], F32))
        sn = es.enter_context(nc.sbuf_tensor("m1_sin", [64, 512], F32))
        on = es.enter_context(nc.sbuf_tensor("m1_on", [128, 2, 512], BF16))
        orp = es.enter_context(nc.sbuf_tensor("m1_or", [64, 2, 512], BF16))
        vt = es.enter_context(nc.sbuf_tensor("m1_vt", [128, 2, 512], BF16))
        onesB = consts['onesB']
        cx.op('dve', lambda e: e.memset(sqr[:, :], 0.0), w=["m1_sqr"])
        cx.op('dve', lambda e: e.memset(sqk[:, :], 0.0), w=["m1_sqk"])
        pA, pB, pC, pD = ps[0], ps[1], ps[2], ps[3]

        def rope(src, dst_k, dst_ap):
            cx.op('pe', lambda e: e.matmul(pD[0:64, :], lhsT=Rm[:, :], rhs=src[:, :], start=True, stop=True), r=["m1_Rm", "m1_t1"], w=["ps3"])
            cx.op('dve', lambda e: e.tensor_tensor(out=t2[:, :], in0=pD[0:64, :], in1=sn[:, :], op=ALU.mult), r=["ps3", "m1_sin"], w=["m1_t2"])
            cx.op('dve', lambda e: e.tensor_tensor(out=src[:, :], in0=src[:, :], in1=cs[:, :], op=ALU.mult), r=["m1_t1", "m1_cos"], w=["m1_t1"])
            cx.op('dve', lambda e: e.tensor_tensor(out=dst_ap, in0=src[:, :], in1=t2[:, :], op=ALU.add), r=["m1_t1", "m1_t2"], w=[dst_k])

        for sg in range(cfg.NS):
            for blk in range(4):
                c0 = sg * L + blk * 512
                p0 = seg_pos0(cfg, sg) + blk * 512
                cx.dma('sp', lambda e, c0=c0: e.dma_start(out=hb[:, :, :], in_=hT_d[:, :, c0:c0 + 512].rearrange("k p t -> p k t")), w=["m1_hb"])
                cx.dma('sp', lambda e, p0=p0: e.dma_start(out=cs[:, :], in_=ins["rope_cos"][:, p0:p0 + 512]), w=["m1_cos"])
                cx.dma('sp', lambda e, p0=p0: e.dma_start(out=sn[:, :], in_=ins["rope_sin"][:, p0:p0 + 512]), w=["m1_sin"])
                for (wt, wk, nch, dst, dk) in ((wcq, "m1_wcq", 3, cqn, "m1_cqn"), (wckv, "m1_wckv", 2, ckvn, "m1_ckvn")):
                    for n in range(nch):
                        for k in range(8):
                            cx.op('pe', lambda e, wt=wt, n=n, k=k: e.matmul(pA[:, :], lhsT=wt[:, k, n * 128:(n + 1) * 128], rhs=hb[:, k, :], start=(k == 0), stop=(k == 7)),
                                  r=[wk, "m1_hb"], w=["ps0"])
                        cx.op('act', lambda e, n=n: e.activation(out=cq[:, n, :], in_=pA[:, :], func=AF.Copy), r=["ps0"], w=["m1_cq"])
                        cx.op('dve', lambda e, n=n: e.tensor_tensor(out=sqb[:, n, :], in0=pA[:, :], in1=cq[:, n, :], op=ALU.mult), r=["ps0", "m1_cq"], w=["m1_sqb"])
                    for n in range(nch):
                        cx.op('pe', lambda e, n=n, nch=nch: e.matmul(pB[:, :], lhsT=onesB[:, :], rhs=sqb[:, n, :], start=(n == 0), stop=(n == nch - 1)), r=["onesB", "m1_sqb"], w=["ps1"])
                    rstd_tile(cx, r1[:, :], "m1_r1", pB[:, :], "ps1", nch * 128)
                    for n in range(nch):
                        cx.op('dve', lambda e, n=n, dst=dst: e.tensor_tensor(out=dst[:, n, :], in0=cq[:, n, :], in1=r1[:, :], op=ALU.mult), r=["m1_cq", "m1_r1"], w=[dk])
                for k in range(8):
                    cx.op('pe', lambda e, k=k: e.matmul(pC[0:64, :], lhsT=wkpe[:, k, :], rhs=hb[:, k, :], start=(k == 0), stop=(k == 7)), r=["m1_wkpe", "m1_hb"], w=["ps2"])
                cx.op('act', lambda e: e.activation(out=kpe[:, :], in_=pC[0:64, :], func=AF.Copy), r=["ps2"], w=["m1_kpe"])
                cx.op('dve', lambda e: e.tensor_tensor(out=sqk[0:64, :], in0=pC[0:64, :], in1=kpe[:, :], op=ALU.mult), r=["ps2", "m1_kpe"], w=["m1_sqk"])
                cx.op('dve', lambda e: e.tensor_scalar(t1[:, :], kpe[:, :], gk[0:64, 1:2], None, op0=ALU.mult), r=["m1_kpe", "m1_gk"], w=["m1_t1"])
                rope(t1, "m1_krot", krot[:, :])
                for h in range(4):
                    hb2 = h % 2
                    for n in range(3):
                        cx.op('pe', lambda e, n=n, h=h: e.matmul(pA[:, :], lhsT=wuq[:, n, h * 192:h * 192 + 128], rhs=cqn[:, n, :], start=(n == 0), stop=(n == 2)), r=["m1_wuq", "m1_cqn"], w=["ps0"])
                    for n in range(3):
                        cx.op('pe', lambda e, n=n, h=h: e.matmul(pC[0:64, :], lhsT=wuq[:, n, h * 192 + 128:h * 192 + 192], rhs=cqn[:, n, :], start=(n == 0), stop=(n == 2)), r=["m1_wuq", "m1_cqn"], w=["ps2"])
                    cx.op('act', lambda e: e.activation(out=nf[:, :], in_=pA[:, :], func=AF.Copy), r=["ps0"], w=["m1_nf"])
                    cx.op('dve', lambda e: e.tensor_tensor(out=sqb[:, 0, :], in0=pA[:, :], in1=nf[:, :], op=ALU.mult), r=["ps0", "m1_nf"], w=["m1_sqb"])
                    cx.op('act', lambda e: e.activation(out=rf[:, :], in_=pC[0:64, :], func=AF.Copy), r=["ps2"], w=["m1_rf"])
                    cx.op('dve', lambda e: e.tensor_tensor(out=sqr[0:64, :], in0=pC[0:64, :], in1=rf[:, :], op=ALU.mult), r=["ps2", "m1_rf"], w=["m1_sqr"])
                    cx.op('pe', lambda e: e.matmul(pB[:, :], lhsT=onesB[:, :], rhs=sqb[:, 0, :], start=True, stop=False), r=["onesB", "m1_sqb"], w=["ps1"])
                    cx.op('pe', lambda e: e.matmul(pB[:, :], lhsT=onesB[:, :], rhs=sqr[:, :], start=False, stop=True), r=["onesB", "m1_sqr"], w=["ps1"])
                    rstd_tile(cx, r1[:, :], "m1_r1", pB[:, :], "ps1", 192)
                    cx.op('dve', lambda e, hb2=hb2: e.scalar_tensor_tensor(out=on[:, hb2, :], in0=nf[:, :], scalar=gq[:, 0:1], in1=r1[:, :], op0=ALU.mult, op1=ALU.mult), r=["m1_nf", "m1_gq", "m1_r1"], w=[f"m1_on{hb2}"])
                    cx.dma('sp', lambda e, h=h, hb2=hb2, c0=c0: e.dma_start(out=q_d[h, 0:128, c0:c0 + 512], in_=on[:, hb2, :]), r=[f"m1_on{hb2}"], w=[f"d:q:{sg}"])
                    cx.op('dve', lambda e: e.scalar_tensor_tensor(out=t1[:, :], in0=rf[:, :], scalar=gq[0:64, 1:2], in1=r1[0:64, :], op0=ALU.mult, op1=ALU.mult), r=["m1_rf", "m1_gq", "m1_r1"], w=["m1_t1"])
                    rope(t1, f"m1_or{hb2}", orp[:, hb2, :])
                    cx.dma('sp', lambda e, h=h, hb2=hb2, c0=c0: e.dma_start(out=q_d[h, 128:192, c0:c0 + 512], in_=orp[:, hb2, :]), r=[f"m1_or{hb2}"], w=[f"d:q:{sg}"])
                    for n in range(2):
                        cx.op('pe', lambda e, n=n, h=h: e.matmul(pA[:, :], lhsT=wukv[:, n, h * 256:h * 256 + 128], rhs=ckvn[:, n, :], start=(n == 0), stop=(n == 1)), r=["m1_wukv", "m1_ckvn"], w=["ps0"])
                    cx.op('act', lambda e: e.activation(out=nf[:, :], in_=pA[:, :], func=AF.Copy), r=["ps0"], w=["m1_nf"])
                    cx.op('dve', lambda e: e.tensor_tensor(out=sqb[:, 0, :], in0=pA[:, :], in1=nf[:, :], op=ALU.mult), r=["ps0", "m1_nf"], w=["m1_sqb"])
                    cx.op('pe', lambda e: e.matmul(pB[:, :], lhsT=onesB[:, :], rhs=sqb[:, 0, :], start=True, stop=False), r=["onesB", "m1_sqb"], w=["ps1"])
                    cx.op('pe', lambda e: e.matmul(pB[:, :], lhsT=onesB[:, :], rhs=sqk[:, :], start=False, stop=True), r=["onesB", "m1_sqk"], w=["ps1"])
                    rstd_tile(cx, r1[:, :], "m1_r1", pB[:, :], "ps1", 192)
                    hb3 = 1 - hb2
                    cx.op('dve', lambda e, hb3=hb3: e.scalar_tensor_tensor(out=on[:, hb3, :], in0=nf[:, :], scalar=gk[:, 0:1], in1=r1[:, :], op0=ALU.mult, op1=ALU.mult), r=["m1_nf", "m1_gk", "m1_r1"], w=[f"m1_on{hb3}"])
                    cx.dma('sp', lambda e, h=h, hb3=hb3, c0=c0: e.dma_start(out=k_d[h, 0:128, c0:c0 + 512], in_=on[:, hb3, :]), r=[f"m1_on{hb3}"], w=[f"d:k:{sg}"])
                    cx.op('dve', lambda e, hb3=hb3: e.tensor_tensor(out=orp[:, hb3, :], in0=krot[:, :], in1=r1[0:64, :], op=ALU.mult), r=["m1_krot", "m1_r1"], w=[f"m1_or{hb3}"])
                    cx.dma('sp', lambda e, h=h, hb3=hb3, c0=c0: e.dma_start(out=k_d[h, 128:192, c0:c0 + 512], in_=orp[:, hb3, :]), r=[f"m1_or{hb3}"], w=[f"d:k:{sg}"])
                for j in range(4):
                    jb = j % 2
                    for n in range(2):
                        cx.op('pe', lambda e, n=n, j=j: e.matmul(pA[:, :], lhsT=ckvn[:, n, j * 128:(j + 1) * 128], rhs=wv[:, n, :], start=(n == 0), stop=(n == 1)), r=["m1_ckvn", "m1_wv"], w=["ps0"])
                    cx.op('act', lambda e, jb=jb: e.activation(out=vt[:, jb, :], in_=pA[:, :], func=AF.Copy), r=["ps0"], w=[f"m1_vt{jb}"])
                    cx.dma('sp', lambda e, jb=jb, j=j, c0=c0: e.dma_start(out=v_d[c0 + j * 128:c0 + (j + 1) * 128, :], in_=vt[:, jb, :]), r=[f"m1_vt{jb}"], w=[f"d:v:{sg}"])
            cx.epoch()


def pass_attn(cx, cfg, q_d, k_d, v_d, om_d, consts, ps):
    nc = cx.nc
    with ExitStack() as es:
        qn = es.enter_context(nc.sbuf_tensor("a_qn", [128, 4, L], BF16))
        qr = es.enter_context(nc.sbuf_tensor("a_qr", [128, 4, L], BF16))
        kn = es.enter_context(nc.sbuf_tensor("a_kn", [128, 4, L], BF16))
        kr = es.enter_context(nc.sbuf_tensor("a_kr", [128, 4, L], BF16))
        vv = es.enter_context(nc.sbuf_tensor("a_vv", [128, 16, 512], BF16))
        pT = [es.enter_context(nc.sbuf_tensor(f"a_pT{i}", [128, 512], BF16)) for i in range(2)]
        acc_o = es.enter_context(nc.sbuf_tensor("a_acco", [128, 16, 512], F32)) if cfg.P > 1 else None
        acc_d = es.enter_context(nc.sbuf_tensor("a_accd", [128, 16, 512], F32)) if cfg.P > 1 else None
        rec = es.enter_context(nc.sbuf_tensor("a_rec", [128, 512], F32))
        ot = [es.enter_context(nc.sbuf_tensor(f"a_ot{i}", [128, 512], BF16)) for i in range(2)]
        onesB = consts['onesB']
        for sg in range(cfg.NS):
            ctx = chain_of(cfg, sg)
            if sg == 0:
                cx.op('dve', lambda e: e.memset(qr[64:128, :, :], 0.0), w=["a_qr"])
                cx.op('pool', lambda e: e.memset(kr[64:128, :, :], 0.0), w=["a_kr"])
                cx.epoch()
            for h in range(4):
                cx.dma('sp', lambda e, h=h, sg=sg: e.dma_start(out=qn[:, h, :], in_=q_d[h, 0:128, sg * L:(sg + 1) * L]), w=["a_qn"])
                cx.dma('sp', lambda e, h=h, sg=sg: e.dma_start(out=qr[0:64, h, :], in_=q_d[h, 128:192, sg * L:(sg + 1) * L]), w=["a_qr"])
            for ki, ks in enumerate(ctx):
                for h in range(4):
                    cx.dma('sp', lambda e, h=h, ks=ks: e.dma_start(out=kn[:, h, :], in_=k_d[h, 0:128, ks * L:(ks + 1) * L]), w=["a_kn"])
                    cx.dma('sp', lambda e, h=h, ks=ks: e.dma_start(out=kr[0:64, h, :], in_=k_d[h, 128:192, ks * L:(ks + 1) * L]), w=["a_kr"])
                for j4 in range(4):
                    cx.dma('sp', lambda e, j4=j4, ks=ks: e.dma_start(out=vv[:, j4 * 4:(j4 + 1) * 4, :], in_=v_d[ks * L + j4 * 512:ks * L + (j4 + 1) * 512, :].rearrange("(j p) n -> p j n", p=128)), w=["a_vv"])
                for qb in range(4):
                    for h in range(4):
                        i = qb * 4 + h
                        po, pd = ps[2 + (i % 2)], ps[4 + (i % 2)]
                        pok, pdk = f"ps{2 + (i % 2)}", f"ps{4 + (i % 2)}"
                        for kt in range(16):
                            sb = kt % 2
                            pS = ps[sb]
                            cx.op('pe', lambda e, pS=pS, h=h, kt=kt, qb=qb: e.matmul(pS[:, :], lhsT=kn[:, h, kt * 128:(kt + 1) * 128], rhs=qn[:, h, qb * 512:(qb + 1) * 512], start=True, stop=False),
                                  r=["a_kn", "a_qn"], w=[f"ps{sb}"])
                            cx.op('pe', lambda e, pS=pS, h=h, kt=kt, qb=qb: e.matmul(pS[:, :], lhsT=kr[:, h, kt * 128:(kt + 1) * 128], rhs=qr[:, h, qb * 512:(qb + 1) * 512], start=False, stop=True),
                                  r=["a_kr", "a_qr"], w=[f"ps{sb}"])
                            cx.op('act', lambda e, sb=sb, pS=pS: e.activation(out=pT[sb][:, :], in_=pS[:, :], func=AF.Exp), r=[f"ps{sb}"], w=[f"a_pT{sb}"])
                            cx.op('pe', lambda e, po=po, h=h, kt=kt, sb=sb: e.matmul(po[:, :], lhsT=vv[:, kt, h * 128:(h + 1) * 128], rhs=pT[sb][:, :], start=(kt == 0), stop=(kt == 15)),
                                  r=["a_vv", f"a_pT{sb}"], w=[pok])
                            cx.op('pe', lambda e, pd=pd, sb=sb, kt=kt: e.matmul(pd[:, :], lhsT=onesB[:, :], rhs=pT[sb][:, :], start=(kt == 0), stop=(kt == 15)),
                                  r=["onesB", f"a_pT{sb}"], w=[pdk])
                        last = (ki == len(ctx) - 1)
                        if len(ctx) > 1:
                            if ki == 0:
                                cx.op('act', lambda e, po=po, i=i: e.activation(out=acc_o[:, i, :], in_=po[:, :], func=AF.Copy), r=[pok], w=["a_acco"])
                                cx.op('dve', lambda e, pd=pd, i=i: e.tensor_copy(acc_d[:, i, :], pd[:, :]), r=[pdk], w=["a_accd"])
                            else:
                                cx.op('dve', lambda e, po=po, i=i: e.tensor_tensor(out=acc_o[:, i, :], in0=po[:, :], in1=acc_o[:, i, :], op=ALU.add), r=[pok, "a_acco"], w=["a_acco"])
                                cx.op('dve', lambda e, pd=pd, i=i: e.tensor_tensor(out=acc_d[:, i, :], in0=pd[:, :], in1=acc_d[:, i, :], op=ALU.add), r=[pdk, "a_accd"], w=["a_accd"])
                        if last:
                            ob = i % 2
                            if len(ctx) > 1:
                                cx.op('dve', lambda e, i=i: e.reciprocal(rec[:, :], acc_d[:, i, :]), r=["a_accd"], w=["a_rec"])
                                cx.op('dve', lambda e, i=i, ob=ob: e.tensor_tensor(out=ot[ob][:, :], in0=acc_o[:, i, :], in1=rec[:, :], op=ALU.mult), r=["a_acco", "a_rec"], w=[f"a_ot{ob}"])
                            else:
                                cx.op('dve', lambda e, pd=pd: e.reciprocal(rec[:, :], pd[:, :]), r=[pdk], w=["a_rec"])
                                cx.op('dve', lambda e, po=po, ob=ob: e.tensor_tensor(out=ot[ob][:, :], in0=po[:, :], in1=rec[:, :], op=ALU.mult), r=[pok, "a_rec"], w=[f"a_ot{ob}"])
                            c0 = sg * L + qb * 512
                            cx.dma('sp', lambda e, h=h, ob=ob, c0=c0: e.dma_start(out=om_d[h, :, c0:c0 + 512], in_=ot[ob][:, :]), r=[f"a_ot{ob}"], w=[f"d:om:{sg}"])
                cx.epoch()


class Tail:
    def __init__(s, cx, es, pfx, w_out_src, router_src, gffn_col, stg):
        nc = cx.nc
        s.cx, s.pfx = cx, pfx
        s.wo = load_w(cx, es, f"{pfx}_wo", w_out_src, D, D, stg=stg)
        s.wr = es.enter_context(nc.sbuf_tensor(f"{pfx}_wr", [128, 8, NE], F32))
        cx.dma('sp', lambda e: e.dma_start(out=s.wr[:, :, :], in_=router_src.rearrange("(k p) n -> p k n", p=128)), w=[f"{pfx}_wr"])
        for k in range(8):
            cx.op('dve', lambda e, k=k: e.tensor_scalar(s.wr[:, k, :], s.wr[:, k, :], gffn_col[:, k:k + 1], None, op0=ALU.mult), r=[f"{pfx}_wr"], w=[f"{pfx}_wr"])
        s.xt = [es.enter_context(nc.sbuf_tensor(f"{pfx}_xt{i}", [128, D], F32)) for i in range(2)]
        s.hn = es.enter_context(nc.sbuf_tensor(f"{pfx}_hn", [128, D], F32))
        s.hnb = es.enter_context(nc.sbuf_tensor(f"{pfx}_hnb", [128, D], BF16))
        s.hnT = es.enter_context(nc.sbuf_tensor(f"{pfx}_hnT", [128, 8, 128], F32))
        s.sm = es.enter_context(nc.sbuf_tensor(f"{pfx}_sm", [128, 4], F32))
        s.ee = es.enter_context(nc.sbuf_tensor(f"{pfx}_ee", [128, NE], F32))

    def run(s, mix, mk, c_off, t0, x_src, x1_d, hn_d, aff_sb, aff_d, consts, ps):
        cx, pfx = s.cx, s.pfx
        identF = consts['identF']
        for j in range(4):
            b = j % 2
            xt, xk = s.xt[b], f"{pfx}_xt{b}"
            r0 = t0 + j * 128
            cx.dma('sp', lambda e, xt=xt, r0=r0: e.dma_start(out=xt[:, :], in_=x_src[r0:r0 + 128, :]), w=[xk])
            for dh in range(2):
                po, pk = ps[dh], f"ps{dh}"
                for k in range(8):
                    cx.op('pe', lambda e, po=po, k=k, j=j, dh=dh: e.matmul(po[:, :], lhsT=mix[:, k, c_off + j * 128:c_off + (j + 1) * 128], rhs=s.wo[:, k, dh * 512:(dh + 1) * 512], start=(k == 0), stop=(k == 7)),
                          r=[mk, f"{pfx}_wo"], w=[pk])
                cx.op('dve', lambda e, po=po, dh=dh, xt=xt: e.tensor_tensor(out=xt[:, dh * 512:(dh + 1) * 512], in0=po[:, :], in1=xt[:, dh * 512:(dh + 1) * 512], op=ALU.add), r=[pk, xk], w=[xk])
            cx.dma('sp', lambda e, xt=xt, r0=r0: e.dma_start(out=x1_d[r0:r0 + 128, :], in_=xt[:, :]), r=[xk], w=[f"d:x1:{r0}"])
            cx.op('act', lambda e, xt=xt: e.activation(out=s.hn[:, :], in_=xt[:, :], func=AF.Square, accum_out=s.sm[:, 0:1]), r=[xk], w=[f"{pfx}_hn", f"{pfx}_sm"])
            rstd_from_ssq(cx, (s.sm[:, 1:2], f"{pfx}_sm"), (s.sm[:, 0:1], f"{pfx}_sm"), D, None)
            cx.op('dve', lambda e, xt=xt: e.tensor_scalar(s.hn[:, :], xt[:, :], s.sm[:, 1:2], None, op0=ALU.mult), r=[xk, f"{pfx}_sm"], w=[f"{pfx}_hn"])
            cx.op('act', lambda e: e.activation(out=s.hnb[:, :], in_=s.hn[:, :], func=AF.Copy), r=[f"{pfx}_hn"], w=[f"{pfx}_hnb"])
            cx.dma('sp', lambda e, r0=r0: e.dma_start(out=hn_d[r0:r0 + 128, :], in_=s.hnb[:, :]), r=[f"{pfx}_hnb"], w=[f"d:hn:{r0}"])
            for half in range(2):
                pt, pk = ps[2 + half], f"ps{2 + half}"
                for kk in range(4):
                    k = half * 4 + kk
                    cx.op('pe', lambda e, pt=pt, kk=kk, k=k: e.transpose(pt[:, kk * 128:(kk + 1) * 128], s.hn[:, k * 128:(k + 1) * 128], identF[:, :]), r=[f"{pfx}_hn", "identF"], w=[pk])
                if half == 0:
                    cx.op('act', lambda e, pt=pt: e.activation(out=s.hnT[:, 0:4, :], in_=pt[:, :].rearrange("p (k t) -> p k t", k=4), func=AF.Copy), r=[pk], w=[f"{pfx}_hnT"])
                else:
                    cx.op('dve', lambda e, pt=pt: e.tensor_copy(s.hnT[:, 4:8, :], pt[:, :].rearrange("p (k t) -> p k t", k=4)), r=[pk], w=[f"{pfx}_hnT"])
            pl = ps[4]
            for k in range(8):
                cx.op('pe', lambda e, k=k: e.matmul(pl[:, 0:NE], lhsT=s.hnT[:, k, :], rhs=s.wr[:, k, :], start=(k == 0), stop=(k == 7)), r=[f"{pfx}_hnT", f"{pfx}_wr"], w=["ps4"])
            cx.op('act', lambda e: e.activation(out=s.ee[:, :], in_=pl[:, 0:NE], func=AF.Exp, accum_out=s.sm[:, 2:3]), r=["ps4"], w=[f"{pfx}_ee", f"{pfx}_sm"])
            cx.op('dve', lambda e: e.reciprocal(s.sm[:, 3:4], s.sm[:, 2:3]), r=[f"{pfx}_sm"], w=[f"{pfx}_sm"])
            ti = r0 // 128
            cx.op('dve', lambda e, ti=ti: e.tensor_scalar(aff_sb[:, ti, :], s.ee[:, :], s.sm[:, 3:4], None, op0=ALU.mult), r=[f"{pfx}_ee", f"{pfx}_sm"], w=["aff_sb"])
            cx.dma('sp', lambda e, ti=ti, r0=r0: e.dma_start(out=aff_d[r0:r0 + 128, 0:NE], in_=aff_sb[:, ti, :]), r=["aff_sb"], w=[f"d:aff:{r0}"])


def pass_combine(cx, cfg, x_src, hT_d, o_d, om_d, x1_d, hn_d, aff_sb, aff_d, ins, consts, ps, gmix, gffn):
    nc = cx.nc
    with ExitStack() as es:
        stg = [es.enter_context(nc.sbuf_tensor(f"c_stg{i}", [128, 1024], F32)) for i in range(2)]
        wg = load_w(cx, es, "c_wg", ins["ev_w_in"][:, 1024:1536], D, 512, gcol=gmix, stg=stg)
        tail = Tail(cx, es, "c", ins["ev_w_out"], ins["moe_router"][0], gffn, stg)
        gain = es.enter_context(nc.sbuf_tensor("c_gain", [128, 1], F32))
        cx.dma('sp', lambda e: e.dma_start(out=gain[:, :], in_=ins["hg_out_gain"].rearrange("(p o) -> p o", o=1)), w=["c_gain"])
        hb = es.enter_context(nc.sbuf_tensor("c_hb", [128, 8, 512], BF16))
        of = es.enter_context(nc.sbuf_tensor("c_of", [64, 8, 512], BF16))
        ob = es.enter_context(nc.sbuf_tensor("c_ob", [64, 8, 512], BF16))
        sg_ = es.enter_context(nc.sbuf_tensor("c_sg", [128, 512], F32))
        ohf = es.enter_context(nc.sbuf_tensor("c_ohf", [128, 512], F32))
        sq = es.enter_context(nc.sbuf_tensor("c_sq", [128, 512], BF16))
        r1 = es.enter_context(nc.sbuf_tensor("c_r1", [128, 512], F32))
        mix = es.enter_context(nc.sbuf_tensor("c_mix", [128, 8, 512], BF16))
        identB, J64, onesB = consts['identB'], consts['J64'], consts['onesB']
        for sg in range(cfg.NS):
            if sg < cfg.P:
                cbase, nchain, rbase = sg * 32, cfg.P * 32, 0
            else:
                cbase, nchain, rbase = 0, 32, sg * L
            for blk in range(4):
                t0 = sg * L + blk * 512
                cx.dma('sp', lambda e, t0=t0: e.dma_start(out=hb[:, :, :], in_=hT_d[:, :, t0:t0 + 512].rearrange("k p t -> p k t")), w=["c_hb"])
                cx.dma('sp', lambda e, t0=t0: e.dma_start(out=of[:, :, :], in_=o_d[0][t0:t0 + 512, :].rearrange("(c t) n -> t c n", t=64)), w=["c_of"])
                c_lo = cbase + blk * 8
                rr = rbase + (nchain - 1 - c_lo - 7) * 64
                cx.dma('sp', lambda e, rr=rr: e.dma_start(out=ob[:, :, :], in_=o_d[1][rr:rr + 512, :].rearrange("(c t) n -> t c n", t=64)), w=["c_ob"])
                cx.dma('sp', lambda e, t0=t0: e.dma_start(out=mix[:, 4:8, :], in_=om_d[:, :, t0:t0 + 512].rearrange("h p t -> p h t")), w=["c_mix"])
                for h in range(4):
                    pg, po, pss = ps[0], ps[1], ps[2]
                    for k in range(8):
                        cx.op('pe', lambda e, k=k, h=h: e.matmul(pg[:, :], lhsT=wg[:, k, h * 128:(h + 1) * 128], rhs=hb[:, k, :], start=(k == 0), stop=(k == 7)), r=["c_wg", "c_hb"], w=["ps0"])
                    cx.op('act', lambda e: e.activation(out=sg_[:, :], in_=pg[:, :], func=AF.Silu), r=["ps0"], w=["c_sg"])
                    for i in range(8):
                        cx.op('pe', lambda e, i=i, h=h: e.matmul(po[:, i * 64:(i + 1) * 64], lhsT=of[:, i, h * 128:(h + 1) * 128], rhs=identB[0:64, 0:64], start=True, stop=False), r=["c_of", "identB"], w=["ps1"])
                        cx.op('pe', lambda e, i=i, h=h: e.matmul(po[:, i * 64:(i + 1) * 64], lhsT=ob[:, 7 - i, h * 128:(h + 1) * 128], rhs=J64[:, :], start=False, stop=True), r=["c_ob", "J64"], w=["ps1"])
                    cx.op('act', lambda e: e.activation(out=ohf[:, :], in_=po[:, :], func=AF.Copy), r=["ps1"], w=["c_ohf"])
                    cx.op('dve', lambda e: e.tensor_tensor(out=sq[:, :], in0=po[:, :], in1=ohf[:, :], op=ALU.mult), r=["ps1", "c_ohf"], w=["c_sq"])
                    cx.op('pe', lambda e: e.matmul(pss[:, :], lhsT=onesB[:, :], rhs=sq[:, :], start=True, stop=True), r=["onesB", "c_sq"], w=["ps2"])
                    rstd_tile(cx, r1[:, :], "c_r1", pss[:, :], "ps2", 128)
                    cx.op('dve', lambda e: e.tensor_tensor(out=ohf[:, :], in0=ohf[:, :], in1=r1[:, :], op=ALU.mult), r=["c_ohf", "c_r1"], w=["c_ohf"])
                    cx.op('dve', lambda e, h=h: e.scalar_tensor_tensor(out=mix[:, h, :], in0=ohf[:, :], scalar=gain[:, 0:1], in1=sg_[:, :], op0=ALU.mult, op1=ALU.mult), r=["c_ohf", "c_gain", "c_sg"], w=["c_mix"])
                tail.run(mix, "c_mix", 0, t0, x_src, x1_d, hn_d, aff_sb, aff_d, consts, ps)
            cx.epoch()


def pass_moe(cx, cfg, layer, aff_sb, aff_d, hn_d, xacc_d, pinc_d, ins, consts, ps, gffn):
    nc = cx.nc
    NT = cfg.NT
    groups = [(a * (L // 128), b * (L // 128)) for (a, b) in cfg.groups if b > a]
    caps = [((b - a) * 128) // 8 for (a, b) in groups]
    nst = [c // 128 for c in caps]
    NSL = sum(nst)
    ntmax = max(b - a for (a, b) in groups)
    pfx = f"e{layer}"
    with ExitStack() as es0:
        idx_sb = es0.enter_context(nc.sbuf_tensor(f"{pfx}_idx", [128, NE, NSL], I32))
        with ExitStack() as es:
            thr = es.enter_context(nc.sbuf_tensor(f"{pfx}_thr", [128, NE], F32))
            hi = es.enter_context(nc.sbuf_tensor(f"{pfx}_hi", [128, NE], F32))
            mid = es.enter_context(nc.sbuf_tensor(f"{pfx}_mid", [128, NE], F32))
            sel = es.enter_context(nc.sbuf_tensor(f"{pfx}_sel", [128, NE], F32))
            d1 = es.enter_context(nc.sbuf_tensor(f"{pfx}_d1", [128, NE], F32))
            cntp = es.enter_context(nc.sbuf_tensor(f"{pfx}_cntp", [128, NE], F32))
            cmp_ = es.enter_context(nc.sbuf_tensor(f"{pfx}_cmp", [128, ntmax, NE], BF16))
            tot = es.enter_context(nc.sbuf_tensor(f"{pfx}_tot", [128, NE, ntmax], F32))
            incl = es.enter_context(nc.sbuf_tensor(f"{pfx}_incl", [128, NE, ntmax], F32))
            pin = [es.enter_context(nc.sbuf_tensor(f"{pfx}_pin{i}", [128, 4, 128], F32)) for i in range(2)]
            junk = [es.enter_context(nc.sbuf_tensor(f"{pfx}_junk{i}", [128, max(ntmax, 128)], F32)) for i in range(4)]
            ts = [es.enter_context(nc.sbuf_tensor(f"{pfx}_ts{i}", [128, 8], F32)) for i in range(4)]
            rowi = [es.enter_context(nc.sbuf_tensor(f"{pfx}_rowi{i}", [128, 1], I32)) for i in range(4)]
            prow = [es.enter_context(nc.sbuf_tensor(f"{pfx}_prow{i}", [128, 128], F32)) for i in range(4)]
            onesB, onesF, linc, slotid, ones512 = consts['onesB'], consts['onesF'], consts['linc'], consts['slotid'], consts['ones512']
            soff = 0
            for gi, (tg0, tg1) in enumerate(groups):
                nt = tg1 - tg0
                kk = float(caps[gi])
                cx.op('dve', lambda e: e.memset(thr[:, :], 0.0), w=[f"{pfx}_thr"])
                cx.op('dve', lambda e: e.memset(hi[:, :], 1.0), w=[f"{pfx}_hi"])
                for it in range(30):
                    cx.op('dve', lambda e: e.tensor_tensor(out=mid[:, :], in0=thr[:, :], in1=hi[:, :], op=ALU.add), r=[f"{pfx}_thr", f"{pfx}_hi"], w=[f"{pfx}_mid"])
                    cx.op('dve', lambda e: e.tensor_scalar(mid[:, :], mid[:, :], 0.5, None, op0=ALU.mult), r=[f"{pfx}_mid"], w=[f"{pfx}_mid"])
                    cx.op('dve', lambda e, tg0=tg0, tg1=tg1, nt=nt: e.tensor_tensor(out=cmp_[:, 0:nt, :], in0=aff_sb[:, tg0:tg1, :], in1=mid[:, :].unsqueeze(1).to_broadcast([128, nt, NE]), op=ALU.is_ge),
                          r=["aff_sb", f"{pfx}_mid"], w=[f"{pfx}_cmp"])
                    cx.op('dve', lambda e, nt=nt: e.tensor_reduce(out=cntp[:, :], in_=cmp_[:, 0:nt, :].rearrange("p t e -> p e t"), axis=AX.X, op=ALU.add), r=[f"{pfx}_cmp"], w=[f"{pfx}_cntp"])
                    cx.op('pe', lambda e: e.matmul(ps[0][:, 0:NE], lhsT=onesF[:, :], rhs=cntp[:, :], start=True, stop=True), r=["onesF", f"{pfx}_cntp"], w=["ps0"])
                    cx.op('dve', lambda e, kk=kk: e.tensor_scalar(sel[:, :], ps[0][:, 0:NE], kk, None, op0=ALU.is_ge), r=["ps0"], w=[f"{pfx}_sel"])
                    cx.op('dve', lambda e: e.tensor_tensor(out=d1[:, :], in0=mid[:, :], in1=thr[:, :], op=ALU.subtract), r=[f"{pfx}_mid", f"{pfx}_thr"], w=[f"{pfx}_d1"])
                    cx.op('dve', lambda e: e.tensor_tensor(out=d1[:, :], in0=d1[:, :], in1=sel[:, :], op=ALU.mult), r=[f"{pfx}_d1", f"{pfx}_sel"], w=[f"{pfx}_d1"])
                    cx.op('dve', lambda e: e.tensor_tensor(out=thr[:, :], in0=thr[:, :], in1=d1[:, :], op=ALU.add), r=[f"{pfx}_thr", f"{pfx}_d1"], w=[f"{pfx}_thr"])
                    cx.op('dve', lambda e: e.tensor_tensor(out=d1[:, :], in0=hi[:, :], in1=mid[:, :], op=ALU.subtract), r=[f"{pfx}_hi", f"{pfx}_mid"], w=[f"{pfx}_d1"])
                    cx.op('dve', lambda e: e.tensor_tensor(out=d1[:, :], in0=d1[:, :], in1=sel[:, :], op=ALU.mult), r=[f"{pfx}_d1", f"{pfx}_sel"], w=[f"{pfx}_d1"])
                    cx.op('dve', lambda e: e.tensor_tensor(out=hi[:, :], in0=mid[:, :], in1=d1[:, :], op=ALU.add), r=[f"{pfx}_mid", f"{pfx}_d1"], w=[f"{pfx}_hi"])
                if MSTOP <= 1:
                    cx.epoch(); continue
                cx.op('dve', lambda e, tg0=tg0, tg1=tg1, nt=nt: e.tensor_tensor(out=cmp_[:, 0:nt, :], in0=aff_sb[:, tg0:tg1, :], in1=thr[:, :].unsqueeze(1).to_broadcast([128, nt, NE]), op=ALU.is_ge),
                      r=["aff_sb", f"{pfx}_thr"], w=[f"{pfx}_cmp"])
                for ci, a in enumerate(range(0, nt, 32)):
                    na = min(32, nt - a)
                    ncol = na * NE
                    mb = cmp_[:, a:a + na, :].rearrange("p t e -> p (t e)")
                    cx.op('pe', lambda e, mb=mb, ncol=ncol: e.matmul(ps[1][:, 0:ncol], lhsT=onesB[:, :], rhs=mb, start=True, stop=True), r=["onesB", f"{pfx}_cmp"], w=["ps1"])
                    cx.op('act', lambda e, a=a, na=na, ncol=ncol: e.activation(out=tot[:, :, a:a + na], in_=ps[1][:, 0:ncol].rearrange("p (t e) -> p e t", e=NE), func=AF.Copy), r=["ps1"], w=[f"{pfx}_tot"])
                    pb = ci % 2
                    for i in range(ncol // 128):
                        cx.op('pe', lambda e, mb=mb, i=i, pb=pb: e.matmul(ps[2 + pb][:, i * 128:(i + 1) * 128], lhsT=mb[:, i * 128:(i + 1) * 128], rhs=linc[:, :], start=True, stop=True), r=[f"{pfx}_cmp", "linc"], w=[f"ps{2 + pb}"])
                    ni = ncol // 128
                    cx.op('dve', lambda e, pb=pb, ni=ni: e.tensor_copy(pin[pb][:, 0:ni, :], ps[2 + pb][:, 0:ni * 128].rearrange("p (i t) -> p i t", t=128)), r=[f"ps{2 + pb}"], w=[f"{pfx}_pin{pb}"])
                    rb = (tg0 + a) * NE
                    cx.dma('sp', lambda e, pb=pb, ni=ni, rb=rb: e.dma_start(out=pinc_d[rb:rb + ni * 128, :].rearrange("(i p) t -> p i t", p=128), in_=pin[pb][:, 0:ni, :]), r=[f"{pfx}_pin{pb}"], w=["d:pinc"])
                for ex in range(NE):
                    cx.op('dve', lambda e, ex=ex, nt=nt: e.tensor_tensor_scan(out=incl[:, ex, 0:nt], data0=ones512[:, 0:nt], data1=tot[:, ex, 0:nt], initial=0.0, op0=ALU.mult, op1=ALU.add),
                          r=[f"{pfx}_tot", "ones512"], w=[f"{pfx}_incl"])
                if MSTOP <= 2:
                    cx.epoch(); continue
                n_ = 0
                for ex in range(NE):
                    for J in range(nst[gi]):
                        b = n_ % 4
                        n_ += 1
                        t_, tk = ts[b], f"{pfx}_ts{b}"
                        jv = slotid[:, J:J + 1]
                        cx.op('dve', lambda e, ex=ex, nt=nt, b=b, t_=t_, jv=jv: e.tensor_scalar(junk[b][:, 0:nt], incl[:, ex, 0:nt], jv, None, op0=ALU.is_le, op1=ALU.add, accum_out=t_[:, 0:1]),
                              r=[f"{pfx}_incl", "slotid"], w=[f"{pfx}_junk{b}", tk])
                        cx.op('dve', lambda e, ex=ex, nt=nt, b=b, t_=t_, jv=jv: e.scalar_tensor_tensor(out=junk[b][:, 0:nt], in0=incl[:, ex, 0:nt], scalar=jv, in1=tot[:, ex, 0:nt], op0=ALU.is_le, op1=ALU.mult, accum_out=t_[:, 1:2]),
                              r=[f"{pfx}_incl", f"{pfx}_tot", "slotid"], w=[f"{pfx}_junk{b}", tk])
                        cx.op('dve', lambda e, t_=t_, jv=jv: e.tensor_tensor(out=t_[:, 2:3], in0=jv, in1=t_[:, 1:2], op=ALU.subtract), r=[tk, "slotid"], w=[tk])
                        cx.op('dve', lambda e, t_=t_, b=b, ex=ex, tg0=tg0: e.tensor_scalar(rowi[b][:, :], t_[:, 0:1], float(NE), float(ex + tg0 * NE), op0=ALU.mult, op1=ALU.add), r=[tk], w=[f"{pfx}_rowi{b}"])
                        cx.dma('pool', lambda e, b=b: e.indirect_dma_start(out=prow[b][:, :], out_offset=None, in_=pinc_d[:, :], in_offset=bass.IndirectOffsetOnAxis(ap=rowi[b][:, 0:1], axis=0), bounds_check=NT * NE - 1, oob_is_err=False),
                               r=[f"{pfx}_rowi{b}", "d:pinc"], w=[f"{pfx}_prow{b}"])
                        cx.op('dve', lambda e, b=b, t_=t_: e.tensor_scalar(junk[b][:, 0:128], prow[b][:, :], t_[:, 2:3], None, op0=ALU.is_le, op1=ALU.add, accum_out=t_[:, 3:4]),
                              r=[f"{pfx}_prow{b}", tk], w=[f"{pfx}_junk{b}", tk])
                        cx.op('dve', lambda e, t_=t_, tg0=tg0: e.tensor_scalar(t_[:, 4:5], t_[:, 0:1], 128.0, float(tg0 * 128), op0=ALU.mult, op1=ALU.add), r=[tk], w=[tk])
                        cx.op('dve', lambda e, t_=t_, ex=ex, J=J, soff=soff: e.tensor_tensor(out=idx_sb[:, ex, soff + J:soff + J + 1], in0=t_[:, 4:5], in1=t_[:, 3:4], op=ALU.add), r=[tk], w=[f"{pfx}_idx"])
                soff += nst[gi]
                cx.epoch()
        if consts.get('idx_dbg') is not None and layer == 0:
            cx.dma('sp', lambda e: e.dma_start(out=consts['idx_dbg'][:, :], in_=idx_sb[:, :, :].rearrange("p e s -> p (e s)")), r=[f"{pfx}_idx"], w=["d:idxdbg"])
            cx.epoch()
        if MSTOP <= 3:
            return
        with ExitStack() as es:
            stg = [es.enter_context(nc.sbuf_tensor(f"{pfx}_stg{i}", [128, 1024], F32)) for i in range(2)]
            xe = [es.enter_context(nc.sbuf_tensor(f"{pfx}_xe{i}", [128, D], BF16)) for i in range(4)]
            ga = [es.enter_context(nc.sbuf_tensor(f"{pfx}_ga{i}", [128, 128], F32)) for i in range(4)]
            xeT = es.enter_context(nc.sbuf_tensor(f"{pfx}_xeT", [128, 8, 512], BF16))
            hid = es.enter_context(nc.sbuf_tensor(f"{pfx}_hid", [128, 8, 512], BF16))
            sl = es.enter_context(nc.sbuf_tensor(f"{pfx}_sl", [128, 512], F32))
            ye = [es.enter_context(nc.sbuf_tensor(f"{pfx}_ye{i}", [128, D], F32)) for i in range(2)]
            identB = consts['identB']
            psb = consts['psb']
            blocks = [list(range(a, min(a + 4, NSL))) for a in range(0, NSL, 4)]
            for ex in range(NEXP):
                with ExitStack() as esw:
                    wg = load_w(cx, esw, f"{pfx}_wg{ex}", ins["moe_w_gate"][layer, ex], D, D, gcol=gffn, stg=stg)
                    wu = load_w(cx, esw, f"{pfx}_wu{ex}", ins["moe_w_up"][layer, ex], D, D, gcol=gffn, stg=stg)
                    wd = load_w(cx, esw, f"{pfx}_wd{ex}", ins["moe_w_down"][layer, ex], D, D, stg=stg)
                    for blk in blocks:
                        N = len(blk) * 128
                        for si, sidx in enumerate(blk):
                            b = si % 4
                            ix = idx_sb[:, ex, sidx:sidx + 1]
                            cx.dma('pool', lambda e, b=b, ix=ix: e.indirect_dma_start(out=xe[b][:, :], out_offset=None, in_=hn_d[:, :], in_offset=bass.IndirectOffsetOnAxis(ap=ix, axis=0), bounds_check=cfg.T - 1, oob_is_err=False),
                                   r=[f"{pfx}_idx", "d:hn"], w=[f"{pfx}_xe{b}"])
                            cx.dma('pool', lambda e, b=b, ix=ix: e.indirect_dma_start(out=ga[b][:, :], out_offset=None, in_=aff_d[:, :], in_offset=bass.IndirectOffsetOnAxis(ap=ix, axis=0), bounds_check=cfg.T - 1, oob_is_err=False),
                                   r=[f"{pfx}_idx", "d:aff"], w=[f"{pfx}_ga{b}"])
                            for half in range(2):
                                pt, pk = psb[half], f"psb{half}"
                                for kq in range(4):
                                    k = half * 4 + kq
                                    cx.op('pe', lambda e, pt=pt, kq=kq, k=k, b=b: e.transpose(pt[:, kq * 128:(kq + 1) * 128], xe[b][:, k * 128:(k + 1) * 128], identB[:, :]), r=[f"{pfx}_xe{b}", "identB"], w=[pk])
                                eng = 'act' if half == 0 else 'dve'
                                if half == 0:
                                    cx.op('act', lambda e, pt=pt, si=si: e.activation(out=xeT[:, 0:4, si * 128:(si + 1) * 128], in_=pt[:, :].rearrange("p (k t) -> p k t", k=4), func=AF.Copy), r=[pk], w=[f"{pfx}_xeT"])
                                else:
                                    cx.op('dve', lambda e, pt=pt, si=si: e.tensor_copy(xeT[:, 4:8, si * 128:(si + 1) * 128], pt[:, :].rearrange("p (k t) -> p k t", k=4)), r=[pk], w=[f"{pfx}_xeT"])
                        if FSTOP <= 1:
                            continue
                        for f in range(8):
                            pg, pu = ps[0], ps[1]
                            for k in range(8):
                                cx.op('pe', lambda e, k=k, f=f, N=N: e.matmul(pg[:, 0:N], lhsT=wg[:, k, f * 128:(f + 1) * 128], rhs=xeT[:, k, 0:N], start=(k == 0), stop=(k == 7)), r=[f"{pfx}_wg{ex}", f"{pfx}_xeT"], w=["ps0"])
                            for k in range(8):
                                cx.op('pe', lambda e, k=k, f=f, N=N: e.matmul(pu[:, 0:N], lhsT=wu[:, k, f * 128:(f + 1) * 128], rhs=xeT[:, k, 0:N], start=(k == 0), stop=(k == 7)), r=[f"{pfx}_wu{ex}", f"{pfx}_xeT"], w=["ps1"])
                            cx.op('act', lambda e, N=N: e.activation(out=sl[:, 0:N], in_=pg[:, 0:N], func=AF.Silu), r=["ps0"], w=[f"{pfx}_sl"])
                            cx.op('dve', lambda e, f=f, N=N: e.tensor_tensor(out=hid[:, f, 0:N], in0=pu[:, 0:N], in1=sl[:, 0:N], op=ALU.mult), r=["ps1", f"{pfx}_sl"], w=[f"{pfx}_hid"])
                        for si, sidx in enumerate(blk):
                            b = si % 4
                            yb = si % 2
                            for dh in range(2):
                                pd, pk = ps[2 + dh], f"ps{2 + dh}"
                                for f in range(8):
                                    cx.op('pe', lambda e, pd=pd, f=f, si=si, dh=dh: e.matmul(pd[:, :], lhsT=hid[:, f, si * 128:(si + 1) * 128], rhs=wd[:, f, dh * 512:(dh + 1) * 512], start=(f == 0), stop=(f == 7)), r=[f"{pfx}_hid", f"{pfx}_wd{ex}"], w=[pk])
                                if dh == 0:
                                    cx.op('act', lambda e, pd=pd, yb=yb, b=b: e.activation(out=ye[yb][:, 0:512], in_=pd[:, :], func=AF.Copy, scale=ga[b][:, ex:ex + 1]), r=[pk, f"{pfx}_ga{b}"], w=[f"{pfx}_ye{yb}"])
                                else:
                                    cx.op('dve', lambda e, pd=pd, yb=yb, b=b: e.tensor_scalar(ye[yb][:, 512:1024], pd[:, :], ga[b][:, ex:ex + 1], None, op0=ALU.mult), r=[pk, f"{pfx}_ga{b}"], w=[f"{pfx}_ye{yb}"])
                            ix = idx_sb[:, ex, sidx:sidx + 1]
                            if FSTOP <= 3:
                                continue
                            cx.dma('pool', lambda e, yb=yb, ix=ix: e.indirect_dma_start(out=xacc_d[:, :], out_offset=bass.IndirectOffsetOnAxis(ap=ix, axis=0), in_=ye[yb][:, :], in_offset=None, bounds_check=cfg.T - 1, oob_is_err=False, compute_op=ALU.add),
                                   r=[f"{pfx}_ye{yb}", f"{pfx}_idx", "d:xacc"], w=["d:xacc"])
                    cx.epoch()


def pass_r1(cx, cfg, hT_d, gl_d, xb_d, ins, consts, ps, gmix):
    nc = cx.nc
    with ExitStack() as es:
        stg = [es.enter_context(nc.sbuf_tensor(f"r1_stg{i}", [128, 1024], F32)) for i in range(2)]
        w = load_w(cx, es, "r1_w", ins["od_w_in"], D, 2048, gcol=gmix, stg=stg)
        hb = es.enter_context(nc.sbuf_tensor("r1_hb", [128, 8, 512], BF16))
        gl = es.enter_context(nc.sbuf_tensor("r1_gl", [128, 8, 512], BF16))
        xb = es.enter_context(nc.sbuf_tensor("r1_xb", [128, 8, 512], F32))
        for sg in range(cfg.NS):
            for blk in range(4):
                t0 = sg * L + blk * 512
                cx.dma('sp', lambda e, t0=t0: e.dma_start(out=hb[:, :, :], in_=hT_d[:, :, t0:t0 + 512].rearrange("k p t -> p k t")), w=["r1_hb"])
                for n in range(16):
                    pp, pk = ps[n % 2], f"ps{n % 2}"
                    for k in range(8):
                        cx.op('pe', lambda e, pp=pp, n=n, k=k: e.matmul(pp[:, :], lhsT=w[:, k, n * 128:(n + 1) * 128], rhs=hb[:, k, :], start=(k == 0), stop=(k == 7)), r=["r1_w", "r1_hb"], w=[pk])
                    if n < 8:
                        cx.op('act', lambda e, pp=pp, n=n: e.activation(out=gl[:, n, :], in_=pp[:, :], func=AF.Gelu), r=[pk], w=["r1_gl"])
                    else:
                        cx.op('dve', lambda e, pp=pp, n=n: e.tensor_copy(xb[:, n - 8, :], pp[:, :]), r=[pk], w=["r1_xb"])
                cx.dma('sp', lambda e, t0=t0: e.dma_start(out=gl_d[:, :, t0:t0 + 512].rearrange("k p t -> p k t"), in_=gl[:, :, :]), r=["r1_gl"], w=[f"d:gl:{sg}"])
                for hx in range(2):
                    cx.dma('sp', lambda e, t0=t0, hx=hx: e.dma_start(out=xb_d[hx][:, :, t0:t0 + 512].rearrange("k p t -> p k t"), in_=xb[:, hx * 4:(hx + 1) * 4, :]), r=["r1_xb"], w=[f"d:xb:{sg}:{hx}"])
            cx.epoch()


def pass_r2(cx, cfg, dirn, xb_d, gl_d, hf_d, x_src, x3_d, hn_d, aff_sb, aff_d, ins, consts, ps, gffn):
    nc = cx.nc
    pf = f"r2{dirn}"
    with ExitStack() as es:
        stg = [es.enter_context(nc.sbuf_tensor(f"{pf}_stg{i}", [128, 1024], F32)) for i in range(2)]
        wa = load_w(cx, es, f"{pf}_wa", ins["rg_w_a"][dirn].rearrange("n k j -> (n k) j"), D, 256, stg=stg)
        wx = load_w(cx, es, f"{pf}_wx", ins["rg_w_x"][dirn].rearrange("n k j -> (n k) j"), D, 256, stg=stg)
        ba = load_cols(cx, es, f"{pf}_ba", ins["rg_b_a"][dirn, :], D)
        bx = load_cols(cx, es, f"{pf}_bx", ins["rg_b_x"][dirn, :], D)
        c8 = load_cols(cx, es, f"{pf}_c8", ins["rg_lambda"][dirn, :], D)
        cx.op('act', lambda e: e.activation(out=c8[:, :], in_=c8[:, :], func=AF.Exp, scale=-1.0), r=[f"{pf}_c8"], w=[f"{pf}_c8"])
        cx.op('act', lambda e: e.activation(out=c8[:, :], in_=c8[:, :], func=AF.Ln, bias=1.0), r=[f"{pf}_c8"], w=[f"{pf}_c8"])
        cx.op('dve', lambda e: e.tensor_scalar(c8[:, :], c8[:, :], -8.0, None, op0=ALU.mult), r=[f"{pf}_c8"], w=[f"{pf}_c8"])
        cw = [load_cols(cx, es, f"{pf}_cw{j}", ins["od_conv_w"][j, :], D) for j in range(4)]
        cb = load_cols(cx, es, f"{pf}_cb", ins["od_conv_b"], D)
        tail = Tail(cx, es, pf, ins["od_w_out"], ins["moe_router"][1], gffn, stg) if dirn else None
        xp = [es.enter_context(nc.sbuf_tensor(f"{pf}_xp0", [128, L + 4], F32))] * 2
        xcb = es.enter_context(nc.sbuf_tensor(f"{pf}_xcb", [128, 8, L], BF16))
        rr = es.enter_context(nc.sbuf_tensor(f"{pf}_r", [128, L], F32))
        ii = es.enter_context(nc.sbuf_tensor(f"{pf}_i", [128, L], F32))
        tt = es.enter_context(nc.sbuf_tensor(f"{pf}_t", [128, L], F32))
        acc = tt
        hh = tt
        carry = es.enter_context(nc.sbuf_tensor(f"{pf}_carry", [128, 8], F32))
        hb16 = [es.enter_context(nc.sbuf_tensor(f"{pf}_hb0", [128, L], BF16))] * 2
        glt = [es.enter_context(nc.sbuf_tensor(f"{pf}_gl0", [128, L], BF16))] * 2 if dirn else None
        yT = es.enter_context(nc.sbuf_tensor(f"{pf}_yT", [128, 8, L], BF16)) if dirn else None
        segs = list(range(cfg.NS))
        if dirn:
            segs = list(reversed(range(cfg.P))) + list(range(cfg.P, cfg.NS))
        for sg in segs:
            inchain = sg < cfg.P
            has_l = inchain and sg > 0
            has_r = inchain and sg < cfg.P - 1
            first = (not inchain) or (sg == (cfg.P - 1 if dirn else 0))
            t0 = sg * L
            if first:
                cx.op('dve', lambda e: e.memset(carry[:, :], 0.0), w=[f"{pf}_carry"])
            for ch in range(8):
                b = ch % 2
                xk = f"{pf}_xp0"
                cx.op('pool', lambda e, b=b: e.memset(xp[b][:, 0:2], 0.0), w=[xk])
                cx.op('pool', lambda e, b=b: e.memset(xp[b][:, L + 2:L + 4], 0.0), w=[xk])
                lo = t0 - 2 if has_l else t0
                hi = t0 + L + 1 if has_r else t0 + L
                c_lo = 0 if has_l else 2
                cx.dma('sp', lambda e, b=b, ch=ch, lo=lo, hi=hi, c_lo=c_lo: e.dma_start(out=xp[b][:, c_lo:c_lo + (hi - lo)], in_=xb_d[ch // 4][ch % 4, :, lo:hi]), w=[xk])
                cx.op('dve', lambda e, b=b, ch=ch: e.tensor_scalar(acc[:, :], xp[b][:, 0:L], cw[0][:, ch:ch + 1], cb[:, ch:ch + 1], op0=ALU.mult, op1=ALU.add), r=[xk, f"{pf}_cw0", f"{pf}_cb"], w=[f"{pf}_t"])
                for j in (1, 2):
                    cx.op('dve', lambda e, b=b, ch=ch, j=j: e.scalar_tensor_tensor(out=acc[:, :], in0=xp[b][:, j:j + L], scalar=cw[j][:, ch:ch + 1], in1=acc[:, :], op0=ALU.mult, op1=ALU.add), r=[xk, f"{pf}_cw{j}", f"{pf}_t"], w=[f"{pf}_t"])
                cx.op('dve', lambda e, b=b, ch=ch: e.scalar_tensor_tensor(out=xcb[:, ch, :], in0=xp[b][:, 3:3 + L], scalar=cw[3][:, ch:ch + 1], in1=acc[:, :], op0=ALU.mult, op1=ALU.add), r=[xk, f"{pf}_cw3", f"{pf}_t"], w=[f"{pf}_xcb"])
            for co in range(8):
                n, jh = co // 2, co % 2
                b = co % 2
                for blk in range(4):
                    pa, px = ps[(blk % 2) * 2], ps[(blk % 2) * 2 + 1]
                    pak, pxk = f"ps{(blk % 2) * 2}", f"ps{(blk % 2) * 2 + 1}"
                    for kc in range(2):
                        cx.op('pe', lambda e, pa=pa, kc=kc, n=n, jh=jh, blk=blk: e.matmul(pa[:, :], lhsT=wa[:, n * 2 + kc, jh * 128:(jh + 1) * 128], rhs=xcb[:, n * 2 + kc, blk * 512:(blk + 1) * 512], start=(kc == 0), stop=(kc == 1)), r=[f"{pf}_wa", f"{pf}_xcb"], w=[pak])
                    for kc in range(2):
                        cx.op('pe', lambda e, px=px, kc=kc, n=n, jh=jh, blk=blk: e.matmul(px[:, :], lhsT=wx[:, n * 2 + kc, jh * 128:(jh + 1) * 128], rhs=xcb[:, n * 2 + kc, blk * 512:(blk + 1) * 512], start=(kc == 0), stop=(kc == 1)), r=[f"{pf}_wx", f"{pf}_xcb"], w=[pxk])
                    cx.op('act', lambda e, pa=pa, blk=blk, co=co: e.activation(out=rr[:, blk * 512:(blk + 1) * 512], in_=pa[:, :], func=AF.Sigmoid, bias=ba[:, co:co + 1]), r=[pak, f"{pf}_ba"], w=[f"{pf}_r"])
                    cx.op('act', lambda e, px=px, blk=blk, co=co: e.activation(out=ii[:, blk * 512:(blk + 1) * 512], in_=px[:, :], func=AF.Sigmoid, bias=bx[:, co:co + 1]), r=[pxk, f"{pf}_bx"], w=[f"{pf}_i"])
                cx.op('act', lambda e, co=co: e.activation(out=rr[:, :], in_=rr[:, :], func=AF.Exp, scale=c8[:, co:co + 1]), r=[f"{pf}_r", f"{pf}_c8"], w=[f"{pf}_r"])
                cx.op('dve', lambda e: e.tensor_tensor(out=tt[:, :], in0=rr[:, :], in1=rr[:, :], op=ALU.mult), r=[f"{pf}_r"], w=[f"{pf}_t"])
                cx.op('dve', lambda e: e.tensor_scalar(tt[:, :], tt[:, :], -1.0, 1.0, op0=ALU.mult, op1=ALU.add), r=[f"{pf}_t"], w=[f"{pf}_t"])
                cx.op('act', lambda e: e.activation(out=tt[:, :], in_=tt[:, :], func=AF.Sqrt), r=[f"{pf}_t"], w=[f"{pf}_t"])
                cx.op('dve', lambda e: e.tensor_tensor(out=ii[:, :], in0=tt[:, :], in1=ii[:, :], op=ALU.mult), r=[f"{pf}_t", f"{pf}_i"], w=[f"{pf}_i"])
                cx.op('dve', lambda e, co=co: e.tensor_tensor(out=ii[:, :], in0=ii[:, :], in1=xcb[:, co, :], op=ALU.mult), r=[f"{pf}_i", f"{pf}_xcb"], w=[f"{pf}_i"])
                if dirn == 0:
                    cx.op('dve', lambda e, co=co: e.tensor_tensor_scan(out=hh[:, :], data0=rr[:, :], data1=ii[:, :], initial=carry[:, co:co + 1], op0=ALU.mult, op1=ALU.add), r=[f"{pf}_r", f"{pf}_i", f"{pf}_carry"], w=[f"{pf}_t"])
                    cx.op('dve', lambda e, co=co: e.tensor_copy(carry[:, co:co + 1], hh[:, L - 1:L]), r=[f"{pf}_t"], w=[f"{pf}_carry"])
                    cx.op('act', lambda e, b=b: e.activation(out=hb16[b][:, :], in_=hh[:, :], func=AF.Copy), r=[f"{pf}_t"], w=[f"{pf}_hb0"])
                    cx.dma('sp', lambda e, b=b, co=co, t0=t0: e.dma_start(out=hf_d[co, :, t0:t0 + L], in_=hb16[b][:, :]), r=[f"{pf}_hb0"], w=[f"d:hf:{sg}"])
                else:
                    cx.op('dve', lambda e, co=co: e.tensor_tensor_scan(out=hh[:, ::-1], data0=rr[:, ::-1], data1=ii[:, ::-1], initial=carry[:, co:co + 1], op0=ALU.mult, op1=ALU.add), r=[f"{pf}_r", f"{pf}_i", f"{pf}_carry"], w=[f"{pf}_t"])
                    cx.op('dve', lambda e, co=co: e.tensor_copy(carry[:, co:co + 1], hh[:, 0:1]), r=[f"{pf}_t"], w=[f"{pf}_carry"])
                    cx.dma('sp', lambda e, b=b, co=co, t0=t0: e.dma_start(out=hb16[b][:, :], in_=hf_d[co, :, t0:t0 + L]), w=[f"{pf}_hb0"])
                    cx.dma('sp', lambda e, b=b, co=co, t0=t0: e.dma_start(out=glt[b][:, :], in_=gl_d[co, :, t0:t0 + L]), w=[f"{pf}_gl0"])
                    cx.op('dve', lambda e, b=b: e.tensor_tensor(out=hh[:, :], in0=hh[:, :], in1=hb16[b][:, :], op=ALU.add), r=[f"{pf}_t", f"{pf}_hb0"], w=[f"{pf}_t"])
                    cx.op('dve', lambda e, b=b, co=co: e.tensor_tensor(out=yT[:, co, :], in0=hh[:, :], in1=glt[b][:, :], op=ALU.mult), r=[f"{pf}_t", f"{pf}_gl0"], w=[f"{pf}_yT"])
            if dirn:
                for blk in range(4):
                    tail.run(yT, f"{pf}_yT", blk * 512, t0 + blk * 512, x_src, x3_d, hn_d, aff_sb, aff_d, consts, ps)
            cx.epoch()


def make_consts(cx, es):
    nc = cx.nc
    c = {}
    iot = es.enter_context(nc.sbuf_tensor("c_iot", [128, 128], F32))
    cx.op('pool', lambda e: e.iota(iot[:, :], [[1, 128]], base=0, channel_multiplier=-1, allow_small_or_imprecise_dtypes=True), w=["c_iot"])
    identF = es.enter_context(nc.sbuf_tensor("identF", [128, 128], F32))
    identB = es.enter_context(nc.sbuf_tensor("identB", [128, 128], BF16))
    cx.op('dve', lambda e: e.tensor_scalar(identF[:, :], iot[:, :], 0.0, None, op0=ALU.is_equal), r=["c_iot"], w=["identF"])
    cx.op('dve', lambda e: e.tensor_copy(identB[:, :], identF[:, :]), r=["identF"], w=["identB"])
    iot2 = es.enter_context(nc.sbuf_tensor("c_iot2", [128, 128], F32))
    cx.op('pool', lambda e: e.iota(iot2[:, :], [[1, 128]], base=0, channel_multiplier=1, allow_small_or_imprecise_dtypes=True), w=["c_iot2"])
    J64 = es.enter_context(nc.sbuf_tensor("J64", [64, 64], BF16))
    cx.op('dve', lambda e: e.tensor_scalar(J64[:, :], iot2[0:64, 0:64], 63.0, None, op0=ALU.is_equal), r=["c_iot2"], w=["J64"])
    cmask = es.enter_context(nc.sbuf_tensor("cmask", [64, 64], F32))
    cx.op('dve', lambda e: e.tensor_scalar(cmask[:, :], iot[0:64, 0:64], 0.0, None, op0=ALU.is_ge), r=["c_iot"], w=["cmask"])
    ltri = es.enter_context(nc.sbuf_tensor("ltri", [128, 128], BF16))
    cx.op('dve', lambda e: e.tensor_scalar(ltri[:, :], iot[:, :], 0.0, None, op0=ALU.is_gt), r=["c_iot"], w=["ltri"])
    onesB = es.enter_context(nc.sbuf_tensor("onesB", [128, 128], BF16))
    cx.op('dve', lambda e: e.memset(onesB[:, :], 1.0), w=["onesB"])
    onesF = es.enter_context(nc.sbuf_tensor("onesF", [128, 128], F32))
    cx.op('dve', lambda e: e.memset(onesF[:, :], 1.0), w=["onesF"])
    smask = es.enter_context(nc.sbuf_tensor("smask", [128, 1024], F32))
    cx.op('dve', lambda e: e.memset(smask[:, :], 1.0), w=["smask"])
    cx.op('dve', lambda e: e.memset(smask[:, :].rearrange("p (c t) -> p c t", t=64)[:, :, 0:1], 0.0), w=["smask"])
    linc = es.enter_context(nc.sbuf_tensor("linc", [128, 128], BF16))
    cx.op('dve', lambda e: e.tensor_scalar(linc[:, :], iot[:, :], 0.0, None, op0=ALU.is_ge), r=["c_iot"], w=["linc"])
    ones512 = es.enter_context(nc.sbuf_tensor("ones512", [128, 512], F32))
    cx.op('dve', lambda e: e.memset(ones512[:, :], 1.0), w=["ones512"])
    slotid = es.enter_context(nc.sbuf_tensor("slotid", [128, 64], F32))
    cx.op('pool', lambda e: e.iota(slotid[:, :], [[128, 64]], base=0, channel_multiplier=1, allow_small_or_imprecise_dtypes=True), w=["slotid"])
    c.update(linc=linc, ones512=ones512, slotid=slotid)
    c.update(identF=identF, identB=identB, J64=J64, cmask=cmask, ltri=ltri, onesB=onesB, onesF=onesF, smask=smask, iot=iot)
    return c


def build(P, B, debug=False, stages=99):
    cfg = Cfg(P, B)
    T = cfg.T
    nc = bass.Bass("TRN2", target_bir_lowering=False)
    dt = lambda name, shape, dtype=F32, kind="ExternalInput": nc.dram_tensor(name, list(shape), dtype, kind=kind).ap()
    x_in = dt("x_in", [T, D])
    ins = {}
    for name, shape in [("norm_mix", [2, D]), ("norm_ffn", [2, D]), ("ev_w_in", [D, 3264]), ("ev_w_out", [D, D]),
                        ("hg_lb_logits", [2, 2, 512]), ("hg_out_gain", [128]), ("mla_q_norm", [384]), ("mla_kv_norm", [256]),
                        ("mla_w_uq", [384, 768]), ("mla_w_ukv", [256, 1024]), ("mla_q_gain", [192]), ("mla_k_gain", [192]),
                        ("od_w_in", [D, 2048]), ("od_conv_w", [4, D]), ("od_conv_b", [D]), ("rg_w_a", [2, 4, 256, 256]),
                        ("rg_b_a", [2, D]), ("rg_w_x", [2, 4, 256, 256]), ("rg_b_x", [2, D]), ("rg_lambda", [2, D]),
                        ("od_w_out", [D, D]), ("moe_router", [2, D, NE]), ("moe_w_gate", [2, NE, D, D]),
                        ("moe_w_up", [2, NE, D, D]), ("moe_w_down", [2, NE, D, D]), ("rope_cos", [64, P * L if P else L]),
                        ("rope_sin", [64, P * L if P else L])]:
        ins[name] = dt(name, shape)
    y_out = dt("y_out", [T, D], kind="ExternalOutput")
    hT_d = dt("hT_d", [8, 128, T], BF16, kind=("ExternalOutput" if debug else "Internal"))
    o_d = [dt(f"o_d{i}", [T, 512], BF16, kind=("ExternalOutput" if debug else "Internal")) for i in range(2)]
    dbg = "ExternalOutput" if debug else "Internal"
    q_d = dt("q_d", [4, 192, T], BF16, kind="Internal")
    k_d = dt("k_d", [4, 192, T], BF16, kind="Internal")
    v_d = dt("v_d", [T, 512], BF16, kind="Internal")
    om_d = dt("om_d", [4, 128, T], BF16, kind=dbg)
    x1_d = y_out
    hn_d = dt("hn_d", [T, D], BF16, kind="Internal")
    aff_d = dt("aff_d", [T, 128], F32, kind=dbg)
    pinc_d = dt("pinc_d", [cfg.NT * NE, 128], F32, kind="Internal")
    gl_d = dt("gl_d", [8, 128, T], BF16, kind="Internal")
    xb_d = [dt(f"xb_d{i}", [4, 128, T], F32, kind="Internal") for i in range(2)]
    hf_d = dt("hf_d", [8, 128, T], BF16, kind="Internal")
    nsl_dbg = sum(((b - a) * L // 8) // 128 for (a, b) in cfg.groups if b > a)
    idx_dbg = dt("idx_dbg", [128, NE * nsl_dbg], I32, kind="ExternalOutput") if debug else None
    with ExitStack() as es:
        cx = Cx(nc, es)
        ps = [es.enter_context(nc.psum_tensor(f"ps{i}", [128, 512], F32)) for i in range(6)]
        psb = [es.enter_context(nc.psum_tensor(f"psb{i}", [128, 512], BF16)) for i in range(2)]
        consts = make_consts(cx, es)
        consts['psb'] = psb
        consts['idx_dbg'] = idx_dbg
        gmix = [load_cols(cx, es, f"gmix{l}", ins["norm_mix"][l, :], D) for l in range(2)]
        gffn = [load_cols(cx, es, f"gffn{l}", ins["norm_ffn"][l, :], D) for l in range(2)]
        lbT = []
        for d_ in range(2):
            l0 = load_cols(cx, es, f"lbl0_{d_}", ins["hg_lb_logits"][d_, 0, :], 512)
            l1 = load_cols(cx, es, f"lbl1_{d_}", ins["hg_lb_logits"][d_, 1, :], 512)
            lb = es.enter_context(nc.sbuf_tensor(f"lb_{d_}", [128, 4], F32))
            lbm = es.enter_context(nc.sbuf_tensor(f"lbm_{d_}", [128, 4], F32))
            cx.op('dve', lambda e, l0=l0, l1=l1, lb=lb: e.tensor_tensor(out=lb[:, :], in0=l0[:, :], in1=l1[:, :], op=ALU.subtract), r=[f"lbl0_{d_}", f"lbl1_{d_}"], w=["lbT"])
            cx.op('act', lambda e, lb=lb: e.activation(out=lb[:, :], in_=lb[:, :], func=AF.Sigmoid), r=["lbT"], w=["lbT"])
            cx.op('dve', lambda e, lb=lb, lbm=lbm: e.tensor_scalar(lbm[:, :], lb[:, :], -1.0, 1.0, op0=ALU.mult, op1=ALU.add), r=["lbT"], w=["lbT"])
            lbT.append((lb, lbm))
        cx.epoch()
        x_rows = lambda t0, n: x_in[t0:t0 + n, :]
        if stages >= 1:
            pass_h(cx, cfg, x_rows, hT_d, consts, ps)
        if stages >= 2:
            pass_hgrn(cx, cfg, hT_d, o_d[0], ins["ev_w_in"], lbT, 0, consts, ps, gmix[0], stop=HSTOP)
        if stages >= 3:
            pass_hgrn(cx, cfg, hT_d, o_d[1], ins["ev_w_in"], lbT, 1, consts, ps, gmix[0])
        if stages >= 4:
            pass_mla_prep(cx, cfg, hT_d, q_d, k_d, v_d, ins, consts, ps, gmix[0])
        if stages >= 5:
            pass_attn(cx, cfg, q_d, k_d, v_d, om_d, consts, ps)
        if stages >= 6:
            with nc.sbuf_tensor("aff_sb0", [128, cfg.NT, NE], F32) as aff_sb:
                pass_combine(cx, cfg, x_in, hT_d, o_d, om_d, x1_d, hn_d, aff_sb, aff_d, ins, consts, ps, gmix[0], gffn[0])
                if stages >= 7:
                    pass_moe(cx, cfg, 0, aff_sb, aff_d, hn_d, x1_d, pinc_d, ins, consts, ps, gffn[0])
        if stages >= 8:
            pass_h(cx, cfg, lambda t0, n: x1_d[t0:t0 + n, :], hT_d, consts, ps, tag="b")
            pass_r1(cx, cfg, hT_d, gl_d, xb_d, ins, consts, ps, gmix[1])
            pass_r2(cx, cfg, 0, xb_d, gl_d, hf_d, x1_d, y_out, hn_d, None, aff_d, ins, consts, ps, gffn[1])
            with nc.sbuf_tensor("aff_sb1", [128, cfg.NT, NE], F32) as aff_sb:
                pass_r2(cx, cfg, 1, xb_d, gl_d, hf_d, x1_d, y_out, hn_d, aff_sb, aff_d, ins, consts, ps, gffn[1])
                if stages >= 9:
                    pass_moe(cx, cfg, 1, aff_sb, aff_d, hn_d, y_out, pinc_d, ins, consts, ps, gffn[1])
        cx.drain()
        print("instructions:", cx.ninst)
    return nc, cfg


def host_layout(inputs, P, B):
    g = lambda k: np.ascontiguousarray(np.asarray(inputs[k], dtype=np.float32))
    m = {"x_in": np.concatenate([g("x_prompt").reshape(-1, D), g("x_sample").reshape(-1, D)], 0)}
    for k in ["norm_mix", "norm_ffn", "hg_lb_logits", "moe_router", "moe_w_gate", "moe_w_up", "moe_w_down"]:
        m[k] = g(k)
    for k in ["ev_w_in", "ev_w_out", "hg_out_gain", "mla_q_norm", "mla_kv_norm", "mla_w_uq", "mla_w_ukv", "mla_q_gain", "mla_k_gain",
              "od_w_in", "od_w_out", "od_conv_w", "od_conv_b", "rg_w_a", "rg_b_a", "rg_w_x", "rg_b_x", "rg_lambda"]:
        a = g(k)
        m[k] = np.ascontiguousarray(a.reshape(a.shape[1:]))
    Lp = max(P, 1) * L
    pos = np.arange(Lp, dtype=np.float32)
    inv = (1.0 / (10000.0 ** (np.arange(0, 64, 2, dtype=np.float32) / 64))).astype(np.float32)
    ang = pos[None, :] * inv[:, None]
    m["rope_cos"] = np.concatenate([np.cos(ang), np.cos(ang)], 0).astype(np.float32)
    m["rope_sin"] = np.concatenate([np.sin(ang), np.sin(ang)], 0).astype(np.float32)
    return m


def kernel(**inputs):
    P, B = 8, 32
    nc, cfg = build(P, B)
    m = host_layout(inputs, P, B)
    res = run_bass_kernel_spmd(nc, [m], core_ids=[0])
    y = np.asarray(res.results[0]["y_out"], dtype=np.float32)
    return (y[:P * L].reshape(1, P * L, D).copy(), y[P * L:].reshape(B, L, D).copy())
```

```python
import numpy as np
from contextlib import ExitStack
import concourse.bass as bass
import concourse.mybir as mybir
from concourse.bass_utils import run_bass_kernel_spmd

F32 = mybir.dt.float32
BF16 = mybir.dt.bfloat16
I32 = mybir.dt.int32
AF = mybir.ActivationFunctionType
ALU = mybir.AluOpType
AX = mybir.AxisListType

D = 1024
L = 2048
EPS = 1e-6
NE = 16
BIG = 1.0e6
import os
HSTOP = int(os.environ.get('HSTOP', '99'))
MSTOP = int(os.environ.get('MSTOP', '99'))
FSTOP = int(os.environ.get('FSTOP', '99'))
NEXP = int(os.environ.get('NEXP', '16'))


class Cx:
    ENG = ('pe', 'dve', 'act', 'pool')

    def __init__(s, nc, es):
        s.nc = nc
        s.e = {'pe': nc.tensor, 'dve': nc.vector, 'act': nc.scalar, 'pool': nc.gpsimd, 'sp': nc.sync}
        s.sem = {}
        for k in s.ENG:
            s.sem[k] = es.enter_context(nc.semaphore(f"c_{k}"))
        s.cnt = {k: 0 for k in s.ENG}
        s.ch = []
        for q, n in (('sp', 8), ('pool', 8)):
            for i in range(n):
                nm = f"d_{q}{i}"
                s.sem[nm] = es.enter_context(nc.semaphore(nm))
                s.ch.append({'q': q, 'name': nm, 'cnt': 0})
        s.rr = {'sp': 0, 'pool': 0}
        s.bar = [es.enter_context(nc.semaphore(f"bar{i}")) for i in range(4)]
        s.par = 0
        s.seen = {k: {} for k in ('pe', 'dve', 'act', 'pool', 'sp')}
        s.lw = {}
        s.rd = {}
        s.ninst = 0
        s.nwait = 0
        s.npool = 0
        s.nopt = es.enter_context(nc.sbuf_tensor("cx_nop", [128, 1], F32))

    def _wait(s, eng, sn, v):
        if v <= 0:
            return
        if s.seen[eng].get(sn, 0) >= v:
            return
        s.e[eng].wait_ge(s.sem[sn], v)
        s.seen[eng][sn] = v
        s.nwait += 1

    def _deps(s, eng, r, w):
        need = {}
        for k in r:
            ev = s.lw.get(k)
            if ev:
                need[ev[0]] = max(need.get(ev[0], 0), ev[1])
        for k in w:
            ev = s.lw.get(k)
            if ev:
                need[ev[0]] = max(need.get(ev[0], 0), ev[1])
            for sn, v in s.rd.get(k, {}).items():
                need[sn] = max(need.get(sn, 0), v)
        for sn, v in need.items():
            if eng == 'pe' and sn == 'pe':
                continue
            s._wait(eng, sn, v)

    def _rec(s, ev, r, w):
        for k in w:
            s.lw[k] = ev
            s.rd[k] = {}
        for k in r:
            d = s.rd.setdefault(k, {})
            d[ev[0]] = max(d.get(ev[0], 0), ev[1])

    def op(s, eng, fn, r=(), w=()):
        s._deps(eng, r, w)
        inst = fn(s.e[eng])
        s.cnt[eng] += 1
        assert s.cnt[eng] < 32000, "epoch too long"
        inst.then_inc(s.sem[eng], 1)
        s._rec((eng, s.cnt[eng]), r, w)
        s.ninst += 1

    def dma(s, q, fn, r=(), w=()):
        chs = [c for c in s.ch if c['q'] == q]
        c = chs[s.rr[q] % len(chs)]
        s.rr[q] += 1
        nw0 = s.nwait
        s._wait(q, c['name'], c['cnt'])
        s._deps(q, r, w)
        if q == 'pool':
            s.npool += 1
            probe = s.e['pool'].alloc_register(f"cxprobe{s.npool}")
            s.e['pool'].free_register(probe)
            inst = fn(s.e[q])
            try:
                s.e['pool'].free_register(probe)
            except Exception:
                pass
        else:
            inst = fn(s.e[q])
        c['cnt'] += 16
        assert c['cnt'] < 32000, "epoch too long (dma)"
        inst.then_inc(s.sem[c['name']], 16)
        s._rec((c['name'], c['cnt']), r, w)
        s.ninst += 1

    def drain(s):
        for k in s.ENG:
            s._wait(k, k, s.cnt[k])
        for c in s.ch:
            s._wait(c['q'], c['name'], c['cnt'])

    def epoch(s):
        s.drain()
        A, B = s.bar[2 * s.par], s.bar[2 * s.par + 1]
        A2, B2 = s.bar[2 * (1 - s.par)], s.bar[2 * (1 - s.par) + 1]
        allk = ('pe', 'dve', 'act', 'pool', 'sp')
        for k in allk:
            s.e[k].sem_inc(A, 1)
        m = s.e['sp']
        m.wait_ge(A, 5)
        for k in s.ENG:
            m.sem_clear(s.sem[k])
        for c in s.ch:
            if c['q'] != 'pool':
                m.sem_clear(s.sem[c['name']])
        m.sem_clear(A2)
        m.sem_clear(B2)
        m.sem_inc(B, 1)
        for k in allk:
            s.e[k].wait_ge(B, 1)
        s.cnt = {k: 0 for k in s.ENG}
        for c in s.ch:
            if c['q'] != 'pool':
                c['cnt'] = 0
        s.seen = {k: {} for k in allk}
        s.lw = {}
        s.rd = {}
        s.par ^= 1


class Cfg:
    def __init__(s, P, B):
        s.P = P
        s.B = B
        s.NS = P + B
        s.T = s.NS * L
        s.NT = s.T // 128
        s.groups = [(0, P), (P, P + B)]


def chain_of(cfg, sg):
    if sg < cfg.P:
        return list(range(cfg.P))
    return [sg]


def load_w(cx, es, name, src, K, N, gcol=None, stg=None, dtype=BF16):
    nc = cx.nc
    kc = (K + 127) // 128
    wt = es.enter_context(nc.sbuf_tensor(name, [128, kc, N], dtype))
    for k in range(kc):
        rows = min(128, K - k * 128)
        for n0 in range(0, N, 1024):
            nn = min(1024, N - n0)
            sk = f"wstg{(k + n0 // 1024) % 2}"
            st = stg[(k + n0 // 1024) % 2]
            cx.dma('sp', lambda e, st=st, k=k, rows=rows, n0=n0, nn=nn: e.dma_start(out=st[0:rows, 0:nn], in_=src[k * 128:k * 128 + rows, n0:n0 + nn]),
                   w=[sk])
            if gcol is None:
                cx.op('pool', lambda e, st=st, k=k, rows=rows, n0=n0, nn=nn: e.tensor_copy(wt[0:rows, k, n0:n0 + nn], st[0:rows, 0:nn]),
                      r=[sk], w=[name])
            else:
                cx.op('dve', lambda e, st=st, k=k, rows=rows, n0=n0, nn=nn: e.tensor_scalar(wt[0:rows, k, n0:n0 + nn], st[0:rows, 0:nn], gcol[0:rows, k:k + 1], None, op0=ALU.mult),
                      r=[sk], w=[name])
    return wt


def load_cols(cx, es, name, src_vec, n):
    nc = cx.nc
    kc = n // 128
    t = es.enter_context(nc.sbuf_tensor(name, [128, kc], F32))
    cx.dma('sp', lambda e: e.dma_start(out=t[:, :], in_=src_vec.rearrange("(k p) -> p k", p=128), allow_slow_non_contiguous=True), w=[name])
    return t


def rstd_from_ssq(cx, out, ssq, n, tmpk):
    (o_ap, o_k), (s_ap, s_k) = out, ssq
    cx.op('dve', lambda e: e.tensor_scalar(o_ap, s_ap, 1.0 / n, EPS, op0=ALU.mult, op1=ALU.add), r=[s_k], w=[o_k])
    cx.op('act', lambda e: e.activation(out=o_ap, in_=o_ap, func=AF.Sqrt), r=[o_k], w=[o_k])
    cx.op('dve', lambda e: e.reciprocal(o_ap, o_ap), r=[o_k], w=[o_k])


def pass_h(cx, cfg, x_rows, hT_d, consts, ps, tag="a"):
    nc = cx.nc
    with ExitStack() as es:
        xt = [es.enter_context(nc.sbuf_tensor(f"h{tag}_xt{i}", [128, D], F32)) for i in range(2)]
        junk = es.enter_context(nc.sbuf_tensor(f"h{tag}_junk", [128, D], BF16))
        ssq = es.enter_context(nc.sbuf_tensor(f"h{tag}_ssq", [128, 2], F32))
        rs = es.enter_context(nc.sbuf_tensor(f"h{tag}_rs", [128, 2], F32))
        hT = [es.enter_context(nc.sbuf_tensor(f"h{tag}_hT{i}", [128, 8, 512], BF16)) for i in range(2)]
        ident = consts['identF']
        for sg in range(cfg.NS):
            for blk in range(L // 512):
                hb = hT[blk % 2]
                hk = f"h_hT{blk % 2}"
                for j4 in range(4):
                    j = blk * 4 + j4
                    b = j % 2
                    t0 = sg * L + j * 128
                    cx.dma('sp', lambda e, b=b, t0=t0: e.dma_start(out=xt[b][:, :], in_=x_rows(t0, 128)), w=[f"h_xt{b}"])
                    cx.op('act', lambda e, b=b: e.activation(out=junk[:, :], in_=xt[b][:, :], func=AF.Square, accum_out=ssq[:, b:b + 1]),
                          r=[f"h_xt{b}"], w=["h_junk", f"h_ssq{b}"])
                    rstd_from_ssq(cx, (rs[:, b:b + 1], f"h_rs{b}"), (ssq[:, b:b + 1], f"h_ssq{b}"), D, None)
                    cx.op('dve', lambda e, b=b: e.tensor_scalar(xt[b][:, :], xt[b][:, :], rs[:, b:b + 1], None, op0=ALU.mult),
                          r=[f"h_rs{b}", f"h_xt{b}"], w=[f"h_xt{b}"])
                    for half in range(2):
                        pt = ps[half]
                        pk = f"ps{half}"
                        for kk in range(4):
                            k = half * 4 + kk
                            cx.op('pe', lambda e, pt=pt, kk=kk, k=k, b=b: e.transpose(pt[:, kk * 128:(kk + 1) * 128], xt[b][:, k * 128:(k + 1) * 128], ident[:, :]),
                                  r=[f"h_xt{b}", "identF"], w=[pk])
                        eng = 'act' if half == 0 else 'dve'
                        if eng == 'act':
                            cx.op('act', lambda e, pt=pt, half=half, j4=j4, hb=hb: e.activation(
                                out=hb[:, half * 4:(half + 1) * 4, j4 * 128:(j4 + 1) * 128],
                                in_=pt[:, :].rearrange("p (k t) -> p k t", k=4), func=AF.Copy), r=[pk], w=[hk])
                        else:
                            cx.op('dve', lambda e, pt=pt, half=half, j4=j4, hb=hb: e.tensor_copy(
                                hb[:, half * 4:(half + 1) * 4, j4 * 128:(j4 + 1) * 128],
                                pt[:, :].rearrange("p (k t) -> p k t", k=4)), r=[pk], w=[hk])
                c0 = sg * L + blk * 512
                cx.dma('sp', lambda e, hb=hb, c0=c0: e.dma_start(out=hT_d[:, :, c0:c0 + 512].rearrange("k p t -> p k t"), in_=hb[:, :, :]),
                       r=[hk], w=[f"d:hT:{sg}"])
            cx.epoch()


def pass_hgrn(cx, cfg, hT_d, o_d, w_in, lbT, dirn, consts, ps, gmix, stop=99):
    nc = cx.nc
    with ExitStack() as es:
        stg = [es.enter_context(nc.sbuf_tensor(f"g{dirn}_stg{i}", [128, 1024], F32)) for i in range(2)]
        wq = load_w(cx, es, f"g{dirn}_wq", w_in[:, 0:512], D, 512, gcol=gmix, stg=stg)
        wi = load_w(cx, es, f"g{dirn}_wi", w_in[:, 512:1024], D, 512, gcol=gmix, stg=stg)
        wf = load_w(cx, es, f"g{dirn}_wf", w_in[:, 1536 + 512 * dirn:2048 + 512 * dirn], D, 512, gcol=gmix, stg=stg)
        hR = es.enter_context(nc.sbuf_tensor(f"g{dirn}_hR", [128, 8, L], BF16))
        hst = [es.enter_context(nc.sbuf_tensor(f"g{dirn}_hst{i}", [128, L], BF16)) for i in range(2)] if dirn else None
        f = es.enter_context(nc.sbuf_tensor(f"g{dirn}_f", [128, 1024], F32))
        bb = es.enter_context(nc.sbuf_tensor(f"g{dirn}_b", [128, 1024], F32))
        ea = es.enter_context(nc.sbuf_tensor(f"g{dirn}_ea", [128, 1024], F32))
        kk_ = es.enter_context(nc.sbuf_tensor(f"g{dirn}_k", [128, 1024], F32))
        qt = es.enter_context(nc.sbuf_tensor(f"g{dirn}_qt", [128, 4, L], BF16))
        kt = es.enter_context(nc.sbuf_tensor(f"g{dirn}_kt", [128, 4, L], BF16))
        kh = es.enter_context(nc.sbuf_tensor(f"g{dirn}_kh", [128, 4, L], BF16))
        ebl = es.enter_context(nc.sbuf_tensor(f"g{dirn}_ebl", [128, 4, 32], F32))
        v = es.enter_context(nc.sbuf_tensor(f"g{dirn}_v", [64, 2, 512], BF16))
        khT = es.enter_context(nc.sbuf_tensor(f"g{dirn}_khT", [64, 2, 512], BF16))
        pT = es.enter_context(nc.sbuf_tensor(f"g{dirn}_pT", [64, 4, 64], BF16))
        osb = es.enter_context(nc.sbuf_tensor(f"g{dirn}_o", [64, 2, 8, 512], BF16))
        otmp = es.enter_context(nc.sbuf_tensor(f"g{dirn}_otmp", [64, 512], F32))
        S = es.enter_context(nc.sbuf_tensor(f"g{dirn}_S", [128, 512], F32))
        Sb = es.enter_context(nc.sbuf_tensor(f"g{dirn}_Sb", [128, 512], BF16))
        identB = consts['identB']
        cmask = consts['cmask']
        smask = consts['smask']
        lb = lbT[dirn]
        segs = list(range(cfg.NS))
        if dirn:
            segs = list(reversed(range(cfg.P))) + list(range(cfg.P, cfg.NS))
        for si, sg in enumerate(segs):
            first = (sg >= cfg.P) or (sg == (cfg.P - 1 if dirn else 0))
            for k in range(8):
                if dirn:
                    cx.dma('sp', lambda e, k=k, sg=sg: e.dma_start(out=hst[k % 2][:, :], in_=hT_d[k, :, sg * L:(sg + 1) * L]), w=[f"g_hst{k % 2}"])
                    cx.op('dve', lambda e, k=k: e.tensor_copy(hR[:, k, :], hst[k % 2][:, ::-1]), r=[f"g_hst{k % 2}"], w=["g_hR"])
                else:
                    cx.dma('sp', lambda e, k=k, sg=sg: e.dma_start(out=hR[:, k, :], in_=hT_d[k, :, sg * L:(sg + 1) * L]), w=["g_hR"])
            hk = "g_hR"
            if first:
                cx.op('dve', lambda e: e.memset(S[:, :], 0.0), w=["g_S0", "g_S1", "g_S2", "g_S3"])
                cx.op('pool', lambda e: e.memset(Sb[:, :], 0.0), w=["g_Sb"])
            if stop <= 0:
                cx.epoch(); continue
            for h in range(4):
                for hf in range(2):
                    c0 = hf * 1024
                    pz = (ps[0], ps[1])
                    pq = (ps[2], ps[3])
                    for nb in range(2):
                        for k in range(8):
                            cx.op('pe', lambda e, nb=nb, k=k, h=h, c0=c0: e.matmul(pz[nb][:, :], lhsT=wf[:, k, h * 128:(h + 1) * 128], rhs=hR[:, k, c0 + nb * 512:c0 + (nb + 1) * 512], start=(k == 0), stop=(k == 7)),
                                  r=[f"g{dirn}_wf", hk], w=[f"ps{nb}"])
                        for k in range(8):
                            cx.op('pe', lambda e, nb=nb, k=k, h=h, c0=c0: e.matmul(pq[nb][:, :], lhsT=wq[:, k, h * 128:(h + 1) * 128], rhs=hR[:, k, c0 + nb * 512:c0 + (nb + 1) * 512], start=(k == 0), stop=(k == 7)),
                                  r=[f"g{dirn}_wq", hk], w=[f"ps{2 + nb}"])
                    for nb in range(2):
                        cx.op('act', lambda e, nb=nb: e.activation(out=f[:, nb * 512:(nb + 1) * 512], in_=pz[nb][:, :], func=AF.Sigmoid), r=[f"ps{nb}"], w=["g_f"])
                    cx.op('dve', lambda e, h=h: e.tensor_scalar(f[:, :], f[:, :], lb[1][:, h:h + 1], lb[0][:, h:h + 1], op0=ALU.mult, op1=ALU.add), r=["g_f", "lbT"], w=["g_f"])
                    cx.op('act', lambda e: e.activation(out=bb[:, :], in_=f[:, :], func=AF.Ln), r=["g_f"], w=["g_b"])
                    cx.op('dve', lambda e: e.tensor_tensor_scan(out=bb[:, :], data0=smask[:, :], data1=bb[:, :], initial=0.0, op0=ALU.mult, op1=ALU.add), r=["g_b", "smask"], w=["g_b"])
                    cx.op('pool', lambda e: e.tensor_scalar(kk_[:, :], f[:, :], -1.0, 1.0, op0=ALU.mult, op1=ALU.add), r=["g_f"], w=["g_k"])
                    cx.op('act', lambda e: e.activation(out=ea[:, :], in_=bb[:, :], func=AF.Exp), r=["g_b"], w=["g_ea"])
                    for nb in range(2):
                        cx.op('dve', lambda e, nb=nb, h=h, c0=c0: e.scalar_tensor_tensor(out=qt[:, h, c0 + nb * 512:c0 + (nb + 1) * 512], in0=pq[nb][:, :], scalar=128.0 ** -0.5, in1=ea[:, nb * 512:(nb + 1) * 512], op0=ALU.mult, op1=ALU.mult),
                              r=[f"ps{2 + nb}", "g_ea"], w=["g_qt"])
                    cx.op('act', lambda e: e.activation(out=ea[:, :], in_=bb[:, :], func=AF.Exp, scale=-1.0), r=["g_b"], w=["g_ea"])
                    cx.op('dve', lambda e, h=h, c0=c0: e.tensor_tensor(out=kt[:, h, c0:c0 + 1024], in0=kk_[:, :], in1=ea[:, :], op=ALU.mult), r=["g_k", "g_ea"], w=["g_kt"])
                    b3 = bb[:, :].rearrange("p (c t) -> p c t", t=64)
                    cx.op('act', lambda e, h=h, hf=hf: e.activation(out=ebl[:, h, hf * 16:(hf + 1) * 16], in_=b3[:, :, 63], func=AF.Exp), r=["g_b"], w=["g_ebl"])
                    cx.op('dve', lambda e: e.tensor_tensor(out=ea[:, :].rearrange("p (c t) -> p c t", t=64), in0=b3[:, :, 63:64].to_broadcast([128, 16, 64]), in1=b3, op=ALU.subtract), r=["g_b"], w=["g_ea"])
                    cx.op('act', lambda e: e.activation(out=ea[:, :], in_=ea[:, :], func=AF.Exp), r=["g_ea"], w=["g_ea"])
                    cx.op('dve', lambda e, h=h, c0=c0: e.tensor_tensor(out=kh[:, h, c0:c0 + 1024], in0=kk_[:, :], in1=ea[:, :], op=ALU.mult), r=["g_k", "g_ea"], w=["g_kh"])
            if stop <= 1:
                cx.epoch(); continue
            if dirn == 0:
                r0 = sg * L
            else:
                r0 = (cfg.P - 1 - sg) * L if sg < cfg.P else sg * L
            for c in range(32):
                cb = c % 2
                pv = ps[4 + cb]
                pvk = f"ps{4 + cb}"
                for k in range(8):
                    cx.op('pe', lambda e, k=k, c=c, pv=pv: e.matmul(pv[0:64, :], lhsT=hR[:, k, c * 64:(c + 1) * 64], rhs=wi[:, k, :], start=(k == 0), stop=(k == 7)),
                          r=[hk, f"g{dirn}_wi"], w=[pvk])
                cx.op('act', lambda e, cb=cb, pv=pv: e.activation(out=v[:, cb, :], in_=pv[0:64, :], func=AF.Copy), r=[pvk], w=[f"g_v{cb}"])
                pk_ = consts['psb'][cb]
                pkk = f"psb{cb}"
                for h in range(4):
                    cx.op('pe', lambda e, h=h, c=c, pk_=pk_: e.transpose(pk_[0:64, h * 128:(h + 1) * 128], kh[:, h, c * 64:(c + 1) * 64], identB[:, :]),
                          r=["g_kh", "identB"], w=[pkk])
                cx.op('pool' if False else 'dve', lambda e, cb=cb, pk_=pk_: e.tensor_copy(khT[:, cb, :], pk_[0:64, :]), r=[pkk], w=[f"g_khT{cb}"])
                psS, pso, psd = ps[0], ps[1], ps[2]
                for h in range(4):
                    cx.op('pe', lambda e, h=h, c=c: e.matmul(psS[0:64, h * 64:(h + 1) * 64], lhsT=kt[:, h, c * 64:(c + 1) * 64], rhs=qt[:, h, c * 64:(c + 1) * 64], start=True, stop=True),
                          r=["g_kt", "g_qt"], w=["ps0"])
                cx.op('dve', lambda e: e.tensor_tensor(out=pT[:, :, :], in0=psS[0:64, 0:256].rearrange("p (h t) -> p h t", h=4), in1=cmask[:, :].unsqueeze(1).to_broadcast([64, 4, 64]), op=ALU.mult),
                      r=["ps0", "cmask"], w=["g_pT"])
                for h in range(4):
                    cx.op('pe', lambda e, h=h, cb=cb: e.matmul(pso[0:64, h * 128:(h + 1) * 128], lhsT=pT[:, h, :], rhs=v[:, cb, h * 128:(h + 1) * 128], start=True, stop=True),
                          r=["g_pT", f"g_v{cb}"], w=["ps1"])
                    cx.op('pe', lambda e, h=h, c=c: e.matmul(ps[3][0:64, h * 128:(h + 1) * 128], lhsT=qt[:, h, c * 64:(c + 1) * 64], rhs=Sb[:, h * 128:(h + 1) * 128], start=True, stop=True),
                          r=["g_qt", "g_Sb"], w=["ps3"])
                ob = (c // 8) % 2
                cx.op('act', lambda e: e.activation(out=otmp[:, :], in_=pso[0:64, :], func=AF.Copy), r=["ps1"], w=["g_otmp"])
                cx.op('dve', lambda e, c=c, ob=ob: e.tensor_tensor(out=osb[:, ob, c % 8, :], in0=ps[3][0:64, :], in1=otmp[:, :], op=ALU.add), r=["ps3", "g_otmp"], w=[f"g_o{ob}"])
                for h in range(4):
                    cx.op('pe', lambda e, h=h, cb=cb: e.matmul(psd[:, h * 128:(h + 1) * 128], lhsT=khT[:, cb, h * 128:(h + 1) * 128], rhs=v[:, cb, h * 128:(h + 1) * 128], start=True, stop=True),
                          r=[f"g_khT{cb}", f"g_v{cb}"], w=["ps2"])
                for h in range(4):
                    cx.op('dve', lambda e, h=h, c=c: e.scalar_tensor_tensor(out=S[:, h * 128:(h + 1) * 128], in0=S[:, h * 128:(h + 1) * 128], scalar=ebl[:, h, c:c + 1], in1=psd[:, h * 128:(h + 1) * 128], op0=ALU.mult, op1=ALU.add),
                          r=[f"g_S{h}", "g_ebl", "ps2"], w=[f"g_S{h}"])
                cx.op('act', lambda e: e.activation(out=Sb[:, :], in_=S[:, :], func=AF.Copy), r=["g_S0", "g_S1", "g_S2", "g_S3"], w=["g_Sb"])
                if c % 8 == 7:
                    rr0 = r0 + (c - 7) * 64
                    cx.dma('sp', lambda e, rr0=rr0, ob=ob: e.dma_start(out=o_d[rr0:rr0 + 512, :].rearrange("(c t) n -> t c n", t=64), in_=osb[:, ob, :, :]), r=[f"g_o{ob}"], w=[f"d:o{dirn}:{sg}:{c}"])
            cx.epoch()


def rstd_tile(cx, out_ap, out_k, ps_ap, ps_k, n):
    cx.op('dve', lambda e: e.tensor_scalar(out_ap, ps_ap, 1.0 / n, EPS, op0=ALU.mult, op1=ALU.add), r=[ps_k], w=[out_k])
    cx.op('act', lambda e: e.activation(out=out_ap, in_=out_ap, func=AF.Sqrt), r=[out_k], w=[out_k])
    cx.op('dve', lambda e: e.reciprocal(out_ap, out_ap), r=[out_k], w=[out_k])


def seg_pos0(cfg, sg):
    return sg * L if sg < cfg.P else 0


def pass_mla_prep(cx, cfg, hT_d, q_d, k_d, v_d, ins, consts, ps, gmix):
    nc = cx.nc
    w_in = ins["ev_w_in"]
    with ExitStack() as es:
        stg = [es.enter_context(nc.sbuf_tensor(f"m1_stg{i}", [128, 1024], F32)) for i in range(2)]
        wcq = load_w(cx, es, "m1_wcq", w_in[:, 2560:2944], D, 384, gcol=gmix, stg=stg)
        wckv = load_w(cx, es, "m1_wckv", w_in[:, 2944:3200], D, 256, gcol=gmix, stg=stg)
        wkpe = load_w(cx, es, "m1_wkpe", w_in[:, 3200:3264], D, 64, gcol=gmix, stg=stg)
        gqn = load_cols(cx, es, "m1_gqn", ins["mla_q_norm"], 384)
        gkvn = load_cols(cx, es, "m1_gkvn", ins["mla_kv_norm"], 256)
        wuq = load_w(cx, es, "m1_wuq", ins["mla_w_uq"], 384, 768, gcol=gqn, stg=stg)
        wukv = load_w(cx, es, "m1_wukv", ins["mla_w_ukv"], 256, 1024, gcol=gkvn, stg=stg)
        wv = es.enter_context(nc.sbuf_tensor("m1_wv", [128, 2, 512], BF16))
        for h in range(4):
            cx.op('dve', lambda e, h=h: e.tensor_copy(wv[:, :, h * 128:(h + 1) * 128], wukv[:, :, h * 256 + 128:h * 256 + 256]), r=["m1_wukv"], w=["m1_wv"])
        gq = es.enter_context(nc.sbuf_tensor("m1_gq", [128, 2], F32))
        gk = es.enter_context(nc.sbuf_tensor("m1_gk", [128, 2], F32))
        for (t, src, nm) in ((gq, ins["mla_q_gain"], "m1_gq"), (gk, ins["mla_k_gain"], "m1_gk")):
            cx.op('dve', lambda e, t=t: e.memset(t[:, :], 0.0), w=[nm])
            cx.dma('sp', lambda e, t=t, src=src: e.dma_start(out=t[:, 0:1], in_=src[0:128].rearrange("(p o) -> p o", o=1)), w=[nm])
            cx.dma('sp', lambda e, t=t, src=src: e.dma_start(out=t[0:64, 1:2], in_=src[128:192].rearrange("(p o) -> p o", o=1)), w=[nm])
        cx.op('dve', lambda e: e.tensor_scalar(gq[:, :], gq[:, :], 192.0 ** -0.5, None, op0=ALU.mult), r=["m1_gq"], w=["m1_gq"])
        Rm = es.enter_context(nc.sbuf_tensor("m1_Rm", [64, 64], F32))
        Rt = es.enter_context(nc.sbuf_tensor("m1_Rt", [64, 64], F32))
        iot = consts['iot']
        cx.op('dve', lambda e: e.tensor_scalar(Rm[:, :], iot[0:64, 0:64], 32.0, None, op0=ALU.is_equal), r=["c_iot"], w=["m1_Rm"])
        cx.op('dve', lambda e: e.tensor_scalar(Rt[:, :], iot[0:64, 0:64], -32.0, None, op0=ALU.is_equal), r=["c_iot"], w=["m1_Rt"])
        cx.op('dve', lambda e: e.tensor_tensor(out=Rm[:, :], in0=Rm[:, :], in1=Rt[:, :], op=ALU.subtract), r=["m1_Rm", "m1_Rt"], w=["m1_Rm"])
        hb = es.enter_context(nc.sbuf_tensor("m1_hb", [128, 8, 512], BF16))
        cq = es.enter_context(nc.sbuf_tensor("m1_cq", [128, 3, 512], F32))
        cqn = es.enter_context(nc.sbuf_tensor("m1_cqn", [128, 3, 512], BF16))
        ckvn = es.enter_context(nc.sbuf_tensor("m1_ckvn", [128, 2, 512], BF16))
        sqb = es.enter_context(nc.sbuf_tensor("m1_sqb", [128, 3, 512], BF16))
        sqr = es.enter_context(nc.sbuf_tensor("m1_sqr", [128, 512], BF16))
        sqk = es.enter_context(nc.sbuf_tensor("m1_sqk", [128, 512], BF16))
        r1 = es.enter_context(nc.sbuf_tensor("m1_r1", [128, 512], F32))
        nf = es.enter_context(nc.sbuf_tensor("m1_nf", [128, 512], F32))
        rf = es.enter_context(nc.sbuf_tensor("m1_rf", [64, 512], F32))
        t1 = es.enter_context(nc.sbuf_tensor("m1_t1", [64, 512], F32))
        t2 = es.enter_context(nc.sbuf_tensor("m1_t2", [64, 512], F32))
        kpe = es.enter_context(nc.sbuf_tensor("m1_kpe", [64, 512], F32))
        krot = es.enter_context(nc.sbuf_tensor("m1_krot", [64, 512], F32))
        cs = es.enter_context(nc.sbuf_tensor("m1_cos", [64, 512], F32))
        sn = es.enter_context(nc.sbuf_tensor("m1_sin", [64, 512], F32))
        on = es.enter_context(nc.sbuf_tensor("m1_on", [128, 2, 512], BF16))
        orp = es.enter_context(nc.sbuf_tensor("m1_or", [64, 2, 512], BF16))
        vt = es.enter_context(nc.sbuf_tensor("m1_vt", [128, 2, 512], BF16))
        onesB = consts['onesB']
        cx.op('dve', lambda e: e.memset(sqr[:, :], 0.0), w=["m1_sqr"])
        cx.op('dve', lambda e: e.memset(sqk[:, :], 0.0), w=["m1_sqk"])
        pA, pB, pC, pD = ps[0], ps[1], ps[2], ps[3]

        def rope(src, dst_k, dst_ap):
            cx.op('pe', lambda e: e.matmul(pD[0:64, :], lhsT=Rm[:, :], rhs=src[:, :], start=True, stop=True), r=["m1_Rm", "m1_t1"], w=["ps3"])
            cx.op('dve', lambda e: e.tensor_tensor(out=t2[:, :], in0=pD[0:64, :], in1=sn[:, :], op=ALU.mult), r=["ps3", "m1_sin"], w=["m1_t2"])
            cx.op('dve', lambda e: e.tensor_tensor(out=src[:, :], in0=src[:, :], in1=cs[:, :], op=ALU.mult), r=["m1_t1", "m1_cos"], w=["m1_t1"])
            cx.op('dve', lambda e: e.tensor_tensor(out=dst_ap, in0=src[:, :], in1=t2[:, :], op=ALU.add), r=["m1_t1", "m1_t2"], w=[dst_k])

        for sg in range(cfg.NS):
            for blk in range(4):
                c0 = sg * L + blk * 512
                p0 = seg_pos0(cfg, sg) + blk * 512
                cx.dma('sp', lambda e, c0=c0: e.dma_start(out=hb[:, :, :], in_=hT_d[:, :, c0:c0 + 512].rearrange("k p t -> p k t")), w=["m1_hb"])
                cx.dma('sp', lambda e, p0=p0: e.dma_start(out=cs[:, :], in_=ins["rope_cos"][:, p0:p0 + 512]), w=["m1_cos"])
                cx.dma('sp', lambda e, p0=p0: e.dma_start(out=sn[:, :], in_=ins["rope_sin"][:, p0:p0 + 512]), w=["m1_sin"])
                for (wt, wk, nch, dst, dk) in ((wcq, "m1_wcq", 3, cqn, "m1_cqn"), (wckv, "m1_wckv", 2, ckvn, "m1_ckvn")):
                    for n in range(nch):
                        for k in range(8):
                            cx.op('pe', lambda e, wt=wt, n=n, k=k: e.matmul(pA[:, :], lhsT=wt[:, k, n * 128:(n + 1) * 128], rhs=hb[:, k, :], start=(k == 0), stop=(k == 7)),
                                  r=[wk, "m1_hb"], w=["ps0"])
                        cx.op('act', lambda e, n=n: e.activation(out=cq[:, n, :], in_=pA[:, :], func=AF.Copy), r=["ps0"], w=["m1_cq"])
                        cx.op('dve', lambda e, n=n: e.tensor_tensor(out=sqb[:, n, :], in0=pA[:, :], in1=cq[:, n, :], op=ALU.mult), r=["ps0", "m1_cq"], w=["m1_sqb"])
                    for n in range(nch):
                        cx.op('pe', lambda e, n=n, nch=nch: e.matmul(pB[:, :], lhsT=onesB[:, :], rhs=sqb[:, n, :], start=(n == 0), stop=(n == nch - 1)), r=["onesB", "m1_sqb"], w=["ps1"])
                    rstd_tile(cx, r1[:, :], "m1_r1", pB[:, :], "ps1", nch * 128)
                    for n in range(nch):
                        cx.op('dve', lambda e, n=n, dst=dst: e.tensor_tensor(out=dst[:, n, :], in0=cq[:, n, :], in1=r1[:, :], op=ALU.mult), r=["m1_cq", "m1_r1"], w=[dk])
                for k in range(8):
                    cx.op('pe', lambda e, k=k: e.matmul(pC[0:64, :], lhsT=wkpe[:, k, :], rhs=hb[:, k, :], start=(k == 0), stop=(k == 7)), r=["m1_wkpe", "m1_hb"], w=["ps2"])
                cx.op('act', lambda e: e.activation(out=kpe[:, :], in_=pC[0:64, :], func=AF.Copy), r=["ps2"], w=["m1_kpe"])
                cx.op('dve', lambda e: e.tensor_tensor(out=sqk[0:64, :], in0=pC[0:64, :], in1=kpe[:, :], op=ALU.mult), r=["ps2", "m1_kpe"], w=["m1_sqk"])
                cx.op('dve', lambda e: e.tensor_scalar(t1[:, :], kpe[:, :], gk[0:64, 1:2], None, op0=ALU.mult), r=["m1_kpe", "m1_gk"], w=["m1_t1"])
                rope(t1, "m1_krot", krot[:, :])
                for h in range(4):
                    hb2 = h % 2
                    for n in range(3):
                        cx.op('pe', lambda e, n=n, h=h: e.matmul(pA[:, :], lhsT=wuq[:, n, h * 192:h * 192 + 128], rhs=cqn[:, n, :], start=(n == 0), stop=(n == 2)), r=["m1_wuq", "m1_cqn"], w=["ps0"])
                    for n in range(3):
                        cx.op('pe', lambda e, n=n, h=h: e.matmul(pC[0:64, :], lhsT=wuq[:, n, h * 192 + 128:h * 192 + 192], rhs=cqn[:, n, :], start=(n == 0), stop=(n == 2)), r=["m1_wuq", "m1_cqn"], w=["ps2"])
                    cx.op('act', lambda e: e.activation(out=nf[:, :], in_=pA[:, :], func=AF.Copy), r=["ps0"], w=["m1_nf"])
                    cx.op('dve', lambda e: e.tensor_tensor(out=sqb[:, 0, :], in0=pA[:, :], in1=nf[:, :], op=ALU.mult), r=["ps0", "m1_nf"], w=["m1_sqb"])
                    cx.op('act', lambda e: e.activation(out=rf[:, :], in_=pC[0:64, :], func=AF.Copy), r=["ps2"], w=["m1_rf"])
                    cx.op('dve', lambda e: e.tensor_tensor(out=sqr[0:64, :], in0=pC[0:64, :], in1=rf[:, :], op=ALU.mult), r=["ps2", "m1_rf"], w=["m1_sqr"])
                    cx.op('pe', lambda e: e.matmul(pB[:, :], lhsT=onesB[:, :], rhs=sqb[:, 0, :], start=True, stop=False), r=["onesB", "m1_sqb"], w=["ps1"])
                    cx.op('pe', lambda e: e.matmul(pB[:, :], lhsT=onesB[:, :], rhs=sqr[:, :], start=False, stop=True), r=["onesB", "m1_sqr"], w=["ps1"])
                    rstd_tile(cx, r1[:, :], "m1_r1", pB[:, :], "ps1", 192)
                    cx.op('dve', lambda e, hb2=hb2: e.scalar_tensor_tensor(out=on[:, hb2, :], in0=nf[:, :], scalar=gq[:, 0:1], in1=r1[:, :], op0=ALU.mult, op1=ALU.mult), r=["m1_nf", "m1_gq", "m1_r1"], w=[f"m1_on{hb2}"])
                    cx.dma('sp', lambda e, h=h, hb2=hb2, c0=c0: e.dma_start(out=q_d[h, 0:128, c0:c0 + 512], in_=on[:, hb2, :]), r=[f"m1_on{hb2}"], w=[f"d:q:{sg}"])
                    cx.op('dve', lambda e: e.scalar_tensor_tensor(out=t1[:, :], in0=rf[:, :], scalar=gq[0:64, 1:2], in1=r1[0:64, :], op0=ALU.mult, op1=ALU.mult), r=["m1_rf", "m1_gq", "m1_r1"], w=["m1_t1"])
                    rope(t1, f"m1_or{hb2}", orp[:, hb2, :])
                    cx.dma('sp', lambda e, h=h, hb2=hb2, c0=c0: e.dma_start(out=q_d[h, 128:192, c0:c0 + 512], in_=orp[:, hb2, :]), r=[f"m1_or{hb2}"], w=[f"d:q:{sg}"])
                    for n in range(2):
                        cx.op('pe', lambda e, n=n, h=h: e.matmul(pA[:, :], lhsT=wukv[:, n, h * 256:h * 256 + 128], rhs=ckvn[:, n, :], start=(n == 0), stop=(n == 1)), r=["m1_wukv", "m1_ckvn"], w=["ps0"])
                    cx.op('act', lambda e: e.activation(out=nf[:, :], in_=pA[:, :], func=AF.Copy), r=["ps0"], w=["m1_nf"])
                    cx.op('dve', lambda e: e.tensor_tensor(out=sqb[:, 0, :], in0=pA[:, :], in1=nf[:, :], op=ALU.mult), r=["ps0", "m1_nf"], w=["m1_sqb"])
                    cx.op('pe', lambda e: e.matmul(pB[:, :], lhsT=onesB[:, :], rhs=sqb[:, 0, :], start=True, stop=False), r=["onesB", "m1_sqb"], w=["ps1"])
                    cx.op('pe', lambda e: e.matmul(pB[:, :], lhsT=onesB[:, :], rhs=sqk[:, :], start=False, stop=True), r=["onesB", "m1_sqk"], w=["ps1"])
                    rstd_tile(cx, r1[:, :], "m1_r1", pB[:, :], "ps1", 192)
                    hb3 = 1 - hb2
                    cx.op('dve', lambda e, hb3=hb3: e.scalar_tensor_tensor(out=on[:, hb3, :], in0=nf[:, :], scalar=gk[:, 0:1], in1=r1[:, :], op0=ALU.mult, op1=ALU.mult), r=["m1_nf", "m1_gk", "m1_r1"], w=[f"m1_on{hb3}"])
                    cx.dma('sp', lambda e, h=h, hb3=hb3, c0=c0: e.dma_start(out=k_d[h, 0:128, c0:c0 + 512], in_=on[:, hb3, :]), r=[f"m1_on{hb3}"], w=[f"d:k:{sg}"])
                    cx.op('dve', lambda e, hb3=hb3: e.tensor_tensor(out=orp[:, hb3, :], in0=krot[:, :], in1=r1[0:64, :], op=ALU.mult), r=["m1_krot", "m1_r1"], w=[f"m1_or{hb3}"])
                    cx.dma('sp', lambda e, h=h, hb3=hb3, c0=c0: e.dma_start(out=k_d[h, 128:192, c0:c0 + 512], in_=orp[:, hb3, :]), r=[f"m1_or{hb3}"], w=[f"d:k:{sg}"])
                for j in range(4):
                    jb = j % 2
                    for n in range(2):
                        cx.op('pe', lambda e, n=n, j=j: e.matmul(pA[:, :], lhsT=ckvn[:, n, j * 128:(j + 1) * 128], rhs=wv[:, n, :], start=(n == 0), stop=(n == 1)), r=["m1_ckvn", "m1_wv"], w=["ps0"])
                    cx.op('act', lambda e, jb=jb: e.activation(out=vt[:, jb, :], in_=pA[:, :], func=AF.Copy), r=["ps0"], w=[f"m1_vt{jb}"])
                    cx.dma('sp', lambda e, jb=jb, j=j, c0=c0: e.dma_start(out=v_d[c0 + j * 128:c0 + (j + 1) * 128, :], in_=vt[:, jb, :]), r=[f"m1_vt{jb}"], w=[f"d:v:{sg}"])
            cx.epoch()


def pass_attn(cx, cfg, q_d, k_d, v_d, om_d, consts, ps):
    nc = cx.nc
    with ExitStack() as es:
        qn = es.enter_context(nc.sbuf_tensor("a_qn", [128, 4, L], BF16))
        qr = es.enter_context(nc.sbuf_tensor("a_qr", [128, 4, L], BF16))
        kn = es.enter_context(nc.sbuf_tensor("a_kn", [128, 4, L], BF16))
        kr = es.enter_context(nc.sbuf_tensor("a_kr", [128, 4, L], BF16))
        vv = es.enter_context(nc.sbuf_tensor("a_vv", [128, 16, 512], BF16))
        pT = [es.enter_context(nc.sbuf_tensor(f"a_pT{i}", [128, 512], BF16)) for i in range(2)]
        acc_o = es.enter_context(nc.sbuf_tensor("a_acco", [128, 16, 512], F32)) if cfg.P > 1 else None
        acc_d = es.enter_context(nc.sbuf_tensor("a_accd", [128, 16, 512], F32)) if cfg.P > 1 else None
        rec = es.enter_context(nc.sbuf_tensor("a_rec", [128, 512], F32))
        ot = [es.enter_context(nc.sbuf_tensor(f"a_ot{i}", [128, 512], BF16)) for i in range(2)]
        onesB = consts['onesB']
        for sg in range(cfg.NS):
            ctx = chain_of(cfg, sg)
            if sg == 0:
                cx.op('dve', lambda e: e.memset(qr[64:128, :, :], 0.0), w=["a_qr"])
                cx.op('pool', lambda e: e.memset(kr[64:128, :, :], 0.0), w=["a_kr"])
                cx.epoch()
            for h in range(4):
                cx.dma('sp', lambda e, h=h, sg=sg: e.dma_start(out=qn[:, h, :], in_=q_d[h, 0:128, sg * L:(sg + 1) * L]), w=["a_qn"])
                cx.dma('sp', lambda e, h=h, sg=sg: e.dma_start(out=qr[0:64, h, :], in_=q_d[h, 128:192, sg * L:(sg + 1) * L]), w=["a_qr"])
            for ki, ks in enumerate(ctx):
                for h in range(4):
                    cx.dma('sp', lambda e, h=h, ks=ks: e.dma_start(out=kn[:, h, :], in_=k_d[h, 0:128, ks * L:(ks + 1) * L]), w=["a_kn"])
                    cx.dma('sp', lambda e, h=h, ks=ks: e.dma_start(out=kr[0:64, h, :], in_=k_d[h, 128:192, ks * L:(ks + 1) * L]), w=["a_kr"])
                for j4 in range(4):
                    cx.dma('sp', lambda e, j4=j4, ks=ks: e.dma_start(out=vv[:, j4 * 4:(j4 + 1) * 4, :], in_=v_d[ks * L + j4 * 512:ks * L + (j4 + 1) * 512, :].rearrange("(j p) n -> p j n", p=128)), w=["a_vv"])
                for qb in range(4):
                    for h in range(4):
                        i = qb * 4 + h
                        po, pd = ps[2 + (i % 2)], ps[4 + (i % 2)]
                        pok, pdk = f"ps{2 + (i % 2)}", f"ps{4 + (i % 2)}"
                        def scores(kt, h=h, qb=qb):
                            sb = kt % 2
                            pS = ps[sb]
                            cx.op('pe', lambda e: e.matmul(pS[:, :], lhsT=kn[:, h, kt * 128:(kt + 1) * 128], rhs=qn[:, h, qb * 512:(qb + 1) * 512], start=True, stop=False),
                                  r=["a_kn", "a_qn"], w=[f"ps{sb}"])
                            cx.op('pe', lambda e: e.matmul(pS[:, :], lhsT=kr[:, h, kt * 128:(kt + 1) * 128], rhs=qr[:, h, qb * 512:(qb + 1) * 512], start=False, stop=True),
                                  r=["a_kr", "a_qr"], w=[f"ps{sb}"])
                        scores(0)
                        for kt in range(16):
                            sb = kt % 2
                            pS = ps[sb]
                            if kt + 1 < 16:
                                scores(kt + 1)
                            cx.op('act', lambda e, sb=sb, pS=pS: e.activation(out=pT[sb][:, :], in_=pS[:, :], func=AF.Exp), r=[f"ps{sb}"], w=[f"a_pT{sb}"])
                            cx.op('pe', lambda e, po=po, h=h, kt=kt, sb=sb: e.matmul(po[:, :], lhsT=vv[:, kt, h * 128:(h + 1) * 128], rhs=pT[sb][:, :], start=(kt == 0), stop=(kt == 15)),
                                  r=["a_vv", f"a_pT{sb}"], w=[pok])
                            cx.op('pe', lambda e, pd=pd, sb=sb, kt=kt: e.matmul(pd[:, :], lhsT=onesB[:, :], rhs=pT[sb][:, :], start=(kt == 0), stop=(kt == 15)),
                                  r=["onesB", f"a_pT{sb}"], w=[pdk])
                        last = (ki == len(ctx) - 1)
                        if len(ctx) > 1:
                            if ki == 0:
                                cx.op('act', lambda e, po=po, i=i: e.activation(out=acc_o[:, i, :], in_=po[:, :], func=AF.Copy), r=[pok], w=["a_acco"])
                                cx.op('dve', lambda e, pd=pd, i=i: e.tensor_copy(acc_d[:, i, :], pd[:, :]), r=[pdk], w=["a_accd"])
                            else:
                                cx.op('dve', lambda e, po=po, i=i: e.tensor_tensor(out=acc_o[:, i, :], in0=po[:, :], in1=acc_o[:, i, :], op=ALU.add), r=[pok, "a_acco"], w=["a_acco"])
                                cx.op('dve', lambda e, pd=pd, i=i: e.tensor_tensor(out=acc_d[:, i, :], in0=pd[:, :], in1=acc_d[:, i, :], op=ALU.add), r=[pdk, "a_accd"], w=["a_accd"])
                        if last:
                            ob = i % 2
                            if len(ctx) > 1:
                                cx.op('dve', lambda e, i=i: e.reciprocal(rec[:, :], acc_d[:, i, :]), r=["a_accd"], w=["a_rec"])
                                cx.op('dve', lambda e, i=i, ob=ob: e.tensor_tensor(out=ot[ob][:, :], in0=acc_o[:, i, :], in1=rec[:, :], op=ALU.mult), r=["a_acco", "a_rec"], w=[f"a_ot{ob}"])
                            else:
                                cx.op('dve', lambda e, pd=pd: e.reciprocal(rec[:, :], pd[:, :]), r=[pdk], w=["a_rec"])
                                cx.op('dve', lambda e, po=po, ob=ob: e.tensor_tensor(out=ot[ob][:, :], in0=po[:, :], in1=rec[:, :], op=ALU.mult), r=[pok, "a_rec"], w=[f"a_ot{ob}"])
                            c0 = sg * L + qb * 512
                            cx.dma('sp', lambda e, h=h, ob=ob, c0=c0: e.dma_start(out=om_d[h, :, c0:c0 + 512], in_=ot[ob][:, :]), r=[f"a_ot{ob}"], w=[f"d:om:{sg}"])
                cx.epoch()


class Tail:
    def __init__(s, cx, es, pfx, w_out_src, router_src, gffn_col, stg):
        nc = cx.nc
        s.cx, s.pfx = cx, pfx
        s.wo = load_w(cx, es, f"{pfx}_wo", w_out_src, D, D, stg=stg)
        s.wr = es.enter_context(nc.sbuf_tensor(f"{pfx}_wr", [128, 8, NE], F32))
        cx.dma('sp', lambda e: e.dma_start(out=s.wr[:, :, :], in_=router_src.rearrange("(k p) n -> p k n", p=128)), w=[f"{pfx}_wr"])
        for k in range(8):
            cx.op('dve', lambda e, k=k: e.tensor_scalar(s.wr[:, k, :], s.wr[:, k, :], gffn_col[:, k:k + 1], None, op0=ALU.mult), r=[f"{pfx}_wr"], w=[f"{pfx}_wr"])
        s.xt = [es.enter_context(nc.sbuf_tensor(f"{pfx}_xt{i}", [128, D], F32)) for i in range(2)]
        s.hn = es.enter_context(nc.sbuf_tensor(f"{pfx}_hn", [128, D], F32))
        s.hnb = es.enter_context(nc.sbuf_tensor(f"{pfx}_hnb", [128, D], BF16))
        s.hnT = es.enter_context(nc.sbuf_tensor(f"{pfx}_hnT", [128, 8, 128], F32))
        s.sm = es.enter_context(nc.sbuf_tensor(f"{pfx}_sm", [128, 4], F32))
        s.ee = es.enter_context(nc.sbuf_tensor(f"{pfx}_ee", [128, NE], F32))

    def run(s, mix, mk, c_off, t0, x_src, x1_d, hn_d, aff_sb, aff_d, consts, ps):
        cx, pfx = s.cx, s.pfx
        identF = consts['identF']
        for j in range(4):
            b = j % 2
            xt, xk = s.xt[b], f"{pfx}_xt{b}"
            r0 = t0 + j * 128
            cx.dma('sp', lambda e, xt=xt, r0=r0: e.dma_start(out=xt[:, :], in_=x_src[r0:r0 + 128, :]), w=[xk])
            for dh in range(2):
                po, pk = ps[dh], f"ps{dh}"
                for k in range(8):
                    cx.op('pe', lambda e, po=po, k=k, j=j, dh=dh: e.matmul(po[:, :], lhsT=mix[:, k, c_off + j * 128:c_off + (j + 1) * 128], rhs=s.wo[:, k, dh * 512:(dh + 1) * 512], start=(k == 0), stop=(k == 7)),
                          r=[mk, f"{pfx}_wo"], w=[pk])
                cx.op('dve', lambda e, po=po, dh=dh, xt=xt: e.tensor_tensor(out=xt[:, dh * 512:(dh + 1) * 512], in0=po[:, :], in1=xt[:, dh * 512:(dh + 1) * 512], op=ALU.add), r=[pk, xk], w=[xk])
            cx.dma('sp', lambda e, xt=xt, r0=r0: e.dma_start(out=x1_d[r0:r0 + 128, :], in_=xt[:, :]), r=[xk], w=[f"d:x1:{r0}"])
            cx.op('act', lambda e, xt=xt: e.activation(out=s.hn[:, :], in_=xt[:, :], func=AF.Square, accum_out=s.sm[:, 0:1]), r=[xk], w=[f"{pfx}_hn", f"{pfx}_sm"])
            rstd_from_ssq(cx, (s.sm[:, 1:2], f"{pfx}_sm"), (s.sm[:, 0:1], f"{pfx}_sm"), D, None)
            cx.op('dve', lambda e, xt=xt: e.tensor_scalar(s.hn[:, :], xt[:, :], s.sm[:, 1:2], None, op0=ALU.mult), r=[xk, f"{pfx}_sm"], w=[f"{pfx}_hn"])
            cx.op('act', lambda e: e.activation(out=s.hnb[:, :], in_=s.hn[:, :], func=AF.Copy), r=[f"{pfx}_hn"], w=[f"{pfx}_hnb"])
            cx.dma('sp', lambda e, r0=r0: e.dma_start(out=hn_d[r0:r0 + 128, :], in_=s.hnb[:, :]), r=[f"{pfx}_hnb"], w=[f"d:hn:{r0}"])
            for half in range(2):
                pt, pk = ps[2 + half], f"ps{2 + half}"
                for kk in range(4):
                    k = half * 4 + kk
                    cx.op('pe', lambda e, pt=pt, kk=kk, k=k: e.transpose(pt[:, kk * 128:(kk + 1) * 128], s.hn[:, k * 128:(k + 1) * 128], identF[:, :]), r=[f"{pfx}_hn", "identF"], w=[pk])
                if half == 0:
                    cx.op('act', lambda e, pt=pt: e.activation(out=s.hnT[:, 0:4, :], in_=pt[:, :].rearrange("p (k t) -> p k t", k=4), func=AF.Copy), r=[pk], w=[f"{pfx}_hnT"])
                else:
                    cx.op('dve', lambda e, pt=pt: e.tensor_copy(s.hnT[:, 4:8, :], pt[:, :].rearrange("p (k t) -> p k t", k=4)), r=[pk], w=[f"{pfx}_hnT"])
            pl = ps[4]
            for k in range(8):
                cx.op('pe', lambda e, k=k: e.matmul(pl[:, 0:NE], lhsT=s.hnT[:, k, :], rhs=s.wr[:, k, :], start=(k == 0), stop=(k == 7)), r=[f"{pfx}_hnT", f"{pfx}_wr"], w=["ps4"])
            cx.op('act', lambda e: e.activation(out=s.ee[:, :], in_=pl[:, 0:NE], func=AF.Exp, accum_out=s.sm[:, 2:3]), r=["ps4"], w=[f"{pfx}_ee", f"{pfx}_sm"])
            cx.op('dve', lambda e: e.reciprocal(s.sm[:, 3:4], s.sm[:, 2:3]), r=[f"{pfx}_sm"], w=[f"{pfx}_sm"])
            ti = r0 // 128
            cx.op('dve', lambda e, ti=ti: e.tensor_scalar(aff_sb[:, ti, :], s.ee[:, :], s.sm[:, 3:4], None, op0=ALU.mult), r=[f"{pfx}_ee", f"{pfx}_sm"], w=["aff_sb"])
            cx.dma('sp', lambda e, ti=ti, r0=r0: e.dma_start(out=aff_d[r0:r0 + 128, 0:NE], in_=aff_sb[:, ti, :]), r=["aff_sb"], w=[f"d:aff:{r0}"])


def pass_combine(cx, cfg, x_src, hT_d, o_d, om_d, x1_d, hn_d, aff_sb, aff_d, ins, consts, ps, gmix, gffn):
    nc = cx.nc
    with ExitStack() as es:
        stg = [es.enter_context(nc.sbuf_tensor(f"c_stg{i}", [128, 1024], F32)) for i in range(2)]
        wg = load_w(cx, es, "c_wg", ins["ev_w_in"][:, 1024:1536], D, 512, gcol=gmix, stg=stg)
        tail = Tail(cx, es, "c", ins["ev_w_out"], ins["moe_router"][0], gffn, stg)
        gain = es.enter_context(nc.sbuf_tensor("c_gain", [128, 1], F32))
        cx.dma('sp', lambda e: e.dma_start(out=gain[:, :], in_=ins["hg_out_gain"].rearrange("(p o) -> p o", o=1)), w=["c_gain"])
        hb = es.enter_context(nc.sbuf_tensor("c_hb", [128, 8, 512], BF16))
        of = es.enter_context(nc.sbuf_tensor("c_of", [64, 8, 512], BF16))
        ob = es.enter_context(nc.sbuf_tensor("c_ob", [64, 8, 512], BF16))
        sg_ = es.enter_context(nc.sbuf_tensor("c_sg", [128, 512], F32))
        ohf = es.enter_context(nc.sbuf_tensor("c_ohf", [128, 512], F32))
        sq = es.enter_context(nc.sbuf_tensor("c_sq", [128, 512], BF16))
        r1 = es.enter_context(nc.sbuf_tensor("c_r1", [128, 512], F32))
        mix = es.enter_context(nc.sbuf_tensor("c_mix", [128, 8, 512], BF16))
        identB, J64, onesB = consts['identB'], consts['J64'], consts['onesB']
        for sg in range(cfg.NS):
            if sg < cfg.P:
                cbase, nchain, rbase = sg * 32, cfg.P * 32, 0
            else:
                cbase, nchain, rbase = 0, 32, sg * L
            for blk in range(4):
                t0 = sg * L + blk * 512
                cx.dma('sp', lambda e, t0=t0: e.dma_start(out=hb[:, :, :], in_=hT_d[:, :, t0:t0 + 512].rearrange("k p t -> p k t")), w=["c_hb"])
                cx.dma('sp', lambda e, t0=t0: e.dma_start(out=of[:, :, :], in_=o_d[0][t0:t0 + 512, :].rearrange("(c t) n -> t c n", t=64)), w=["c_of"])
                c_lo = cbase + blk * 8
                rr = rbase + (nchain - 1 - c_lo - 7) * 64
                cx.dma('sp', lambda e, rr=rr: e.dma_start(out=ob[:, :, :], in_=o_d[1][rr:rr + 512, :].rearrange("(c t) n -> t c n", t=64)), w=["c_ob"])
                cx.dma('sp', lambda e, t0=t0: e.dma_start(out=mix[:, 4:8, :], in_=om_d[:, :, t0:t0 + 512].rearrange("h p t -> p h t")), w=["c_mix"])
                for h in range(4):
                    pg, po, pss = ps[0], ps[1], ps[2]
                    for k in range(8):
                        cx.op('pe', lambda e, k=k, h=h: e.matmul(pg[:, :], lhsT=wg[:, k, h * 128:(h + 1) * 128], rhs=hb[:, k, :], start=(k == 0), stop=(k == 7)), r=["c_wg", "c_hb"], w=["ps0"])
                    cx.op('act', lambda e: e.activation(out=sg_[:, :], in_=pg[:, :], func=AF.Silu), r=["ps0"], w=["c_sg"])
                    for i in range(8):
                        cx.op('pe', lambda e, i=i, h=h: e.matmul(po[:, i * 64:(i + 1) * 64], lhsT=of[:, i, h * 128:(h + 1) * 128], rhs=identB[0:64, 0:64], start=True, stop=False), r=["c_of", "identB"], w=["ps1"])
                        cx.op('pe', lambda e, i=i, h=h: e.matmul(po[:, i * 64:(i + 1) * 64], lhsT=ob[:, 7 - i, h * 128:(h + 1) * 128], rhs=J64[:, :], start=False, stop=True), r=["c_ob", "J64"], w=["ps1"])
                    cx.op('act', lambda e: e.activation(out=ohf[:, :], in_=po[:, :], func=AF.Copy), r=["ps1"], w=["c_ohf"])
                    cx.op('dve', lambda e: e.tensor_tensor(out=sq[:, :], in0=po[:, :], in1=ohf[:, :], op=ALU.mult), r=["ps1", "c_ohf"], w=["c_sq"])
                    cx.op('pe', lambda e: e.matmul(pss[:, :], lhsT=onesB[:, :], rhs=sq[:, :], start=True, stop=True), r=["onesB", "c_sq"], w=["ps2"])
                    rstd_tile(cx, r1[:, :], "c_r1", pss[:, :], "ps2", 128)
                    cx.op('dve', lambda e: e.tensor_tensor(out=ohf[:, :], in0=ohf[:, :], in1=r1[:, :], op=ALU.mult), r=["c_ohf", "c_r1"], w=["c_ohf"])
                    cx.op('dve', lambda e, h=h: e.scalar_tensor_tensor(out=mix[:, h, :], in0=ohf[:, :], scalar=gain[:, 0:1], in1=sg_[:, :], op0=ALU.mult, op1=ALU.mult), r=["c_ohf", "c_gain", "c_sg"], w=["c_mix"])
                tail.run(mix, "c_mix", 0, t0, x_src, x1_d, hn_d, aff_sb, aff_d, consts, ps)
            cx.epoch()


def pass_moe(cx, cfg, layer, aff_sb, aff_d, hn_d, xacc_d, pinc_d, ins, consts, ps, gffn):
    nc = cx.nc
    NT = cfg.NT
    groups = [(a * (L // 128), b * (L // 128)) for (a, b) in cfg.groups if b > a]
    caps = [((b - a) * 128) // 8 for (a, b) in groups]
    nst = [c // 128 for c in caps]
    NSL = sum(nst)
    ntmax = max(b - a for (a, b) in groups)
    pfx = f"e{layer}"
    with ExitStack() as es0:
        idx_sb = es0.enter_context(nc.sbuf_tensor(f"{pfx}_idx", [128, NE, NSL], I32))
        with ExitStack() as es:
            thr = es.enter_context(nc.sbuf_tensor(f"{pfx}_thr", [128, NE], F32))
            hi = es.enter_context(nc.sbuf_tensor(f"{pfx}_hi", [128, NE], F32))
            mid = es.enter_context(nc.sbuf_tensor(f"{pfx}_mid", [128, NE], F32))
            sel = es.enter_context(nc.sbuf_tensor(f"{pfx}_sel", [128, NE], F32))
            d1 = es.enter_context(nc.sbuf_tensor(f"{pfx}_d1", [128, NE], F32))
            cntp = es.enter_context(nc.sbuf_tensor(f"{pfx}_cntp", [128, NE], F32))
            cmp_ = es.enter_context(nc.sbuf_tensor(f"{pfx}_cmp", [128, ntmax, NE], BF16))
            tot = es.enter_context(nc.sbuf_tensor(f"{pfx}_tot", [128, NE, ntmax], F32))
            incl = es.enter_context(nc.sbuf_tensor(f"{pfx}_incl", [128, NE, ntmax], F32))
            pin = [es.enter_context(nc.sbuf_tensor(f"{pfx}_pin{i}", [128, 4, 128], F32)) for i in range(2)]
            junk = [es.enter_context(nc.sbuf_tensor(f"{pfx}_junk{i}", [128, max(ntmax, 128)], F32)) for i in range(4)]
            ts = [es.enter_context(nc.sbuf_tensor(f"{pfx}_ts{i}", [128, 8], F32)) for i in range(4)]
            rowi = [es.enter_context(nc.sbuf_tensor(f"{pfx}_rowi{i}", [128, 1], I32)) for i in range(4)]
            prow = [es.enter_context(nc.sbuf_tensor(f"{pfx}_prow{i}", [128, 128], F32)) for i in range(4)]
            onesB, onesF, linc, slotid, ones512 = consts['onesB'], consts['onesF'], consts['linc'], consts['slotid'], consts['ones512']
            soff = 0
            for gi, (tg0, tg1) in enumerate(groups):
                nt = tg1 - tg0
                kk = float(caps[gi])
                cx.op('dve', lambda e: e.memset(thr[:, :], 0.0), w=[f"{pfx}_thr"])
                cx.op('dve', lambda e: e.memset(hi[:, :], 1.0), w=[f"{pfx}_hi"])
                for it in range(30):
                    cx.op('dve', lambda e: e.tensor_tensor(out=mid[:, :], in0=thr[:, :], in1=hi[:, :], op=ALU.add), r=[f"{pfx}_thr", f"{pfx}_hi"], w=[f"{pfx}_mid"])
                    cx.op('dve', lambda e: e.tensor_scalar(mid[:, :], mid[:, :], 0.5, None, op0=ALU.mult), r=[f"{pfx}_mid"], w=[f"{pfx}_mid"])
                    cx.op('dve', lambda e, tg0=tg0, tg1=tg1, nt=nt: e.tensor_tensor(out=cmp_[:, 0:nt, :], in0=aff_sb[:, tg0:tg1, :], in1=mid[:, :].unsqueeze(1).to_broadcast([128, nt, NE]), op=ALU.is_ge),
                          r=["aff_sb", f"{pfx}_mid"], w=[f"{pfx}_cmp"])
                    cx.op('dve', lambda e, nt=nt: e.tensor_reduce(out=cntp[:, :], in_=cmp_[:, 0:nt, :].rearrange("p t e -> p e t"), axis=AX.X, op=ALU.add), r=[f"{pfx}_cmp"], w=[f"{pfx}_cntp"])
                    cx.op('pe', lambda e: e.matmul(ps[0][:, 0:NE], lhsT=onesF[:, :], rhs=cntp[:, :], start=True, stop=True), r=["onesF", f"{pfx}_cntp"], w=["ps0"])
                    cx.op('dve', lambda e, kk=kk: e.tensor_scalar(sel[:, :], ps[0][:, 0:NE], kk, None, op0=ALU.is_ge), r=["ps0"], w=[f"{pfx}_sel"])
                    cx.op('dve', lambda e: e.tensor_tensor(out=d1[:, :], in0=mid[:, :], in1=thr[:, :], op=ALU.subtract), r=[f"{pfx}_mid", f"{pfx}_thr"], w=[f"{pfx}_d1"])
                    cx.op('dve', lambda e: e.tensor_tensor(out=d1[:, :], in0=d1[:, :], in1=sel[:, :], op=ALU.mult), r=[f"{pfx}_d1", f"{pfx}_sel"], w=[f"{pfx}_d1"])
                    cx.op('dve', lambda e: e.tensor_tensor(out=thr[:, :], in0=thr[:, :], in1=d1[:, :], op=ALU.add), r=[f"{pfx}_thr", f"{pfx}_d1"], w=[f"{pfx}_thr"])
                    cx.op('dve', lambda e: e.tensor_tensor(out=d1[:, :], in0=hi[:, :], in1=mid[:, :], op=ALU.subtract), r=[f"{pfx}_hi", f"{pfx}_mid"], w=[f"{pfx}_d1"])
                    cx.op('dve', lambda e: e.tensor_tensor(out=d1[:, :], in0=d1[:, :], in1=sel[:, :], op=ALU.mult), r=[f"{pfx}_d1", f"{pfx}_sel"], w=[f"{pfx}_d1"])
                    cx.op('dve', lambda e: e.tensor_tensor(out=hi[:, :], in0=mid[:, :], in1=d1[:, :], op=ALU.add), r=[f"{pfx}_mid", f"{pfx}_d1"], w=[f"{pfx}_hi"])
                if MSTOP <= 1:
                    cx.epoch(); continue
                cx.op('dve', lambda e, tg0=tg0, tg1=tg1, nt=nt: e.tensor_tensor(out=cmp_[:, 0:nt, :], in0=aff_sb[:, tg0:tg1, :], in1=thr[:, :].unsqueeze(1).to_broadcast([128, nt, NE]), op=ALU.is_ge),
                      r=["aff_sb", f"{pfx}_thr"], w=[f"{pfx}_cmp"])
                for ci, a in enumerate(range(0, nt, 32)):
                    na = min(32, nt - a)
                    ncol = na * NE
                    mb = cmp_[:, a:a + na, :].rearrange("p t e -> p (t e)")
                    cx.op('pe', lambda e, mb=mb, ncol=ncol: e.matmul(ps[1][:, 0:ncol], lhsT=onesB[:, :], rhs=mb, start=True, stop=True), r=["onesB", f"{pfx}_cmp"], w=["ps1"])
                    cx.op('act', lambda e, a=a, na=na, ncol=ncol: e.activation(out=tot[:, :, a:a + na], in_=ps[1][:, 0:ncol].rearrange("p (t e) -> p e t", e=NE), func=AF.Copy), r=["ps1"], w=[f"{pfx}_tot"])
                    pb = ci % 2
                    for i in range(ncol // 128):
                        cx.op('pe', lambda e, mb=mb, i=i, pb=pb: e.matmul(ps[2 + pb][:, i * 128:(i + 1) * 128], lhsT=mb[:, i * 128:(i + 1) * 128], rhs=linc[:, :], start=True, stop=True), r=[f"{pfx}_cmp", "linc"], w=[f"ps{2 + pb}"])
                    ni = ncol // 128
                    cx.op('dve', lambda e, pb=pb, ni=ni: e.tensor_copy(pin[pb][:, 0:ni, :], ps[2 + pb][:, 0:ni * 128].rearrange("p (i t) -> p i t", t=128)), r=[f"ps{2 + pb}"], w=[f"{pfx}_pin{pb}"])
                    rb = (tg0 + a) * NE
                    cx.dma('sp', lambda e, pb=pb, ni=ni, rb=rb: e.dma_start(out=pinc_d[rb:rb + ni * 128, :].rearrange("(i p) t -> p i t", p=128), in_=pin[pb][:, 0:ni, :]), r=[f"{pfx}_pin{pb}"], w=["d:pinc"])
                for ex in range(NE):
                    cx.op('dve', lambda e, ex=ex, nt=nt: e.tensor_tensor_scan(out=incl[:, ex, 0:nt], data0=ones512[:, 0:nt], data1=tot[:, ex, 0:nt], initial=0.0, op0=ALU.mult, op1=ALU.add),
                          r=[f"{pfx}_tot", "ones512"], w=[f"{pfx}_incl"])
                if MSTOP <= 2:
                    cx.epoch(); continue
                n_ = 0
                for ex in range(NE):
                    for J in range(nst[gi]):
                        b = n_ % 4
                        n_ += 1
                        t_, tk = ts[b], f"{pfx}_ts{b}"
                        jv = slotid[:, J:J + 1]
                        cx.op('dve', lambda e, ex=ex, nt=nt, b=b, t_=t_, jv=jv: e.tensor_scalar(junk[b][:, 0:nt], incl[:, ex, 0:nt], jv, None, op0=ALU.is_le, op1=ALU.add, accum_out=t_[:, 0:1]),
                              r=[f"{pfx}_incl", "slotid"], w=[f"{pfx}_junk{b}", tk])
                        cx.op('dve', lambda e, ex=ex, nt=nt, b=b, t_=t_, jv=jv: e.scalar_tensor_tensor(out=junk[b][:, 0:nt], in0=incl[:, ex, 0:nt], scalar=jv, in1=tot[:, ex, 0:nt], op0=ALU.is_le, op1=ALU.mult, accum_out=t_[:, 1:2]),
                              r=[f"{pfx}_incl", f"{pfx}_tot", "slotid"], w=[f"{pfx}_junk{b}", tk])
                        cx.op('dve', lambda e, t_=t_, jv=jv: e.tensor_tensor(out=t_[:, 2:3], in0=jv, in1=t_[:, 1:2], op=ALU.subtract), r=[tk, "slotid"], w=[tk])
                        cx.op('dve', lambda e, t_=t_, b=b, ex=ex, tg0=tg0: e.tensor_scalar(rowi[b][:, :], t_[:, 0:1], float(NE), float(ex + tg0 * NE), op0=ALU.mult, op1=ALU.add), r=[tk], w=[f"{pfx}_rowi{b}"])
                        cx.dma('pool', lambda e, b=b: e.indirect_dma_start(out=prow[b][:, :], out_offset=None, in_=pinc_d[:, :], in_offset=bass.IndirectOffsetOnAxis(ap=rowi[b][:, 0:1], axis=0), bounds_check=NT * NE - 1, oob_is_err=False),
                               r=[f"{pfx}_rowi{b}", "d:pinc"], w=[f"{pfx}_prow{b}"])
                        cx.op('dve', lambda e, b=b, t_=t_: e.tensor_scalar(junk[b][:, 0:128], prow[b][:, :], t_[:, 2:3], None, op0=ALU.is_le, op1=ALU.add, accum_out=t_[:, 3:4]),
                              r=[f"{pfx}_prow{b}", tk], w=[f"{pfx}_junk{b}", tk])
                        cx.op('dve', lambda e, t_=t_, tg0=tg0: e.tensor_scalar(t_[:, 4:5], t_[:, 0:1], 128.0, float(tg0 * 128), op0=ALU.mult, op1=ALU.add), r=[tk], w=[tk])
                        cx.op('dve', lambda e, t_=t_, ex=ex, J=J, soff=soff: e.tensor_tensor(out=idx_sb[:, ex, soff + J:soff + J + 1], in0=t_[:, 4:5], in1=t_[:, 3:4], op=ALU.add), r=[tk], w=[f"{pfx}_idx"])
                soff += nst[gi]
                cx.epoch()
        if consts.get('idx_dbg') is not None and layer == 0:
            cx.dma('sp', lambda e: e.dma_start(out=consts['idx_dbg'][:, :], in_=idx_sb[:, :, :].rearrange("p e s -> p (e s)")), r=[f"{pfx}_idx"], w=["d:idxdbg"])
            cx.epoch()
        if MSTOP <= 3:
            return
        with ExitStack() as es:
            stg = [es.enter_context(nc.sbuf_tensor(f"{pfx}_stg{i}", [128, 1024], F32)) for i in range(2)]
            xe = [es.enter_context(nc.sbuf_tensor(f"{pfx}_xe{i}", [128, D], BF16)) for i in range(4)]
            ga = [es.enter_context(nc.sbuf_tensor(f"{pfx}_ga{i}", [128, 128], F32)) for i in range(4)]
            xeT = es.enter_context(nc.sbuf_tensor(f"{pfx}_xeT", [128, 8, 512], BF16))
            hid = es.enter_context(nc.sbuf_tensor(f"{pfx}_hid", [128, 8, 512], BF16))
            sl2 = [es.enter_context(nc.sbuf_tensor(f"{pfx}_sl{i}", [128, 512], F32)) for i in range(2)]
            ye = [es.enter_context(nc.sbuf_tensor(f"{pfx}_ye{i}", [128, D], F32)) for i in range(2)]
            identB = consts['identB']
            psb = consts['psb']
            blocks = [list(range(a, min(a + 4, NSL))) for a in range(0, NSL, 4)]
            for ex in range(NEXP):
                with ExitStack() as esw:
                    wg = load_w(cx, esw, f"{pfx}_wg{ex}", ins["moe_w_gate"][layer, ex], D, D, gcol=gffn, stg=stg)
                    wu = load_w(cx, esw, f"{pfx}_wu{ex}", ins["moe_w_up"][layer, ex], D, D, gcol=gffn, stg=stg)
                    wd = load_w(cx, esw, f"{pfx}_wd{ex}", ins["moe_w_down"][layer, ex], D, D, stg=stg)
                    for blk in blocks:
                        N = len(blk) * 128
                        for si, sidx in enumerate(blk):
                            b = si % 4
                            ix = idx_sb[:, ex, sidx:sidx + 1]
                            cx.dma('pool', lambda e, b=b, ix=ix: e.indirect_dma_start(out=xe[b][:, :], out_offset=None, in_=hn_d[:, :], in_offset=bass.IndirectOffsetOnAxis(ap=ix, axis=0), bounds_check=cfg.T - 1, oob_is_err=False),
                                   r=[f"{pfx}_idx", "d:hn"], w=[f"{pfx}_xe{b}"])
                            cx.dma('pool', lambda e, b=b, ix=ix: e.indirect_dma_start(out=ga[b][:, :], out_offset=None, in_=aff_d[:, :], in_offset=bass.IndirectOffsetOnAxis(ap=ix, axis=0), bounds_check=cfg.T - 1, oob_is_err=False),
                                   r=[f"{pfx}_idx", "d:aff"], w=[f"{pfx}_ga{b}"])
                            for half in range(2):
                                pt, pk = psb[half], f"psb{half}"
                                for kq in range(4):
                                    k = half * 4 + kq
                                    cx.op('pe', lambda e, pt=pt, kq=kq, k=k, b=b: e.transpose(pt[:, kq * 128:(kq + 1) * 128], xe[b][:, k * 128:(k + 1) * 128], identB[:, :]), r=[f"{pfx}_xe{b}", "identB"], w=[pk])
                                eng = 'act' if half == 0 else 'dve'
                                if half == 0:
                                    cx.op('act', lambda e, pt=pt, si=si: e.activation(out=xeT[:, 0:4, si * 128:(si + 1) * 128], in_=pt[:, :].rearrange("p (k t) -> p k t", k=4), func=AF.Copy), r=[pk], w=[f"{pfx}_xeT"])
                                else:
                                    cx.op('dve', lambda e, pt=pt, si=si: e.tensor_copy(xeT[:, 4:8, si * 128:(si + 1) * 128], pt[:, :].rearrange("p (k t) -> p k t", k=4)), r=[pk], w=[f"{pfx}_xeT"])
                        if FSTOP <= 1:
                            continue
                        for f in range(8):
                            fb = (f % 2) * 2
                            pg, pu = ps[fb], ps[fb + 1]
                            pgk, puk = f"ps{fb}", f"ps{fb + 1}"
                            slb, slk = sl2[f % 2], f"{pfx}_sl{f % 2}"
                            for k in range(8):
                                cx.op('pe', lambda e, k=k, f=f, N=N, pg=pg: e.matmul(pg[:, 0:N], lhsT=wg[:, k, f * 128:(f + 1) * 128], rhs=xeT[:, k, 0:N], start=(k == 0), stop=(k == 7)), r=[f"{pfx}_wg{ex}", f"{pfx}_xeT"], w=[pgk])
                            for k in range(8):
                                cx.op('pe', lambda e, k=k, f=f, N=N, pu=pu: e.matmul(pu[:, 0:N], lhsT=wu[:, k, f * 128:(f + 1) * 128], rhs=xeT[:, k, 0:N], start=(k == 0), stop=(k == 7)), r=[f"{pfx}_wu{ex}", f"{pfx}_xeT"], w=[puk])
                            cx.op('act', lambda e, N=N, pg=pg, slb=slb: e.activation(out=slb[:, 0:N], in_=pg[:, 0:N], func=AF.Silu), r=[pgk], w=[slk])
                            cx.op('dve', lambda e, f=f, N=N, pu=pu, slb=slb: e.tensor_tensor(out=hid[:, f, 0:N], in0=pu[:, 0:N], in1=slb[:, 0:N], op=ALU.mult), r=[puk, slk], w=[f"{pfx}_hid{f}"])
                        for si, sidx in enumerate(blk):
                            b = si % 4
                            yb = si % 2
                            for dh in range(2):
                                pd, pk = ps[4 + dh], f"ps{4 + dh}"
                                for f in range(8):
                                    cx.op('pe', lambda e, pd=pd, f=f, si=si, dh=dh: e.matmul(pd[:, :], lhsT=hid[:, f, si * 128:(si + 1) * 128], rhs=wd[:, f, dh * 512:(dh + 1) * 512], start=(f == 0), stop=(f == 7)), r=[f"{pfx}_hid{f}", f"{pfx}_wd{ex}"], w=[pk])
                                if dh == 0:
                                    cx.op('act', lambda e, pd=pd, yb=yb, b=b: e.activation(out=ye[yb][:, 0:512], in_=pd[:, :], func=AF.Copy, scale=ga[b][:, ex:ex + 1]), r=[pk, f"{pfx}_ga{b}"], w=[f"{pfx}_ye{yb}"])
                                else:
                                    cx.op('dve', lambda e, pd=pd, yb=yb, b=b: e.tensor_scalar(ye[yb][:, 512:1024], pd[:, :], ga[b][:, ex:ex + 1], None, op0=ALU.mult), r=[pk, f"{pfx}_ga{b}"], w=[f"{pfx}_ye{yb}"])
                            ix = idx_sb[:, ex, sidx:sidx + 1]
                            if FSTOP <= 3:
                                continue
                            cx.dma('pool', lambda e, yb=yb, ix=ix: e.indirect_dma_start(out=xacc_d[:, :], out_offset=bass.IndirectOffsetOnAxis(ap=ix, axis=0), in_=ye[yb][:, :], in_offset=None, bounds_check=cfg.T - 1, oob_is_err=False, compute_op=ALU.add),
                                   r=[f"{pfx}_ye{yb}", f"{pfx}_idx", "d:xacc"], w=["d:xacc"])
                    cx.epoch()


def pass_r1(cx, cfg, hT_d, gl_d, xb_d, ins, consts, ps, gmix):
    nc = cx.nc
    with ExitStack() as es:
        stg = [es.enter_context(nc.sbuf_tensor(f"r1_stg{i}", [128, 1024], F32)) for i in range(2)]
        w = load_w(cx, es, "r1_w", ins["od_w_in"], D, 2048, gcol=gmix, stg=stg)
        hb = es.enter_context(nc.sbuf_tensor("r1_hb", [128, 8, 512], BF16))
        gl = es.enter_context(nc.sbuf_tensor("r1_gl", [128, 8, 512], BF16))
        xb = es.enter_context(nc.sbuf_tensor("r1_xb", [128, 8, 512], F32))
        for sg in range(cfg.NS):
            for blk in range(4):
                t0 = sg * L + blk * 512
                cx.dma('sp', lambda e, t0=t0: e.dma_start(out=hb[:, :, :], in_=hT_d[:, :, t0:t0 + 512].rearrange("k p t -> p k t")), w=["r1_hb"])
                for n in range(16):
                    pp, pk = ps[n % 2], f"ps{n % 2}"
                    for k in range(8):
                        cx.op('pe', lambda e, pp=pp, n=n, k=k: e.matmul(pp[:, :], lhsT=w[:, k, n * 128:(n + 1) * 128], rhs=hb[:, k, :], start=(k == 0), stop=(k == 7)), r=["r1_w", "r1_hb"], w=[pk])
                    if n < 8:
                        cx.op('act', lambda e, pp=pp, n=n: e.activation(out=gl[:, n, :], in_=pp[:, :], func=AF.Gelu), r=[pk], w=["r1_gl"])
                    else:
                        cx.op('dve', lambda e, pp=pp, n=n: e.tensor_copy(xb[:, n - 8, :], pp[:, :]), r=[pk], w=["r1_xb"])
                cx.dma('sp', lambda e, t0=t0: e.dma_start(out=gl_d[:, :, t0:t0 + 512].rearrange("k p t -> p k t"), in_=gl[:, :, :]), r=["r1_gl"], w=[f"d:gl:{sg}"])
                for hx in range(2):
                    cx.dma('sp', lambda e, t0=t0, hx=hx: e.dma_start(out=xb_d[hx][:, :, t0:t0 + 512].rearrange("k p t -> p k t"), in_=xb[:, hx * 4:(hx + 1) * 4, :]), r=["r1_xb"], w=[f"d:xb:{sg}:{hx}"])
            cx.epoch()


def pass_r2(cx, cfg, dirn, xb_d, gl_d, hf_d, x_src, x3_d, hn_d, aff_sb, aff_d, ins, consts, ps, gffn):
    nc = cx.nc
    pf = f"r2{dirn}"
    with ExitStack() as es:
        stg = [es.enter_context(nc.sbuf_tensor(f"{pf}_stg{i}", [128, 1024], F32)) for i in range(2)]
        wa = load_w(cx, es, f"{pf}_wa", ins["rg_w_a"][dirn].rearrange("n k j -> (n k) j"), D, 256, stg=stg)
        wx = load_w(cx, es, f"{pf}_wx", ins["rg_w_x"][dirn].rearrange("n k j -> (n k) j"), D, 256, stg=stg)
        ba = load_cols(cx, es, f"{pf}_ba", ins["rg_b_a"][dirn, :], D)
        bx = load_cols(cx, es, f"{pf}_bx", ins["rg_b_x"][dirn, :], D)
        c8 = load_cols(cx, es, f"{pf}_c8", ins["rg_lambda"][dirn, :], D)
        cx.op('act', lambda e: e.activation(out=c8[:, :], in_=c8[:, :], func=AF.Exp, scale=-1.0), r=[f"{pf}_c8"], w=[f"{pf}_c8"])
        cx.op('act', lambda e: e.activation(out=c8[:, :], in_=c8[:, :], func=AF.Ln, bias=1.0), r=[f"{pf}_c8"], w=[f"{pf}_c8"])
        cx.op('dve', lambda e: e.tensor_scalar(c8[:, :], c8[:, :], -8.0, None, op0=ALU.mult), r=[f"{pf}_c8"], w=[f"{pf}_c8"])
        cw = [load_cols(cx, es, f"{pf}_cw{j}", ins["od_conv_w"][j, :], D) for j in range(4)]
        cb = load_cols(cx, es, f"{pf}_cb", ins["od_conv_b"], D)
        tail = Tail(cx, es, pf, ins["od_w_out"], ins["moe_router"][1], gffn, stg) if dirn else None
        xp = [es.enter_context(nc.sbuf_tensor(f"{pf}_xp0", [128, L + 4], F32))] * 2
        xcb = es.enter_context(nc.sbuf_tensor(f"{pf}_xcb", [128, 8, L], BF16))
        rr = es.enter_context(nc.sbuf_tensor(f"{pf}_r", [128, L], F32))
        ii = es.enter_context(nc.sbuf_tensor(f"{pf}_i", [128, L], F32))
        tt = es.enter_context(nc.sbuf_tensor(f"{pf}_t", [128, L], F32))
        acc = tt
        hh = tt
        carry = es.enter_context(nc.sbuf_tensor(f"{pf}_carry", [128, 8], F32))
        hb16 = [es.enter_context(nc.sbuf_tensor(f"{pf}_hb0", [128, L], BF16))] * 2
        glt = [es.enter_context(nc.sbuf_tensor(f"{pf}_gl0", [128, L], BF16))] * 2 if dirn else None
        yT = es.enter_context(nc.sbuf_tensor(f"{pf}_yT", [128, 8, L], BF16)) if dirn else None
        segs = list(range(cfg.NS))
        if dirn:
            segs = list(reversed(range(cfg.P))) + list(range(cfg.P, cfg.NS))
        for sg in segs:
            inchain = sg < cfg.P
            has_l = inchain and sg > 0
            has_r = inchain and sg < cfg.P - 1
            first = (not inchain) or (sg == (cfg.P - 1 if dirn else 0))
            t0 = sg * L
            if first:
                cx.op('dve', lambda e: e.memset(carry[:, :], 0.0), w=[f"{pf}_carry"])
            for ch in range(8):
                b = ch % 2
                xk = f"{pf}_xp0"
                cx.op('pool', lambda e, b=b: e.memset(xp[b][:, 0:2], 0.0), w=[xk])
                cx.op('pool', lambda e, b=b: e.memset(xp[b][:, L + 2:L + 4], 0.0), w=[xk])
                lo = t0 - 2 if has_l else t0
                hi = t0 + L + 1 if has_r else t0 + L
                c_lo = 0 if has_l else 2
                cx.dma('sp', lambda e, b=b, ch=ch, lo=lo, hi=hi, c_lo=c_lo: e.dma_start(out=xp[b][:, c_lo:c_lo + (hi - lo)], in_=xb_d[ch // 4][ch % 4, :, lo:hi]), w=[xk])
                cx.op('dve', lambda e, b=b, ch=ch: e.tensor_scalar(acc[:, :], xp[b][:, 0:L], cw[0][:, ch:ch + 1], cb[:, ch:ch + 1], op0=ALU.mult, op1=ALU.add), r=[xk, f"{pf}_cw0", f"{pf}_cb"], w=[f"{pf}_t"])
                for j in (1, 2):
                    cx.op('dve', lambda e, b=b, ch=ch, j=j: e.scalar_tensor_tensor(out=acc[:, :], in0=xp[b][:, j:j + L], scalar=cw[j][:, ch:ch + 1], in1=acc[:, :], op0=ALU.mult, op1=ALU.add), r=[xk, f"{pf}_cw{j}", f"{pf}_t"], w=[f"{pf}_t"])
                cx.op('dve', lambda e, b=b, ch=ch: e.scalar_tensor_tensor(out=xcb[:, ch, :], in0=xp[b][:, 3:3 + L], scalar=cw[3][:, ch:ch + 1], in1=acc[:, :], op0=ALU.mult, op1=ALU.add), r=[xk, f"{pf}_cw3", f"{pf}_t"], w=[f"{pf}_xcb"])
            for co in range(8):
                n, jh = co // 2, co % 2
                b = co % 2
                for blk in range(4):
                    pa, px = ps[(blk % 2) * 2], ps[(blk % 2) * 2 + 1]
                    pak, pxk = f"ps{(blk % 2) * 2}", f"ps{(blk % 2) * 2 + 1}"
                    for kc in range(2):
                        cx.op('pe', lambda e, pa=pa, kc=kc, n=n, jh=jh, blk=blk: e.matmul(pa[:, :], lhsT=wa[:, n * 2 + kc, jh * 128:(jh + 1) * 128], rhs=xcb[:, n * 2 + kc, blk * 512:(blk + 1) * 512], start=(kc == 0), stop=(kc == 1)), r=[f"{pf}_wa", f"{pf}_xcb"], w=[pak])
                    for kc in range(2):
                        cx.op('pe', lambda e, px=px, kc=kc, n=n, jh=jh, blk=blk: e.matmul(px[:, :], lhsT=wx[:, n * 2 + kc, jh * 128:(jh + 1) * 128], rhs=xcb[:, n * 2 + kc, blk * 512:(blk + 1) * 512], start=(kc == 0), stop=(kc == 1)), r=[f"{pf}_wx", f"{pf}_xcb"], w=[pxk])
                    cx.op('act', lambda e, pa=pa, blk=blk, co=co: e.activation(out=rr[:, blk * 512:(blk + 1) * 512], in_=pa[:, :], func=AF.Sigmoid, bias=ba[:, co:co + 1]), r=[pak, f"{pf}_ba"], w=[f"{pf}_r"])
                    cx.op('act', lambda e, px=px, blk=blk, co=co: e.activation(out=ii[:, blk * 512:(blk + 1) * 512], in_=px[:, :], func=AF.Sigmoid, bias=bx[:, co:co + 1]), r=[pxk, f"{pf}_bx"], w=[f"{pf}_i"])
                cx.op('act', lambda e, co=co: e.activation(out=rr[:, :], in_=rr[:, :], func=AF.Exp, scale=c8[:, co:co + 1]), r=[f"{pf}_r", f"{pf}_c8"], w=[f"{pf}_r"])
                cx.op('act', lambda e: e.activation(out=tt[:, :], in_=rr[:, :], func=AF.Square), r=[f"{pf}_r"], w=[f"{pf}_t"])
                cx.op('act', lambda e: e.activation(out=tt[:, :], in_=tt[:, :], func=AF.Sqrt, scale=-1.0, bias=1.0), r=[f"{pf}_t"], w=[f"{pf}_t"])
                cx.op('dve', lambda e: e.tensor_tensor(out=ii[:, :], in0=tt[:, :], in1=ii[:, :], op=ALU.mult), r=[f"{pf}_t", f"{pf}_i"], w=[f"{pf}_i"])
                cx.op('dve', lambda e, co=co: e.tensor_tensor(out=ii[:, :], in0=ii[:, :], in1=xcb[:, co, :], op=ALU.mult), r=[f"{pf}_i", f"{pf}_xcb"], w=[f"{pf}_i"])
                if dirn == 0:
                    cx.op('dve', lambda e, co=co: e.tensor_tensor_scan(out=hh[:, :], data0=rr[:, :], data1=ii[:, :], initial=carry[:, co:co + 1], op0=ALU.mult, op1=ALU.add), r=[f"{pf}_r", f"{pf}_i", f"{pf}_carry"], w=[f"{pf}_t"])
                    cx.op('dve', lambda e, co=co: e.tensor_copy(carry[:, co:co + 1], hh[:, L - 1:L]), r=[f"{pf}_t"], w=[f"{pf}_carry"])
                    cx.op('act', lambda e, b=b: e.activation(out=hb16[b][:, :], in_=hh[:, :], func=AF.Copy), r=[f"{pf}_t"], w=[f"{pf}_hb0"])
                    cx.dma('sp', lambda e, b=b, co=co, t0=t0: e.dma_start(out=hf_d[co, :, t0:t0 + L], in_=hb16[b][:, :]), r=[f"{pf}_hb0"], w=[f"d:hf:{sg}"])
                else:
                    cx.op('dve', lambda e, co=co: e.tensor_tensor_scan(out=hh[:, ::-1], data0=rr[:, ::-1], data1=ii[:, ::-1], initial=carry[:, co:co + 1], op0=ALU.mult, op1=ALU.add), r=[f"{pf}_r", f"{pf}_i", f"{pf}_carry"], w=[f"{pf}_t"])
                    cx.op('dve', lambda e, co=co: e.tensor_copy(carry[:, co:co + 1], hh[:, 0:1]), r=[f"{pf}_t"], w=[f"{pf}_carry"])
                    cx.dma('sp', lambda e, b=b, co=co, t0=t0: e.dma_start(out=hb16[b][:, :], in_=hf_d[co, :, t0:t0 + L]), w=[f"{pf}_hb0"])
                    cx.dma('sp', lambda e, b=b, co=co, t0=t0: e.dma_start(out=glt[b][:, :], in_=gl_d[co, :, t0:t0 + L]), w=[f"{pf}_gl0"])
                    cx.op('dve', lambda e, b=b: e.tensor_tensor(out=hh[:, :], in0=hh[:, :], in1=hb16[b][:, :], op=ALU.add), r=[f"{pf}_t", f"{pf}_hb0"], w=[f"{pf}_t"])
                    cx.op('dve', lambda e, b=b, co=co: e.tensor_tensor(out=yT[:, co, :], in0=hh[:, :], in1=glt[b][:, :], op=ALU.mult), r=[f"{pf}_t", f"{pf}_gl0"], w=[f"{pf}_yT"])
            if dirn:
                for blk in range(4):
                    tail.run(yT, f"{pf}_yT", blk * 512, t0 + blk * 512, x_src, x3_d, hn_d, aff_sb, aff_d, consts, ps)
            cx.epoch()


def make_consts(cx, es):
    nc = cx.nc
    c = {}
    iot = es.enter_context(nc.sbuf_tensor("c_iot", [128, 128], F32))
    cx.op('pool', lambda e: e.iota(iot[:, :], [[1, 128]], base=0, channel_multiplier=-1, allow_small_or_imprecise_dtypes=True), w=["c_iot"])
    identF = es.enter_context(nc.sbuf_tensor("identF", [128, 128], F32))
    identB = es.enter_context(nc.sbuf_tensor("identB", [128, 128], BF16))
    cx.op('dve', lambda e: e.tensor_scalar(identF[:, :], iot[:, :], 0.0, None, op0=ALU.is_equal), r=["c_iot"], w=["identF"])
    cx.op('dve', lambda e: e.tensor_copy(identB[:, :], identF[:, :]), r=["identF"], w=["identB"])
    iot2 = es.enter_context(nc.sbuf_tensor("c_iot2", [128, 128], F32))
    cx.op('pool', lambda e: e.iota(iot2[:, :], [[1, 128]], base=0, channel_multiplier=1, allow_small_or_imprecise_dtypes=True), w=["c_iot2"])
    J64 = es.enter_context(nc.sbuf_tensor("J64", [64, 64], BF16))
    cx.op('dve', lambda e: e.tensor_scalar(J64[:, :], iot2[0:64, 0:64], 63.0, None, op0=ALU.is_equal), r=["c_iot2"], w=["J64"])
    cmask = es.enter_context(nc.sbuf_tensor("cmask", [64, 64], F32))
    cx.op('dve', lambda e: e.tensor_scalar(cmask[:, :], iot[0:64, 0:64], 0.0, None, op0=ALU.is_ge), r=["c_iot"], w=["cmask"])
    ltri = es.enter_context(nc.sbuf_tensor("ltri", [128, 128], BF16))
    cx.op('dve', lambda e: e.tensor_scalar(ltri[:, :], iot[:, :], 0.0, None, op0=ALU.is_gt), r=["c_iot"], w=["ltri"])
    onesB = es.enter_context(nc.sbuf_tensor("onesB", [128, 128], BF16))
    cx.op('dve', lambda e: e.memset(onesB[:, :], 1.0), w=["onesB"])
    onesF = es.enter_context(nc.sbuf_tensor("onesF", [128, 128], F32))
    cx.op('dve', lambda e: e.memset(onesF[:, :], 1.0), w=["onesF"])
    smask = es.enter_context(nc.sbuf_tensor("smask", [128, 1024], F32))
    cx.op('dve', lambda e: e.memset(smask[:, :], 1.0), w=["smask"])
    cx.op('dve', lambda e: e.memset(smask[:, :].rearrange("p (c t) -> p c t", t=64)[:, :, 0:1], 0.0), w=["smask"])
    linc = es.enter_context(nc.sbuf_tensor("linc", [128, 128], BF16))
    cx.op('dve', lambda e: e.tensor_scalar(linc[:, :], iot[:, :], 0.0, None, op0=ALU.is_ge), r=["c_iot"], w=["linc"])
    ones512 = es.enter_context(nc.sbuf_tensor("ones512", [128, 512], F32))
    cx.op('dve', lambda e: e.memset(ones512[:, :], 1.0), w=["ones512"])
    slotid = es.enter_context(nc.sbuf_tensor("slotid", [128, 64], F32))
    cx.op('pool', lambda e: e.iota(slotid[:, :], [[128, 64]], base=0, channel_multiplier=1, allow_small_or_imprecise_dtypes=True), w=["slotid"])
    c.update(linc=linc, ones512=ones512, slotid=slotid)
    c.update(identF=identF, identB=identB, J64=J64, cmask=cmask, ltri=ltri, onesB=onesB, onesF=onesF, smask=smask, iot=iot)
    return c


def build(P, B, debug=False, stages=99):
    cfg = Cfg(P, B)
    T = cfg.T
    nc = bass.Bass("TRN2", target_bir_lowering=False)
    dt = lambda name, shape, dtype=F32, kind="ExternalInput": nc.dram_tensor(name, list(shape), dtype, kind=kind).ap()
    x_in = dt("x_in", [T, D])
    ins = {}
    for name, shape in [("norm_mix", [2, D]), ("norm_ffn", [2, D]), ("ev_w_in", [D, 3264]), ("ev_w_out", [D, D]),
                        ("hg_lb_logits", [2, 2, 512]), ("hg_out_gain", [128]), ("mla_q_norm", [384]), ("mla_kv_norm", [256]),
                        ("mla_w_uq", [384, 768]), ("mla_w_ukv", [256, 1024]), ("mla_q_gain", [192]), ("mla_k_gain", [192]),
                        ("od_w_in", [D, 2048]), ("od_conv_w", [4, D]), ("od_conv_b", [D]), ("rg_w_a", [2, 4, 256, 256]),
                        ("rg_b_a", [2, D]), ("rg_w_x", [2, 4, 256, 256]), ("rg_b_x", [2, D]), ("rg_lambda", [2, D]),
                        ("od_w_out", [D, D]), ("moe_router", [2, D, NE]), ("moe_w_gate", [2, NE, D, D]),
                        ("moe_w_up", [2, NE, D, D]), ("moe_w_down", [2, NE, D, D]), ("rope_cos", [64, P * L if P else L]),
                        ("rope_sin", [64, P * L if P else L])]:
        ins[name] = dt(name, shape)
    y_out = dt("y_out", [T, D], kind="ExternalOutput")
    hT_d = dt("hT_d", [8, 128, T], BF16, kind=("ExternalOutput" if debug else "Internal"))
    o_d = [dt(f"o_d{i}", [T, 512], BF16, kind=("ExternalOutput" if debug else "Internal")) for i in range(2)]
    dbg = "ExternalOutput" if debug else "Internal"
    q_d = dt("q_d", [4, 192, T], BF16, kind="Internal")
    k_d = dt("k_d", [4, 192, T], BF16, kind="Internal")
    v_d = dt("v_d", [T, 512], BF16, kind="Internal")
    om_d = dt("om_d", [4, 128, T], BF16, kind=dbg)
    x1_d = y_out
    hn_d = dt("hn_d", [T, D], BF16, kind="Internal")
    aff_d = dt("aff_d", [T, 128], F32, kind=dbg)
    pinc_d = dt("pinc_d", [cfg.NT * NE, 128], F32, kind="Internal")
    gl_d = dt("gl_d", [8, 128, T], BF16, kind="Internal")
    xb_d = [dt(f"xb_d{i}", [4, 128, T], F32, kind="Internal") for i in range(2)]
    hf_d = dt("hf_d", [8, 128, T], BF16, kind="Internal")
    nsl_dbg = sum(((b - a) * L // 8) // 128 for (a, b) in cfg.groups if b > a)
    idx_dbg = dt("idx_dbg", [128, NE * nsl_dbg], I32, kind="ExternalOutput") if debug else None
    with ExitStack() as es:
        cx = Cx(nc, es)
        ps = [es.enter_context(nc.psum_tensor(f"ps{i}", [128, 512], F32)) for i in range(6)]
        psb = [es.enter_context(nc.psum_tensor(f"psb{i}", [128, 512], BF16)) for i in range(2)]
        consts = make_consts(cx, es)
        consts['psb'] = psb
        consts['idx_dbg'] = idx_dbg
        gmix = [load_cols(cx, es, f"gmix{l}", ins["norm_mix"][l, :], D) for l in range(2)]
        gffn = [load_cols(cx, es, f"gffn{l}", ins["norm_ffn"][l, :], D) for l in range(2)]
        lbT = []
        for d_ in range(2):
            l0 = load_cols(cx, es, f"lbl0_{d_}", ins["hg_lb_logits"][d_, 0, :], 512)
            l1 = load_cols(cx, es, f"lbl1_{d_}", ins["hg_lb_logits"][d_, 1, :], 512)
            lb = es.enter_context(nc.sbuf_tensor(f"lb_{d_}", [128, 4], F32))
            lbm = es.enter_context(nc.sbuf_tensor(f"lbm_{d_}", [128, 4], F32))
            cx.op('dve', lambda e, l0=l0, l1=l1, lb=lb: e.tensor_tensor(out=lb[:, :], in0=l0[:, :], in1=l1[:, :], op=ALU.subtract), r=[f"lbl0_{d_}", f"lbl1_{d_}"], w=["lbT"])
            cx.op('act', lambda e, lb=lb: e.activation(out=lb[:, :], in_=lb[:, :], func=AF.Sigmoid), r=["lbT"], w=["lbT"])
            cx.op('dve', lambda e, lb=lb, lbm=lbm: e.tensor_scalar(lbm[:, :], lb[:, :], -1.0, 1.0, op0=ALU.mult, op1=ALU.add), r=["lbT"], w=["lbT"])
            lbT.append((lb, lbm))
        cx.epoch()
        x_rows = lambda t0, n: x_in[t0:t0 + n, :]
        if stages >= 1:
            pass_h(cx, cfg, x_rows, hT_d, consts, ps)
        if stages >= 2:
            pass_hgrn(cx, cfg, hT_d, o_d[0], ins["ev_w_in"], lbT, 0, consts, ps, gmix[0], stop=HSTOP)
        if stages >= 3:
            pass_hgrn(cx, cfg, hT_d, o_d[1], ins["ev_w_in"], lbT, 1, consts, ps, gmix[0])
        if stages >= 4:
            pass_mla_prep(cx, cfg, hT_d, q_d, k_d, v_d, ins, consts, ps, gmix[0])
        if stages >= 5:
            pass_attn(cx, cfg, q_d, k_d, v_d, om_d, consts, ps)
        if stages >= 6:
            with nc.sbuf_tensor("aff_sb0", [128, cfg.NT, NE], F32) as aff_sb:
                pass_combine(cx, cfg, x_in, hT_d, o_d, om_d, x1_d, hn_d, aff_sb, aff_d, ins, consts, ps, gmix[0], gffn[0])
                if stages >= 7:
                    pass_moe(cx, cfg, 0, aff_sb, aff_d, hn_d, x1_d, pinc_d, ins, consts, ps, gffn[0])
        if stages >= 8:
            pass_h(cx, cfg, lambda t0, n: x1_d[t0:t0 + n, :], hT_d, consts, ps, tag="b")
            pass_r1(cx, cfg, hT_d, gl_d, xb_d, ins, consts, ps, gmix[1])
            pass_r2(cx, cfg, 0, xb_d, gl_d, hf_d, x1_d, y_out, hn_d, None, aff_d, ins, consts, ps, gffn[1])
            with nc.sbuf_tensor("aff_sb1", [128, cfg.NT, NE], F32) as aff_sb:
                pass_r2(cx, cfg, 1, xb_d, gl_d, hf_d, x1_d, y_out, hn_d, aff_sb, aff_d, ins, consts, ps, gffn[1])
                if stages >= 9:
                    pass_moe(cx, cfg, 1, aff_sb, aff_d, hn_d, y_out, pinc_d, ins, consts, ps, gffn[1])
        cx.drain()
        print("instructions:", cx.ninst)
    return nc, cfg


def host_layout(inputs, P, B):
    g = lambda k: np.ascontiguousarray(np.asarray(inputs[k], dtype=np.float32))
    m = {"x_in": np.concatenate([g("x_prompt").reshape(-1, D), g("x_sample").reshape(-1, D)], 0)}
    for k in ["norm_mix", "norm_ffn", "hg_lb_logits", "moe_router", "moe_w_gate", "moe_w_up", "moe_w_down"]:
        m[k] = g(k)
    for k in ["ev_w_in", "ev_w_out", "hg_out_gain", "mla_q_norm", "mla_kv_norm", "mla_w_uq", "mla_w_ukv", "mla_q_gain", "mla_k_gain",
              "od_w_in", "od_w_out", "od_conv_w", "od_conv_b", "rg_w_a", "rg_b_a", "rg_w_x", "rg_b_x", "rg_lambda"]:
        a = g(k)
        m[k] = np.ascontiguousarray(a.reshape(a.shape[1:]))
    Lp = max(P, 1) * L
    pos = np.arange(Lp, dtype=np.float32)
    inv = (1.0 / (10000.0 ** (np.arange(0, 64, 2, dtype=np.float32) / 64))).astype(np.float32)
    ang = pos[None, :] * inv[:, None]
    m["rope_cos"] = np.concatenate([np.cos(ang), np.cos(ang)], 0).astype(np.float32)
    m["rope_sin"] = np.concatenate([np.sin(ang), np.sin(ang)], 0).astype(np.float32)
    return m


def kernel(**inputs):
    P, B = 8, 32
    nc, cfg = build(P, B)
    m = host_layout(inputs, P, B)
    res = run_bass_kernel_spmd(nc, [m], core_ids=[0])
    y = np.asarray(res.results[0]["y_out"], dtype=np.float32)
    return (y[:P * L].reshape(1, P * L, D).copy(), y[P * L:].reshape(B, L, D).copy())
```

```python
import numpy as np
from contextlib import ExitStack
import concourse.bass as bass
import concourse.mybir as mybir
from concourse.bass_utils import run_bass_kernel_spmd

F32 = mybir.dt.float32
BF16 = mybir.dt.bfloat16
I32 = mybir.dt.int32
AF = mybir.ActivationFunctionType
ALU = mybir.AluOpType
AX = mybir.AxisListType

D = 1024
L = 2048
EPS = 1e-6
NE = 16
BIG = 1.0e6
import os
HSTOP = int(os.environ.get('HSTOP', '99'))
MSTOP = int(os.environ.get('MSTOP', '99'))
FSTOP = int(os.environ.get('FSTOP', '99'))
NEXP = int(os.environ.get('NEXP', '16'))


class Cx:
    ENG = ('pe', 'dve', 'act', 'pool')

    def __init__(s, nc, es):
        s.nc = nc
        s.e = {'pe': nc.tensor, 'dve': nc.vector, 'act': nc.scalar, 'pool': nc.gpsimd, 'sp': nc.sync}
        s.sem = {}
        for k in s.ENG:
            s.sem[k] = es.enter_context(nc.semaphore(f"c_{k}"))
        s.cnt = {k: 0 for k in s.ENG}
        s.ch = []
        for q, n in (('sp', 8), ('pool', 8)):
            for i in range(n):
                nm = f"d_{q}{i}"
                s.sem[nm] = es.enter_context(nc.semaphore(nm))
                s.ch.append({'q': q, 'name': nm, 'cnt': 0})
        s.rr = {'sp': 0, 'pool': 0}
        s.bar = [es.enter_context(nc.semaphore(f"bar{i}")) for i in range(4)]
        s.par = 0
        s.seen = {k: {} for k in ('pe', 'dve', 'act', 'pool', 'sp')}
        s.lw = {}
        s.rd = {}
        s.ninst = 0
        s.nwait = 0
        s.npool = 0
        s.nopt = es.enter_context(nc.sbuf_tensor("cx_nop", [128, 1], F32))

    def _wait(s, eng, sn, v):
        if v <= 0:
            return
        if s.seen[eng].get(sn, 0) >= v:
            return
        s.e[eng].wait_ge(s.sem[sn], v)
        s.seen[eng][sn] = v
        s.nwait += 1

    def _deps(s, eng, r, w):
        need = {}
        for k in r:
            ev = s.lw.get(k)
            if ev:
                need[ev[0]] = max(need.get(ev[0], 0), ev[1])
        for k in w:
            ev = s.lw.get(k)
            if ev:
                need[ev[0]] = max(need.get(ev[0], 0), ev[1])
            for sn, v in s.rd.get(k, {}).items():
                need[sn] = max(need.get(sn, 0), v)
        for sn, v in need.items():
            if eng == 'pe' and sn == 'pe':
                continue
            s._wait(eng, sn, v)

    def _rec(s, ev, r, w):
        for k in w:
            s.lw[k] = ev
            s.rd[k] = {}
        for k in r:
            d = s.rd.setdefault(k, {})
            d[ev[0]] = max(d.get(ev[0], 0), ev[1])

    def op(s, eng, fn, r=(), w=()):
        s._deps(eng, r, w)
        inst = fn(s.e[eng])
        s.cnt[eng] += 1
        assert s.cnt[eng] < 32000, "epoch too long"
        inst.then_inc(s.sem[eng], 1)
        s._rec((eng, s.cnt[eng]), r, w)
        s.ninst += 1

    def dma(s, q, fn, r=(), w=()):
        chs = [c for c in s.ch if c['q'] == q]
        c = chs[s.rr[q] % len(chs)]
        s.rr[q] += 1
        nw0 = s.nwait
        s._wait(q, c['name'], c['cnt'])
        s._deps(q, r, w)
        if q == 'pool':
            s.npool += 1
            probe = s.e['pool'].alloc_register(f"cxprobe{s.npool}")
            s.e['pool'].free_register(probe)
            inst = fn(s.e[q])
            try:
                s.e['pool'].free_register(probe)
            except Exception:
                pass
        else:
            inst = fn(s.e[q])
        c['cnt'] += 16
        assert c['cnt'] < 32000, "epoch too long (dma)"
        inst.then_inc(s.sem[c['name']], 16)
        s._rec((c['name'], c['cnt']), r, w)
        s.ninst += 1

    def drain(s):
        for k in s.ENG:
            s._wait(k, k, s.cnt[k])
        for c in s.ch:
            s._wait(c['q'], c['name'], c['cnt'])

    def epoch(s):
        s.drain()
        A, B = s.bar[2 * s.par], s.bar[2 * s.par + 1]
        A2, B2 = s.bar[2 * (1 - s.par)], s.bar[2 * (1 - s.par) + 1]
        allk = ('pe', 'dve', 'act', 'pool', 'sp')
        for k in allk:
            s.e[k].sem_inc(A, 1)
        m = s.e['sp']
        m.wait_ge(A, 5)
        for k in s.ENG:
            m.sem_clear(s.sem[k])
        for c in s.ch:
            if c['q'] != 'pool':
                m.sem_clear(s.sem[c['name']])
        m.sem_clear(A2)
        m.sem_clear(B2)
        m.sem_inc(B, 1)
        for k in allk:
            s.e[k].wait_ge(B, 1)
        s.cnt = {k: 0 for k in s.ENG}
        for c in s.ch:
            if c['q'] != 'pool':
                c['cnt'] = 0
        s.seen = {k: {} for k in allk}
        s.lw = {}
        s.rd = {}
        s.par ^= 1


class Cfg:
    def __init__(s, P, B):
        s.P = P
        s.B = B
        s.NS = P + B
        s.T = s.NS * L
        s.NT = s.T // 128
        s.groups = [(0, P), (P, P + B)]


def chain_of(cfg, sg):
    if sg < cfg.P:
        return list(range(cfg.P))
    return [sg]


def load_w(cx, es, name, src, K, N, gcol=None, stg=None, dtype=BF16):
    nc = cx.nc
    kc = (K + 127) // 128
    wt = es.enter_context(nc.sbuf_tensor(name, [128, kc, N], dtype))
    for k in range(kc):
        rows = min(128, K - k * 128)
        for n0 in range(0, N, 1024):
            nn = min(1024, N - n0)
            sk = f"wstg{(k + n0 // 1024) % 2}"
            st = stg[(k + n0 // 1024) % 2]
            cx.dma('sp', lambda e, st=st, k=k, rows=rows, n0=n0, nn=nn: e.dma_start(out=st[0:rows, 0:nn], in_=src[k * 128:k * 128 + rows, n0:n0 + nn]),
                   w=[sk])
            if gcol is None:
                cx.op('pool', lambda e, st=st, k=k, rows=rows, n0=n0, nn=nn: e.tensor_copy(wt[0:rows, k, n0:n0 + nn], st[0:rows, 0:nn]),
                      r=[sk], w=[name])
            else:
                cx.op('dve', lambda e, st=st, k=k, rows=rows, n0=n0, nn=nn: e.tensor_scalar(wt[0:rows, k, n0:n0 + nn], st[0:rows, 0:nn], gcol[0:rows, k:k + 1], None, op0=ALU.mult),
                      r=[sk], w=[name])
    return wt


def load_cols(cx, es, name, src_vec, n):
    nc = cx.nc
    kc = n // 128
    t = es.enter_context(nc.sbuf_tensor(name, [128, kc], F32))
    cx.dma('sp', lambda e: e.dma_start(out=t[:, :], in_=src_vec.rearrange("(k p) -> p k", p=128), allow_slow_non_contiguous=True), w=[name])
    return t


def rstd_from_ssq(cx, out, ssq, n, tmpk):
    (o_ap, o_k), (s_ap, s_k) = out, ssq
    cx.op('dve', lambda e: e.tensor_scalar(o_ap, s_ap, 1.0 / n, EPS, op0=ALU.mult, op1=ALU.add), r=[s_k], w=[o_k])
    cx.op('act', lambda e: e.activation(out=o_ap, in_=o_ap, func=AF.Sqrt), r=[o_k], w=[o_k])
    cx.op('dve', lambda e: e.reciprocal(o_ap, o_ap), r=[o_k], w=[o_k])


def pass_h(cx, cfg, x_rows, hT_d, consts, ps, tag="a"):
    nc = cx.nc
    with ExitStack() as es:
        xt = [es.enter_context(nc.sbuf_tensor(f"h{tag}_xt{i}", [128, D], F32)) for i in range(2)]
        junk = es.enter_context(nc.sbuf_tensor(f"h{tag}_junk", [128, D], BF16))
        ssq = es.enter_context(nc.sbuf_tensor(f"h{tag}_ssq", [128, 2], F32))
        rs = es.enter_context(nc.sbuf_tensor(f"h{tag}_rs", [128, 2], F32))
        hT = [es.enter_context(nc.sbuf_tensor(f"h{tag}_hT{i}", [128, 8, 512], BF16)) for i in range(2)]
        ident = consts['identF']
        for sg in range(cfg.NS):
            for blk in range(L // 512):
                hb = hT[blk % 2]
                hk = f"h_hT{blk % 2}"
                for j4 in range(4):
                    j = blk * 4 + j4
                    b = j % 2
                    t0 = sg * L + j * 128
                    cx.dma('sp', lambda e, b=b, t0=t0: e.dma_start(out=xt[b][:, :], in_=x_rows(t0, 128)), w=[f"h_xt{b}"])
                    cx.op('act', lambda e, b=b: e.activation(out=junk[:, :], in_=xt[b][:, :], func=AF.Square, accum_out=ssq[:, b:b + 1]),
                          r=[f"h_xt{b}"], w=["h_junk", f"h_ssq{b}"])
                    rstd_from_ssq(cx, (rs[:, b:b + 1], f"h_rs{b}"), (ssq[:, b:b + 1], f"h_ssq{b}"), D, None)
                    cx.op('dve', lambda e, b=b: e.tensor_scalar(xt[b][:, :], xt[b][:, :], rs[:, b:b + 1], None, op0=ALU.mult),
                          r=[f"h_rs{b}", f"h_xt{b}"], w=[f"h_xt{b}"])
                    for half in range(2):
                        pt = ps[half]
                        pk = f"ps{half}"
                        for kk in range(4):
                            k = half * 4 + kk
                            cx.op('pe', lambda e, pt=pt, kk=kk, k=k, b=b: e.transpose(pt[:, kk * 128:(kk + 1) * 128], xt[b][:, k * 128:(k + 1) * 128], ident[:, :]),
                                  r=[f"h_xt{b}", "identF"], w=[pk])
                        eng = 'act' if half == 0 else 'dve'
                        if eng == 'act':
                            cx.op('act', lambda e, pt=pt, half=half, j4=j4, hb=hb: e.activation(
                                out=hb[:, half * 4:(half + 1) * 4, j4 * 128:(j4 + 1) * 128],
                                in_=pt[:, :].rearrange("p (k t) -> p k t", k=4), func=AF.Copy), r=[pk], w=[hk])
                        else:
                            cx.op('dve', lambda e, pt=pt, half=half, j4=j4, hb=hb: e.tensor_copy(
                                hb[:, half * 4:(half + 1) * 4, j4 * 128:(j4 + 1) * 128],
                                pt[:, :].rearrange("p (k t) -> p k t", k=4)), r=[pk], w=[hk])
                c0 = sg * L + blk * 512
                cx.dma('sp', lambda e, hb=hb, c0=c0: e.dma_start(out=hT_d[:, :, c0:c0 + 512].rearrange("k p t -> p k t"), in_=hb[:, :, :]),
                       r=[hk], w=[f"d:hT:{sg}"])
            cx.epoch()


def pass_hgrn(cx, cfg, hT_d, o_d, w_in, lbT, dirn, consts, ps, gmix, stop=99):
    nc = cx.nc
    with ExitStack() as es:
        stg = [es.enter_context(nc.sbuf_tensor(f"g{dirn}_stg{i}", [128, 1024], F32)) for i in range(2)]
        wq = load_w(cx, es, f"g{dirn}_wq", w_in[:, 0:512], D, 512, gcol=gmix, stg=stg)
        wi = load_w(cx, es, f"g{dirn}_wi", w_in[:, 512:1024], D, 512, gcol=gmix, stg=stg)
        wf = load_w(cx, es, f"g{dirn}_wf", w_in[:, 1536 + 512 * dirn:2048 + 512 * dirn], D, 512, gcol=gmix, stg=stg)
        hR = es.enter_context(nc.sbuf_tensor(f"g{dirn}_hR", [128, 8, L], BF16))
        hst = [es.enter_context(nc.sbuf_tensor(f"g{dirn}_hst{i}", [128, L], BF16)) for i in range(2)] if dirn else None
        f = es.enter_context(nc.sbuf_tensor(f"g{dirn}_f", [128, 1024], F32))
        bb = es.enter_context(nc.sbuf_tensor(f"g{dirn}_b", [128, 1024], F32))
        ea = es.enter_context(nc.sbuf_tensor(f"g{dirn}_ea", [128, 1024], F32))
        kk_ = es.enter_context(nc.sbuf_tensor(f"g{dirn}_k", [128, 1024], F32))
        qt = es.enter_context(nc.sbuf_tensor(f"g{dirn}_qt", [128, 4, L], BF16))
        kt = es.enter_context(nc.sbuf_tensor(f"g{dirn}_kt", [128, 4, L], BF16))
        kh = es.enter_context(nc.sbuf_tensor(f"g{dirn}_kh", [128, 4, L], BF16))
        ebl = es.enter_context(nc.sbuf_tensor(f"g{dirn}_ebl", [128, 4, 32], F32))
        v = es.enter_context(nc.sbuf_tensor(f"g{dirn}_v", [64, 2, 512], BF16))
        khT = es.enter_context(nc.sbuf_tensor(f"g{dirn}_khT", [64, 2, 512], BF16))
        pT = es.enter_context(nc.sbuf_tensor(f"g{dirn}_pT", [64, 4, 64], BF16))
        osb = es.enter_context(nc.sbuf_tensor(f"g{dirn}_o", [64, 2, 8, 512], BF16))
        otmp = es.enter_context(nc.sbuf_tensor(f"g{dirn}_otmp", [64, 512], F32))
        S = es.enter_context(nc.sbuf_tensor(f"g{dirn}_S", [128, 512], F32))
        Sb = es.enter_context(nc.sbuf_tensor(f"g{dirn}_Sb", [128, 512], BF16))
        identB = consts['identB']
        cmask = consts['cmask']
        smask = consts['smask']
        lb = lbT[dirn]
        segs = list(range(cfg.NS))
        if dirn:
            segs = list(reversed(range(cfg.P))) + list(range(cfg.P, cfg.NS))
        for si, sg in enumerate(segs):
            first = (sg >= cfg.P) or (sg == (cfg.P - 1 if dirn else 0))
            for k in range(8):
                if dirn:
                    cx.dma('sp', lambda e, k=k, sg=sg: e.dma_start(out=hst[k % 2][:, :], in_=hT_d[k, :, sg * L:(sg + 1) * L]), w=[f"g_hst{k % 2}"])
                    cx.op('dve', lambda e, k=k: e.tensor_copy(hR[:, k, :], hst[k % 2][:, ::-1]), r=[f"g_hst{k % 2}"], w=["g_hR"])
                else:
                    cx.dma('sp', lambda e, k=k, sg=sg: e.dma_start(out=hR[:, k, :], in_=hT_d[k, :, sg * L:(sg + 1) * L]), w=["g_hR"])
            hk = "g_hR"
            if first:
                cx.op('dve', lambda e: e.memset(S[:, :], 0.0), w=["g_S0", "g_S1", "g_S2", "g_S3"])
                cx.op('pool', lambda e: e.memset(Sb[:, :], 0.0), w=["g_Sb"])
            if stop <= 0:
                cx.epoch(); continue
            for h in range(4):
                for hf in range(2):
                    c0 = hf * 1024
                    pz = (ps[0], ps[1])
                    pq = (ps[2], ps[3])
                    for nb in range(2):
                        for k in range(8):
                            cx.op('pe', lambda e, nb=nb, k=k, h=h, c0=c0: e.matmul(pz[nb][:, :], lhsT=wf[:, k, h * 128:(h + 1) * 128], rhs=hR[:, k, c0 + nb * 512:c0 + (nb + 1) * 512], start=(k == 0), stop=(k == 7)),
                                  r=[f"g{dirn}_wf", hk], w=[f"ps{nb}"])
                        for k in range(8):
                            cx.op('pe', lambda e, nb=nb, k=k, h=h, c0=c0: e.matmul(pq[nb][:, :], lhsT=wq[:, k, h * 128:(h + 1) * 128], rhs=hR[:, k, c0 + nb * 512:c0 + (nb + 1) * 512], start=(k == 0), stop=(k == 7)),
                                  r=[f"g{dirn}_wq", hk], w=[f"ps{2 + nb}"])
                    for nb in range(2):
                        cx.op('act', lambda e, nb=nb: e.activation(out=f[:, nb * 512:(nb + 1) * 512], in_=pz[nb][:, :], func=AF.Sigmoid), r=[f"ps{nb}"], w=["g_f"])
                    cx.op('dve', lambda e, h=h: e.tensor_scalar(f[:, :], f[:, :], lb[1][:, h:h + 1], lb[0][:, h:h + 1], op0=ALU.mult, op1=ALU.add), r=["g_f", "lbT"], w=["g_f"])
                    cx.op('act', lambda e: e.activation(out=bb[:, :], in_=f[:, :], func=AF.Ln), r=["g_f"], w=["g_b"])
                    cx.op('dve', lambda e: e.tensor_tensor_scan(out=bb[:, :], data0=smask[:, :], data1=bb[:, :], initial=0.0, op0=ALU.mult, op1=ALU.add), r=["g_b", "smask"], w=["g_b"])
                    cx.op('pool', lambda e: e.tensor_scalar(kk_[:, :], f[:, :], -1.0, 1.0, op0=ALU.mult, op1=ALU.add), r=["g_f"], w=["g_k"])
                    cx.op('act', lambda e: e.activation(out=ea[:, :], in_=bb[:, :], func=AF.Exp), r=["g_b"], w=["g_ea"])
                    for nb in range(2):
                        cx.op('dve', lambda e, nb=nb, h=h, c0=c0: e.scalar_tensor_tensor(out=qt[:, h, c0 + nb * 512:c0 + (nb + 1) * 512], in0=pq[nb][:, :], scalar=128.0 ** -0.5, in1=ea[:, nb * 512:(nb + 1) * 512], op0=ALU.mult, op1=ALU.mult),
                              r=[f"ps{2 + nb}", "g_ea"], w=["g_qt"])
                    cx.op('act', lambda e: e.activation(out=ea[:, :], in_=bb[:, :], func=AF.Exp, scale=-1.0), r=["g_b"], w=["g_ea"])
                    cx.op('dve', lambda e, h=h, c0=c0: e.tensor_tensor(out=kt[:, h, c0:c0 + 1024], in0=kk_[:, :], in1=ea[:, :], op=ALU.mult), r=["g_k", "g_ea"], w=["g_kt"])
                    b3 = bb[:, :].rearrange("p (c t) -> p c t", t=64)
                    cx.op('act', lambda e, h=h, hf=hf: e.activation(out=ebl[:, h, hf * 16:(hf + 1) * 16], in_=b3[:, :, 63], func=AF.Exp), r=["g_b"], w=["g_ebl"])
                    cx.op('dve', lambda e: e.tensor_tensor(out=ea[:, :].rearrange("p (c t) -> p c t", t=64), in0=b3[:, :, 63:64].to_broadcast([128, 16, 64]), in1=b3, op=ALU.subtract), r=["g_b"], w=["g_ea"])
                    cx.op('act', lambda e: e.activation(out=ea[:, :], in_=ea[:, :], func=AF.Exp), r=["g_ea"], w=["g_ea"])
                    cx.op('dve', lambda e, h=h, c0=c0: e.tensor_tensor(out=kh[:, h, c0:c0 + 1024], in0=kk_[:, :], in1=ea[:, :], op=ALU.mult), r=["g_k", "g_ea"], w=["g_kh"])
            if stop <= 1:
                cx.epoch(); continue
            if dirn == 0:
                r0 = sg * L
            else:
                r0 = (cfg.P - 1 - sg) * L if sg < cfg.P else sg * L
            for c in range(32):
                cb = c % 2
                pv = ps[4 + cb]
                pvk = f"ps{4 + cb}"
                for k in range(8):
                    cx.op('pe', lambda e, k=k, c=c, pv=pv: e.matmul(pv[0:64, :], lhsT=hR[:, k, c * 64:(c + 1) * 64], rhs=wi[:, k, :], start=(k == 0), stop=(k == 7)),
                          r=[hk, f"g{dirn}_wi"], w=[pvk])
                cx.op('act', lambda e, cb=cb, pv=pv: e.activation(out=v[:, cb, :], in_=pv[0:64, :], func=AF.Copy), r=[pvk], w=[f"g_v{cb}"])
                pk_ = consts['psb'][cb]
                pkk = f"psb{cb}"
                for h in range(4):
                    cx.op('pe', lambda e, h=h, c=c, pk_=pk_: e.transpose(pk_[0:64, h * 128:(h + 1) * 128], kh[:, h, c * 64:(c + 1) * 64], identB[:, :]),
                          r=["g_kh", "identB"], w=[pkk])
                cx.op('pool' if False else 'dve', lambda e, cb=cb, pk_=pk_: e.tensor_copy(khT[:, cb, :], pk_[0:64, :]), r=[pkk], w=[f"g_khT{cb}"])
                psS, pso, psd = ps[0], ps[1], ps[2]
                for h in range(4):
                    cx.op('pe', lambda e, h=h, c=c: e.matmul(psS[0:64, h * 64:(h + 1) * 64], lhsT=kt[:, h, c * 64:(c + 1) * 64], rhs=qt[:, h, c * 64:(c + 1) * 64], start=True, stop=True),
                          r=["g_kt", "g_qt"], w=["ps0"])
                cx.op('dve', lambda e: e.tensor_tensor(out=pT[:, :, :], in0=psS[0:64, 0:256].rearrange("p (h t) -> p h t", h=4), in1=cmask[:, :].unsqueeze(1).to_broadcast([64, 4, 64]), op=ALU.mult),
                      r=["ps0", "cmask"], w=["g_pT"])
                for h in range(4):
                    cx.op('pe', lambda e, h=h, cb=cb: e.matmul(pso[0:64, h * 128:(h + 1) * 128], lhsT=pT[:, h, :], rhs=v[:, cb, h * 128:(h + 1) * 128], start=True, stop=True),
                          r=["g_pT", f"g_v{cb}"], w=["ps1"])
                    cx.op('pe', lambda e, h=h, c=c: e.matmul(ps[3][0:64, h * 128:(h + 1) * 128], lhsT=qt[:, h, c * 64:(c + 1) * 64], rhs=Sb[:, h * 128:(h + 1) * 128], start=True, stop=True),
                          r=["g_qt", "g_Sb"], w=["ps3"])
                ob = (c // 8) % 2
                cx.op('act', lambda e: e.activation(out=otmp[:, :], in_=pso[0:64, :], func=AF.Copy), r=["ps1"], w=["g_otmp"])
                cx.op('dve', lambda e, c=c, ob=ob: e.tensor_tensor(out=osb[:, ob, c % 8, :], in0=ps[3][0:64, :], in1=otmp[:, :], op=ALU.add), r=["ps3", "g_otmp"], w=[f"g_o{ob}"])
                for h in range(4):
                    cx.op('pe', lambda e, h=h, cb=cb: e.matmul(psd[:, h * 128:(h + 1) * 128], lhsT=khT[:, cb, h * 128:(h + 1) * 128], rhs=v[:, cb, h * 128:(h + 1) * 128], start=True, stop=True),
                          r=[f"g_khT{cb}", f"g_v{cb}"], w=["ps2"])
                for h in range(4):
                    cx.op('dve', lambda e, h=h, c=c: e.scalar_tensor_tensor(out=S[:, h * 128:(h + 1) * 128], in0=S[:, h * 128:(h + 1) * 128], scalar=ebl[:, h, c:c + 1], in1=psd[:, h * 128:(h + 1) * 128], op0=ALU.mult, op1=ALU.add),
                          r=[f"g_S{h}", "g_ebl", "ps2"], w=[f"g_S{h}"])
                cx.op('act', lambda e: e.activation(out=Sb[:, :], in_=S[:, :], func=AF.Copy), r=["g_S0", "g_S1", "g_S2", "g_S3"], w=["g_Sb"])
                if c % 8 == 7:
                    rr0 = r0 + (c - 7) * 64
                    cx.dma('sp', lambda e, rr0=rr0, ob=ob: e.dma_start(out=o_d[rr0:rr0 + 512, :].rearrange("(c t) n -> t c n", t=64), in_=osb[:, ob, :, :]), r=[f"g_o{ob}"], w=[f"d:o{dirn}:{sg}:{c}"])
            cx.epoch()


def rstd_tile(cx, out_ap, out_k, ps_ap, ps_k, n):
    cx.op('dve', lambda e: e.tensor_scalar(out_ap, ps_ap, 1.0 / n, EPS, op0=ALU.mult, op1=ALU.add), r=[ps_k], w=[out_k])
    cx.op('act', lambda e: e.activation(out=out_ap, in_=out_ap, func=AF.Sqrt), r=[out_k], w=[out_k])
    cx.op('dve', lambda e: e.reciprocal(out_ap, out_ap), r=[out_k], w=[out_k])


def seg_pos0(cfg, sg):
    return sg * L if sg < cfg.P else 0


def pass_mla_prep(cx, cfg, hT_d, q_d, k_d, v_d, ins, consts, ps, gmix):
    nc = cx.nc
    w_in = ins["ev_w_in"]
    with ExitStack() as es:
        stg = [es.enter_context(nc.sbuf_tensor(f"m1_stg{i}", [128, 1024], F32)) for i in range(2)]
        wcq = load_w(cx, es, "m1_wcq", w_in[:, 2560:2944], D, 384, gcol=gmix, stg=stg)
        wckv = load_w(cx, es, "m1_wckv", w_in[:, 2944:3200], D, 256, gcol=gmix, stg=stg)
        wkpe = load_w(cx, es, "m1_wkpe", w_in[:, 3200:3264], D, 64, gcol=gmix, stg=stg)
        gqn = load_cols(cx, es, "m1_gqn", ins["mla_q_norm"], 384)
        gkvn = load_cols(cx, es, "m1_gkvn", ins["mla_kv_norm"], 256)
        wuq = load_w(cx, es, "m1_wuq", ins["mla_w_uq"], 384, 768, gcol=gqn, stg=stg)
        wukv = load_w(cx, es, "m1_wukv", ins["mla_w_ukv"], 256, 1024, gcol=gkvn, stg=stg)
        wv = es.enter_context(nc.sbuf_tensor("m1_wv", [128, 2, 512], BF16))
        for h in range(4):
            cx.op('dve', lambda e, h=h: e.tensor_copy(wv[:, :, h * 128:(h + 1) * 128], wukv[:, :, h * 256 + 128:h * 256 + 256]), r=["m1_wukv"], w=["m1_wv"])
        gq = es.enter_context(nc.sbuf_tensor("m1_gq", [128, 2], F32))
        gk = es.enter_context(nc.sbuf_tensor("m1_gk", [128, 2], F32))
        for (t, src, nm) in ((gq, ins["mla_q_gain"], "m1_gq"), (gk, ins["mla_k_gain"], "m1_gk")):
            cx.op('dve', lambda e, t=t: e.memset(t[:, :], 0.0), w=[nm])
            cx.dma('sp', lambda e, t=t, src=src: e.dma_start(out=t[:, 0:1], in_=src[0:128].rearrange("(p o) -> p o", o=1)), w=[nm])
            cx.dma('sp', lambda e, t=t, src=src: e.dma_start(out=t[0:64, 1:2], in_=src[128:192].rearrange("(p o) -> p o", o=1)), w=[nm])
        cx.op('dve', lambda e: e.tensor_scalar(gq[:, :], gq[:, :], 192.0 ** -0.5, None, op0=ALU.mult), r=["m1_gq"], w=["m1_gq"])
        Rm = es.enter_context(nc.sbuf_tensor("m1_Rm", [64, 64], F32))
        Rt = es.enter_context(nc.sbuf_tensor("m1_Rt", [64, 64], F32))
        iot = consts['iot']
        cx.op('dve', lambda e: e.tensor_scalar(Rm[:, :], iot[0:64, 0:64], 32.0, None, op0=ALU.is_equal), r=["c_iot"], w=["m1_Rm"])
        cx.op('dve', lambda e: e.tensor_scalar(Rt[:, :], iot[0:64, 0:64], -32.0, None, op0=ALU.is_equal), r=["c_iot"], w=["m1_Rt"])
        cx.op('dve', lambda e: e.tensor_tensor(out=Rm[:, :], in0=Rm[:, :], in1=Rt[:, :], op=ALU.subtract), r=["m1_Rm", "m1_Rt"], w=["m1_Rm"])
        hb = es.enter_context(nc.sbuf_tensor("m1_hb", [128, 8, 512], BF16))
        cq = es.enter_context(nc.sbuf_tensor("m1_cq", [128, 3, 512], F32))
        cqn = es.enter_context(nc.sbuf_tensor("m1_cqn", [128, 3, 512], BF16))
        ckvn = es.enter_context(nc.sbuf_tensor("m1_ckvn", [128, 2, 512], BF16))
        sqb = es.enter_context(nc.sbuf_tensor("m1_sqb", [128, 3, 512], BF16))
        sqr = es.enter_context(nc.sbuf_tensor("m1_sqr", [128, 512], BF16))
        sqk = es.enter_context(nc.sbuf_tensor("m1_sqk", [128, 512], BF16))
        r1 = es.enter_context(nc.sbuf_tensor("m1_r1", [128, 512], F32))
        nf = es.enter_context(nc.sbuf_tensor("m1_nf", [128, 512], F32))
        rf = es.enter_context(nc.sbuf_tensor("m1_rf", [64, 512], F32))
        t1 = es.enter_context(nc.sbuf_tensor("m1_t1", [64, 512], F32))
        t2 = es.enter_context(nc.sbuf_tensor("m1_t2", [64, 512], F32))
        kpe = es.enter_context(nc.sbuf_tensor("m1_kpe", [64, 512], F32))
        krot = es.enter_context(nc.sbuf_tensor("m1_krot", [64, 512], F32))
        cs = es.enter_context(nc.sbuf_tensor("m1_cos", [64, 512], F32))
        sn = es.enter_context(nc.sbuf_tensor("m1_sin", [64, 512], F32))
        on = es.enter_context(nc.sbuf_tensor("m1_on", [128, 2, 512], BF16))
        orp = es.enter_context(nc.sbuf_tensor("m1_or", [64, 2, 512], BF16))
        vt = es.enter_context(nc.sbuf_tensor("m1_vt", [128, 2, 512], BF16))
        onesB = consts['onesB']
        cx.op('dve', lambda e: e.memset(sqr[:, :], 0.0), w=["m1_sqr"])
        cx.op('dve', lambda e: e.memset(sqk[:, :], 0.0), w=["m1_sqk"])
        pA, pB, pC, pD = ps[0], ps[1], ps[2], ps[3]

        def rope(src, dst_k, dst_ap):
            cx.op('pe', lambda e: e.matmul(pD[0:64, :], lhsT=Rm[:, :], rhs=src[:, :], start=True, stop=True), r=["m1_Rm", "m1_t1"], w=["ps3"])
            cx.op('dve', lambda e: e.tensor_tensor(out=t2[:, :], in0=pD[0:64, :], in1=sn[:, :], op=ALU.mult), r=["ps3", "m1_sin"], w=["m1_t2"])
            cx.op('dve', lambda e: e.tensor_tensor(out=src[:, :], in0=src[:, :], in1=cs[:, :], op=ALU.mult), r=["m1_t1", "m1_cos"], w=["m1_t1"])
            cx.op('dve', lambda e: e.tensor_tensor(out=dst_ap, in0=src[:, :], in1=t2[:, :], op=ALU.add), r=["m1_t1", "m1_t2"], w=[dst_k])

        for sg in range(cfg.NS):
            for blk in range(4):
                c0 = sg * L + blk * 512
                p0 = seg_pos0(cfg, sg) + blk * 512
                cx.dma('sp', lambda e, c0=c0: e.dma_start(out=hb[:, :, :], in_=hT_d[:, :, c0:c0 + 512].rearrange("k p t -> p k t")), w=["m1_hb"])
                cx.dma('sp', lambda e, p0=p0: e.dma_start(out=cs[:, :], in_=ins["rope_cos"][:, p0:p0 + 512]), w=["m1_cos"])
                cx.dma('sp', lambda e, p0=p0: e.dma_start(out=sn[:, :], in_=ins["rope_sin"][:, p0:p0 + 512]), w=["m1_sin"])
                for (wt, wk, nch, dst, dk) in ((wcq, "m1_wcq", 3, cqn, "m1_cqn"), (wckv, "m1_wckv", 2, ckvn, "m1_ckvn")):
                    for n in range(nch):
                        for k in range(8):
                            cx.op('pe', lambda e, wt=wt, n=n, k=k: e.matmul(pA[:, :], lhsT=wt[:, k, n * 128:(n + 1) * 128], rhs=hb[:, k, :], start=(k == 0), stop=(k == 7)),
                                  r=[wk, "m1_hb"], w=["ps0"])
                        cx.op('act', lambda e, n=n: e.activation(out=cq[:, n, :], in_=pA[:, :], func=AF.Copy), r=["ps0"], w=["m1_cq"])
                        cx.op('dve', lambda e, n=n: e.tensor_tensor(out=sqb[:, n, :], in0=pA[:, :], in1=cq[:, n, :], op=ALU.mult), r=["ps0", "m1_cq"], w=["m1_sqb"])
                    for n in range(nch):
                        cx.op('pe', lambda e, n=n, nch=nch: e.matmul(pB[:, :], lhsT=onesB[:, :], rhs=sqb[:, n, :], start=(n == 0), stop=(n == nch - 1)), r=["onesB", "m1_sqb"], w=["ps1"])
                    rstd_tile(cx, r1[:, :], "m1_r1", pB[:, :], "ps1", nch * 128)
                    for n in range(nch):
                        cx.op('dve', lambda e, n=n, dst=dst: e.tensor_tensor(out=dst[:, n, :], in0=cq[:, n, :], in1=r1[:, :], op=ALU.mult), r=["m1_cq", "m1_r1"], w=[dk])
                for k in range(8):
                    cx.op('pe', lambda e, k=k: e.matmul(pC[0:64, :], lhsT=wkpe[:, k, :], rhs=hb[:, k, :], start=(k == 0), stop=(k == 7)), r=["m1_wkpe", "m1_hb"], w=["ps2"])
                cx.op('act', lambda e: e.activation(out=kpe[:, :], in_=pC[0:64, :], func=AF.Copy), r=["ps2"], w=["m1_kpe"])
                cx.op('dve', lambda e: e.tensor_tensor(out=sqk[0:64, :], in0=pC[0:64, :], in1=kpe[:, :], op=ALU.mult), r=["ps2", "m1_kpe"], w=["m1_sqk"])
                cx.op('dve', lambda e: e.tensor_scalar(t1[:, :], kpe[:, :], gk[0:64, 1:2], None, op0=ALU.mult), r=["m1_kpe", "m1_gk"], w=["m1_t1"])
                rope(t1, "m1_krot", krot[:, :])
                for h in range(4):
                    hb2 = h % 2
                    for n in range(3):
                        cx.op('pe', lambda e, n=n, h=h: e.matmul(pA[:, :], lhsT=wuq[:, n, h * 192:h * 192 + 128], rhs=cqn[:, n, :], start=(n == 0), stop=(n == 2)), r=["m1_wuq", "m1_cqn"], w=["ps0"])
                    for n in range(3):
                        cx.op('pe', lambda e, n=n, h=h: e.matmul(pC[0:64, :], lhsT=wuq[:, n, h * 192 + 128:h * 192 + 192], rhs=cqn[:, n, :], start=(n == 0), stop=(n == 2)), r=["m1_wuq", "m1_cqn"], w=["ps2"])
                    cx.op('act', lambda e: e.activation(out=nf[:, :], in_=pA[:, :], func=AF.Copy), r=["ps0"], w=["m1_nf"])
                    cx.op('dve', lambda e: e.tensor_tensor(out=sqb[:, 0, :], in0=pA[:, :], in1=nf[:, :], op=ALU.mult), r=["ps0", "m1_nf"], w=["m1_sqb"])
                    cx.op('act', lambda e: e.activation(out=rf[:, :], in_=pC[0:64, :], func=AF.Copy), r=["ps2"], w=["m1_rf"])
                    cx.op('dve', lambda e: e.tensor_tensor(out=sqr[0:64, :], in0=pC[0:64, :], in1=rf[:, :], op=ALU.mult), r=["ps2", "m1_rf"], w=["m1_sqr"])
                    cx.op('pe', lambda e: e.matmul(pB[:, :], lhsT=onesB[:, :], rhs=sqb[:, 0, :], start=True, stop=False), r=["onesB", "m1_sqb"], w=["ps1"])
                    cx.op('pe', lambda e: e.matmul(pB[:, :], lhsT=onesB[:, :], rhs=sqr[:, :], start=False, stop=True), r=["onesB", "m1_sqr"], w=["ps1"])
                    rstd_tile(cx, r1[:, :], "m1_r1", pB[:, :], "ps1", 192)
                    cx.op('dve', lambda e, hb2=hb2: e.scalar_tensor_tensor(out=on[:, hb2, :], in0=nf[:, :], scalar=gq[:, 0:1], in1=r1[:, :], op0=ALU.mult, op1=ALU.mult), r=["m1_nf", "m1_gq", "m1_r1"], w=[f"m1_on{hb2}"])
                    cx.dma('sp', lambda e, h=h, hb2=hb2, c0=c0: e.dma_start(out=q_d[h, 0:128, c0:c0 + 512], in_=on[:, hb2, :]), r=[f"m1_on{hb2}"], w=[f"d:q:{sg}"])
                    cx.op('dve', lambda e: e.scalar_tensor_tensor(out=t1[:, :], in0=rf[:, :], scalar=gq[0:64, 1:2], in1=r1[0:64, :], op0=ALU.mult, op1=ALU.mult), r=["m1_rf", "m1_gq", "m1_r1"], w=["m1_t1"])
                    rope(t1, f"m1_or{hb2}", orp[:, hb2, :])
                    cx.dma('sp', lambda e, h=h, hb2=hb2, c0=c0: e.dma_start(out=q_d[h, 128:192, c0:c0 + 512], in_=orp[:, hb2, :]), r=[f"m1_or{hb2}"], w=[f"d:q:{sg}"])
                    for n in range(2):
                        cx.op('pe', lambda e, n=n, h=h: e.matmul(pA[:, :], lhsT=wukv[:, n, h * 256:h * 256 + 128], rhs=ckvn[:, n, :], start=(n == 0), stop=(n == 1)), r=["m1_wukv", "m1_ckvn"], w=["ps0"])
                    cx.op('act', lambda e: e.activation(out=nf[:, :], in_=pA[:, :], func=AF.Copy), r=["ps0"], w=["m1_nf"])
                    cx.op('dve', lambda e: e.tensor_tensor(out=sqb[:, 0, :], in0=pA[:, :], in1=nf[:, :], op=ALU.mult), r=["ps0", "m1_nf"], w=["m1_sqb"])
                    cx.op('pe', lambda e: e.matmul(pB[:, :], lhsT=onesB[:, :], rhs=sqb[:, 0, :], start=True, stop=False), r=["onesB", "m1_sqb"], w=["ps1"])
                    cx.op('pe', lambda e: e.matmul(pB[:, :], lhsT=onesB[:, :], rhs=sqk[:, :], start=False, stop=True), r=["onesB", "m1_sqk"], w=["ps1"])
                    rstd_tile(cx, r1[:, :], "m1_r1", pB[:, :], "ps1", 192)
                    hb3 = 1 - hb2
                    cx.op('dve', lambda e, hb3=hb3: e.scalar_tensor_tensor(out=on[:, hb3, :], in0=nf[:, :], scalar=gk[:, 0:1], in1=r1[:, :], op0=ALU.mult, op1=ALU.mult), r=["m1_nf", "m1_gk", "m1_r1"], w=[f"m1_on{hb3}"])
                    cx.dma('sp', lambda e, h=h, hb3=hb3, c0=c0: e.dma_start(out=k_d[h, 0:128, c0:c0 + 512], in_=on[:, hb3, :]), r=[f"m1_on{hb3}"], w=[f"d:k:{sg}"])
                    cx.op('dve', lambda e, hb3=hb3: e.tensor_tensor(out=orp[:, hb3, :], in0=krot[:, :], in1=r1[0:64, :], op=ALU.mult), r=["m1_krot", "m1_r1"], w=[f"m1_or{hb3}"])
                    cx.dma('sp', lambda e, h=h, hb3=hb3, c0=c0: e.dma_start(out=k_d[h, 128:192, c0:c0 + 512], in_=orp[:, hb3, :]), r=[f"m1_or{hb3}"], w=[f"d:k:{sg}"])
                for j in range(4):
                    jb = j % 2
                    for n in range(2):
                        cx.op('pe', lambda e, n=n, j=j: e.matmul(pA[:, :], lhsT=ckvn[:, n, j * 128:(j + 1) * 128], rhs=wv[:, n, :], start=(n == 0), stop=(n == 1)), r=["m1_ckvn", "m1_wv"], w=["ps0"])
                    cx.op('act', lambda e, jb=jb: e.activation(out=vt[:, jb, :], in_=pA[:, :], func=AF.Copy), r=["ps0"], w=[f"m1_vt{jb}"])
                    cx.dma('sp', lambda e, jb=jb, j=j, c0=c0: e.dma_start(out=v_d[c0 + j * 128:c0 + (j + 1) * 128, :], in_=vt[:, jb, :]), r=[f"m1_vt{jb}"], w=[f"d:v:{sg}"])
            cx.epoch()


def pass_attn(cx, cfg, q_d, k_d, v_d, om_d, consts, ps):
    nc = cx.nc
    with ExitStack() as es:
        qn = es.enter_context(nc.sbuf_tensor("a_qn", [128, 4, L], BF16))
        qr = es.enter_context(nc.sbuf_tensor("a_qr", [128, 4, L], BF16))
        kn = es.enter_context(nc.sbuf_tensor("a_kn", [128, 4, L], BF16))
        kr = es.enter_context(nc.sbuf_tensor("a_kr", [128, 4, L], BF16))
        vv = es.enter_context(nc.sbuf_tensor("a_vv", [128, 16, 512], BF16))
        pT = [es.enter_context(nc.sbuf_tensor(f"a_pT{i}", [128, 512], BF16)) for i in range(2)]
        acc_o = es.enter_context(nc.sbuf_tensor("a_acco", [128, 16, 512], F32)) if cfg.P > 1 else None
        acc_d = es.enter_context(nc.sbuf_tensor("a_accd", [128, 16, 512], F32)) if cfg.P > 1 else None
        rec = es.enter_context(nc.sbuf_tensor("a_rec", [128, 512], F32))
        ot = [es.enter_context(nc.sbuf_tensor(f"a_ot{i}", [128, 512], BF16)) for i in range(2)]
        onesB = consts['onesB']
        for sg in range(cfg.NS):
            ctx = chain_of(cfg, sg)
            if sg == 0:
                cx.op('dve', lambda e: e.memset(qr[64:128, :, :], 0.0), w=["a_qr"])
                cx.op('pool', lambda e: e.memset(kr[64:128, :, :], 0.0), w=["a_kr"])
                cx.epoch()
            for h in range(4):
                cx.dma('sp', lambda e, h=h, sg=sg: e.dma_start(out=qn[:, h, :], in_=q_d[h, 0:128, sg * L:(sg + 1) * L]), w=["a_qn"])
                cx.dma('sp', lambda e, h=h, sg=sg: e.dma_start(out=qr[0:64, h, :], in_=q_d[h, 128:192, sg * L:(sg + 1) * L]), w=["a_qr"])
            for ki, ks in enumerate(ctx):
                for h in range(4):
                    cx.dma('sp', lambda e, h=h, ks=ks: e.dma_start(out=kn[:, h, :], in_=k_d[h, 0:128, ks * L:(ks + 1) * L]), w=["a_kn"])
                    cx.dma('sp', lambda e, h=h, ks=ks: e.dma_start(out=kr[0:64, h, :], in_=k_d[h, 128:192, ks * L:(ks + 1) * L]), w=["a_kr"])
                for j4 in range(4):
                    cx.dma('sp', lambda e, j4=j4, ks=ks: e.dma_start(out=vv[:, j4 * 4:(j4 + 1) * 4, :], in_=v_d[ks * L + j4 * 512:ks * L + (j4 + 1) * 512, :].rearrange("(j p) n -> p j n", p=128)), w=["a_vv"])
                for qb in range(4):
                    for h in range(4):
                        i = qb * 4 + h
                        po, pd = ps[2 + (i % 2)], ps[4 + (i % 2)]
                        pok, pdk = f"ps{2 + (i % 2)}", f"ps{4 + (i % 2)}"
                        def scores(kt, h=h, qb=qb):
                            sb = kt % 2
                            pS = ps[sb]
                            cx.op('pe', lambda e: e.matmul(pS[:, :], lhsT=kn[:, h, kt * 128:(kt + 1) * 128], rhs=qn[:, h, qb * 512:(qb + 1) * 512], start=True, stop=False),
                                  r=["a_kn", "a_qn"], w=[f"ps{sb}"])
                            cx.op('pe', lambda e: e.matmul(pS[:, :], lhsT=kr[:, h, kt * 128:(kt + 1) * 128], rhs=qr[:, h, qb * 512:(qb + 1) * 512], start=False, stop=True),
                                  r=["a_kr", "a_qr"], w=[f"ps{sb}"])
                        scores(0)
                        for kt in range(16):
                            sb = kt % 2
                            pS = ps[sb]
                            if kt + 1 < 16:
                                scores(kt + 1)
                            cx.op('act', lambda e, sb=sb, pS=pS: e.activation(out=pT[sb][:, :], in_=pS[:, :], func=AF.Exp), r=[f"ps{sb}"], w=[f"a_pT{sb}"])
                            cx.op('pe', lambda e, po=po, h=h, kt=kt, sb=sb: e.matmul(po[:, :], lhsT=vv[:, kt, h * 128:(h + 1) * 128], rhs=pT[sb][:, :], start=(kt == 0), stop=(kt == 15)),
                                  r=["a_vv", f"a_pT{sb}"], w=[pok])
                            cx.op('pe', lambda e, pd=pd, sb=sb, kt=kt: e.matmul(pd[:, :], lhsT=onesB[:, :], rhs=pT[sb][:, :], start=(kt == 0), stop=(kt == 15)),
                                  r=["onesB", f"a_pT{sb}"], w=[pdk])
                        last = (ki == len(ctx) - 1)
                        if len(ctx) > 1:
                            if ki == 0:
                                cx.op('act', lambda e, po=po, i=i: e.activation(out=acc_o[:, i, :], in_=po[:, :], func=AF.Copy), r=[pok], w=["a_acco"])
                                cx.op('dve', lambda e, pd=pd, i=i: e.tensor_copy(acc_d[:, i, :], pd[:, :]), r=[pdk], w=["a_accd"])
                            else:
                                cx.op('dve', lambda e, po=po, i=i: e.tensor_tensor(out=acc_o[:, i, :], in0=po[:, :], in1=acc_o[:, i, :], op=ALU.add), r=[pok, "a_acco"], w=["a_acco"])
                                cx.op('dve', lambda e, pd=pd, i=i: e.tensor_tensor(out=acc_d[:, i, :], in0=pd[:, :], in1=acc_d[:, i, :], op=ALU.add), r=[pdk, "a_accd"], w=["a_accd"])
                        if last:
                            ob = i % 2
                            if len(ctx) > 1:
                                cx.op('dve', lambda e, i=i: e.reciprocal(rec[:, :], acc_d[:, i, :]), r=["a_accd"], w=["a_rec"])
                                cx.op('dve', lambda e, i=i, ob=ob: e.tensor_tensor(out=ot[ob][:, :], in0=acc_o[:, i, :], in1=rec[:, :], op=ALU.mult), r=["a_acco", "a_rec"], w=[f"a_ot{ob}"])
                            else:
                                cx.op('dve', lambda e, pd=pd: e.reciprocal(rec[:, :], pd[:, :]), r=[pdk], w=["a_rec"])
                                cx.op('dve', lambda e, po=po, ob=ob: e.tensor_tensor(out=ot[ob][:, :], in0=po[:, :], in1=rec[:, :], op=ALU.mult), r=[pok, "a_rec"], w=[f"a_ot{ob}"])
                            c0 = sg * L + qb * 512
                            cx.dma('sp', lambda e, h=h, ob=ob, c0=c0: e.dma_start(out=om_d[h, :, c0:c0 + 512], in_=ot[ob][:, :]), r=[f"a_ot{ob}"], w=[f"d:om:{sg}"])
                cx.epoch()


class Tail:
    def __init__(s, cx, es, pfx, w_out_src, router_src, gffn_col, stg):
        nc = cx.nc
        s.cx, s.pfx = cx, pfx
        s.wo = load_w(cx, es, f"{pfx}_wo", w_out_src, D, D, stg=stg)
        s.wr = es.enter_context(nc.sbuf_tensor(f"{pfx}_wr", [128, 8, NE], F32))
        cx.dma('sp', lambda e: e.dma_start(out=s.wr[:, :, :], in_=router_src.rearrange("(k p) n -> p k n", p=128)), w=[f"{pfx}_wr"])
        for k in range(8):
            cx.op('dve', lambda e, k=k: e.tensor_scalar(s.wr[:, k, :], s.wr[:, k, :], gffn_col[:, k:k + 1], None, op0=ALU.mult), r=[f"{pfx}_wr"], w=[f"{pfx}_wr"])
        s.xt = [es.enter_context(nc.sbuf_tensor(f"{pfx}_xt{i}", [128, D], F32)) for i in range(2)]
        s.hn = [es.enter_context(nc.sbuf_tensor(f"{pfx}_hn{i}", [128, D], F32)) for i in range(2)]
        s.hnb = [es.enter_context(nc.sbuf_tensor(f"{pfx}_hnb{i}", [128, D], BF16)) for i in range(2)]
        s.hnT = es.enter_context(nc.sbuf_tensor(f"{pfx}_hnT", [128, 8, 128], F32))
        s.sma = [es.enter_context(nc.sbuf_tensor(f"{pfx}_sma{i}", [128, 2], F32)) for i in range(2)]
        s.sm = es.enter_context(nc.sbuf_tensor(f"{pfx}_sm", [128, 4], F32))
        s.ee = es.enter_context(nc.sbuf_tensor(f"{pfx}_ee", [128, NE], F32))

    def run(s, mix, mk, c_off, t0, x_src, x1_d, hn_d, aff_sb, aff_d, consts, ps):
        cx, pfx = s.cx, s.pfx
        identF = consts['identF']

        def stage_a(j):
            b = j % 2
            xt, xk = s.xt[b], f"{pfx}_xt{b}"
            hn, hk = s.hn[b], f"{pfx}_hn{b}"
            hnb, hbk = s.hnb[b], f"{pfx}_hnb{b}"
            sma, sk = s.sma[b], f"{pfx}_sma{b}"
            r0 = t0 + j * 128
            cx.dma('sp', lambda e: e.dma_start(out=xt[:, :], in_=x_src[r0:r0 + 128, :]), w=[xk])
            for dh in range(2):
                po, pk = ps[dh], f"ps{dh}"
                for k in range(8):
                    cx.op('pe', lambda e, po=po, k=k, dh=dh: e.matmul(po[:, :], lhsT=mix[:, k, c_off + j * 128:c_off + (j + 1) * 128], rhs=s.wo[:, k, dh * 512:(dh + 1) * 512], start=(k == 0), stop=(k == 7)),
                          r=[mk, f"{pfx}_wo"], w=[pk])
                cx.op('dve', lambda e, po=po, dh=dh: e.tensor_tensor(out=xt[:, dh * 512:(dh + 1) * 512], in0=po[:, :], in1=xt[:, dh * 512:(dh + 1) * 512], op=ALU.add), r=[pk, xk], w=[xk])
            cx.dma('sp', lambda e: e.dma_start(out=x1_d[r0:r0 + 128, :], in_=xt[:, :]), r=[xk], w=[f"d:x1:{r0}"])
            cx.op('act', lambda e: e.activation(out=hn[:, :], in_=xt[:, :], func=AF.Square, accum_out=sma[:, 0:1]), r=[xk], w=[hk, sk])
            rstd_from_ssq(cx, (sma[:, 1:2], sk), (sma[:, 0:1], sk), D, None)
            cx.op('dve', lambda e: e.tensor_scalar(hn[:, :], xt[:, :], sma[:, 1:2], None, op0=ALU.mult), r=[xk, sk], w=[hk])
            cx.op('act', lambda e: e.activation(out=hnb[:, :], in_=hn[:, :], func=AF.Copy), r=[hk], w=[hbk])
            cx.dma('sp', lambda e: e.dma_start(out=hn_d[r0:r0 + 128, :], in_=hnb[:, :]), r=[hbk], w=[f"d:hn:{r0}"])

        def stage_b(j):
            b = j % 2
            hn, hk = s.hn[b], f"{pfx}_hn{b}"
            r0 = t0 + j * 128
            for half in range(2):
                pt, pk = ps[2 + half], f"ps{2 + half}"
                for kk in range(4):
                    k = half * 4 + kk
                    cx.op('pe', lambda e, pt=pt, kk=kk, k=k: e.transpose(pt[:, kk * 128:(kk + 1) * 128], hn[:, k * 128:(k + 1) * 128], identF[:, :]), r=[hk, "identF"], w=[pk])
                if half == 0:
                    cx.op('act', lambda e, pt=pt: e.activation(out=s.hnT[:, 0:4, :], in_=pt[:, :].rearrange("p (k t) -> p k t", k=4), func=AF.Copy), r=[pk], w=[f"{pfx}_hnT"])
                else:
                    cx.op('dve', lambda e, pt=pt: e.tensor_copy(s.hnT[:, 4:8, :], pt[:, :].rearrange("p (k t) -> p k t", k=4)), r=[pk], w=[f"{pfx}_hnT"])
            pl = ps[4]
            for k in range(8):
                cx.op('pe', lambda e, k=k: e.matmul(pl[:, 0:NE], lhsT=s.hnT[:, k, :], rhs=s.wr[:, k, :], start=(k == 0), stop=(k == 7)), r=[f"{pfx}_hnT", f"{pfx}_wr"], w=["ps4"])
            cx.op('act', lambda e: e.activation(out=s.ee[:, :], in_=pl[:, 0:NE], func=AF.Exp, accum_out=s.sm[:, 2:3]), r=["ps4"], w=[f"{pfx}_ee", f"{pfx}_sm"])
            cx.op('dve', lambda e: e.reciprocal(s.sm[:, 3:4], s.sm[:, 2:3]), r=[f"{pfx}_sm"], w=[f"{pfx}_sm"])
            ti = r0 // 128
            cx.op('dve', lambda e: e.tensor_scalar(aff_sb[:, ti, :], s.ee[:, :], s.sm[:, 3:4], None, op0=ALU.mult), r=[f"{pfx}_ee", f"{pfx}_sm"], w=["aff_sb"])
            cx.dma('sp', lambda e: e.dma_start(out=aff_d[r0:r0 + 128, 0:NE], in_=aff_sb[:, ti, :]), r=["aff_sb"], w=[f"d:aff:{r0}"])

        stage_a(0)
        for j in range(4):
            if j + 1 < 4:
                stage_a(j + 1)
            stage_b(j)


def pass_combine(cx, cfg, x_src, hT_d, o_d, om_d, x1_d, hn_d, aff_sb, aff_d, ins, consts, ps, gmix, gffn):
    nc = cx.nc
    with ExitStack() as es:
        stg = [es.enter_context(nc.sbuf_tensor(f"c_stg{i}", [128, 1024], F32)) for i in range(2)]
        wg = load_w(cx, es, "c_wg", ins["ev_w_in"][:, 1024:1536], D, 512, gcol=gmix, stg=stg)
        tail = Tail(cx, es, "c", ins["ev_w_out"], ins["moe_router"][0], gffn, stg)
        gain = es.enter_context(nc.sbuf_tensor("c_gain", [128, 1], F32))
        cx.dma('sp', lambda e: e.dma_start(out=gain[:, :], in_=ins["hg_out_gain"].rearrange("(p o) -> p o", o=1)), w=["c_gain"])
        hb = es.enter_context(nc.sbuf_tensor("c_hb", [128, 8, 512], BF16))
        of = es.enter_context(nc.sbuf_tensor("c_of", [64, 8, 512], BF16))
        ob = es.enter_context(nc.sbuf_tensor("c_ob", [64, 8, 512], BF16))
        sg_ = es.enter_context(nc.sbuf_tensor("c_sg", [128, 512], F32))
        ohf = es.enter_context(nc.sbuf_tensor("c_ohf", [128, 512], F32))
        sq = es.enter_context(nc.sbuf_tensor("c_sq", [128, 512], BF16))
        r1 = es.enter_context(nc.sbuf_tensor("c_r1", [128, 512], F32))
        mix = es.enter_context(nc.sbuf_tensor("c_mix", [128, 8, 512], BF16))
        identB, J64, onesB = consts['identB'], consts['J64'], consts['onesB']
        for sg in range(cfg.NS):
            if sg < cfg.P:
                cbase, nchain, rbase = sg * 32, cfg.P * 32, 0
            else:
                cbase, nchain, rbase = 0, 32, sg * L
            for blk in range(4):
                t0 = sg * L + blk * 512
                cx.dma('sp', lambda e, t0=t0: e.dma_start(out=hb[:, :, :], in_=hT_d[:, :, t0:t0 + 512].rearrange("k p t -> p k t")), w=["c_hb"])
                cx.dma('sp', lambda e, t0=t0: e.dma_start(out=of[:, :, :], in_=o_d[0][t0:t0 + 512, :].rearrange("(c t) n -> t c n", t=64)), w=["c_of"])
                c_lo = cbase + blk * 8
                rr = rbase + (nchain - 1 - c_lo - 7) * 64
                cx.dma('sp', lambda e, rr=rr: e.dma_start(out=ob[:, :, :], in_=o_d[1][rr:rr + 512, :].rearrange("(c t) n -> t c n", t=64)), w=["c_ob"])
                cx.dma('sp', lambda e, t0=t0: e.dma_start(out=mix[:, 4:8, :], in_=om_d[:, :, t0:t0 + 512].rearrange("h p t -> p h t")), w=["c_mix"])
                for h in range(4):
                    pg, po, pss = ps[0], ps[1], ps[2]
                    for k in range(8):
                        cx.op('pe', lambda e, k=k, h=h: e.matmul(pg[:, :], lhsT=wg[:, k, h * 128:(h + 1) * 128], rhs=hb[:, k, :], start=(k == 0), stop=(k == 7)), r=["c_wg", "c_hb"], w=["ps0"])
                    cx.op('act', lambda e: e.activation(out=sg_[:, :], in_=pg[:, :], func=AF.Silu), r=["ps0"], w=["c_sg"])
                    for i in range(8):
                        cx.op('pe', lambda e, i=i, h=h: e.matmul(po[:, i * 64:(i + 1) * 64], lhsT=of[:, i, h * 128:(h + 1) * 128], rhs=identB[0:64, 0:64], start=True, stop=False), r=["c_of", "identB"], w=["ps1"])
                        cx.op('pe', lambda e, i=i, h=h: e.matmul(po[:, i * 64:(i + 1) * 64], lhsT=ob[:, 7 - i, h * 128:(h + 1) * 128], rhs=J64[:, :], start=False, stop=True), r=["c_ob", "J64"], w=["ps1"])
                    cx.op('act', lambda e: e.activation(out=ohf[:, :], in_=po[:, :], func=AF.Copy), r=["ps1"], w=["c_ohf"])
                    cx.op('dve', lambda e: e.tensor_tensor(out=sq[:, :], in0=po[:, :], in1=ohf[:, :], op=ALU.mult), r=["ps1", "c_ohf"], w=["c_sq"])
                    cx.op('pe', lambda e: e.matmul(pss[:, :], lhsT=onesB[:, :], rhs=sq[:, :], start=True, stop=True), r=["onesB", "c_sq"], w=["ps2"])
                    rstd_tile(cx, r1[:, :], "c_r1", pss[:, :], "ps2", 128)
                    cx.op('dve', lambda e: e.tensor_tensor(out=ohf[:, :], in0=ohf[:, :], in1=r1[:, :], op=ALU.mult), r=["c_ohf", "c_r1"], w=["c_ohf"])
                    cx.op('dve', lambda e, h=h: e.scalar_tensor_tensor(out=mix[:, h, :], in0=ohf[:, :], scalar=gain[:, 0:1], in1=sg_[:, :], op0=ALU.mult, op1=ALU.mult), r=["c_ohf", "c_gain", "c_sg"], w=["c_mix"])
                tail.run(mix, "c_mix", 0, t0, x_src, x1_d, hn_d, aff_sb, aff_d, consts, ps)
            cx.epoch()


def pass_moe(cx, cfg, layer, aff_sb, aff_d, hn_d, xacc_d, pinc_d, ins, consts, ps, gffn):
    nc = cx.nc
    NT = cfg.NT
    groups = [(a * (L // 128), b * (L // 128)) for (a, b) in cfg.groups if b > a]
    caps = [((b - a) * 128) // 8 for (a, b) in groups]
    nst = [c // 128 for c in caps]
    NSL = sum(nst)
    ntmax = max(b - a for (a, b) in groups)
    pfx = f"e{layer}"
    with ExitStack() as es0:
        idx_sb = es0.enter_context(nc.sbuf_tensor(f"{pfx}_idx", [128, NE, NSL], I32))
        with ExitStack() as es:
            thr = es.enter_context(nc.sbuf_tensor(f"{pfx}_thr", [128, NE], F32))
            hi = es.enter_context(nc.sbuf_tensor(f"{pfx}_hi", [128, NE], F32))
            mid = es.enter_context(nc.sbuf_tensor(f"{pfx}_mid", [128, NE], F32))
            sel = es.enter_context(nc.sbuf_tensor(f"{pfx}_sel", [128, NE], F32))
            d1 = es.enter_context(nc.sbuf_tensor(f"{pfx}_d1", [128, NE], F32))
            cntp = es.enter_context(nc.sbuf_tensor(f"{pfx}_cntp", [128, NE], F32))
            cmp_ = es.enter_context(nc.sbuf_tensor(f"{pfx}_cmp", [128, ntmax, NE], BF16))
            tot = es.enter_context(nc.sbuf_tensor(f"{pfx}_tot", [128, NE, ntmax], F32))
            incl = es.enter_context(nc.sbuf_tensor(f"{pfx}_incl", [128, NE, ntmax], F32))
            pin = [es.enter_context(nc.sbuf_tensor(f"{pfx}_pin{i}", [128, 4, 128], F32)) for i in range(2)]
            junk = [es.enter_context(nc.sbuf_tensor(f"{pfx}_junk{i}", [128, max(ntmax, 128)], F32)) for i in range(4)]
            ts = [es.enter_context(nc.sbuf_tensor(f"{pfx}_ts{i}", [128, 8], F32)) for i in range(4)]
            rowi = [es.enter_context(nc.sbuf_tensor(f"{pfx}_rowi{i}", [128, 1], I32)) for i in range(4)]
            prow = [es.enter_context(nc.sbuf_tensor(f"{pfx}_prow{i}", [128, 128], F32)) for i in range(4)]
            onesB, onesF, linc, slotid, ones512 = consts['onesB'], consts['onesF'], consts['linc'], consts['slotid'], consts['ones512']
            soff = 0
            for gi, (tg0, tg1) in enumerate(groups):
                nt = tg1 - tg0
                kk = float(caps[gi])
                cx.op('dve', lambda e: e.memset(thr[:, :], 0.0), w=[f"{pfx}_thr"])
                cx.op('dve', lambda e: e.memset(hi[:, :], 1.0), w=[f"{pfx}_hi"])
                for it in range(30):
                    cx.op('dve', lambda e: e.tensor_tensor(out=mid[:, :], in0=thr[:, :], in1=hi[:, :], op=ALU.add), r=[f"{pfx}_thr", f"{pfx}_hi"], w=[f"{pfx}_mid"])
                    cx.op('dve', lambda e: e.tensor_scalar(mid[:, :], mid[:, :], 0.5, None, op0=ALU.mult), r=[f"{pfx}_mid"], w=[f"{pfx}_mid"])
                    cx.op('dve', lambda e, tg0=tg0, tg1=tg1, nt=nt: e.tensor_tensor(out=cmp_[:, 0:nt, :], in0=aff_sb[:, tg0:tg1, :], in1=mid[:, :].unsqueeze(1).to_broadcast([128, nt, NE]), op=ALU.is_ge),
                          r=["aff_sb", f"{pfx}_mid"], w=[f"{pfx}_cmp"])
                    cx.op('dve', lambda e, nt=nt: e.tensor_reduce(out=cntp[:, :], in_=cmp_[:, 0:nt, :].rearrange("p t e -> p e t"), axis=AX.X, op=ALU.add), r=[f"{pfx}_cmp"], w=[f"{pfx}_cntp"])
                    cx.op('pe', lambda e: e.matmul(ps[0][:, 0:NE], lhsT=onesF[:, :], rhs=cntp[:, :], start=True, stop=True), r=["onesF", f"{pfx}_cntp"], w=["ps0"])
                    cx.op('dve', lambda e, kk=kk: e.tensor_scalar(sel[:, :], ps[0][:, 0:NE], kk, None, op0=ALU.is_ge), r=["ps0"], w=[f"{pfx}_sel"])
                    cx.op('dve', lambda e: e.tensor_tensor(out=d1[:, :], in0=mid[:, :], in1=thr[:, :], op=ALU.subtract), r=[f"{pfx}_mid", f"{pfx}_thr"], w=[f"{pfx}_d1"])
                    cx.op('dve', lambda e: e.tensor_tensor(out=d1[:, :], in0=d1[:, :], in1=sel[:, :], op=ALU.mult), r=[f"{pfx}_d1", f"{pfx}_sel"], w=[f"{pfx}_d1"])
                    cx.op('dve', lambda e: e.tensor_tensor(out=thr[:, :], in0=thr[:, :], in1=d1[:, :], op=ALU.add), r=[f"{pfx}_thr", f"{pfx}_d1"], w=[f"{pfx}_thr"])
                    cx.op('dve', lambda e: e.tensor_tensor(out=d1[:, :], in0=hi[:, :], in1=mid[:, :], op=ALU.subtract), r=[f"{pfx}_hi", f"{pfx}_mid"], w=[f"{pfx}_d1"])
                    cx.op('dve', lambda e: e.tensor_tensor(out=d1[:, :], in0=d1[:, :], in1=sel[:, :], op=ALU.mult), r=[f"{pfx}_d1", f"{pfx}_sel"], w=[f"{pfx}_d1"])
                    cx.op('dve', lambda e: e.tensor_tensor(out=hi[:, :], in0=mid[:, :], in1=d1[:, :], op=ALU.add), r=[f"{pfx}_mid", f"{pfx}_d1"], w=[f"{pfx}_hi"])
                if MSTOP <= 1:
                    cx.epoch(); continue
                cx.op('dve', lambda e, tg0=tg0, tg1=tg1, nt=nt: e.tensor_tensor(out=cmp_[:, 0:nt, :], in0=aff_sb[:, tg0:tg1, :], in1=thr[:, :].unsqueeze(1).to_broadcast([128, nt, NE]), op=ALU.is_ge),
                      r=["aff_sb", f"{pfx}_thr"], w=[f"{pfx}_cmp"])
                for ci, a in enumerate(range(0, nt, 32)):
                    na = min(32, nt - a)
                    ncol = na * NE
                    mb = cmp_[:, a:a + na, :].rearrange("p t e -> p (t e)")
                    cx.op('pe', lambda e, mb=mb, ncol=ncol: e.matmul(ps[1][:, 0:ncol], lhsT=onesB[:, :], rhs=mb, start=True, stop=True), r=["onesB", f"{pfx}_cmp"], w=["ps1"])
                    cx.op('act', lambda e, a=a, na=na, ncol=ncol: e.activation(out=tot[:, :, a:a + na], in_=ps[1][:, 0:ncol].rearrange("p (t e) -> p e t", e=NE), func=AF.Copy), r=["ps1"], w=[f"{pfx}_tot"])
                    pb = ci % 2
                    for i in range(ncol // 128):
                        cx.op('pe', lambda e, mb=mb, i=i, pb=pb: e.matmul(ps[2 + pb][:, i * 128:(i + 1) * 128], lhsT=mb[:, i * 128:(i + 1) * 128], rhs=linc[:, :], start=True, stop=True), r=[f"{pfx}_cmp", "linc"], w=[f"ps{2 + pb}"])
                    ni = ncol // 128
                    cx.op('dve', lambda e, pb=pb, ni=ni: e.tensor_copy(pin[pb][:, 0:ni, :], ps[2 + pb][:, 0:ni * 128].rearrange("p (i t) -> p i t", t=128)), r=[f"ps{2 + pb}"], w=[f"{pfx}_pin{pb}"])
                    rb = (tg0 + a) * NE
                    cx.dma('sp', lambda e, pb=pb, ni=ni, rb=rb: e.dma_start(out=pinc_d[rb:rb + ni * 128, :].rearrange("(i p) t -> p i t", p=128), in_=pin[pb][:, 0:ni, :]), r=[f"{pfx}_pin{pb}"], w=["d:pinc"])
                for ex in range(NE):
                    cx.op('dve', lambda e, ex=ex, nt=nt: e.tensor_tensor_scan(out=incl[:, ex, 0:nt], data0=ones512[:, 0:nt], data1=tot[:, ex, 0:nt], initial=0.0, op0=ALU.mult, op1=ALU.add),
                          r=[f"{pfx}_tot", "ones512"], w=[f"{pfx}_incl"])
                if MSTOP <= 2:
                    cx.epoch(); continue
                n_ = 0
                for ex in range(NE):
                    for J in range(nst[gi]):
                        b = n_ % 4
                        n_ += 1
                        t_, tk = ts[b], f"{pfx}_ts{b}"
                        jv = slotid[:, J:J + 1]
                        cx.op('dve', lambda e, ex=ex, nt=nt, b=b, t_=t_, jv=jv: e.tensor_scalar(junk[b][:, 0:nt], incl[:, ex, 0:nt], jv, None, op0=ALU.is_le, op1=ALU.add, accum_out=t_[:, 0:1]),
                              r=[f"{pfx}_incl", "slotid"], w=[f"{pfx}_junk{b}", tk])
                        cx.op('dve', lambda e, ex=ex, nt=nt, b=b, t_=t_, jv=jv: e.scalar_tensor_tensor(out=junk[b][:, 0:nt], in0=incl[:, ex, 0:nt], scalar=jv, in1=tot[:, ex, 0:nt], op0=ALU.is_le, op1=ALU.mult, accum_out=t_[:, 1:2]),
                              r=[f"{pfx}_incl", f"{pfx}_tot", "slotid"], w=[f"{pfx}_junk{b}", tk])
                        cx.op('dve', lambda e, t_=t_, jv=jv: e.tensor_tensor(out=t_[:, 2:3], in0=jv, in1=t_[:, 1:2], op=ALU.subtract), r=[tk, "slotid"], w=[tk])
                        cx.op('dve', lambda e, t_=t_, b=b, ex=ex, tg0=tg0: e.tensor_scalar(rowi[b][:, :], t_[:, 0:1], float(NE), float(ex + tg0 * NE), op0=ALU.mult, op1=ALU.add), r=[tk], w=[f"{pfx}_rowi{b}"])
                        cx.dma('pool', lambda e, b=b: e.indirect_dma_start(out=prow[b][:, :], out_offset=None, in_=pinc_d[:, :], in_offset=bass.IndirectOffsetOnAxis(ap=rowi[b][:, 0:1], axis=0), bounds_check=NT * NE - 1, oob_is_err=False),
                               r=[f"{pfx}_rowi{b}", "d:pinc"], w=[f"{pfx}_prow{b}"])
                        cx.op('dve', lambda e, b=b, t_=t_: e.tensor_scalar(junk[b][:, 0:128], prow[b][:, :], t_[:, 2:3], None, op0=ALU.is_le, op1=ALU.add, accum_out=t_[:, 3:4]),
                              r=[f"{pfx}_prow{b}", tk], w=[f"{pfx}_junk{b}", tk])
                        cx.op('dve', lambda e, t_=t_, tg0=tg0: e.tensor_scalar(t_[:, 4:5], t_[:, 0:1], 128.0, float(tg0 * 128), op0=ALU.mult, op1=ALU.add), r=[tk], w=[tk])
                        cx.op('dve', lambda e, t_=t_, ex=ex, J=J, soff=soff: e.tensor_tensor(out=idx_sb[:, ex, soff + J:soff + J + 1], in0=t_[:, 4:5], in1=t_[:, 3:4], op=ALU.add), r=[tk], w=[f"{pfx}_idx"])
                soff += nst[gi]
                cx.epoch()
        if consts.get('idx_dbg') is not None and layer == 0:
            cx.dma('sp', lambda e: e.dma_start(out=consts['idx_dbg'][:, :], in_=idx_sb[:, :, :].rearrange("p e s -> p (e s)")), r=[f"{pfx}_idx"], w=["d:idxdbg"])
            cx.epoch()
        if MSTOP <= 3:
            return
        with ExitStack() as es:
            stg = [es.enter_context(nc.sbuf_tensor(f"{pfx}_stg{i}", [128, 1024], F32)) for i in range(2)]
            xe = [es.enter_context(nc.sbuf_tensor(f"{pfx}_xe{i}", [128, D], BF16)) for i in range(4)]
            ga = [es.enter_context(nc.sbuf_tensor(f"{pfx}_ga{i}", [128, 128], F32)) for i in range(4)]
            xeT = es.enter_context(nc.sbuf_tensor(f"{pfx}_xeT", [128, 8, 512], BF16))
            hid = es.enter_context(nc.sbuf_tensor(f"{pfx}_hid", [128, 8, 512], BF16))
            sl2 = [es.enter_context(nc.sbuf_tensor(f"{pfx}_sl{i}", [128, 512], F32)) for i in range(2)]
            ye = [es.enter_context(nc.sbuf_tensor(f"{pfx}_ye{i}", [128, D], F32)) for i in range(2)]
            identB = consts['identB']
            psb = consts['psb']
            blocks = [list(range(a, min(a + 4, NSL))) for a in range(0, NSL, 4)]
            for ex in range(NEXP):
                with ExitStack() as esw:
                    wg = load_w(cx, esw, f"{pfx}_wg{ex}", ins["moe_w_gate"][layer, ex], D, D, gcol=gffn, stg=stg)
                    wu = load_w(cx, esw, f"{pfx}_wu{ex}", ins["moe_w_up"][layer, ex], D, D, gcol=gffn, stg=stg)
                    wd = load_w(cx, esw, f"{pfx}_wd{ex}", ins["moe_w_down"][layer, ex], D, D, stg=stg)
                    for blk in blocks:
                        N = len(blk) * 128
                        for si, sidx in enumerate(blk):
                            b = si % 4
                            ix = idx_sb[:, ex, sidx:sidx + 1]
                            cx.dma('pool', lambda e, b=b, ix=ix: e.indirect_dma_start(out=xe[b][:, :], out_offset=None, in_=hn_d[:, :], in_offset=bass.IndirectOffsetOnAxis(ap=ix, axis=0), bounds_check=cfg.T - 1, oob_is_err=False),
                                   r=[f"{pfx}_idx", "d:hn"], w=[f"{pfx}_xe{b}"])
                            cx.dma('pool', lambda e, b=b, ix=ix: e.indirect_dma_start(out=ga[b][:, :], out_offset=None, in_=aff_d[:, :], in_offset=bass.IndirectOffsetOnAxis(ap=ix, axis=0), bounds_check=cfg.T - 1, oob_is_err=False),
                                   r=[f"{pfx}_idx", "d:aff"], w=[f"{pfx}_ga{b}"])
                            for half in range(2):
                                pt, pk = psb[half], f"psb{half}"
                                for kq in range(4):
                                    k = half * 4 + kq
                                    cx.op('pe', lambda e, pt=pt, kq=kq, k=k, b=b: e.transpose(pt[:, kq * 128:(kq + 1) * 128], xe[b][:, k * 128:(k + 1) * 128], identB[:, :]), r=[f"{pfx}_xe{b}", "identB"], w=[pk])
                                eng = 'act' if half == 0 else 'dve'
                                if half == 0:
                                    cx.op('act', lambda e, pt=pt, si=si: e.activation(out=xeT[:, 0:4, si * 128:(si + 1) * 128], in_=pt[:, :].rearrange("p (k t) -> p k t", k=4), func=AF.Copy), r=[pk], w=[f"{pfx}_xeT"])
                                else:
                                    cx.op('dve', lambda e, pt=pt, si=si: e.tensor_copy(xeT[:, 4:8, si * 128:(si + 1) * 128], pt[:, :].rearrange("p (k t) -> p k t", k=4)), r=[pk], w=[f"{pfx}_xeT"])
                        if FSTOP <= 1:
                            continue
                        for f in range(8):
                            fb = (f % 2) * 2
                            pg, pu = ps[fb], ps[fb + 1]
                            pgk, puk = f"ps{fb}", f"ps{fb + 1}"
                            slb, slk = sl2[f % 2], f"{pfx}_sl{f % 2}"
                            for k in range(8):
                                cx.op('pe', lambda e, k=k, f=f, N=N, pg=pg: e.matmul(pg[:, 0:N], lhsT=wg[:, k, f * 128:(f + 1) * 128], rhs=xeT[:, k, 0:N], start=(k == 0), stop=(k == 7)), r=[f"{pfx}_wg{ex}", f"{pfx}_xeT"], w=[pgk])
                            for k in range(8):
                                cx.op('pe', lambda e, k=k, f=f, N=N, pu=pu: e.matmul(pu[:, 0:N], lhsT=wu[:, k, f * 128:(f + 1) * 128], rhs=xeT[:, k, 0:N], start=(k == 0), stop=(k == 7)), r=[f"{pfx}_wu{ex}", f"{pfx}_xeT"], w=[puk])
                            cx.op('act', lambda e, N=N, pg=pg, slb=slb: e.activation(out=slb[:, 0:N], in_=pg[:, 0:N], func=AF.Silu), r=[pgk], w=[slk])
                            cx.op('dve', lambda e, f=f, N=N, pu=pu, slb=slb: e.tensor_tensor(out=hid[:, f, 0:N], in0=pu[:, 0:N], in1=slb[:, 0:N], op=ALU.mult), r=[puk, slk], w=[f"{pfx}_hid{f}"])
                        for si, sidx in enumerate(blk):
                            b = si % 4
                            yb = si % 2
                            for dh in range(2):
                                pd, pk = ps[4 + dh], f"ps{4 + dh}"
                                for f in range(8):
                                    cx.op('pe', lambda e, pd=pd, f=f, si=si, dh=dh: e.matmul(pd[:, :], lhsT=hid[:, f, si * 128:(si + 1) * 128], rhs=wd[:, f, dh * 512:(dh + 1) * 512], start=(f == 0), stop=(f == 7)), r=[f"{pfx}_hid{f}", f"{pfx}_wd{ex}"], w=[pk])
                                if dh == 0:
                                    cx.op('act', lambda e, pd=pd, yb=yb, b=b: e.activation(out=ye[yb][:, 0:512], in_=pd[:, :], func=AF.Copy, scale=ga[b][:, ex:ex + 1]), r=[pk, f"{pfx}_ga{b}"], w=[f"{pfx}_ye{yb}"])
                                else:
                                    cx.op('dve', lambda e, pd=pd, yb=yb, b=b: e.tensor_scalar(ye[yb][:, 512:1024], pd[:, :], ga[b][:, ex:ex + 1], None, op0=ALU.mult), r=[pk, f"{pfx}_ga{b}"], w=[f"{pfx}_ye{yb}"])
                            ix = idx_sb[:, ex, sidx:sidx + 1]
                            if FSTOP <= 3:
                                continue
                            cx.dma('pool', lambda e, yb=yb, ix=ix: e.indirect_dma_start(out=xacc_d[:, :], out_offset=bass.IndirectOffsetOnAxis(ap=ix, axis=0), in_=ye[yb][:, :], in_offset=None, bounds_check=cfg.T - 1, oob_is_err=False, compute_op=ALU.add),
                                   r=[f"{pfx}_ye{yb}", f"{pfx}_idx", "d:xacc"], w=["d:xacc"])
                    cx.epoch()


def pass_r1(cx, cfg, hT_d, gl_d, xb_d, ins, consts, ps, gmix):
    nc = cx.nc
    with ExitStack() as es:
        stg = [es.enter_context(nc.sbuf_tensor(f"r1_stg{i}", [128, 1024], F32)) for i in range(2)]
        w = load_w(cx, es, "r1_w", ins["od_w_in"], D, 2048, gcol=gmix, stg=stg)
        hb = es.enter_context(nc.sbuf_tensor("r1_hb", [128, 8, 512], BF16))
        gl = es.enter_context(nc.sbuf_tensor("r1_gl", [128, 8, 512], BF16))
        xb = es.enter_context(nc.sbuf_tensor("r1_xb", [128, 8, 512], F32))
        for sg in range(cfg.NS):
            for blk in range(4):
                t0 = sg * L + blk * 512
                cx.dma('sp', lambda e, t0=t0: e.dma_start(out=hb[:, :, :], in_=hT_d[:, :, t0:t0 + 512].rearrange("k p t -> p k t")), w=["r1_hb"])
                for n in range(16):
                    pp, pk = ps[n % 2], f"ps{n % 2}"
                    for k in range(8):
                        cx.op('pe', lambda e, pp=pp, n=n, k=k: e.matmul(pp[:, :], lhsT=w[:, k, n * 128:(n + 1) * 128], rhs=hb[:, k, :], start=(k == 0), stop=(k == 7)), r=["r1_w", "r1_hb"], w=[pk])
                    if n < 8:
                        cx.op('act', lambda e, pp=pp, n=n: e.activation(out=gl[:, n, :], in_=pp[:, :], func=AF.Gelu), r=[pk], w=["r1_gl"])
                    else:
                        cx.op('dve', lambda e, pp=pp, n=n: e.tensor_copy(xb[:, n - 8, :], pp[:, :]), r=[pk], w=["r1_xb"])
                cx.dma('sp', lambda e, t0=t0: e.dma_start(out=gl_d[:, :, t0:t0 + 512].rearrange("k p t -> p k t"), in_=gl[:, :, :]), r=["r1_gl"], w=[f"d:gl:{sg}"])
                for hx in range(2):
                    cx.dma('sp', lambda e, t0=t0, hx=hx: e.dma_start(out=xb_d[hx][:, :, t0:t0 + 512].rearrange("k p t -> p k t"), in_=xb[:, hx * 4:(hx + 1) * 4, :]), r=["r1_xb"], w=[f"d:xb:{sg}:{hx}"])
            cx.epoch()


def pass_r2(cx, cfg, dirn, xb_d, gl_d, hf_d, x_src, x3_d, hn_d, aff_sb, aff_d, ins, consts, ps, gffn):
    nc = cx.nc
    pf = f"r2{dirn}"
    with ExitStack() as es:
        xp = [es.enter_context(nc.sbuf_tensor(f"{pf}_xp0", [128, L + 4], F32))] * 2
        stg = [xp[0][:, 0:1024], xp[0][:, 1024:2048]]
        wa = load_w(cx, es, f"{pf}_wa", ins["rg_w_a"][dirn].rearrange("n k j -> (n k) j"), D, 256, stg=stg)
        wx = load_w(cx, es, f"{pf}_wx", ins["rg_w_x"][dirn].rearrange("n k j -> (n k) j"), D, 256, stg=stg)
        ba = load_cols(cx, es, f"{pf}_ba", ins["rg_b_a"][dirn, :], D)
        bx = load_cols(cx, es, f"{pf}_bx", ins["rg_b_x"][dirn, :], D)
        c8 = load_cols(cx, es, f"{pf}_c8", ins["rg_lambda"][dirn, :], D)
        cx.op('act', lambda e: e.activation(out=c8[:, :], in_=c8[:, :], func=AF.Exp, scale=-1.0), r=[f"{pf}_c8"], w=[f"{pf}_c8"])
        cx.op('act', lambda e: e.activation(out=c8[:, :], in_=c8[:, :], func=AF.Ln, bias=1.0), r=[f"{pf}_c8"], w=[f"{pf}_c8"])
        cx.op('dve', lambda e: e.tensor_scalar(c8[:, :], c8[:, :], -8.0, None, op0=ALU.mult), r=[f"{pf}_c8"], w=[f"{pf}_c8"])
        cw = [load_cols(cx, es, f"{pf}_cw{j}", ins["od_conv_w"][j, :], D) for j in range(4)]
        cb = load_cols(cx, es, f"{pf}_cb", ins["od_conv_b"], D)
        tail = Tail(cx, es, pf, ins["od_w_out"], ins["moe_router"][1], gffn, stg) if dirn else None
        cx.epoch()
        xcb = es.enter_context(nc.sbuf_tensor(f"{pf}_xcb", [128, 8, L], BF16))
        rr = es.enter_context(nc.sbuf_tensor(f"{pf}_r", [128, L], F32))
        ii = es.enter_context(nc.sbuf_tensor(f"{pf}_i", [128, L], F32))
        tt = es.enter_context(nc.sbuf_tensor(f"{pf}_t", [128, L], F32))
        acc = tt
        hh = tt
        carry = es.enter_context(nc.sbuf_tensor(f"{pf}_carry", [128, 8], F32))
        hb16 = [es.enter_context(nc.sbuf_tensor(f"{pf}_hb0", [128, L], BF16))] * 2
        glt = [es.enter_context(nc.sbuf_tensor(f"{pf}_gl0", [128, L], BF16))] * 2 if dirn else None
        yT = es.enter_context(nc.sbuf_tensor(f"{pf}_yT", [128, 8, L], BF16)) if dirn else None
        segs = list(range(cfg.NS))
        if dirn:
            segs = list(reversed(range(cfg.P))) + list(range(cfg.P, cfg.NS))
        for sg in segs:
            inchain = sg < cfg.P
            has_l = inchain and sg > 0
            has_r = inchain and sg < cfg.P - 1
            first = (not inchain) or (sg == (cfg.P - 1 if dirn else 0))
            t0 = sg * L
            if first:
                cx.op('dve', lambda e: e.memset(carry[:, :], 0.0), w=[f"{pf}_carry"])
            for ch in range(8):
                b = ch % 2
                xk = f"{pf}_xp0"
                cx.op('pool', lambda e, b=b: e.memset(xp[b][:, 0:2], 0.0), w=[xk])
                cx.op('pool', lambda e, b=b: e.memset(xp[b][:, L + 2:L + 4], 0.0), w=[xk])
                lo = t0 - 2 if has_l else t0
                hi = t0 + L + 1 if has_r else t0 + L
                c_lo = 0 if has_l else 2
                cx.dma('sp', lambda e, b=b, ch=ch, lo=lo, hi=hi, c_lo=c_lo: e.dma_start(out=xp[b][:, c_lo:c_lo + (hi - lo)], in_=xb_d[ch // 4][ch % 4, :, lo:hi]), w=[xk])
                cx.op('dve', lambda e, b=b, ch=ch: e.tensor_scalar(acc[:, :], xp[b][:, 0:L], cw[0][:, ch:ch + 1], cb[:, ch:ch + 1], op0=ALU.mult, op1=ALU.add), r=[xk, f"{pf}_cw0", f"{pf}_cb"], w=[f"{pf}_t"])
                for j in (1, 2):
                    cx.op('dve', lambda e, b=b, ch=ch, j=j: e.scalar_tensor_tensor(out=acc[:, :], in0=xp[b][:, j:j + L], scalar=cw[j][:, ch:ch + 1], in1=acc[:, :], op0=ALU.mult, op1=ALU.add), r=[xk, f"{pf}_cw{j}", f"{pf}_t"], w=[f"{pf}_t"])
                cx.op('dve', lambda e, b=b, ch=ch: e.scalar_tensor_tensor(out=xcb[:, ch, :], in0=xp[b][:, 3:3 + L], scalar=cw[3][:, ch:ch + 1], in1=acc[:, :], op0=ALU.mult, op1=ALU.add), r=[xk, f"{pf}_cw3", f"{pf}_t"], w=[f"{pf}_xcb"])
            for co in range(8):
                n, jh = co // 2, co % 2
                b = co % 2
                for blk in range(4):
                    pa, px = ps[(blk % 2) * 2], ps[(blk % 2) * 2 + 1]
                    pak, pxk = f"ps{(blk % 2) * 2}", f"ps{(blk % 2) * 2 + 1}"
                    for kc in range(2):
                        cx.op('pe', lambda e, pa=pa, kc=kc, n=n, jh=jh, blk=blk: e.matmul(pa[:, :], lhsT=wa[:, n * 2 + kc, jh * 128:(jh + 1) * 128], rhs=xcb[:, n * 2 + kc, blk * 512:(blk + 1) * 512], start=(kc == 0), stop=(kc == 1)), r=[f"{pf}_wa", f"{pf}_xcb"], w=[pak])
                    for kc in range(2):
                        cx.op('pe', lambda e, px=px, kc=kc, n=n, jh=jh, blk=blk: e.matmul(px[:, :], lhsT=wx[:, n * 2 + kc, jh * 128:(jh + 1) * 128], rhs=xcb[:, n * 2 + kc, blk * 512:(blk + 1) * 512], start=(kc == 0), stop=(kc == 1)), r=[f"{pf}_wx", f"{pf}_xcb"], w=[pxk])
                    cx.op('act', lambda e, pa=pa, blk=blk, co=co: e.activation(out=rr[:, blk * 512:(blk + 1) * 512], in_=pa[:, :], func=AF.Sigmoid, bias=ba[:, co:co + 1]), r=[pak, f"{pf}_ba"], w=[f"{pf}_r"])
                    cx.op('act', lambda e, px=px, blk=blk, co=co: e.activation(out=ii[:, blk * 512:(blk + 1) * 512], in_=px[:, :], func=AF.Sigmoid, bias=bx[:, co:co + 1]), r=[pxk, f"{pf}_bx"], w=[f"{pf}_i"])
                cx.op('act', lambda e, co=co: e.activation(out=rr[:, :], in_=rr[:, :], func=AF.Exp, scale=c8[:, co:co + 1]), r=[f"{pf}_r", f"{pf}_c8"], w=[f"{pf}_r"])
                cx.op('act', lambda e: e.activation(out=tt[:, :], in_=rr[:, :], func=AF.Square), r=[f"{pf}_r"], w=[f"{pf}_t"])
                cx.op('act', lambda e: e.activation(out=tt[:, :], in_=tt[:, :], func=AF.Sqrt, scale=-1.0, bias=1.0), r=[f"{pf}_t"], w=[f"{pf}_t"])
                cx.op('dve', lambda e: e.tensor_tensor(out=ii[:, :], in0=tt[:, :], in1=ii[:, :], op=ALU.mult), r=[f"{pf}_t", f"{pf}_i"], w=[f"{pf}_i"])
                cx.op('dve', lambda e, co=co: e.tensor_tensor(out=ii[:, :], in0=ii[:, :], in1=xcb[:, co, :], op=ALU.mult), r=[f"{pf}_i", f"{pf}_xcb"], w=[f"{pf}_i"])
                if dirn == 0:
                    cx.op('dve', lambda e, co=co: e.tensor_tensor_scan(out=hh[:, :], data0=rr[:, :], data1=ii[:, :], initial=carry[:, co:co + 1], op0=ALU.mult, op1=ALU.add), r=[f"{pf}_r", f"{pf}_i", f"{pf}_carry"], w=[f"{pf}_t"])
                    cx.op('dve', lambda e, co=co: e.tensor_copy(carry[:, co:co + 1], hh[:, L - 1:L]), r=[f"{pf}_t"], w=[f"{pf}_carry"])
                    cx.op('act', lambda e, b=b: e.activation(out=hb16[b][:, :], in_=hh[:, :], func=AF.Copy), r=[f"{pf}_t"], w=[f"{pf}_hb0"])
                    cx.dma('sp', lambda e, b=b, co=co, t0=t0: e.dma_start(out=hf_d[co, :, t0:t0 + L], in_=hb16[b][:, :]), r=[f"{pf}_hb0"], w=[f"d:hf:{sg}"])
                else:
                    cx.op('dve', lambda e, co=co: e.tensor_tensor_scan(out=hh[:, ::-1], data0=rr[:, ::-1], data1=ii[:, ::-1], initial=carry[:, co:co + 1], op0=ALU.mult, op1=ALU.add), r=[f"{pf}_r", f"{pf}_i", f"{pf}_carry"], w=[f"{pf}_t"])
                    cx.op('dve', lambda e, co=co: e.tensor_copy(carry[:, co:co + 1], hh[:, 0:1]), r=[f"{pf}_t"], w=[f"{pf}_carry"])
                    cx.dma('sp', lambda e, b=b, co=co, t0=t0: e.dma_start(out=hb16[b][:, :], in_=hf_d[co, :, t0:t0 + L]), w=[f"{pf}_hb0"])
                    cx.dma('sp', lambda e, b=b, co=co, t0=t0: e.dma_start(out=glt[b][:, :], in_=gl_d[co, :, t0:t0 + L]), w=[f"{pf}_gl0"])
                    cx.op('dve', lambda e, b=b: e.tensor_tensor(out=hh[:, :], in0=hh[:, :], in1=hb16[b][:, :], op=ALU.add), r=[f"{pf}_t", f"{pf}_hb0"], w=[f"{pf}_t"])
                    cx.op('dve', lambda e, b=b, co=co: e.tensor_tensor(out=yT[:, co, :], in0=hh[:, :], in1=glt[b][:, :], op=ALU.mult), r=[f"{pf}_t", f"{pf}_gl0"], w=[f"{pf}_yT"])
            if dirn:
                for blk in range(4):
                    tail.run(yT, f"{pf}_yT", blk * 512, t0 + blk * 512, x_src, x3_d, hn_d, aff_sb, aff_d, consts, ps)
            cx.epoch()


def make_consts(cx, es):
    nc = cx.nc
    c = {}
    iot = es.enter_context(nc.sbuf_tensor("c_iot", [128, 128], F32))
    cx.op('pool', lambda e: e.iota(iot[:, :], [[1, 128]], base=0, channel_multiplier=-1, allow_small_or_imprecise_dtypes=True), w=["c_iot"])
    identF = es.enter_context(nc.sbuf_tensor("identF", [128, 128], F32))
    identB = es.enter_context(nc.sbuf_tensor("identB", [128, 128], BF16))
    cx.op('dve', lambda e: e.tensor_scalar(identF[:, :], iot[:, :], 0.0, None, op0=ALU.is_equal), r=["c_iot"], w=["identF"])
    cx.op('dve', lambda e: e.tensor_copy(identB[:, :], identF[:, :]), r=["identF"], w=["identB"])
    iot2 = es.enter_context(nc.sbuf_tensor("c_iot2", [128, 128], F32))
    cx.op('pool', lambda e: e.iota(iot2[:, :], [[1, 128]], base=0, channel_multiplier=1, allow_small_or_imprecise_dtypes=True), w=["c_iot2"])
    J64 = es.enter_context(nc.sbuf_tensor("J64", [64, 64], BF16))
    cx.op('dve', lambda e: e.tensor_scalar(J64[:, :], iot2[0:64, 0:64], 63.0, None, op0=ALU.is_equal), r=["c_iot2"], w=["J64"])
    cmask = es.enter_context(nc.sbuf_tensor("cmask", [64, 64], F32))
    cx.op('dve', lambda e: e.tensor_scalar(cmask[:, :], iot[0:64, 0:64], 0.0, None, op0=ALU.is_ge), r=["c_iot"], w=["cmask"])
    ltri = es.enter_context(nc.sbuf_tensor("ltri", [128, 128], BF16))
    cx.op('dve', lambda e: e.tensor_scalar(ltri[:, :], iot[:, :], 0.0, None, op0=ALU.is_gt), r=["c_iot"], w=["ltri"])
    onesB = es.enter_context(nc.sbuf_tensor("onesB", [128, 128], BF16))
    cx.op('dve', lambda e: e.memset(onesB[:, :], 1.0), w=["onesB"])
    onesF = es.enter_context(nc.sbuf_tensor("onesF", [128, 128], F32))
    cx.op('dve', lambda e: e.memset(onesF[:, :], 1.0), w=["onesF"])
    smask = es.enter_context(nc.sbuf_tensor("smask", [128, 1024], F32))
    cx.op('dve', lambda e: e.memset(smask[:, :], 1.0), w=["smask"])
    cx.op('dve', lambda e: e.memset(smask[:, :].rearrange("p (c t) -> p c t", t=64)[:, :, 0:1], 0.0), w=["smask"])
    linc = es.enter_context(nc.sbuf_tensor("linc", [128, 128], BF16))
    cx.op('dve', lambda e: e.tensor_scalar(linc[:, :], iot[:, :], 0.0, None, op0=ALU.is_ge), r=["c_iot"], w=["linc"])
    ones512 = es.enter_context(nc.sbuf_tensor("ones512", [128, 512], F32))
    cx.op('dve', lambda e: e.memset(ones512[:, :], 1.0), w=["ones512"])
    slotid = es.enter_context(nc.sbuf_tensor("slotid", [128, 64], F32))
    cx.op('pool', lambda e: e.iota(slotid[:, :], [[128, 64]], base=0, channel_multiplier=1, allow_small_or_imprecise_dtypes=True), w=["slotid"])
    c.update(linc=linc, ones512=ones512, slotid=slotid)
    c.update(identF=identF, identB=identB, J64=J64, cmask=cmask, ltri=ltri, onesB=onesB, onesF=onesF, smask=smask, iot=iot)
    return c


def build(P, B, debug=False, stages=99):
    cfg = Cfg(P, B)
    T = cfg.T
    nc = bass.Bass("TRN2", target_bir_lowering=False)
    dt = lambda name, shape, dtype=F32, kind="ExternalInput": nc.dram_tensor(name, list(shape), dtype, kind=kind).ap()
    x_in = dt("x_in", [T, D])
    ins = {}
    for name, shape in [("norm_mix", [2, D]), ("norm_ffn", [2, D]), ("ev_w_in", [D, 3264]), ("ev_w_out", [D, D]),
                        ("hg_lb_logits", [2, 2, 512]), ("hg_out_gain", [128]), ("mla_q_norm", [384]), ("mla_kv_norm", [256]),
                        ("mla_w_uq", [384, 768]), ("mla_w_ukv", [256, 1024]), ("mla_q_gain", [192]), ("mla_k_gain", [192]),
                        ("od_w_in", [D, 2048]), ("od_conv_w", [4, D]), ("od_conv_b", [D]), ("rg_w_a", [2, 4, 256, 256]),
                        ("rg_b_a", [2, D]), ("rg_w_x", [2, 4, 256, 256]), ("rg_b_x", [2, D]), ("rg_lambda", [2, D]),
                        ("od_w_out", [D, D]), ("moe_router", [2, D, NE]), ("moe_w_gate", [2, NE, D, D]),
                        ("moe_w_up", [2, NE, D, D]), ("moe_w_down", [2, NE, D, D]), ("rope_cos", [64, P * L if P else L]),
                        ("rope_sin", [64, P * L if P else L])]:
        ins[name] = dt(name, shape)
    y_out = dt("y_out", [T, D], kind="ExternalOutput")
    hT_d = dt("hT_d", [8, 128, T], BF16, kind=("ExternalOutput" if debug else "Internal"))
    o_d = [dt(f"o_d{i}", [T, 512], BF16, kind=("ExternalOutput" if debug else "Internal")) for i in range(2)]
    dbg = "ExternalOutput" if debug else "Internal"
    q_d = dt("q_d", [4, 192, T], BF16, kind="Internal")
    k_d = dt("k_d", [4, 192, T], BF16, kind="Internal")
    v_d = dt("v_d", [T, 512], BF16, kind="Internal")
    om_d = dt("om_d", [4, 128, T], BF16, kind=dbg)
    x1_d = y_out
    hn_d = dt("hn_d", [T, D], BF16, kind="Internal")
    aff_d = dt("aff_d", [T, 128], F32, kind=dbg)
    pinc_d = dt("pinc_d", [cfg.NT * NE, 128], F32, kind="Internal")
    gl_d = dt("gl_d", [8, 128, T], BF16, kind="Internal")
    xb_d = [dt(f"xb_d{i}", [4, 128, T], F32, kind="Internal") for i in range(2)]
    hf_d = dt("hf_d", [8, 128, T], BF16, kind="Internal")
    nsl_dbg = sum(((b - a) * L // 8) // 128 for (a, b) in cfg.groups if b > a)
    idx_dbg = dt("idx_dbg", [128, NE * nsl_dbg], I32, kind="ExternalOutput") if debug else None
    with ExitStack() as es:
        cx = Cx(nc, es)
        ps = [es.enter_context(nc.psum_tensor(f"ps{i}", [128, 512], F32)) for i in range(6)]
        psb = [es.enter_context(nc.psum_tensor(f"psb{i}", [128, 512], BF16)) for i in range(2)]
        consts = make_consts(cx, es)
        consts['psb'] = psb
        consts['idx_dbg'] = idx_dbg
        gmix = [load_cols(cx, es, f"gmix{l}", ins["norm_mix"][l, :], D) for l in range(2)]
        gffn = [load_cols(cx, es, f"gffn{l}", ins["norm_ffn"][l, :], D) for l in range(2)]
        lbT = []
        for d_ in range(2):
            l0 = load_cols(cx, es, f"lbl0_{d_}", ins["hg_lb_logits"][d_, 0, :], 512)
            l1 = load_cols(cx, es, f"lbl1_{d_}", ins["hg_lb_logits"][d_, 1, :], 512)
            lb = es.enter_context(nc.sbuf_tensor(f"lb_{d_}", [128, 4], F32))
            lbm = es.enter_context(nc.sbuf_tensor(f"lbm_{d_}", [128, 4], F32))
            cx.op('dve', lambda e, l0=l0, l1=l1, lb=lb: e.tensor_tensor(out=lb[:, :], in0=l0[:, :], in1=l1[:, :], op=ALU.subtract), r=[f"lbl0_{d_}", f"lbl1_{d_}"], w=["lbT"])
            cx.op('act', lambda e, lb=lb: e.activation(out=lb[:, :], in_=lb[:, :], func=AF.Sigmoid), r=["lbT"], w=["lbT"])
            cx.op('dve', lambda e, lb=lb, lbm=lbm: e.tensor_scalar(lbm[:, :], lb[:, :], -1.0, 1.0, op0=ALU.mult, op1=ALU.add), r=["lbT"], w=["lbT"])
            lbT.append((lb, lbm))
        cx.epoch()
        x_rows = lambda t0, n: x_in[t0:t0 + n, :]
        if stages >= 1:
            pass_h(cx, cfg, x_rows, hT_d, consts, ps)
        if stages >= 2:
            pass_hgrn(cx, cfg, hT_d, o_d[0], ins["ev_w_in"], lbT, 0, consts, ps, gmix[0], stop=HSTOP)
        if stages >= 3:
            pass_hgrn(cx, cfg, hT_d, o_d[1], ins["ev_w_in"], lbT, 1, consts, ps, gmix[0])
        if stages >= 4:
            pass_mla_prep(cx, cfg, hT_d, q_d, k_d, v_d, ins, consts, ps, gmix[0])
        if stages >= 5:
            pass_attn(cx, cfg, q_d, k_d, v_d, om_d, consts, ps)
        if stages >= 6:
            with nc.sbuf_tensor("aff_sb0", [128, cfg.NT, NE], F32) as aff_sb:
                pass_combine(cx, cfg, x_in, hT_d, o_d, om_d, x1_d, hn_d, aff_sb, aff_d, ins, consts, ps, gmix[0], gffn[0])
                if stages >= 7:
                    pass_moe(cx, cfg, 0, aff_sb, aff_d, hn_d, x1_d, pinc_d, ins, consts, ps, gffn[0])
        if stages >= 8:
            pass_h(cx, cfg, lambda t0, n: x1_d[t0:t0 + n, :], hT_d, consts, ps, tag="b")
            pass_r1(cx, cfg, hT_d, gl_d, xb_d, ins, consts, ps, gmix[1])
            pass_r2(cx, cfg, 0, xb_d, gl_d, hf_d, x1_d, y_out, hn_d, None, aff_d, ins, consts, ps, gffn[1])
            with nc.sbuf_tensor("aff_sb1", [128, cfg.NT, NE], F32) as aff_sb:
                pass_r2(cx, cfg, 1, xb_d, gl_d, hf_d, x1_d, y_out, hn_d, aff_sb, aff_d, ins, consts, ps, gffn[1])
                if stages >= 9:
                    pass_moe(cx, cfg, 1, aff_sb, aff_d, hn_d, y_out, pinc_d, ins, consts, ps, gffn[1])
        cx.drain()
        print("instructions:", cx.ninst)
    return nc, cfg


def host_layout(inputs, P, B):
    g = lambda k: np.ascontiguousarray(np.asarray(inputs[k], dtype=np.float32))
    m = {"x_in": np.concatenate([g("x_prompt").reshape(-1, D), g("x_sample").reshape(-1, D)], 0)}
    for k in ["norm_mix", "norm_ffn", "hg_lb_logits", "moe_router", "moe_w_gate", "moe_w_up", "moe_w_down"]:
        m[k] = g(k)
    for k in ["ev_w_in", "ev_w_out", "hg_out_gain", "mla_q_norm", "mla_kv_norm", "mla_w_uq", "mla_w_ukv", "mla_q_gain", "mla_k_gain",
              "od_w_in", "od_w_out", "od_conv_w", "od_conv_b", "rg_w_a", "rg_b_a", "rg_w_x", "rg_b_x", "rg_lambda"]:
        a = g(k)
        m[k] = np.ascontiguousarray(a.reshape(a.shape[1:]))
    Lp = max(P, 1) * L
    pos = np.arange(Lp, dtype=np.float32)
    inv = (1.0 / (10000.0 ** (np.arange(0, 64, 2, dtype=np.float32) / 64))).astype(np.float32)
    ang = pos[None, :] * inv[:, None]
    m["rope_cos"] = np.concatenate([np.cos(ang), np.cos(ang)], 0).astype(np.float32)
    m["rope_sin"] = np.concatenate([np.sin(ang), np.sin(ang)], 0).astype(np.float32)
    return m


def kernel(**inputs):
    P, B = 8, 32
    nc, cfg = build(P, B)
    m = host_layout(inputs, P, B)
    res = run_bass_kernel_spmd(nc, [m], core_ids=[0])
    y = np.asarray(res.results[0]["y_out"], dtype=np.float32)
    return (y[:P * L].reshape(1, P * L, D).copy(), y[P * L:].reshape(B, L, D).copy())
```

```python
import numpy as np
from contextlib import ExitStack
import concourse.bass as bass
import concourse.mybir as mybir
from concourse.bass_utils import run_bass_kernel_spmd

F32 = mybir.dt.float32
BF16 = mybir.dt.bfloat16
I32 = mybir.dt.int32
AF = mybir.ActivationFunctionType
ALU = mybir.AluOpType
AX = mybir.AxisListType

D = 1024
L = 2048
EPS = 1e-6
NE = 16
BIG = 1.0e6
import os
HSTOP = int(os.environ.get('HSTOP', '99'))
MSTOP = int(os.environ.get('MSTOP', '99'))
FSTOP = int(os.environ.get('FSTOP', '99'))
NEXP = int(os.environ.get('NEXP', '16'))


class Cx:
    ENG = ('pe', 'dve', 'act', 'pool')

    def __init__(s, nc, es):
        s.nc = nc
        s.e = {'pe': nc.tensor, 'dve': nc.vector, 'act': nc.scalar, 'pool': nc.gpsimd, 'sp': nc.sync}
        s.sem = {}
        for k in s.ENG:
            s.sem[k] = es.enter_context(nc.semaphore(f"c_{k}"))
        s.cnt = {k: 0 for k in s.ENG}
        s.ch = []
        for q, n in (('sp', 8), ('pool', 8)):
            for i in range(n):
                nm = f"d_{q}{i}"
                s.sem[nm] = es.enter_context(nc.semaphore(nm))
                s.ch.append({'q': q, 'name': nm, 'cnt': 0})
        s.rr = {'sp': 0, 'pool': 0}
        s.bar = [es.enter_context(nc.semaphore(f"bar{i}")) for i in range(4)]
        s.par = 0
        s.seen = {k: {} for k in ('pe', 'dve', 'act', 'pool', 'sp')}
        s.lw = {}
        s.rd = {}
        s.ninst = 0
        s.nwait = 0
        s.npool = 0
        s.nopt = es.enter_context(nc.sbuf_tensor("cx_nop", [128, 1], F32))

    def _wait(s, eng, sn, v):
        if v <= 0:
            return
        if s.seen[eng].get(sn, 0) >= v:
            return
        s.e[eng].wait_ge(s.sem[sn], v)
        s.seen[eng][sn] = v
        s.nwait += 1

    def _deps(s, eng, r, w):
        need = {}
        for k in r:
            ev = s.lw.get(k)
            if ev:
                need[ev[0]] = max(need.get(ev[0], 0), ev[1])
        for k in w:
            ev = s.lw.get(k)
            if ev:
                need[ev[0]] = max(need.get(ev[0], 0), ev[1])
            for sn, v in s.rd.get(k, {}).items():
                need[sn] = max(need.get(sn, 0), v)
        for sn, v in need.items():
            if eng == 'pe' and sn == 'pe':
                continue
            s._wait(eng, sn, v)

    def _rec(s, ev, r, w):
        for k in w:
            s.lw[k] = ev
            s.rd[k] = {}
        for k in r:
            d = s.rd.setdefault(k, {})
            d[ev[0]] = max(d.get(ev[0], 0), ev[1])

    def op(s, eng, fn, r=(), w=()):
        s._deps(eng, r, w)
        inst = fn(s.e[eng])
        s.cnt[eng] += 1
        assert s.cnt[eng] < 32000, "epoch too long"
        inst.then_inc(s.sem[eng], 1)
        s._rec((eng, s.cnt[eng]), r, w)
        s.ninst += 1

    def dma(s, q, fn, r=(), w=()):
        chs = [c for c in s.ch if c['q'] == q]
        c = chs[s.rr[q] % len(chs)]
        s.rr[q] += 1
        nw0 = s.nwait
        s._wait(q, c['name'], c['cnt'])
        s._deps(q, r, w)
        if q == 'pool':
            s.npool += 1
            probe = s.e['pool'].alloc_register(f"cxprobe{s.npool}")
            s.e['pool'].free_register(probe)
            inst = fn(s.e[q])
            try:
                s.e['pool'].free_register(probe)
            except Exception:
                pass
        else:
            inst = fn(s.e[q])
        c['cnt'] += 16
        assert c['cnt'] < 32000, "epoch too long (dma)"
        inst.then_inc(s.sem[c['name']], 16)
        s._rec((c['name'], c['cnt']), r, w)
        s.ninst += 1

    def drain(s):
        for k in s.ENG:
            s._wait(k, k, s.cnt[k])
        for c in s.ch:
            s._wait(c['q'], c['name'], c['cnt'])

    def epoch(s):
        s.drain()
        A, B = s.bar[2 * s.par], s.bar[2 * s.par + 1]
        A2, B2 = s.bar[2 * (1 - s.par)], s.bar[2 * (1 - s.par) + 1]
        allk = ('pe', 'dve', 'act', 'pool', 'sp')
        for k in allk:
            s.e[k].sem_inc(A, 1)
        m = s.e['sp']
        m.wait_ge(A, 5)
        for k in s.ENG:
            m.sem_clear(s.sem[k])
        for c in s.ch:
            if c['q'] != 'pool':
                m.sem_clear(s.sem[c['name']])
        m.sem_clear(A2)
        m.sem_clear(B2)
        m.sem_inc(B, 1)
        for k in allk:
            s.e[k].wait_ge(B, 1)
        s.cnt = {k: 0 for k in s.ENG}
        for c in s.ch:
            if c['q'] != 'pool':
                c['cnt'] = 0
        s.seen = {k: {} for k in allk}
        s.lw = {}
        s.rd = {}
        s.par ^= 1


class Cfg:
    def __init__(s, P, B):
        s.P = P
        s.B = B
        s.NS = P + B
        s.T = s.NS * L
        s.NT = s.T // 128
        s.groups = [(0, P), (P, P + B)]


def chain_of(cfg, sg):
    if sg < cfg.P:
        return list(range(cfg.P))
    return [sg]


def load_w(cx, es, name, src, K, N, gcol=None, stg=None, dtype=BF16):
    nc = cx.nc
    kc = (K + 127) // 128
    wt = es.enter_context(nc.sbuf_tensor(name, [128, kc, N], dtype))
    for k in range(kc):
        rows = min(128, K - k * 128)
        for n0 in range(0, N, 1024):
            nn = min(1024, N - n0)
            sk = f"wstg{(k + n0 // 1024) % 2}"
            st = stg[(k + n0 // 1024) % 2]
            cx.dma('sp', lambda e, st=st, k=k, rows=rows, n0=n0, nn=nn: e.dma_start(out=st[0:rows, 0:nn], in_=src[k * 128:k * 128 + rows, n0:n0 + nn]),
                   w=[sk])
            if gcol is None:
                cx.op('pool', lambda e, st=st, k=k, rows=rows, n0=n0, nn=nn: e.tensor_copy(wt[0:rows, k, n0:n0 + nn], st[0:rows, 0:nn]),
                      r=[sk], w=[name])
            else:
                cx.op('dve', lambda e, st=st, k=k, rows=rows, n0=n0, nn=nn: e.tensor_scalar(wt[0:rows, k, n0:n0 + nn], st[0:rows, 0:nn], gcol[0:rows, k:k + 1], None, op0=ALU.mult),
                      r=[sk], w=[name])
    return wt


def load_cols(cx, es, name, src_vec, n):
    nc = cx.nc
    kc = n // 128
    t = es.enter_context(nc.sbuf_tensor(name, [128, kc], F32))
    cx.dma('sp', lambda e: e.dma_start(out=t[:, :], in_=src_vec.rearrange("(k p) -> p k", p=128), allow_slow_non_contiguous=True), w=[name])
    return t


def rstd_from_ssq(cx, out, ssq, n, tmpk):
    (o_ap, o_k), (s_ap, s_k) = out, ssq
    cx.op('dve', lambda e: e.tensor_scalar(o_ap, s_ap, 1.0 / n, EPS, op0=ALU.mult, op1=ALU.add), r=[s_k], w=[o_k])
    cx.op('act', lambda e: e.activation(out=o_ap, in_=o_ap, func=AF.Sqrt), r=[o_k], w=[o_k])
    cx.op('dve', lambda e: e.reciprocal(o_ap, o_ap), r=[o_k], w=[o_k])


def pass_h(cx, cfg, x_rows, hT_d, consts, ps, tag="a"):
    nc = cx.nc
    with ExitStack() as es:
        xt = [es.enter_context(nc.sbuf_tensor(f"h{tag}_xt{i}", [128, D], F32)) for i in range(2)]
        junk = es.enter_context(nc.sbuf_tensor(f"h{tag}_junk", [128, D], BF16))
        ssq = es.enter_context(nc.sbuf_tensor(f"h{tag}_ssq", [128, 2], F32))
        rs = es.enter_context(nc.sbuf_tensor(f"h{tag}_rs", [128, 2], F32))
        hT = [es.enter_context(nc.sbuf_tensor(f"h{tag}_hT{i}", [128, 8, 512], BF16)) for i in range(2)]
        ident = consts['identF']
        for sg in range(cfg.NS):
            for blk in range(L // 512):
                hb = hT[blk % 2]
                hk = f"h_hT{blk % 2}"
                for j4 in range(4):
                    j = blk * 4 + j4
                    b = j % 2
                    t0 = sg * L + j * 128
                    cx.dma('sp', lambda e, b=b, t0=t0: e.dma_start(out=xt[b][:, :], in_=x_rows(t0, 128)), w=[f"h_xt{b}"])
                    cx.op('act', lambda e, b=b: e.activation(out=junk[:, :], in_=xt[b][:, :], func=AF.Square, accum_out=ssq[:, b:b + 1]),
                          r=[f"h_xt{b}"], w=["h_junk", f"h_ssq{b}"])
                    rstd_from_ssq(cx, (rs[:, b:b + 1], f"h_rs{b}"), (ssq[:, b:b + 1], f"h_ssq{b}"), D, None)
                    cx.op('dve', lambda e, b=b: e.tensor_scalar(xt[b][:, :], xt[b][:, :], rs[:, b:b + 1], None, op0=ALU.mult),
                          r=[f"h_rs{b}", f"h_xt{b}"], w=[f"h_xt{b}"])
                    for half in range(2):
                        pt = ps[half]
                        pk = f"ps{half}"
                        for kk in range(4):
                            k = half * 4 + kk
                            cx.op('pe', lambda e, pt=pt, kk=kk, k=k, b=b: e.transpose(pt[:, kk * 128:(kk + 1) * 128], xt[b][:, k * 128:(k + 1) * 128], ident[:, :]),
                                  r=[f"h_xt{b}", "identF"], w=[pk])
                        eng = 'act' if half == 0 else 'dve'
                        if eng == 'act':
                            cx.op('act', lambda e, pt=pt, half=half, j4=j4, hb=hb: e.activation(
                                out=hb[:, half * 4:(half + 1) * 4, j4 * 128:(j4 + 1) * 128],
                                in_=pt[:, :].rearrange("p (k t) -> p k t", k=4), func=AF.Copy), r=[pk], w=[hk])
                        else:
                            cx.op('dve', lambda e, pt=pt, half=half, j4=j4, hb=hb: e.tensor_copy(
                                hb[:, half * 4:(half + 1) * 4, j4 * 128:(j4 + 1) * 128],
                                pt[:, :].rearrange("p (k t) -> p k t", k=4)), r=[pk], w=[hk])
                c0 = sg * L + blk * 512
                cx.dma('sp', lambda e, hb=hb, c0=c0: e.dma_start(out=hT_d[:, :, c0:c0 + 512].rearrange("k p t -> p k t"), in_=hb[:, :, :]),
                       r=[hk], w=[f"d:hT:{sg}"])
            cx.epoch()


def pass_hgrn(cx, cfg, hT_d, o_d, w_in, lbT, dirn, consts, ps, gmix, stop=99):
    nc = cx.nc
    with ExitStack() as es:
        stg = [es.enter_context(nc.sbuf_tensor(f"g{dirn}_stg{i}", [128, 1024], F32)) for i in range(2)]
        wq = load_w(cx, es, f"g{dirn}_wq", w_in[:, 0:512], D, 512, gcol=gmix, stg=stg)
        wi = load_w(cx, es, f"g{dirn}_wi", w_in[:, 512:1024], D, 512, gcol=gmix, stg=stg)
        wf = load_w(cx, es, f"g{dirn}_wf", w_in[:, 1536 + 512 * dirn:2048 + 512 * dirn], D, 512, gcol=gmix, stg=stg)
        hR = es.enter_context(nc.sbuf_tensor(f"g{dirn}_hR", [128, 8, L], BF16))
        hst = [es.enter_context(nc.sbuf_tensor(f"g{dirn}_hst{i}", [128, L], BF16)) for i in range(2)] if dirn else None
        f = es.enter_context(nc.sbuf_tensor(f"g{dirn}_f", [128, 1024], F32))
        bb = es.enter_context(nc.sbuf_tensor(f"g{dirn}_b", [128, 1024], F32))
        ea = es.enter_context(nc.sbuf_tensor(f"g{dirn}_ea", [128, 1024], F32))
        kk_ = es.enter_context(nc.sbuf_tensor(f"g{dirn}_k", [128, 1024], F32))
        qt = es.enter_context(nc.sbuf_tensor(f"g{dirn}_qt", [128, 4, L], BF16))
        kt = es.enter_context(nc.sbuf_tensor(f"g{dirn}_kt", [128, 4, L], BF16))
        kh = es.enter_context(nc.sbuf_tensor(f"g{dirn}_kh", [128, 4, L], BF16))
        ebl = es.enter_context(nc.sbuf_tensor(f"g{dirn}_ebl", [128, 4, 32], F32))
        v = es.enter_context(nc.sbuf_tensor(f"g{dirn}_v", [64, 2, 512], BF16))
        khT = es.enter_context(nc.sbuf_tensor(f"g{dirn}_khT", [64, 2, 512], BF16))
        pT = es.enter_context(nc.sbuf_tensor(f"g{dirn}_pT", [64, 4, 64], BF16))
        osb = es.enter_context(nc.sbuf_tensor(f"g{dirn}_o", [64, 2, 8, 512], BF16))
        otmp = es.enter_context(nc.sbuf_tensor(f"g{dirn}_otmp", [64, 512], F32))
        S = es.enter_context(nc.sbuf_tensor(f"g{dirn}_S", [128, 512], F32))
        Sb = es.enter_context(nc.sbuf_tensor(f"g{dirn}_Sb", [128, 512], BF16))
        identB = consts['identB']
        cmask = consts['cmask']
        smask = consts['smask']
        lb = lbT[dirn]
        segs = list(range(cfg.NS))
        if dirn:
            segs = list(reversed(range(cfg.P))) + list(range(cfg.P, cfg.NS))
        for si, sg in enumerate(segs):
            first = (sg >= cfg.P) or (sg == (cfg.P - 1 if dirn else 0))
            for k in range(8):
                if dirn:
                    cx.dma('sp', lambda e, k=k, sg=sg: e.dma_start(out=hst[k % 2][:, :], in_=hT_d[k, :, sg * L:(sg + 1) * L]), w=[f"g_hst{k % 2}"])
                    cx.op('dve', lambda e, k=k: e.tensor_copy(hR[:, k, :], hst[k % 2][:, ::-1]), r=[f"g_hst{k % 2}"], w=["g_hR"])
                else:
                    cx.dma('sp', lambda e, k=k, sg=sg: e.dma_start(out=hR[:, k, :], in_=hT_d[k, :, sg * L:(sg + 1) * L]), w=["g_hR"])
            hk = "g_hR"
            if first:
                cx.op('dve', lambda e: e.memset(S[:, :], 0.0), w=["g_S0", "g_S1", "g_S2", "g_S3"])
                cx.op('pool', lambda e: e.memset(Sb[:, :], 0.0), w=["g_Sb"])
            if stop <= 0:
                cx.epoch(); continue
            for h in range(4):
                for hf in range(2):
                    c0 = hf * 1024
                    pz = (ps[0], ps[1])
                    pq = (ps[2], ps[3])
                    for nb in range(2):
                        for k in range(8):
                            cx.op('pe', lambda e, nb=nb, k=k, h=h, c0=c0: e.matmul(pz[nb][:, :], lhsT=wf[:, k, h * 128:(h + 1) * 128], rhs=hR[:, k, c0 + nb * 512:c0 + (nb + 1) * 512], start=(k == 0), stop=(k == 7)),
                                  r=[f"g{dirn}_wf", hk], w=[f"ps{nb}"])
                        for k in range(8):
                            cx.op('pe', lambda e, nb=nb, k=k, h=h, c0=c0: e.matmul(pq[nb][:, :], lhsT=wq[:, k, h * 128:(h + 1) * 128], rhs=hR[:, k, c0 + nb * 512:c0 + (nb + 1) * 512], start=(k == 0), stop=(k == 7)),
                                  r=[f"g{dirn}_wq", hk], w=[f"ps{2 + nb}"])
                    for nb in range(2):
                        cx.op('act', lambda e, nb=nb: e.activation(out=f[:, nb * 512:(nb + 1) * 512], in_=pz[nb][:, :], func=AF.Sigmoid), r=[f"ps{nb}"], w=["g_f"])
                    cx.op('dve', lambda e, h=h: e.tensor_scalar(f[:, :], f[:, :], lb[1][:, h:h + 1], lb[0][:, h:h + 1], op0=ALU.mult, op1=ALU.add), r=["g_f", "lbT"], w=["g_f"])
                    cx.op('act', lambda e: e.activation(out=bb[:, :], in_=f[:, :], func=AF.Ln), r=["g_f"], w=["g_b"])
                    cx.op('dve', lambda e: e.tensor_tensor_scan(out=bb[:, :], data0=smask[:, :], data1=bb[:, :], initial=0.0, op0=ALU.mult, op1=ALU.add), r=["g_b", "smask"], w=["g_b"])
                    cx.op('pool', lambda e: e.tensor_scalar(kk_[:, :], f[:, :], -1.0, 1.0, op0=ALU.mult, op1=ALU.add), r=["g_f"], w=["g_k"])
                    cx.op('act', lambda e: e.activation(out=ea[:, :], in_=bb[:, :], func=AF.Exp), r=["g_b"], w=["g_ea"])
                    for nb in range(2):
                        cx.op('dve', lambda e, nb=nb, h=h, c0=c0: e.scalar_tensor_tensor(out=qt[:, h, c0 + nb * 512:c0 + (nb + 1) * 512], in0=pq[nb][:, :], scalar=128.0 ** -0.5, in1=ea[:, nb * 512:(nb + 1) * 512], op0=ALU.mult, op1=ALU.mult),
                              r=[f"ps{2 + nb}", "g_ea"], w=["g_qt"])
                    cx.op('act', lambda e: e.activation(out=ea[:, :], in_=bb[:, :], func=AF.Exp, scale=-1.0), r=["g_b"], w=["g_ea"])
                    cx.op('dve', lambda e, h=h, c0=c0: e.tensor_tensor(out=kt[:, h, c0:c0 + 1024], in0=kk_[:, :], in1=ea[:, :], op=ALU.mult), r=["g_k", "g_ea"], w=["g_kt"])
                    b3 = bb[:, :].rearrange("p (c t) -> p c t", t=64)
                    cx.op('act', lambda e, h=h, hf=hf: e.activation(out=ebl[:, h, hf * 16:(hf + 1) * 16], in_=b3[:, :, 63], func=AF.Exp), r=["g_b"], w=["g_ebl"])
                    cx.op('dve', lambda e: e.tensor_tensor(out=ea[:, :].rearrange("p (c t) -> p c t", t=64), in0=b3[:, :, 63:64].to_broadcast([128, 16, 64]), in1=b3, op=ALU.subtract), r=["g_b"], w=["g_ea"])
                    cx.op('act', lambda e: e.activation(out=ea[:, :], in_=ea[:, :], func=AF.Exp), r=["g_ea"], w=["g_ea"])
                    cx.op('dve', lambda e, h=h, c0=c0: e.tensor_tensor(out=kh[:, h, c0:c0 + 1024], in0=kk_[:, :], in1=ea[:, :], op=ALU.mult), r=["g_k", "g_ea"], w=["g_kh"])
            if stop <= 1:
                cx.epoch(); continue
            if dirn == 0:
                r0 = sg * L
            else:
                r0 = (cfg.P - 1 - sg) * L if sg < cfg.P else sg * L
            for c in range(32):
                cb = c % 2
                pv = ps[4 + cb]
                pvk = f"ps{4 + cb}"
                for k in range(8):
                    cx.op('pe', lambda e, k=k, c=c, pv=pv: e.matmul(pv[0:64, :], lhsT=hR[:, k, c * 64:(c + 1) * 64], rhs=wi[:, k, :], start=(k == 0), stop=(k == 7)),
                          r=[hk, f"g{dirn}_wi"], w=[pvk])
                cx.op('act', lambda e, cb=cb, pv=pv: e.activation(out=v[:, cb, :], in_=pv[0:64, :], func=AF.Copy), r=[pvk], w=[f"g_v{cb}"])
                pk_ = consts['psb'][cb]
                pkk = f"psb{cb}"
                for h in range(4):
                    cx.op('pe', lambda e, h=h, c=c, pk_=pk_: e.transpose(pk_[0:64, h * 128:(h + 1) * 128], kh[:, h, c * 64:(c + 1) * 64], identB[:, :]),
                          r=["g_kh", "identB"], w=[pkk])
                cx.op('pool' if False else 'dve', lambda e, cb=cb, pk_=pk_: e.tensor_copy(khT[:, cb, :], pk_[0:64, :]), r=[pkk], w=[f"g_khT{cb}"])
                psS, pso, psd = ps[0], ps[1], ps[2]
                for h in range(4):
                    cx.op('pe', lambda e, h=h, c=c: e.matmul(psS[0:64, h * 64:(h + 1) * 64], lhsT=kt[:, h, c * 64:(c + 1) * 64], rhs=qt[:, h, c * 64:(c + 1) * 64], start=True, stop=True),
                          r=["g_kt", "g_qt"], w=["ps0"])
                cx.op('dve', lambda e: e.tensor_tensor(out=pT[:, :, :], in0=psS[0:64, 0:256].rearrange("p (h t) -> p h t", h=4), in1=cmask[:, :].unsqueeze(1).to_broadcast([64, 4, 64]), op=ALU.mult),
                      r=["ps0", "cmask"], w=["g_pT"])
                for h in range(4):
                    cx.op('pe', lambda e, h=h, cb=cb: e.matmul(pso[0:64, h * 128:(h + 1) * 128], lhsT=pT[:, h, :], rhs=v[:, cb, h * 128:(h + 1) * 128], start=True, stop=True),
                          r=["g_pT", f"g_v{cb}"], w=["ps1"])
                    cx.op('pe', lambda e, h=h, c=c: e.matmul(ps[3][0:64, h * 128:(h + 1) * 128], lhsT=qt[:, h, c * 64:(c + 1) * 64], rhs=Sb[:, h * 128:(h + 1) * 128], start=True, stop=True),
                          r=["g_qt", "g_Sb"], w=["ps3"])
                ob = (c // 8) % 2
                cx.op('act', lambda e: e.activation(out=otmp[:, :], in_=pso[0:64, :], func=AF.Copy), r=["ps1"], w=["g_otmp"])
                cx.op('dve', lambda e, c=c, ob=ob: e.tensor_tensor(out=osb[:, ob, c % 8, :], in0=ps[3][0:64, :], in1=otmp[:, :], op=ALU.add), r=["ps3", "g_otmp"], w=[f"g_o{ob}"])
                for h in range(4):
                    cx.op('pe', lambda e, h=h, cb=cb: e.matmul(psd[:, h * 128:(h + 1) * 128], lhsT=khT[:, cb, h * 128:(h + 1) * 128], rhs=v[:, cb, h * 128:(h + 1) * 128], start=True, stop=True),
                          r=[f"g_khT{cb}", f"g_v{cb}"], w=["ps2"])
                for h in range(4):
                    cx.op('dve', lambda e, h=h, c=c: e.scalar_tensor_tensor(out=S[:, h * 128:(h + 1) * 128], in0=S[:, h * 128:(h + 1) * 128], scalar=ebl[:, h, c:c + 1], in1=psd[:, h * 128:(h + 1) * 128], op0=ALU.mult, op1=ALU.add),
                          r=[f"g_S{h}", "g_ebl", "ps2"], w=[f"g_S{h}"])
                cx.op('act', lambda e: e.activation(out=Sb[:, :], in_=S[:, :], func=AF.Copy), r=["g_S0", "g_S1", "g_S2", "g_S3"], w=["g_Sb"])
                if c % 8 == 7:
                    rr0 = r0 + (c - 7) * 64
                    cx.dma('sp', lambda e, rr0=rr0, ob=ob: e.dma_start(out=o_d[rr0:rr0 + 512, :].rearrange("(c t) n -> t c n", t=64), in_=osb[:, ob, :, :]), r=[f"g_o{ob}"], w=[f"d:o{dirn}:{sg}:{c}"])
            cx.epoch()


def rstd_tile(cx, out_ap, out_k, ps_ap, ps_k, n):
    cx.op('dve', lambda e: e.tensor_scalar(out_ap, ps_ap, 1.0 / n, EPS, op0=ALU.mult, op1=ALU.add), r=[ps_k], w=[out_k])
    cx.op('act', lambda e: e.activation(out=out_ap, in_=out_ap, func=AF.Sqrt), r=[out_k], w=[out_k])
    cx.op('dve', lambda e: e.reciprocal(out_ap, out_ap), r=[out_k], w=[out_k])


def seg_pos0(cfg, sg):
    return sg * L if sg < cfg.P else 0


def pass_mla_prep(cx, cfg, hT_d, q_d, k_d, v_d, ins, consts, ps, gmix):
    nc = cx.nc
    w_in = ins["ev_w_in"]
    with ExitStack() as es:
        stg = [es.enter_context(nc.sbuf_tensor(f"m1_stg{i}", [128, 1024], F32)) for i in range(2)]
        wcq = load_w(cx, es, "m1_wcq", w_in[:, 2560:2944], D, 384, gcol=gmix, stg=stg)
        wckv = load_w(cx, es, "m1_wckv", w_in[:, 2944:3200], D, 256, gcol=gmix, stg=stg)
        wkpe = load_w(cx, es, "m1_wkpe", w_in[:, 3200:3264], D, 64, gcol=gmix, stg=stg)
        gqn = load_cols(cx, es, "m1_gqn", ins["mla_q_norm"], 384)
        gkvn = load_cols(cx, es, "m1_gkvn", ins["mla_kv_norm"], 256)
        wuq = load_w(cx, es, "m1_wuq", ins["mla_w_uq"], 384, 768, gcol=gqn, stg=stg)
        wukv = load_w(cx, es, "m1_wukv", ins["mla_w_ukv"], 256, 1024, gcol=gkvn, stg=stg)
        wv = es.enter_context(nc.sbuf_tensor("m1_wv", [128, 2, 512], BF16))
        for h in range(4):
            cx.op('dve', lambda e, h=h: e.tensor_copy(wv[:, :, h * 128:(h + 1) * 128], wukv[:, :, h * 256 + 128:h * 256 + 256]), r=["m1_wukv"], w=["m1_wv"])
        gq = es.enter_context(nc.sbuf_tensor("m1_gq", [128, 2], F32))
        gk = es.enter_context(nc.sbuf_tensor("m1_gk", [128, 2], F32))
        for (t, src, nm) in ((gq, ins["mla_q_gain"], "m1_gq"), (gk, ins["mla_k_gain"], "m1_gk")):
            cx.op('dve', lambda e, t=t: e.memset(t[:, :], 0.0), w=[nm])
            cx.dma('sp', lambda e, t=t, src=src: e.dma_start(out=t[:, 0:1], in_=src[0:128].rearrange("(p o) -> p o", o=1)), w=[nm])
            cx.dma('sp', lambda e, t=t, src=src: e.dma_start(out=t[0:64, 1:2], in_=src[128:192].rearrange("(p o) -> p o", o=1)), w=[nm])
        cx.op('dve', lambda e: e.tensor_scalar(gq[:, :], gq[:, :], 192.0 ** -0.5, None, op0=ALU.mult), r=["m1_gq"], w=["m1_gq"])
        Rm = es.enter_context(nc.sbuf_tensor("m1_Rm", [64, 64], F32))
        Rt = es.enter_context(nc.sbuf_tensor("m1_Rt", [64, 64], F32))
        iot = consts['iot']
        cx.op('dve', lambda e: e.tensor_scalar(Rm[:, :], iot[0:64, 0:64], 32.0, None, op0=ALU.is_equal), r=["c_iot"], w=["m1_Rm"])
        cx.op('dve', lambda e: e.tensor_scalar(Rt[:, :], iot[0:64, 0:64], -32.0, None, op0=ALU.is_equal), r=["c_iot"], w=["m1_Rt"])
        cx.op('dve', lambda e: e.tensor_tensor(out=Rm[:, :], in0=Rm[:, :], in1=Rt[:, :], op=ALU.subtract), r=["m1_Rm", "m1_Rt"], w=["m1_Rm"])
        hb = es.enter_context(nc.sbuf_tensor("m1_hb", [128, 8, 512], BF16))
        cq = es.enter_context(nc.sbuf_tensor("m1_cq", [128, 3, 512], F32))
        cqn = es.enter_context(nc.sbuf_tensor("m1_cqn", [128, 3, 512], BF16))
        ckvn = es.enter_context(nc.sbuf_tensor("m1_ckvn", [128, 2, 512], BF16))
        sqb = es.enter_context(nc.sbuf_tensor("m1_sqb", [128, 3, 512], BF16))
        sqr = es.enter_context(nc.sbuf_tensor("m1_sqr", [128, 512], BF16))
        sqk = es.enter_context(nc.sbuf_tensor("m1_sqk", [128, 512], BF16))
        r1 = es.enter_context(nc.sbuf_tensor("m1_r1", [128, 512], F32))
        nf = es.enter_context(nc.sbuf_tensor("m1_nf", [128, 512], F32))
        rf = es.enter_context(nc.sbuf_tensor("m1_rf", [64, 512], F32))
        t1 = es.enter_context(nc.sbuf_tensor("m1_t1", [64, 512], F32))
        t2 = es.enter_context(nc.sbuf_tensor("m1_t2", [64, 512], F32))
        kpe = es.enter_context(nc.sbuf_tensor("m1_kpe", [64, 512], F32))
        krot = es.enter_context(nc.sbuf_tensor("m1_krot", [64, 512], F32))
        cs = es.enter_context(nc.sbuf_tensor("m1_cos", [64, 512], F32))
        sn = es.enter_context(nc.sbuf_tensor("m1_sin", [64, 512], F32))
        on = es.enter_context(nc.sbuf_tensor("m1_on", [128, 2, 512], BF16))
        orp = es.enter_context(nc.sbuf_tensor("m1_or", [64, 2, 512], BF16))
        vt = es.enter_context(nc.sbuf_tensor("m1_vt", [128, 2, 512], BF16))
        onesB = consts['onesB']
        cx.op('dve', lambda e: e.memset(sqr[:, :], 0.0), w=["m1_sqr"])
        cx.op('dve', lambda e: e.memset(sqk[:, :], 0.0), w=["m1_sqk"])
        pA, pB, pC, pD = ps[0], ps[1], ps[2], ps[3]

        def rope(src, dst_k, dst_ap):
            cx.op('pe', lambda e: e.matmul(pD[0:64, :], lhsT=Rm[:, :], rhs=src[:, :], start=True, stop=True), r=["m1_Rm", "m1_t1"], w=["ps3"])
            cx.op('dve', lambda e: e.tensor_tensor(out=t2[:, :], in0=pD[0:64, :], in1=sn[:, :], op=ALU.mult), r=["ps3", "m1_sin"], w=["m1_t2"])
            cx.op('dve', lambda e: e.tensor_tensor(out=src[:, :], in0=src[:, :], in1=cs[:, :], op=ALU.mult), r=["m1_t1", "m1_cos"], w=["m1_t1"])
            cx.op('dve', lambda e: e.tensor_tensor(out=dst_ap, in0=src[:, :], in1=t2[:, :], op=ALU.add), r=["m1_t1", "m1_t2"], w=[dst_k])

        for sg in range(cfg.NS):
            for blk in range(4):
                c0 = sg * L + blk * 512
                p0 = seg_pos0(cfg, sg) + blk * 512
                cx.dma('sp', lambda e, c0=c0: e.dma_start(out=hb[:, :, :], in_=hT_d[:, :, c0:c0 + 512].rearrange("k p t -> p k t")), w=["m1_hb"])
                cx.dma('sp', lambda e, p0=p0: e.dma_start(out=cs[:, :], in_=ins["rope_cos"][:, p0:p0 + 512]), w=["m1_cos"])
                cx.dma('sp', lambda e, p0=p0: e.dma_start(out=sn[:, :], in_=ins["rope_sin"][:, p0:p0 + 512]), w=["m1_sin"])
                for (wt, wk, nch, dst, dk) in ((wcq, "m1_wcq", 3, cqn, "m1_cqn"), (wckv, "m1_wckv", 2, ckvn, "m1_ckvn")):
                    for n in range(nch):
                        for k in range(8):
                            cx.op('pe', lambda e, wt=wt, n=n, k=k: e.matmul(pA[:, :], lhsT=wt[:, k, n * 128:(n + 1) * 128], rhs=hb[:, k, :], start=(k == 0), stop=(k == 7)),
                                  r=[wk, "m1_hb"], w=["ps0"])
                        cx.op('act', lambda e, n=n: e.activation(out=cq[:, n, :], in_=pA[:, :], func=AF.Copy), r=["ps0"], w=["m1_cq"])
                        cx.op('dve', lambda e, n=n: e.tensor_tensor(out=sqb[:, n, :], in0=pA[:, :], in1=cq[:, n, :], op=ALU.mult), r=["ps0", "m1_cq"], w=["m1_sqb"])
                    for n in range(nch):
                        cx.op('pe', lambda e, n=n, nch=nch: e.matmul(pB[:, :], lhsT=onesB[:, :], rhs=sqb[:, n, :], start=(n == 0), stop=(n == nch - 1)), r=["onesB", "m1_sqb"], w=["ps1"])
                    rstd_tile(cx, r1[:, :], "m1_r1", pB[:, :], "ps1", nch * 128)
                    for n in range(nch):
                        cx.op('dve', lambda e, n=n, dst=dst: e.tensor_tensor(out=dst[:, n, :], in0=cq[:, n, :], in1=r1[:, :], op=ALU.mult), r=["m1_cq", "m1_r1"], w=[dk])
                for k in range(8):
                    cx.op('pe', lambda e, k=k: e.matmul(pC[0:64, :], lhsT=wkpe[:, k, :], rhs=hb[:, k, :], start=(k == 0), stop=(k == 7)), r=["m1_wkpe", "m1_hb"], w=["ps2"])
                cx.op('act', lambda e: e.activation(out=kpe[:, :], in_=pC[0:64, :], func=AF.Copy), r=["ps2"], w=["m1_kpe"])
                cx.op('dve', lambda e: e.tensor_tensor(out=sqk[0:64, :], in0=pC[0:64, :], in1=kpe[:, :], op=ALU.mult), r=["ps2", "m1_kpe"], w=["m1_sqk"])
                cx.op('dve', lambda e: e.tensor_scalar(t1[:, :], kpe[:, :], gk[0:64, 1:2], None, op0=ALU.mult), r=["m1_kpe", "m1_gk"], w=["m1_t1"])
                rope(t1, "m1_krot", krot[:, :])
                for h in range(4):
                    hb2 = h % 2
                    for n in range(3):
                        cx.op('pe', lambda e, n=n, h=h: e.matmul(pA[:, :], lhsT=wuq[:, n, h * 192:h * 192 + 128], rhs=cqn[:, n, :], start=(n == 0), stop=(n == 2)), r=["m1_wuq", "m1_cqn"], w=["ps0"])
                    for n in range(3):
                        cx.op('pe', lambda e, n=n, h=h: e.matmul(pC[0:64, :], lhsT=wuq[:, n, h * 192 + 128:h * 192 + 192], rhs=cqn[:, n, :], start=(n == 0), stop=(n == 2)), r=["m1_wuq", "m1_cqn"], w=["ps2"])
                    cx.op('act', lambda e: e.activation(out=nf[:, :], in_=pA[:, :], func=AF.Copy), r=["ps0"], w=["m1_nf"])
                    cx.op('dve', lambda e: e.tensor_tensor(out=sqb[:, 0, :], in0=pA[:, :], in1=nf[:, :], op=ALU.mult), r=["ps0", "m1_nf"], w=["m1_sqb"])
                    cx.op('act', lambda e: e.activation(out=rf[:, :], in_=pC[0:64, :], func=AF.Copy), r=["ps2"], w=["m1_rf"])
                    cx.op('dve', lambda e: e.tensor_tensor(out=sqr[0:64, :], in0=pC[0:64, :], in1=rf[:, :], op=ALU.mult), r=["ps2", "m1_rf"], w=["m1_sqr"])
                    cx.op('pe', lambda e: e.matmul(pB[:, :], lhsT=onesB[:, :], rhs=sqb[:, 0, :], start=True, stop=False), r=["onesB", "m1_sqb"], w=["ps1"])
                    cx.op('pe', lambda e: e.matmul(pB[:, :], lhsT=onesB[:, :], rhs=sqr[:, :], start=False, stop=True), r=["onesB", "m1_sqr"], w=["ps1"])
                    rstd_tile(cx, r1[:, :], "m1_r1", pB[:, :], "ps1", 192)
                    cx.op('dve', lambda e, hb2=hb2: e.scalar_tensor_tensor(out=on[:, hb2, :], in0=nf[:, :], scalar=gq[:, 0:1], in1=r1[:, :], op0=ALU.mult, op1=ALU.mult), r=["m1_nf", "m1_gq", "m1_r1"], w=[f"m1_on{hb2}"])
                    cx.dma('sp', lambda e, h=h, hb2=hb2, c0=c0: e.dma_start(out=q_d[h, 0:128, c0:c0 + 512], in_=on[:, hb2, :]), r=[f"m1_on{hb2}"], w=[f"d:q:{sg}"])
                    cx.op('dve', lambda e: e.scalar_tensor_tensor(out=t1[:, :], in0=rf[:, :], scalar=gq[0:64, 1:2], in1=r1[0:64, :], op0=ALU.mult, op1=ALU.mult), r=["m1_rf", "m1_gq", "m1_r1"], w=["m1_t1"])
                    rope(t1, f"m1_or{hb2}", orp[:, hb2, :])
                    cx.dma('sp', lambda e, h=h, hb2=hb2, c0=c0: e.dma_start(out=q_d[h, 128:192, c0:c0 + 512], in_=orp[:, hb2, :]), r=[f"m1_or{hb2}"], w=[f"d:q:{sg}"])
                    for n in range(2):
                        cx.op('pe', lambda e, n=n, h=h: e.matmul(pA[:, :], lhsT=wukv[:, n, h * 256:h * 256 + 128], rhs=ckvn[:, n, :], start=(n == 0), stop=(n == 1)), r=["m1_wukv", "m1_ckvn"], w=["ps0"])
                    cx.op('act', lambda e: e.activation(out=nf[:, :], in_=pA[:, :], func=AF.Copy), r=["ps0"], w=["m1_nf"])
                    cx.op('dve', lambda e: e.tensor_tensor(out=sqb[:, 0, :], in0=pA[:, :], in1=nf[:, :], op=ALU.mult), r=["ps0", "m1_nf"], w=["m1_sqb"])
                    cx.op('pe', lambda e: e.matmul(pB[:, :], lhsT=onesB[:, :], rhs=sqb[:, 0, :], start=True, stop=False), r=["onesB", "m1_sqb"], w=["ps1"])
                    cx.op('pe', lambda e: e.matmul(pB[:, :], lhsT=onesB[:, :], rhs=sqk[:, :], start=False, stop=True), r=["onesB", "m1_sqk"], w=["ps1"])
                    rstd_tile(cx, r1[:, :], "m1_r1", pB[:, :], "ps1", 192)
                    hb3 = 1 - hb2
                    cx.op('dve', lambda e, hb3=hb3: e.scalar_tensor_tensor(out=on[:, hb3, :], in0=nf[:, :], scalar=gk[:, 0:1], in1=r1[:, :], op0=ALU.mult, op1=ALU.mult), r=["m1_nf", "m1_gk", "m1_r1"], w=[f"m1_on{hb3}"])
                    cx.dma('sp', lambda e, h=h, hb3=hb3, c0=c0: e.dma_start(out=k_d[h, 0:128, c0:c0 + 512], in_=on[:, hb3, :]), r=[f"m1_on{hb3}"], w=[f"d:k:{sg}"])
                    cx.op('dve', lambda e, hb3=hb3: e.tensor_tensor(out=orp[:, hb3, :], in0=krot[:, :], in1=r1[0:64, :], op=ALU.mult), r=["m1_krot", "m1_r1"], w=[f"m1_or{hb3}"])
                    cx.dma('sp', lambda e, h=h, hb3=hb3, c0=c0: e.dma_start(out=k_d[h, 128:192, c0:c0 + 512], in_=orp[:, hb3, :]), r=[f"m1_or{hb3}"], w=[f"d:k:{sg}"])
                for j in range(4):
                    jb = j % 2
                    for n in range(2):
                        cx.op('pe', lambda e, n=n, j=j: e.matmul(pA[:, :], lhsT=ckvn[:, n, j * 128:(j + 1) * 128], rhs=wv[:, n, :], start=(n == 0), stop=(n == 1)), r=["m1_ckvn", "m1_wv"], w=["ps0"])
                    cx.op('act', lambda e, jb=jb: e.activation(out=vt[:, jb, :], in_=pA[:, :], func=AF.Copy), r=["ps0"], w=[f"m1_vt{jb}"])
                    cx.dma('sp', lambda e, jb=jb, j=j, c0=c0: e.dma_start(out=v_d[c0 + j * 128:c0 + (j + 1) * 128, :], in_=vt[:, jb, :]), r=[f"m1_vt{jb}"], w=[f"d:v:{sg}"])
            cx.epoch()


def pass_attn(cx, cfg, q_d, k_d, v_d, om_d, consts, ps):
    nc = cx.nc
    with ExitStack() as es:
        qn = es.enter_context(nc.sbuf_tensor("a_qn", [128, 4, L], BF16))
        qr = es.enter_context(nc.sbuf_tensor("a_qr", [128, 4, L], BF16))
        kn = es.enter_context(nc.sbuf_tensor("a_kn", [128, 4, L], BF16))
        kr = es.enter_context(nc.sbuf_tensor("a_kr", [128, 4, L], BF16))
        vv = es.enter_context(nc.sbuf_tensor("a_vv", [128, 16, 512], BF16))
        pT = [es.enter_context(nc.sbuf_tensor(f"a_pT{i}", [128, 512], BF16)) for i in range(2)]
        acc_o = es.enter_context(nc.sbuf_tensor("a_acco", [128, 16, 512], F32)) if cfg.P > 1 else None
        acc_d = es.enter_context(nc.sbuf_tensor("a_accd", [128, 16, 512], F32)) if cfg.P > 1 else None
        rec = es.enter_context(nc.sbuf_tensor("a_rec", [128, 512], F32))
        ot = [es.enter_context(nc.sbuf_tensor(f"a_ot{i}", [128, 512], BF16)) for i in range(2)]
        onesB = consts['onesB']
        for sg in range(cfg.NS):
            ctx = chain_of(cfg, sg)
            if sg == 0:
                cx.op('dve', lambda e: e.memset(qr[64:128, :, :], 0.0), w=["a_qr"])
                cx.op('pool', lambda e: e.memset(kr[64:128, :, :], 0.0), w=["a_kr"])
                cx.epoch()
            for h in range(4):
                cx.dma('sp', lambda e, h=h, sg=sg: e.dma_start(out=qn[:, h, :], in_=q_d[h, 0:128, sg * L:(sg + 1) * L]), w=["a_qn"])
                cx.dma('sp', lambda e, h=h, sg=sg: e.dma_start(out=qr[0:64, h, :], in_=q_d[h, 128:192, sg * L:(sg + 1) * L]), w=["a_qr"])
            for ki, ks in enumerate(ctx):
                for h in range(4):
                    cx.dma('sp', lambda e, h=h, ks=ks: e.dma_start(out=kn[:, h, :], in_=k_d[h, 0:128, ks * L:(ks + 1) * L]), w=["a_kn"])
                    cx.dma('sp', lambda e, h=h, ks=ks: e.dma_start(out=kr[0:64, h, :], in_=k_d[h, 128:192, ks * L:(ks + 1) * L]), w=["a_kr"])
                for j4 in range(4):
                    cx.dma('sp', lambda e, j4=j4, ks=ks: e.dma_start(out=vv[:, j4 * 4:(j4 + 1) * 4, :], in_=v_d[ks * L + j4 * 512:ks * L + (j4 + 1) * 512, :].rearrange("(j p) n -> p j n", p=128)), w=["a_vv"])
                for qb in range(4):
                    for h in range(4):
                        i = qb * 4 + h
                        po, pd = ps[2 + (i % 2)], ps[4 + (i % 2)]
                        pok, pdk = f"ps{2 + (i % 2)}", f"ps{4 + (i % 2)}"
                        def scores(kt, h=h, qb=qb):
                            sb = kt % 2
                            pS = ps[sb]
                            cx.op('pe', lambda e: e.matmul(pS[:, :], lhsT=kn[:, h, kt * 128:(kt + 1) * 128], rhs=qn[:, h, qb * 512:(qb + 1) * 512], start=True, stop=False),
                                  r=["a_kn", "a_qn"], w=[f"ps{sb}"])
                            cx.op('pe', lambda e: e.matmul(pS[:, :], lhsT=kr[:, h, kt * 128:(kt + 1) * 128], rhs=qr[:, h, qb * 512:(qb + 1) * 512], start=False, stop=True),
                                  r=["a_kr", "a_qr"], w=[f"ps{sb}"])
                        if qb == 0 and h == 0:
                            scores(0)
                        for kt in range(16):
                            sb = kt % 2
                            pS = ps[sb]
                            if kt + 1 < 16:
                                scores(kt + 1)
                            elif not (qb == 3 and h == 3):
                                nq, nh = (qb, h + 1) if h < 3 else (qb + 1, 0)
                                scores(0, h=nh, qb=nq)
                            cx.op('act', lambda e, sb=sb, pS=pS: e.activation(out=pT[sb][:, :], in_=pS[:, :], func=AF.Exp), r=[f"ps{sb}"], w=[f"a_pT{sb}"])
                            cx.op('pe', lambda e, po=po, h=h, kt=kt, sb=sb: e.matmul(po[:, :], lhsT=vv[:, kt, h * 128:(h + 1) * 128], rhs=pT[sb][:, :], start=(kt == 0), stop=(kt == 15)),
                                  r=["a_vv", f"a_pT{sb}"], w=[pok])
                            cx.op('pe', lambda e, pd=pd, sb=sb, kt=kt: e.matmul(pd[:, :], lhsT=onesB[:, :], rhs=pT[sb][:, :], start=(kt == 0), stop=(kt == 15)),
                                  r=["onesB", f"a_pT{sb}"], w=[pdk])
                        last = (ki == len(ctx) - 1)
                        if len(ctx) > 1:
                            if ki == 0:
                                cx.op('act', lambda e, po=po, i=i: e.activation(out=acc_o[:, i, :], in_=po[:, :], func=AF.Copy), r=[pok], w=["a_acco"])
                                cx.op('dve', lambda e, pd=pd, i=i: e.tensor_copy(acc_d[:, i, :], pd[:, :]), r=[pdk], w=["a_accd"])
                            else:
                                cx.op('dve', lambda e, po=po, i=i: e.tensor_tensor(out=acc_o[:, i, :], in0=po[:, :], in1=acc_o[:, i, :], op=ALU.add), r=[pok, "a_acco"], w=["a_acco"])
                                cx.op('dve', lambda e, pd=pd, i=i: e.tensor_tensor(out=acc_d[:, i, :], in0=pd[:, :], in1=acc_d[:, i, :], op=ALU.add), r=[pdk, "a_accd"], w=["a_accd"])
                        if last:
                            ob = i % 2
                            if len(ctx) > 1:
                                cx.op('dve', lambda e, i=i: e.reciprocal(rec[:, :], acc_d[:, i, :]), r=["a_accd"], w=["a_rec"])
                                cx.op('dve', lambda e, i=i, ob=ob: e.tensor_tensor(out=ot[ob][:, :], in0=acc_o[:, i, :], in1=rec[:, :], op=ALU.mult), r=["a_acco", "a_rec"], w=[f"a_ot{ob}"])
                            else:
                                cx.op('dve', lambda e, pd=pd: e.reciprocal(rec[:, :], pd[:, :]), r=[pdk], w=["a_rec"])
                                cx.op('dve', lambda e, po=po, ob=ob: e.tensor_tensor(out=ot[ob][:, :], in0=po[:, :], in1=rec[:, :], op=ALU.mult), r=[pok, "a_rec"], w=[f"a_ot{ob}"])
                            c0 = sg * L + qb * 512
                            cx.dma('sp', lambda e, h=h, ob=ob, c0=c0: e.dma_start(out=om_d[h, :, c0:c0 + 512], in_=ot[ob][:, :]), r=[f"a_ot{ob}"], w=[f"d:om:{sg}"])
                cx.epoch()


class Tail:
    def __init__(s, cx, es, pfx, w_out_src, router_src, gffn_col, stg):
        nc = cx.nc
        s.cx, s.pfx = cx, pfx
        s.wo = load_w(cx, es, f"{pfx}_wo", w_out_src, D, D, stg=stg)
        s.wr = es.enter_context(nc.sbuf_tensor(f"{pfx}_wr", [128, 8, NE], F32))
        cx.dma('sp', lambda e: e.dma_start(out=s.wr[:, :, :], in_=router_src.rearrange("(k p) n -> p k n", p=128)), w=[f"{pfx}_wr"])
        for k in range(8):
            cx.op('dve', lambda e, k=k: e.tensor_scalar(s.wr[:, k, :], s.wr[:, k, :], gffn_col[:, k:k + 1], None, op0=ALU.mult), r=[f"{pfx}_wr"], w=[f"{pfx}_wr"])
        s.xt = [es.enter_context(nc.sbuf_tensor(f"{pfx}_xt{i}", [128, D], F32)) for i in range(2)]
        s.hn = [es.enter_context(nc.sbuf_tensor(f"{pfx}_hn{i}", [128, D], F32)) for i in range(2)]
        s.hnb = [es.enter_context(nc.sbuf_tensor(f"{pfx}_hnb{i}", [128, D], BF16)) for i in range(2)]
        s.hnT = es.enter_context(nc.sbuf_tensor(f"{pfx}_hnT", [128, 8, 128], F32))
        s.sma = [es.enter_context(nc.sbuf_tensor(f"{pfx}_sma{i}", [128, 2], F32)) for i in range(2)]
        s.sm = es.enter_context(nc.sbuf_tensor(f"{pfx}_sm", [128, 4], F32))
        s.ee = es.enter_context(nc.sbuf_tensor(f"{pfx}_ee", [128, NE], F32))

    def run(s, mix, mk, c_off, t0, x_src, x1_d, hn_d, aff_sb, aff_d, consts, ps):
        cx, pfx = s.cx, s.pfx
        identF = consts['identF']

        def stage_a(j):
            b = j % 2
            xt, xk = s.xt[b], f"{pfx}_xt{b}"
            hn, hk = s.hn[b], f"{pfx}_hn{b}"
            hnb, hbk = s.hnb[b], f"{pfx}_hnb{b}"
            sma, sk = s.sma[b], f"{pfx}_sma{b}"
            r0 = t0 + j * 128
            cx.dma('sp', lambda e: e.dma_start(out=xt[:, :], in_=x_src[r0:r0 + 128, :]), w=[xk])
            for dh in range(2):
                po, pk = ps[dh], f"ps{dh}"
                for k in range(8):
                    cx.op('pe', lambda e, po=po, k=k, dh=dh: e.matmul(po[:, :], lhsT=mix[:, k, c_off + j * 128:c_off + (j + 1) * 128], rhs=s.wo[:, k, dh * 512:(dh + 1) * 512], start=(k == 0), stop=(k == 7)),
                          r=[mk, f"{pfx}_wo"], w=[pk])
                cx.op('dve', lambda e, po=po, dh=dh: e.tensor_tensor(out=xt[:, dh * 512:(dh + 1) * 512], in0=po[:, :], in1=xt[:, dh * 512:(dh + 1) * 512], op=ALU.add), r=[pk, xk], w=[xk])
            cx.dma('sp', lambda e: e.dma_start(out=x1_d[r0:r0 + 128, :], in_=xt[:, :]), r=[xk], w=[f"d:x1:{r0}"])
            cx.op('act', lambda e: e.activation(out=hn[:, :], in_=xt[:, :], func=AF.Square, accum_out=sma[:, 0:1]), r=[xk], w=[hk, sk])
            rstd_from_ssq(cx, (sma[:, 1:2], sk), (sma[:, 0:1], sk), D, None)
            cx.op('dve', lambda e: e.tensor_scalar(hn[:, :], xt[:, :], sma[:, 1:2], None, op0=ALU.mult), r=[xk, sk], w=[hk])
            cx.op('act', lambda e: e.activation(out=hnb[:, :], in_=hn[:, :], func=AF.Copy), r=[hk], w=[hbk])
            cx.dma('sp', lambda e: e.dma_start(out=hn_d[r0:r0 + 128, :], in_=hnb[:, :]), r=[hbk], w=[f"d:hn:{r0}"])

        def stage_b(j):
            b = j % 2
            hn, hk = s.hn[b], f"{pfx}_hn{b}"
            r0 = t0 + j * 128
            for half in range(2):
                pt, pk = ps[2 + half], f"ps{2 + half}"
                for kk in range(4):
                    k = half * 4 + kk
                    cx.op('pe', lambda e, pt=pt, kk=kk, k=k: e.transpose(pt[:, kk * 128:(kk + 1) * 128], hn[:, k * 128:(k + 1) * 128], identF[:, :]), r=[hk, "identF"], w=[pk])
                if half == 0:
                    cx.op('act', lambda e, pt=pt: e.activation(out=s.hnT[:, 0:4, :], in_=pt[:, :].rearrange("p (k t) -> p k t", k=4), func=AF.Copy), r=[pk], w=[f"{pfx}_hnT"])
                else:
                    cx.op('dve', lambda e, pt=pt: e.tensor_copy(s.hnT[:, 4:8, :], pt[:, :].rearrange("p (k t) -> p k t", k=4)), r=[pk], w=[f"{pfx}_hnT"])
            pl = ps[4]
            for k in range(8):
                cx.op('pe', lambda e, k=k: e.matmul(pl[:, 0:NE], lhsT=s.hnT[:, k, :], rhs=s.wr[:, k, :], start=(k == 0), stop=(k == 7)), r=[f"{pfx}_hnT", f"{pfx}_wr"], w=["ps4"])
            cx.op('act', lambda e: e.activation(out=s.ee[:, :], in_=pl[:, 0:NE], func=AF.Exp, accum_out=s.sm[:, 2:3]), r=["ps4"], w=[f"{pfx}_ee", f"{pfx}_sm"])
            cx.op('dve', lambda e: e.reciprocal(s.sm[:, 3:4], s.sm[:, 2:3]), r=[f"{pfx}_sm"], w=[f"{pfx}_sm"])
            ti = r0 // 128
            cx.op('dve', lambda e: e.tensor_scalar(aff_sb[:, ti, :], s.ee[:, :], s.sm[:, 3:4], None, op0=ALU.mult), r=[f"{pfx}_ee", f"{pfx}_sm"], w=["aff_sb"])
            cx.dma('sp', lambda e: e.dma_start(out=aff_d[r0:r0 + 128, 0:NE], in_=aff_sb[:, ti, :]), r=["aff_sb"], w=[f"d:aff:{r0}"])

        stage_a(0)
        for j in range(4):
            if j + 1 < 4:
                stage_a(j + 1)
            stage_b(j)


def pass_combine(cx, cfg, x_src, hT_d, o_d, om_d, x1_d, hn_d, aff_sb, aff_d, ins, consts, ps, gmix, gffn):
    nc = cx.nc
    with ExitStack() as es:
        stg = [es.enter_context(nc.sbuf_tensor(f"c_stg{i}", [128, 1024], F32)) for i in range(2)]
        wg = load_w(cx, es, "c_wg", ins["ev_w_in"][:, 1024:1536], D, 512, gcol=gmix, stg=stg)
        tail = Tail(cx, es, "c", ins["ev_w_out"], ins["moe_router"][0], gffn, stg)
        gain = es.enter_context(nc.sbuf_tensor("c_gain", [128, 1], F32))
        cx.dma('sp', lambda e: e.dma_start(out=gain[:, :], in_=ins["hg_out_gain"].rearrange("(p o) -> p o", o=1)), w=["c_gain"])
        hb = es.enter_context(nc.sbuf_tensor("c_hb", [128, 8, 512], BF16))
        of = es.enter_context(nc.sbuf_tensor("c_of", [64, 8, 512], BF16))
        ob = es.enter_context(nc.sbuf_tensor("c_ob", [64, 8, 512], BF16))
        sg_ = es.enter_context(nc.sbuf_tensor("c_sg", [128, 512], F32))
        ohf = es.enter_context(nc.sbuf_tensor("c_ohf", [128, 512], F32))
        sq = es.enter_context(nc.sbuf_tensor("c_sq", [128, 512], BF16))
        r1 = es.enter_context(nc.sbuf_tensor("c_r1", [128, 512], F32))
        mix = es.enter_context(nc.sbuf_tensor("c_mix", [128, 8, 512], BF16))
        identB, J64, onesB = consts['identB'], consts['J64'], consts['onesB']
        for sg in range(cfg.NS):
            if sg < cfg.P:
                cbase, nchain, rbase = sg * 32, cfg.P * 32, 0
            else:
                cbase, nchain, rbase = 0, 32, sg * L
            for blk in range(4):
                t0 = sg * L + blk * 512
                cx.dma('sp', lambda e, t0=t0: e.dma_start(out=hb[:, :, :], in_=hT_d[:, :, t0:t0 + 512].rearrange("k p t -> p k t")), w=["c_hb"])
                cx.dma('sp', lambda e, t0=t0: e.dma_start(out=of[:, :, :], in_=o_d[0][t0:t0 + 512, :].rearrange("(c t) n -> t c n", t=64)), w=["c_of"])
                c_lo = cbase + blk * 8
                rr = rbase + (nchain - 1 - c_lo - 7) * 64
                cx.dma('sp', lambda e, rr=rr: e.dma_start(out=ob[:, :, :], in_=o_d[1][rr:rr + 512, :].rearrange("(c t) n -> t c n", t=64)), w=["c_ob"])
                cx.dma('sp', lambda e, t0=t0: e.dma_start(out=mix[:, 4:8, :], in_=om_d[:, :, t0:t0 + 512].rearrange("h p t -> p h t")), w=["c_mix"])
                for h in range(4):
                    pg, po, pss = ps[0], ps[1], ps[2]
                    for k in range(8):
                        cx.op('pe', lambda e, k=k, h=h: e.matmul(pg[:, :], lhsT=wg[:, k, h * 128:(h + 1) * 128], rhs=hb[:, k, :], start=(k == 0), stop=(k == 7)), r=["c_wg", "c_hb"], w=["ps0"])
                    cx.op('act', lambda e: e.activation(out=sg_[:, :], in_=pg[:, :], func=AF.Silu), r=["ps0"], w=["c_sg"])
                    for i in range(8):
                        cx.op('pe', lambda e, i=i, h=h: e.matmul(po[:, i * 64:(i + 1) * 64], lhsT=of[:, i, h * 128:(h + 1) * 128], rhs=identB[0:64, 0:64], start=True, stop=False), r=["c_of", "identB"], w=["ps1"])
                        cx.op('pe', lambda e, i=i, h=h: e.matmul(po[:, i * 64:(i + 1) * 64], lhsT=ob[:, 7 - i, h * 128:(h + 1) * 128], rhs=J64[:, :], start=False, stop=True), r=["c_ob", "J64"], w=["ps1"])
                    cx.op('act', lambda e: e.activation(out=ohf[:, :], in_=po[:, :], func=AF.Copy), r=["ps1"], w=["c_ohf"])
                    cx.op('dve', lambda e: e.tensor_tensor(out=sq[:, :], in0=po[:, :], in1=ohf[:, :], op=ALU.mult), r=["ps1", "c_ohf"], w=["c_sq"])
                    cx.op('pe', lambda e: e.matmul(pss[:, :], lhsT=onesB[:, :], rhs=sq[:, :], start=True, stop=True), r=["onesB", "c_sq"], w=["ps2"])
                    rstd_tile(cx, r1[:, :], "c_r1", pss[:, :], "ps2", 128)
                    cx.op('dve', lambda e: e.tensor_tensor(out=ohf[:, :], in0=ohf[:, :], in1=r1[:, :], op=ALU.mult), r=["c_ohf", "c_r1"], w=["c_ohf"])
                    cx.op('dve', lambda e, h=h: e.scalar_tensor_tensor(out=mix[:, h, :], in0=ohf[:, :], scalar=gain[:, 0:1], in1=sg_[:, :], op0=ALU.mult, op1=ALU.mult), r=["c_ohf", "c_gain", "c_sg"], w=["c_mix"])
                tail.run(mix, "c_mix", 0, t0, x_src, x1_d, hn_d, aff_sb, aff_d, consts, ps)
            cx.epoch()


def pass_moe(cx, cfg, layer, aff_sb, aff_d, hn_d, xacc_d, pinc_d, ins, consts, ps, gffn):
    nc = cx.nc
    NT = cfg.NT
    groups = [(a * (L // 128), b * (L // 128)) for (a, b) in cfg.groups if b > a]
    caps = [((b - a) * 128) // 8 for (a, b) in groups]
    nst = [c // 128 for c in caps]
    NSL = sum(nst)
    ntmax = max(b - a for (a, b) in groups)
    pfx = f"e{layer}"
    with ExitStack() as es0:
        idx_sb = es0.enter_context(nc.sbuf_tensor(f"{pfx}_idx", [128, NE, NSL], I32))
        with ExitStack() as es:
            thr = es.enter_context(nc.sbuf_tensor(f"{pfx}_thr", [128, NE], F32))
            hi = es.enter_context(nc.sbuf_tensor(f"{pfx}_hi", [128, NE], F32))
            mid = es.enter_context(nc.sbuf_tensor(f"{pfx}_mid", [128, NE], F32))
            sel = es.enter_context(nc.sbuf_tensor(f"{pfx}_sel", [128, NE], F32))
            d1 = es.enter_context(nc.sbuf_tensor(f"{pfx}_d1", [128, NE], F32))
            cntp = es.enter_context(nc.sbuf_tensor(f"{pfx}_cntp", [128, NE], F32))
            cmp_ = es.enter_context(nc.sbuf_tensor(f"{pfx}_cmp", [128, ntmax, NE], BF16))
            tot = es.enter_context(nc.sbuf_tensor(f"{pfx}_tot", [128, NE, ntmax], F32))
            incl = es.enter_context(nc.sbuf_tensor(f"{pfx}_incl", [128, NE, ntmax], F32))
            pin = [es.enter_context(nc.sbuf_tensor(f"{pfx}_pin{i}", [128, 4, 128], F32)) for i in range(2)]
            junk = [es.enter_context(nc.sbuf_tensor(f"{pfx}_junk{i}", [128, max(ntmax, 128)], F32)) for i in range(4)]
            ts = [es.enter_context(nc.sbuf_tensor(f"{pfx}_ts{i}", [128, 8], F32)) for i in range(4)]
            rowi = [es.enter_context(nc.sbuf_tensor(f"{pfx}_rowi{i}", [128, 1], I32)) for i in range(4)]
            prow = [es.enter_context(nc.sbuf_tensor(f"{pfx}_prow{i}", [128, 128], F32)) for i in range(4)]
            onesB, onesF, linc, slotid, ones512 = consts['onesB'], consts['onesF'], consts['linc'], consts['slotid'], consts['ones512']
            soff = 0
            for gi, (tg0, tg1) in enumerate(groups):
                nt = tg1 - tg0
                kk = float(caps[gi])
                cx.op('dve', lambda e: e.memset(thr[:, :], 0.0), w=[f"{pfx}_thr"])
                cx.op('dve', lambda e: e.memset(hi[:, :], 1.0), w=[f"{pfx}_hi"])
                for it in range(30):
                    cx.op('dve', lambda e: e.tensor_tensor(out=mid[:, :], in0=thr[:, :], in1=hi[:, :], op=ALU.add), r=[f"{pfx}_thr", f"{pfx}_hi"], w=[f"{pfx}_mid"])
                    cx.op('dve', lambda e: e.tensor_scalar(mid[:, :], mid[:, :], 0.5, None, op0=ALU.mult), r=[f"{pfx}_mid"], w=[f"{pfx}_mid"])
                    cx.op('dve', lambda e, tg0=tg0, tg1=tg1, nt=nt: e.tensor_tensor(out=cmp_[:, 0:nt, :], in0=aff_sb[:, tg0:tg1, :], in1=mid[:, :].unsqueeze(1).to_broadcast([128, nt, NE]), op=ALU.is_ge),
                          r=["aff_sb", f"{pfx}_mid"], w=[f"{pfx}_cmp"])
                    cx.op('dve', lambda e, nt=nt: e.tensor_reduce(out=cntp[:, :], in_=cmp_[:, 0:nt, :].rearrange("p t e -> p e t"), axis=AX.X, op=ALU.add), r=[f"{pfx}_cmp"], w=[f"{pfx}_cntp"])
                    cx.op('pe', lambda e: e.matmul(ps[0][:, 0:NE], lhsT=onesF[:, :], rhs=cntp[:, :], start=True, stop=True), r=["onesF", f"{pfx}_cntp"], w=["ps0"])
                    cx.op('dve', lambda e, kk=kk: e.tensor_scalar(sel[:, :], ps[0][:, 0:NE], kk, None, op0=ALU.is_ge), r=["ps0"], w=[f"{pfx}_sel"])
                    cx.op('dve', lambda e: e.tensor_tensor(out=d1[:, :], in0=mid[:, :], in1=thr[:, :], op=ALU.subtract), r=[f"{pfx}_mid", f"{pfx}_thr"], w=[f"{pfx}_d1"])
                    cx.op('dve', lambda e: e.tensor_tensor(out=d1[:, :], in0=d1[:, :], in1=sel[:, :], op=ALU.mult), r=[f"{pfx}_d1", f"{pfx}_sel"], w=[f"{pfx}_d1"])
                    cx.op('dve', lambda e: e.tensor_tensor(out=thr[:, :], in0=thr[:, :], in1=d1[:, :], op=ALU.add), r=[f"{pfx}_thr", f"{pfx}_d1"], w=[f"{pfx}_thr"])
                    cx.op('dve', lambda e: e.tensor_tensor(out=d1[:, :], in0=hi[:, :], in1=mid[:, :], op=ALU.subtract), r=[f"{pfx}_hi", f"{pfx}_mid"], w=[f"{pfx}_d1"])
                    cx.op('dve', lambda e: e.tensor_tensor(out=d1[:, :], in0=d1[:, :], in1=sel[:, :], op=ALU.mult), r=[f"{pfx}_d1", f"{pfx}_sel"], w=[f"{pfx}_d1"])
                    cx.op('dve', lambda e: e.tensor_tensor(out=hi[:, :], in0=mid[:, :], in1=d1[:, :], op=ALU.add), r=[f"{pfx}_mid", f"{pfx}_d1"], w=[f"{pfx}_hi"])
                if MSTOP <= 1:
                    cx.epoch(); continue
                cx.op('dve', lambda e, tg0=tg0, tg1=tg1, nt=nt: e.tensor_tensor(out=cmp_[:, 0:nt, :], in0=aff_sb[:, tg0:tg1, :], in1=thr[:, :].unsqueeze(1).to_broadcast([128, nt, NE]), op=ALU.is_ge),
                      r=["aff_sb", f"{pfx}_thr"], w=[f"{pfx}_cmp"])
                for ci, a in enumerate(range(0, nt, 32)):
                    na = min(32, nt - a)
                    ncol = na * NE
                    mb = cmp_[:, a:a + na, :].rearrange("p t e -> p (t e)")
                    cx.op('pe', lambda e, mb=mb, ncol=ncol: e.matmul(ps[1][:, 0:ncol], lhsT=onesB[:, :], rhs=mb, start=True, stop=True), r=["onesB", f"{pfx}_cmp"], w=["ps1"])
                    cx.op('act', lambda e, a=a, na=na, ncol=ncol: e.activation(out=tot[:, :, a:a + na], in_=ps[1][:, 0:ncol].rearrange("p (t e) -> p e t", e=NE), func=AF.Copy), r=["ps1"], w=[f"{pfx}_tot"])
                    pb = ci % 2
                    for i in range(ncol // 128):
                        cx.op('pe', lambda e, mb=mb, i=i, pb=pb: e.matmul(ps[2 + pb][:, i * 128:(i + 1) * 128], lhsT=mb[:, i * 128:(i + 1) * 128], rhs=linc[:, :], start=True, stop=True), r=[f"{pfx}_cmp", "linc"], w=[f"ps{2 + pb}"])
                    ni = ncol // 128
                    cx.op('dve', lambda e, pb=pb, ni=ni: e.tensor_copy(pin[pb][:, 0:ni, :], ps[2 + pb][:, 0:ni * 128].rearrange("p (i t) -> p i t", t=128)), r=[f"ps{2 + pb}"], w=[f"{pfx}_pin{pb}"])
                    rb = (tg0 + a) * NE
                    cx.dma('sp', lambda e, pb=pb, ni=ni, rb=rb: e.dma_start(out=pinc_d[rb:rb + ni * 128, :].rearrange("(i p) t -> p i t", p=128), in_=pin[pb][:, 0:ni, :]), r=[f"{pfx}_pin{pb}"], w=["d:pinc"])
                for ex in range(NE):
                    cx.op('dve', lambda e, ex=ex, nt=nt: e.tensor_tensor_scan(out=incl[:, ex, 0:nt], data0=ones512[:, 0:nt], data1=tot[:, ex, 0:nt], initial=0.0, op0=ALU.mult, op1=ALU.add),
                          r=[f"{pfx}_tot", "ones512"], w=[f"{pfx}_incl"])
                if MSTOP <= 2:
                    cx.epoch(); continue
                n_ = 0
                for ex in range(NE):
                    for J in range(nst[gi]):
                        b = n_ % 4
                        n_ += 1
                        t_, tk = ts[b], f"{pfx}_ts{b}"
                        jv = slotid[:, J:J + 1]
                        cx.op('dve', lambda e, ex=ex, nt=nt, b=b, t_=t_, jv=jv: e.tensor_scalar(junk[b][:, 0:nt], incl[:, ex, 0:nt], jv, None, op0=ALU.is_le, op1=ALU.add, accum_out=t_[:, 0:1]),
                              r=[f"{pfx}_incl", "slotid"], w=[f"{pfx}_junk{b}", tk])
                        cx.op('dve', lambda e, ex=ex, nt=nt, b=b, t_=t_, jv=jv: e.scalar_tensor_tensor(out=junk[b][:, 0:nt], in0=incl[:, ex, 0:nt], scalar=jv, in1=tot[:, ex, 0:nt], op0=ALU.is_le, op1=ALU.mult, accum_out=t_[:, 1:2]),
                              r=[f"{pfx}_incl", f"{pfx}_tot", "slotid"], w=[f"{pfx}_junk{b}", tk])
                        cx.op('dve', lambda e, t_=t_, jv=jv: e.tensor_tensor(out=t_[:, 2:3], in0=jv, in1=t_[:, 1:2], op=ALU.subtract), r=[tk, "slotid"], w=[tk])
                        cx.op('dve', lambda e, t_=t_, b=b, ex=ex, tg0=tg0: e.tensor_scalar(rowi[b][:, :], t_[:, 0:1], float(NE), float(ex + tg0 * NE), op0=ALU.mult, op1=ALU.add), r=[tk], w=[f"{pfx}_rowi{b}"])
                        cx.dma('pool', lambda e, b=b: e.indirect_dma_start(out=prow[b][:, :], out_offset=None, in_=pinc_d[:, :], in_offset=bass.IndirectOffsetOnAxis(ap=rowi[b][:, 0:1], axis=0), bounds_check=NT * NE - 1, oob_is_err=False),
                               r=[f"{pfx}_rowi{b}", "d:pinc"], w=[f"{pfx}_prow{b}"])
                        cx.op('dve', lambda e, b=b, t_=t_: e.tensor_scalar(junk[b][:, 0:128], prow[b][:, :], t_[:, 2:3], None, op0=ALU.is_le, op1=ALU.add, accum_out=t_[:, 3:4]),
                              r=[f"{pfx}_prow{b}", tk], w=[f"{pfx}_junk{b}", tk])
                        cx.op('dve', lambda e, t_=t_, tg0=tg0: e.tensor_scalar(t_[:, 4:5], t_[:, 0:1], 128.0, float(tg0 * 128), op0=ALU.mult, op1=ALU.add), r=[tk], w=[tk])
                        cx.op('dve', lambda e, t_=t_, ex=ex, J=J, soff=soff: e.tensor_tensor(out=idx_sb[:, ex, soff + J:soff + J + 1], in0=t_[:, 4:5], in1=t_[:, 3:4], op=ALU.add), r=[tk], w=[f"{pfx}_idx"])
                soff += nst[gi]
                cx.epoch()
        if consts.get('idx_dbg') is not None and layer == 0:
            cx.dma('sp', lambda e: e.dma_start(out=consts['idx_dbg'][:, :], in_=idx_sb[:, :, :].rearrange("p e s -> p (e s)")), r=[f"{pfx}_idx"], w=["d:idxdbg"])
            cx.epoch()
        if MSTOP <= 3:
            return
        with ExitStack() as es:
            stg = [es.enter_context(nc.sbuf_tensor(f"{pfx}_stg{i}", [128, 1024], F32)) for i in range(2)]
            xe = [es.enter_context(nc.sbuf_tensor(f"{pfx}_xe{i}", [128, D], BF16)) for i in range(4)]
            ga = [es.enter_context(nc.sbuf_tensor(f"{pfx}_ga{i}", [128, 128], F32)) for i in range(4)]
            xeT = es.enter_context(nc.sbuf_tensor(f"{pfx}_xeT", [128, 8, 512], BF16))
            hid = es.enter_context(nc.sbuf_tensor(f"{pfx}_hid", [128, 8, 512], BF16))
            sl2 = [es.enter_context(nc.sbuf_tensor(f"{pfx}_sl{i}", [128, 512], F32)) for i in range(2)]
            ye = [es.enter_context(nc.sbuf_tensor(f"{pfx}_ye{i}", [128, D], F32)) for i in range(2)]
            identB = consts['identB']
            psb = consts['psb']
            blocks = [list(range(a, min(a + 4, NSL))) for a in range(0, NSL, 4)]
            for ex in range(NEXP):
                with ExitStack() as esw:
                    wg = load_w(cx, esw, f"{pfx}_wg{ex}", ins["moe_w_gate"][layer, ex], D, D, gcol=gffn, stg=stg)
                    wu = load_w(cx, esw, f"{pfx}_wu{ex}", ins["moe_w_up"][layer, ex], D, D, gcol=gffn, stg=stg)
                    wd = load_w(cx, esw, f"{pfx}_wd{ex}", ins["moe_w_down"][layer, ex], D, D, stg=stg)
                    for blk in blocks:
                        N = len(blk) * 128
                        for si, sidx in enumerate(blk):
                            b = si % 4
                            ix = idx_sb[:, ex, sidx:sidx + 1]
                            cx.dma('pool', lambda e, b=b, ix=ix: e.indirect_dma_start(out=xe[b][:, :], out_offset=None, in_=hn_d[:, :], in_offset=bass.IndirectOffsetOnAxis(ap=ix, axis=0), bounds_check=cfg.T - 1, oob_is_err=False),
                                   r=[f"{pfx}_idx", "d:hn"], w=[f"{pfx}_xe{b}"])
                            cx.dma('pool', lambda e, b=b, ix=ix: e.indirect_dma_start(out=ga[b][:, :], out_offset=None, in_=aff_d[:, :], in_offset=bass.IndirectOffsetOnAxis(ap=ix, axis=0), bounds_check=cfg.T - 1, oob_is_err=False),
                                   r=[f"{pfx}_idx", "d:aff"], w=[f"{pfx}_ga{b}"])
                            for half in range(2):
                                pt, pk = psb[half], f"psb{half}"
                                for kq in range(4):
                                    k = half * 4 + kq
                                    cx.op('pe', lambda e, pt=pt, kq=kq, k=k, b=b: e.transpose(pt[:, kq * 128:(kq + 1) * 128], xe[b][:, k * 128:(k + 1) * 128], identB[:, :]), r=[f"{pfx}_xe{b}", "identB"], w=[pk])
                                eng = 'act' if half == 0 else 'dve'
                                if half == 0:
                                    cx.op('act', lambda e, pt=pt, si=si: e.activation(out=xeT[:, 0:4, si * 128:(si + 1) * 128], in_=pt[:, :].rearrange("p (k t) -> p k t", k=4), func=AF.Copy), r=[pk], w=[f"{pfx}_xeT"])
                                else:
                                    cx.op('dve', lambda e, pt=pt, si=si: e.tensor_copy(xeT[:, 4:8, si * 128:(si + 1) * 128], pt[:, :].rearrange("p (k t) -> p k t", k=4)), r=[pk], w=[f"{pfx}_xeT"])
                        if FSTOP <= 1:
                            continue
                        for f in range(8):
                            fb = (f % 2) * 2
                            pg, pu = ps[fb], ps[fb + 1]
                            pgk, puk = f"ps{fb}", f"ps{fb + 1}"
                            slb, slk = sl2[f % 2], f"{pfx}_sl{f % 2}"
                            for k in range(8):
                                cx.op('pe', lambda e, k=k, f=f, N=N, pg=pg: e.matmul(pg[:, 0:N], lhsT=wg[:, k, f * 128:(f + 1) * 128], rhs=xeT[:, k, 0:N], start=(k == 0), stop=(k == 7)), r=[f"{pfx}_wg{ex}", f"{pfx}_xeT"], w=[pgk])
                            for k in range(8):
                                cx.op('pe', lambda e, k=k, f=f, N=N, pu=pu: e.matmul(pu[:, 0:N], lhsT=wu[:, k, f * 128:(f + 1) * 128], rhs=xeT[:, k, 0:N], start=(k == 0), stop=(k == 7)), r=[f"{pfx}_wu{ex}", f"{pfx}_xeT"], w=[puk])
                            cx.op('act', lambda e, N=N, pg=pg, slb=slb: e.activation(out=slb[:, 0:N], in_=pg[:, 0:N], func=AF.Silu), r=[pgk], w=[slk])
                            cx.op('dve', lambda e, f=f, N=N, pu=pu, slb=slb: e.tensor_tensor(out=hid[:, f, 0:N], in0=pu[:, 0:N], in1=slb[:, 0:N], op=ALU.mult), r=[puk, slk], w=[f"{pfx}_hid{f}"])
                        for si, sidx in enumerate(blk):
                            b = si % 4
                            yb = si % 2
                            for dh in range(2):
                                pd, pk = ps[4 + dh], f"ps{4 + dh}"
                                for f in range(8):
                                    cx.op('pe', lambda e, pd=pd, f=f, si=si, dh=dh: e.matmul(pd[:, :], lhsT=hid[:, f, si * 128:(si + 1) * 128], rhs=wd[:, f, dh * 512:(dh + 1) * 512], start=(f == 0), stop=(f == 7)), r=[f"{pfx}_hid{f}", f"{pfx}_wd{ex}"], w=[pk])
                                if dh == 0:
                                    cx.op('act', lambda e, pd=pd, yb=yb, b=b: e.activation(out=ye[yb][:, 0:512], in_=pd[:, :], func=AF.Copy, scale=ga[b][:, ex:ex + 1]), r=[pk, f"{pfx}_ga{b}"], w=[f"{pfx}_ye{yb}"])
                                else:
                                    cx.op('dve', lambda e, pd=pd, yb=yb, b=b: e.tensor_scalar(ye[yb][:, 512:1024], pd[:, :], ga[b][:, ex:ex + 1], None, op0=ALU.mult), r=[pk, f"{pfx}_ga{b}"], w=[f"{pfx}_ye{yb}"])
                            ix = idx_sb[:, ex, sidx:sidx + 1]
                            if FSTOP <= 3:
                                continue
                            cx.dma('pool', lambda e, yb=yb, ix=ix: e.indirect_dma_start(out=xacc_d[:, :], out_offset=bass.IndirectOffsetOnAxis(ap=ix, axis=0), in_=ye[yb][:, :], in_offset=None, bounds_check=cfg.T - 1, oob_is_err=False, compute_op=ALU.add),
                                   r=[f"{pfx}_ye{yb}", f"{pfx}_idx", "d:xacc"], w=["d:xacc"])
                    cx.epoch()


def pass_r1(cx, cfg, hT_d, gl_d, xb_d, ins, consts, ps, gmix):
    nc = cx.nc
    with ExitStack() as es:
        stg = [es.enter_context(nc.sbuf_tensor(f"r1_stg{i}", [128, 1024], F32)) for i in range(2)]
        w = load_w(cx, es, "r1_w", ins["od_w_in"], D, 2048, gcol=gmix, stg=stg)
        hb = es.enter_context(nc.sbuf_tensor("r1_hb", [128, 8, 512], BF16))
        gl = es.enter_context(nc.sbuf_tensor("r1_gl", [128, 8, 512], BF16))
        xb = es.enter_context(nc.sbuf_tensor("r1_xb", [128, 8, 512], F32))
        for sg in range(cfg.NS):
            for blk in range(4):
                t0 = sg * L + blk * 512
                cx.dma('sp', lambda e, t0=t0: e.dma_start(out=hb[:, :, :], in_=hT_d[:, :, t0:t0 + 512].rearrange("k p t -> p k t")), w=["r1_hb"])
                for n in range(16):
                    pp, pk = ps[n % 2], f"ps{n % 2}"
                    for k in range(8):
                        cx.op('pe', lambda e, pp=pp, n=n, k=k: e.matmul(pp[:, :], lhsT=w[:, k, n * 128:(n + 1) * 128], rhs=hb[:, k, :], start=(k == 0), stop=(k == 7)), r=["r1_w", "r1_hb"], w=[pk])
                    if n < 8:
                        cx.op('act', lambda e, pp=pp, n=n: e.activation(out=gl[:, n, :], in_=pp[:, :], func=AF.Gelu), r=[pk], w=["r1_gl"])
                    else:
                        cx.op('dve', lambda e, pp=pp, n=n: e.tensor_copy(xb[:, n - 8, :], pp[:, :]), r=[pk], w=["r1_xb"])
                cx.dma('sp', lambda e, t0=t0: e.dma_start(out=gl_d[:, :, t0:t0 + 512].rearrange("k p t -> p k t"), in_=gl[:, :, :]), r=["r1_gl"], w=[f"d:gl:{sg}"])
                for hx in range(2):
                    cx.dma('sp', lambda e, t0=t0, hx=hx: e.dma_start(out=xb_d[hx][:, :, t0:t0 + 512].rearrange("k p t -> p k t"), in_=xb[:, hx * 4:(hx + 1) * 4, :]), r=["r1_xb"], w=[f"d:xb:{sg}:{hx}"])
            cx.epoch()


def pass_r2(cx, cfg, dirn, xb_d, gl_d, hf_d, x_src, x3_d, hn_d, aff_sb, aff_d, ins, consts, ps, gffn):
    nc = cx.nc
    pf = f"r2{dirn}"
    with ExitStack() as es:
        xp = [es.enter_context(nc.sbuf_tensor(f"{pf}_xp0", [128, L + 4], F32))] * 2
        stg = [xp[0][:, 0:1024], xp[0][:, 1024:2048]]
        wa = load_w(cx, es, f"{pf}_wa", ins["rg_w_a"][dirn].rearrange("n k j -> (n k) j"), D, 256, stg=stg)
        wx = load_w(cx, es, f"{pf}_wx", ins["rg_w_x"][dirn].rearrange("n k j -> (n k) j"), D, 256, stg=stg)
        ba = load_cols(cx, es, f"{pf}_ba", ins["rg_b_a"][dirn, :], D)
        bx = load_cols(cx, es, f"{pf}_bx", ins["rg_b_x"][dirn, :], D)
        c8 = load_cols(cx, es, f"{pf}_c8", ins["rg_lambda"][dirn, :], D)
        cx.op('act', lambda e: e.activation(out=c8[:, :], in_=c8[:, :], func=AF.Exp, scale=-1.0), r=[f"{pf}_c8"], w=[f"{pf}_c8"])
        cx.op('act', lambda e: e.activation(out=c8[:, :], in_=c8[:, :], func=AF.Ln, bias=1.0), r=[f"{pf}_c8"], w=[f"{pf}_c8"])
        cx.op('dve', lambda e: e.tensor_scalar(c8[:, :], c8[:, :], -8.0, None, op0=ALU.mult), r=[f"{pf}_c8"], w=[f"{pf}_c8"])
        cw = [load_cols(cx, es, f"{pf}_cw{j}", ins["od_conv_w"][j, :], D) for j in range(4)]
        cb = load_cols(cx, es, f"{pf}_cb", ins["od_conv_b"], D)
        tail = Tail(cx, es, pf, ins["od_w_out"], ins["moe_router"][1], gffn, stg) if dirn else None
        cx.epoch()
        xcb = es.enter_context(nc.sbuf_tensor(f"{pf}_xcb", [128, 8, L], BF16))
        rr = es.enter_context(nc.sbuf_tensor(f"{pf}_r", [128, L], F32))
        ii = es.enter_context(nc.sbuf_tensor(f"{pf}_i", [128, L], F32))
        tt = es.enter_context(nc.sbuf_tensor(f"{pf}_t", [128, L], F32))
        acc = tt
        hh = tt
        carry = es.enter_context(nc.sbuf_tensor(f"{pf}_carry", [128, 8], F32))
        hb16 = [es.enter_context(nc.sbuf_tensor(f"{pf}_hb0", [128, L], BF16))] * 2
        glt = [es.enter_context(nc.sbuf_tensor(f"{pf}_gl0", [128, L], BF16))] * 2 if dirn else None
        yT = es.enter_context(nc.sbuf_tensor(f"{pf}_yT", [128, 8, L], BF16)) if dirn else None
        segs = list(range(cfg.NS))
        if dirn:
            segs = list(reversed(range(cfg.P))) + list(range(cfg.P, cfg.NS))
        for sg in segs:
            inchain = sg < cfg.P
            has_l = inchain and sg > 0
            has_r = inchain and sg < cfg.P - 1
            first = (not inchain) or (sg == (cfg.P - 1 if dirn else 0))
            t0 = sg * L
            if first:
                cx.op('dve', lambda e: e.memset(carry[:, :], 0.0), w=[f"{pf}_carry"])
            for ch in range(8):
                b = ch % 2
                xk = f"{pf}_xp0"
                cx.op('pool', lambda e, b=b: e.memset(xp[b][:, 0:2], 0.0), w=[xk])
                cx.op('pool', lambda e, b=b: e.memset(xp[b][:, L + 2:L + 4], 0.0), w=[xk])
                lo = t0 - 2 if has_l else t0
                hi = t0 + L + 1 if has_r else t0 + L
                c_lo = 0 if has_l else 2
                cx.dma('sp', lambda e, b=b, ch=ch, lo=lo, hi=hi, c_lo=c_lo: e.dma_start(out=xp[b][:, c_lo:c_lo + (hi - lo)], in_=xb_d[ch // 4][ch % 4, :, lo:hi]), w=[xk])
                cx.op('dve', lambda e, b=b, ch=ch: e.tensor_scalar(acc[:, :], xp[b][:, 0:L], cw[0][:, ch:ch + 1], cb[:, ch:ch + 1], op0=ALU.mult, op1=ALU.add), r=[xk, f"{pf}_cw0", f"{pf}_cb"], w=[f"{pf}_t"])
                for j in (1, 2):
                    cx.op('dve', lambda e, b=b, ch=ch, j=j: e.scalar_tensor_tensor(out=acc[:, :], in0=xp[b][:, j:j + L], scalar=cw[j][:, ch:ch + 1], in1=acc[:, :], op0=ALU.mult, op1=ALU.add), r=[xk, f"{pf}_cw{j}", f"{pf}_t"], w=[f"{pf}_t"])
                cx.op('dve', lambda e, b=b, ch=ch: e.scalar_tensor_tensor(out=xcb[:, ch, :], in0=xp[b][:, 3:3 + L], scalar=cw[3][:, ch:ch + 1], in1=acc[:, :], op0=ALU.mult, op1=ALU.add), r=[xk, f"{pf}_cw3", f"{pf}_t"], w=[f"{pf}_xcb"])
            for co in range(8):
                n, jh = co // 2, co % 2
                b = co % 2
                for blk in range(4):
                    pa, px = ps[(blk % 2) * 2], ps[(blk % 2) * 2 + 1]
                    pak, pxk = f"ps{(blk % 2) * 2}", f"ps{(blk % 2) * 2 + 1}"
                    for kc in range(2):
                        cx.op('pe', lambda e, pa=pa, kc=kc, n=n, jh=jh, blk=blk: e.matmul(pa[:, :], lhsT=wa[:, n * 2 + kc, jh * 128:(jh + 1) * 128], rhs=xcb[:, n * 2 + kc, blk * 512:(blk + 1) * 512], start=(kc == 0), stop=(kc == 1)), r=[f"{pf}_wa", f"{pf}_xcb"], w=[pak])
                    for kc in range(2):
                        cx.op('pe', lambda e, px=px, kc=kc, n=n, jh=jh, blk=blk: e.matmul(px[:, :], lhsT=wx[:, n * 2 + kc, jh * 128:(jh + 1) * 128], rhs=xcb[:, n * 2 + kc, blk * 512:(blk + 1) * 512], start=(kc == 0), stop=(kc == 1)), r=[f"{pf}_wx", f"{pf}_xcb"], w=[pxk])
                    cx.op('act', lambda e, pa=pa, blk=blk, co=co: e.activation(out=rr[:, blk * 512:(blk + 1) * 512], in_=pa[:, :], func=AF.Sigmoid, bias=ba[:, co:co + 1]), r=[pak, f"{pf}_ba"], w=[f"{pf}_r"])
                    cx.op('act', lambda e, px=px, blk=blk, co=co: e.activation(out=ii[:, blk * 512:(blk + 1) * 512], in_=px[:, :], func=AF.Sigmoid, bias=bx[:, co:co + 1]), r=[pxk, f"{pf}_bx"], w=[f"{pf}_i"])
                cx.op('act', lambda e, co=co: e.activation(out=rr[:, :], in_=rr[:, :], func=AF.Exp, scale=c8[:, co:co + 1]), r=[f"{pf}_r", f"{pf}_c8"], w=[f"{pf}_r"])
                cx.op('act', lambda e: e.activation(out=tt[:, :], in_=rr[:, :], func=AF.Square), r=[f"{pf}_r"], w=[f"{pf}_t"])
                cx.op('act', lambda e: e.activation(out=tt[:, :], in_=tt[:, :], func=AF.Sqrt, scale=-1.0, bias=1.0), r=[f"{pf}_t"], w=[f"{pf}_t"])
                cx.op('dve', lambda e: e.tensor_tensor(out=ii[:, :], in0=tt[:, :], in1=ii[:, :], op=ALU.mult), r=[f"{pf}_t", f"{pf}_i"], w=[f"{pf}_i"])
                cx.op('dve', lambda e, co=co: e.tensor_tensor(out=ii[:, :], in0=ii[:, :], in1=xcb[:, co, :], op=ALU.mult), r=[f"{pf}_i", f"{pf}_xcb"], w=[f"{pf}_i"])
                if dirn == 0:
                    cx.op('dve', lambda e, co=co: e.tensor_tensor_scan(out=hh[:, :], data0=rr[:, :], data1=ii[:, :], initial=carry[:, co:co + 1], op0=ALU.mult, op1=ALU.add), r=[f"{pf}_r", f"{pf}_i", f"{pf}_carry"], w=[f"{pf}_t"])
                    cx.op('dve', lambda e, co=co: e.tensor_copy(carry[:, co:co + 1], hh[:, L - 1:L]), r=[f"{pf}_t"], w=[f"{pf}_carry"])
                    cx.op('act', lambda e, b=b: e.activation(out=hb16[b][:, :], in_=hh[:, :], func=AF.Copy), r=[f"{pf}_t"], w=[f"{pf}_hb0"])
                    cx.dma('sp', lambda e, b=b, co=co, t0=t0: e.dma_start(out=hf_d[co, :, t0:t0 + L], in_=hb16[b][:, :]), r=[f"{pf}_hb0"], w=[f"d:hf:{sg}"])
                else:
                    cx.op('dve', lambda e, co=co: e.tensor_tensor_scan(out=hh[:, ::-1], data0=rr[:, ::-1], data1=ii[:, ::-1], initial=carry[:, co:co + 1], op0=ALU.mult, op1=ALU.add), r=[f"{pf}_r", f"{pf}_i", f"{pf}_carry"], w=[f"{pf}_t"])
                    cx.op('dve', lambda e, co=co: e.tensor_copy(carry[:, co:co + 1], hh[:, 0:1]), r=[f"{pf}_t"], w=[f"{pf}_carry"])
                    cx.dma('sp', lambda e, b=b, co=co, t0=t0: e.dma_start(out=hb16[b][:, :], in_=hf_d[co, :, t0:t0 + L]), w=[f"{pf}_hb0"])
                    cx.dma('sp', lambda e, b=b, co=co, t0=t0: e.dma_start(out=glt[b][:, :], in_=gl_d[co, :, t0:t0 + L]), w=[f"{pf}_gl0"])
                    cx.op('dve', lambda e, b=b: e.tensor_tensor(out=hh[:, :], in0=hh[:, :], in1=hb16[b][:, :], op=ALU.add), r=[f"{pf}_t", f"{pf}_hb0"], w=[f"{pf}_t"])
                    cx.op('dve', lambda e, b=b, co=co: e.tensor_tensor(out=yT[:, co, :], in0=hh[:, :], in1=glt[b][:, :], op=ALU.mult), r=[f"{pf}_t", f"{pf}_gl0"], w=[f"{pf}_yT"])
            if dirn:
                for blk in range(4):
                    tail.run(yT, f"{pf}_yT", blk * 512, t0 + blk * 512, x_src, x3_d, hn_d, aff_sb, aff_d, consts, ps)
            cx.epoch()


def make_consts(cx, es):
    nc = cx.nc
    c = {}
    iot = es.enter_context(nc.sbuf_tensor("c_iot", [128, 128], F32))
    cx.op('pool', lambda e: e.iota(iot[:, :], [[1, 128]], base=0, channel_multiplier=-1, allow_small_or_imprecise_dtypes=True), w=["c_iot"])
    identF = es.enter_context(nc.sbuf_tensor("identF", [128, 128], F32))
    identB = es.enter_context(nc.sbuf_tensor("identB", [128, 128], BF16))
    cx.op('dve', lambda e: e.tensor_scalar(identF[:, :], iot[:, :], 0.0, None, op0=ALU.is_equal), r=["c_iot"], w=["identF"])
    cx.op('dve', lambda e: e.tensor_copy(identB[:, :], identF[:, :]), r=["identF"], w=["identB"])
    iot2 = es.enter_context(nc.sbuf_tensor("c_iot2", [128, 128], F32))
    cx.op('pool', lambda e: e.iota(iot2[:, :], [[1, 128]], base=0, channel_multiplier=1, allow_small_or_imprecise_dtypes=True), w=["c_iot2"])
    J64 = es.enter_context(nc.sbuf_tensor("J64", [64, 64], BF16))
    cx.op('dve', lambda e: e.tensor_scalar(J64[:, :], iot2[0:64, 0:64], 63.0, None, op0=ALU.is_equal), r=["c_iot2"], w=["J64"])
    cmask = es.enter_context(nc.sbuf_tensor("cmask", [64, 64], F32))
    cx.op('dve', lambda e: e.tensor_scalar(cmask[:, :], iot[0:64, 0:64], 0.0, None, op0=ALU.is_ge), r=["c_iot"], w=["cmask"])
    ltri = es.enter_context(nc.sbuf_tensor("ltri", [128, 128], BF16))
    cx.op('dve', lambda e: e.tensor_scalar(ltri[:, :], iot[:, :], 0.0, None, op0=ALU.is_gt), r=["c_iot"], w=["ltri"])
    onesB = es.enter_context(nc.sbuf_tensor("onesB", [128, 128], BF16))
    cx.op('dve', lambda e: e.memset(onesB[:, :], 1.0), w=["onesB"])
    onesF = es.enter_context(nc.sbuf_tensor("onesF", [128, 128], F32))
    cx.op('dve', lambda e: e.memset(onesF[:, :], 1.0), w=["onesF"])
    smask = es.enter_context(nc.sbuf_tensor("smask", [128, 1024], F32))
    cx.op('dve', lambda e: e.memset(smask[:, :], 1.0), w=["smask"])
    cx.op('dve', lambda e: e.memset(smask[:, :].rearrange("p (c t) -> p c t", t=64)[:, :, 0:1], 0.0), w=["smask"])
    linc = es.enter_context(nc.sbuf_tensor("linc", [128, 128], BF16))
    cx.op('dve', lambda e: e.tensor_scalar(linc[:, :], iot[:, :], 0.0, None, op0=ALU.is_ge), r=["c_iot"], w=["linc"])
    ones512 = es.enter_context(nc.sbuf_tensor("ones512", [128, 512], F32))
    cx.op('dve', lambda e: e.memset(ones512[:, :], 1.0), w=["ones512"])
    slotid = es.enter_context(nc.sbuf_tensor("slotid", [128, 64], F32))
    cx.op('pool', lambda e: e.iota(slotid[:, :], [[128, 64]], base=0, channel_multiplier=1, allow_small_or_imprecise_dtypes=True), w=["slotid"])
    c.update(linc=linc, ones512=ones512, slotid=slotid)
    c.update(identF=identF, identB=identB, J64=J64, cmask=cmask, ltri=ltri, onesB=onesB, onesF=onesF, smask=smask, iot=iot)
    return c


def build(P, B, debug=False, stages=99):
    cfg = Cfg(P, B)
    T = cfg.T
    nc = bass.Bass("TRN2", target_bir_lowering=False)
    dt = lambda name, shape, dtype=F32, kind="ExternalInput": nc.dram_tensor(name, list(shape), dtype, kind=kind).ap()
    x_in = dt("x_in", [T, D])
    ins = {}
    for name, shape in [("norm_mix", [2, D]), ("norm_ffn", [2, D]), ("ev_w_in", [D, 3264]), ("ev_w_out", [D, D]),
                        ("hg_lb_logits", [2, 2, 512]), ("hg_out_gain", [128]), ("mla_q_norm", [384]), ("mla_kv_norm", [256]),
                        ("mla_w_uq", [384, 768]), ("mla_w_ukv", [256, 1024]), ("mla_q_gain", [192]), ("mla_k_gain", [192]),
                        ("od_w_in", [D, 2048]), ("od_conv_w", [4, D]), ("od_conv_b", [D]), ("rg_w_a", [2, 4, 256, 256]),
                        ("rg_b_a", [2, D]), ("rg_w_x", [2, 4, 256, 256]), ("rg_b_x", [2, D]), ("rg_lambda", [2, D]),
                        ("od_w_out", [D, D]), ("moe_router", [2, D, NE]), ("moe_w_gate", [2, NE, D, D]),
                        ("moe_w_up", [2, NE, D, D]), ("moe_w_down", [2, NE, D, D]), ("rope_cos", [64, P * L if P else L]),
                        ("rope_sin", [64, P * L if P else L])]:
        ins[name] = dt(name, shape)
    y_out = dt("y_out", [T, D], kind="ExternalOutput")
    hT_d = dt("hT_d", [8, 128, T], BF16, kind=("ExternalOutput" if debug else "Internal"))
    o_d = [dt(f"o_d{i}", [T, 512], BF16, kind=("ExternalOutput" if debug else "Internal")) for i in range(2)]
    dbg = "ExternalOutput" if debug else "Internal"
    q_d = dt("q_d", [4, 192, T], BF16, kind="Internal")
    k_d = dt("k_d", [4, 192, T], BF16, kind="Internal")
    v_d = dt("v_d", [T, 512], BF16, kind="Internal")
    om_d = dt("om_d", [4, 128, T], BF16, kind=dbg)
    x1_d = y_out
    hn_d = dt("hn_d", [T, D], BF16, kind="Internal")
    aff_d = dt("aff_d", [T, 128], F32, kind=dbg)
    pinc_d = dt("pinc_d", [cfg.NT * NE, 128], F32, kind="Internal")
    gl_d = dt("gl_d", [8, 128, T], BF16, kind="Internal")
    xb_d = [dt(f"xb_d{i}", [4, 128, T], F32, kind="Internal") for i in range(2)]
    hf_d = dt("hf_d", [8, 128, T], BF16, kind="Internal")
    nsl_dbg = sum(((b - a) * L // 8) // 128 for (a, b) in cfg.groups if b > a)
    idx_dbg = dt("idx_dbg", [128, NE * nsl_dbg], I32, kind="ExternalOutput") if debug else None
    with ExitStack() as es:
        cx = Cx(nc, es)
        ps = [es.enter_context(nc.psum_tensor(f"ps{i}", [128, 512], F32)) for i in range(6)]
        psb = [es.enter_context(nc.psum_tensor(f"psb{i}", [128, 512], BF16)) for i in range(2)]
        consts = make_consts(cx, es)
        consts['psb'] = psb
        consts['idx_dbg'] = idx_dbg
        gmix = [load_cols(cx, es, f"gmix{l}", ins["norm_mix"][l, :], D) for l in range(2)]
        gffn = [load_cols(cx, es, f"gffn{l}", ins["norm_ffn"][l, :], D) for l in range(2)]
        lbT = []
        for d_ in range(2):
            l0 = load_cols(cx, es, f"lbl0_{d_}", ins["hg_lb_logits"][d_, 0, :], 512)
            l1 = load_cols(cx, es, f"lbl1_{d_}", ins["hg_lb_logits"][d_, 1, :], 512)
            lb = es.enter_context(nc.sbuf_tensor(f"lb_{d_}", [128, 4], F32))
            lbm = es.enter_context(nc.sbuf_tensor(f"lbm_{d_}", [128, 4], F32))
            cx.op('dve', lambda e, l0=l0, l1=l1, lb=lb: e.tensor_tensor(out=lb[:, :], in0=l0[:, :], in1=l1[:, :], op=ALU.subtract), r=[f"lbl0_{d_}", f"lbl1_{d_}"], w=["lbT"])
            cx.op('act', lambda e, lb=lb: e.activation(out=lb[:, :], in_=lb[:, :], func=AF.Sigmoid), r=["lbT"], w=["lbT"])
            cx.op('dve', lambda e, lb=lb, lbm=lbm: e.tensor_scalar(lbm[:, :], lb[:, :], -1.0, 1.0, op0=ALU.mult, op1=ALU.add), r=["lbT"], w=["lbT"])
            lbT.append((lb, lbm))
        cx.epoch()
        x_rows = lambda t0, n: x_in[t0:t0 + n, :]
        if stages >= 1:
            pass_h(cx, cfg, x_rows, hT_d, consts, ps)
        if stages >= 2:
            pass_hgrn(cx, cfg, hT_d, o_d[0], ins["ev_w_in"], lbT, 0, consts, ps, gmix[0], stop=HSTOP)
        if stages >= 3:
            pass_hgrn(cx, cfg, hT_d, o_d[1], ins["ev_w_in"], lbT, 1, consts, ps, gmix[0])
        if stages >= 4:
            pass_mla_prep(cx, cfg, hT_d, q_d, k_d, v_d, ins, consts, ps, gmix[0])
        if stages >= 5:
            pass_attn(cx, cfg, q_d, k_d, v_d, om_d, consts, ps)
        if stages >= 6:
            with nc.sbuf_tensor("aff_sb0", [128, cfg.NT, NE], F32) as aff_sb:
                pass_combine(cx, cfg, x_in, hT_d, o_d, om_d, x1_d, hn_d, aff_sb, aff_d, ins, consts, ps, gmix[0], gffn[0])
                if stages >= 7:
                    pass_moe(cx, cfg, 0, aff_sb, aff_d, hn_d, x1_d, pinc_d, ins, consts, ps, gffn[0])
        if stages >= 8:
            pass_h(cx, cfg, lambda t0, n: x1_d[t0:t0 + n, :], hT_d, consts, ps, tag="b")
            pass_r1(cx, cfg, hT_d, gl_d, xb_d, ins, consts, ps, gmix[1])
            pass_r2(cx, cfg, 0, xb_d, gl_d, hf_d, x1_d, y_out, hn_d, None, aff_d, ins, consts, ps, gffn[1])
            with nc.sbuf_tensor("aff_sb1", [128, cfg.NT, NE], F32) as aff_sb:
                pass_r2(cx, cfg, 1, xb_d, gl_d, hf_d, x1_d, y_out, hn_d, aff_sb, aff_d, ins, consts, ps, gffn[1])
                if stages >= 9:
                    pass_moe(cx, cfg, 1, aff_sb, aff_d, hn_d, y_out, pinc_d, ins, consts, ps, gffn[1])
        cx.drain()
        print("instructions:", cx.ninst)
    return nc, cfg


def host_layout(inputs, P, B):
    g = lambda k: np.ascontiguousarray(np.asarray(inputs[k], dtype=np.float32))
    m = {"x_in": np.concatenate([g("x_prompt").reshape(-1, D), g("x_sample").reshape(-1, D)], 0)}
    for k in ["norm_mix", "norm_ffn", "hg_lb_logits", "moe_router", "moe_w_gate", "moe_w_up", "moe_w_down"]:
        m[k] = g(k)
    for k in ["ev_w_in", "ev_w_out", "hg_out_gain", "mla_q_norm", "mla_kv_norm", "mla_w_uq", "mla_w_ukv", "mla_q_gain", "mla_k_gain",
              "od_w_in", "od_w_out", "od_conv_w", "od_conv_b", "rg_w_a", "rg_b_a", "rg_w_x", "rg_b_x", "rg_lambda"]:
        a = g(k)
        m[k] = np.ascontiguousarray(a.reshape(a.shape[1:]))
    Lp = max(P, 1) * L
    pos = np.arange(Lp, dtype=np.float32)
    inv = (1.0 / (10000.0 ** (np.arange(0, 64, 2, dtype=np.float32) / 64))).astype(np.float32)
    ang = pos[None, :] * inv[:, None]
    m["rope_cos"] = np.concatenate([np.cos(ang), np.cos(ang)], 0).astype(np.float32)
    m["rope_sin"] = np.concatenate([np.sin(ang), np.sin(ang)], 0).astype(np.float32)
    return m


def kernel(**inputs):
    P, B = 8, 32
    nc, cfg = build(P, B)
    m = host_layout(inputs, P, B)
    res = run_bass_kernel_spmd(nc, [m], core_ids=[0])
    y = np.asarray(res.results[0]["y_out"], dtype=np.float32)
    return (y[:P * L].reshape(1, P * L, D).copy(), y[P * L:].reshape(B, L, D).copy())
```
